# Optimizing a Trainium2 kernel written in Bass

```python
import math
import jax, jax.numpy as jnp
from jax import lax
import numpy as np

D_MODEL = 1024
BATCH = 4
SEQ = 8192
DEPTH = 1

CONV_WIDTH = D_MODEL // 2
CONV_KERNEL = 31
HY_WIDTH = D_MODEL // 2
HY_ORDER = 2
HY_SHORT = 3
HY_EMB = 33
HY_FFN = 64
HY_DECAY_TARGET = 1e-2
HY_FAST_DECAY = 0.3
HY_SLOW_DECAY = 1.5
N_MEM = 256
XA_HEADS = 4
XA_HEAD_DIM = D_MODEL // XA_HEADS
N_EXPERTS = 16
EC_CAPACITY = 2
D_FF_EXPERT = 2 * D_MODEL
COL_GLU = 2 * CONV_WIDTH
COL_HY = 3 * HY_WIDTH
COL_GATE = 2 * D_MODEL
IN_COLS = COL_GLU + COL_HY + COL_GATE
LN_EPS = 1e-5
DN_ALPHA = (2.0 * DEPTH) ** 0.25
DN_BETA = (8.0 * DEPTH) ** -0.25

kernel_name = 'hybrid_conformer_hyena_ec_moe_encoder'


def layer_norm(x, g, b):
    xf = x.astype(jnp.float32)
    mu = jnp.mean(xf, axis=-1, keepdims=True)
    var = jnp.mean(jnp.square(xf - mu), axis=-1, keepdims=True)
    y = (xf - mu) * lax.rsqrt(var + LN_EPS)
    return (y * g.astype(jnp.float32) + b.astype(jnp.float32)).astype(x.dtype)


def depthwise_conv(u, w, b):
    k = w.shape[0]
    pad = (k - 1) // 2
    y = lax.conv_general_dilated(
        u, w[:, None, :].astype(u.dtype), window_strides=(1,), padding=[(pad, k - 1 - pad)],
        dimension_numbers=('NWC', 'WIO', 'NWC'), feature_group_count=u.shape[-1])
    return y + b.astype(u.dtype)


def hyena_positional_features(length):
    n = jnp.arange(length, dtype=jnp.float32)
    t = n / max(length - 1, 1)
    bands = (HY_EMB - 1) // 2
    f = jnp.linspace(1e-4, bands - 1, bands, dtype=jnp.float32)
    ang = (2.0 * math.pi * n / length)[:, None] * f[None, :]
    feat = jnp.concatenate([t[:, None], jnp.cos(ang), -jnp.sin(ang)], axis=-1)
    return feat, t


def hyena_kernels(feat, t, w1, b1, f1, w2, b2, f2, w3):
    f32 = jnp.float32
    h = jnp.sin(f1.astype(f32) * (feat @ w1.astype(f32) + b1.astype(f32)))
    h = jnp.sin(f2.astype(f32) * (h @ w2.astype(f32) + b2.astype(f32)))
    h = h @ w3.astype(f32)
    length = feat.shape[0]
    h = h.reshape(length, HY_ORDER, 2, HY_WIDTH)
    max_decay = math.log(HY_DECAY_TARGET) / HY_FAST_DECAY
    min_decay = math.log(HY_DECAY_TARGET) / HY_SLOW_DECAY
    deltas = jnp.abs(jnp.linspace(min_decay, max_decay, HY_WIDTH, dtype=f32))
    decay = jnp.exp(-t[:, None] * deltas[None, :])
    h = h * decay[:, None, None, :]
    fwd = h[:, :, 0]
    bwd = h[:, :, 1]
    k = jnp.concatenate([fwd, jnp.zeros((1, HY_ORDER, HY_WIDTH), f32), bwd[1:][::-1]], axis=0)
    k = k * lax.rsqrt(jnp.sum(jnp.square(k), axis=0, keepdims=True) + 1e-6)
    return k


def bidirectional_fftconv(u, k):
    n = k.shape[0]
    length = u.shape[1]
    uf = jnp.fft.rfft(u, n=n, axis=1)
    kf = jnp.fft.rfft(k, n=n, axis=0)
    return jnp.fft.irfft(uf * kf[None], n=n, axis=1)[:, :length]


def parallel_mixer(h, w_in, b_gate, conf_dw_w, conf_dw_b, conf_ln_g, conf_ln_b, conf_w_out,
                   hy_short_w, hy_short_b, hy_kern, hy_skip, hy_w_out, w_mix_out):
    proj = h @ w_in
    glu = proj[..., :COL_GLU]
    hy = proj[..., COL_GLU:COL_GLU + COL_HY]
    gate_logits = proj[..., COL_GLU + COL_HY:]
    a, b = jnp.split(glu, 2, axis=-1)
    u = a * jax.nn.sigmoid(b)
    u = depthwise_conv(u, conf_dw_w, conf_dw_b)
    u = jax.nn.silu(layer_norm(u, conf_ln_g, conf_ln_b))
    y_a = u @ conf_w_out
    hy = depthwise_conv(hy, hy_short_w, hy_short_b)
    x1, x2, v = jnp.split(hy, 3, axis=-1)
    z = v.astype(jnp.float32)
    for n, g in enumerate((x1, x2)):
        z = g.astype(jnp.float32) * (bidirectional_fftconv(z, hy_kern[:, n])
                                     + hy_skip[n].astype(jnp.float32) * z)
    y_b = z.astype(h.dtype) @ hy_w_out
    g_a, g_b = jnp.split(jax.nn.sigmoid(gate_logits + b_gate), 2, axis=-1)
    return (g_a * y_a + g_b * y_b) @ w_mix_out


def memory_cross_attention(x, mem, wq, wk, wv, wo):
    bsz, length, d = x.shape
    m = mem.shape[1]
    q = (x @ wq).reshape(bsz, length, XA_HEADS, XA_HEAD_DIM)
    k = (mem @ wk).reshape(bsz, m, XA_HEADS, XA_HEAD_DIM)
    v = (mem @ wv).reshape(bsz, m, XA_HEADS, XA_HEAD_DIM)
    s = jnp.einsum('blhk,bmhk->bhlm', q, k).astype(jnp.float32) * (XA_HEAD_DIM ** -0.5)
    p = jax.nn.softmax(s, axis=-1).astype(v.dtype)
    o = jnp.einsum('bhlm,bmhk->blhk', p, v).reshape(bsz, length, d)
    return o @ wo


def expert_choice_moe(x, w_router, w_gate, w_up, w_down):
    bsz, length, d = x.shape
    cap = max(1, EC_CAPACITY * length // N_EXPERTS)
    logits = jnp.einsum('btd,de->bte', x, w_router).astype(jnp.float32)
    aff = jax.nn.softmax(logits, axis=-1)
    gate, idx = lax.top_k(jnp.swapaxes(aff, 1, 2), cap)
    xg = jax.vmap(lambda xb, ib: xb[ib])(x, idx)
    hg = jnp.einsum('becd,edf->becf', xg, w_gate)
    hu = jnp.einsum('becd,edf->becf', xg, w_up)
    y = jnp.einsum('becf,efd->becd', jax.nn.silu(hg) * hu, w_down)
    y = y * gate[..., None].astype(y.dtype)
    return jax.vmap(lambda ib, yb: jnp.zeros((length, d), yb.dtype)
                    .at[ib.reshape(-1)].add(yb.reshape(-1, d)))(idx, y)


def setup_inputs(seed: int = 0) -> dict:
    key = jax.random.key(seed)
    keys = iter(jax.random.split(key, 48))
    f32 = jnp.float32

    def nrm(shape, scale):
        return jax.random.normal(next(keys), shape, f32) * scale

    def gain(shape):
        return 1.0 + nrm(shape, 0.02)

    def bias(shape):
        return nrm(shape, 0.02)

    ly = (DEPTH,)
    d = D_MODEL
    return {
        'x': nrm((BATCH, SEQ, d), 1.0),
        'mem': nrm((BATCH, N_MEM, d), 1.0),
        'ln_in_g': gain((d,)),
        'ln_in_b': bias((d,)),
        'w_in': nrm(ly + (d, IN_COLS), d ** -0.5),
        'b_gate': bias(ly + (COL_GATE,)),
        'conf_dw_w': nrm(ly + (CONV_KERNEL, CONV_WIDTH), CONV_KERNEL ** -0.5),
        'conf_dw_b': bias(ly + (CONV_WIDTH,)),
        'conf_ln_g': gain(ly + (CONV_WIDTH,)),
        'conf_ln_b': bias(ly + (CONV_WIDTH,)),
        'conf_w_out': nrm(ly + (CONV_WIDTH, d), CONV_WIDTH ** -0.5),
        'hy_short_w': nrm(ly + (HY_SHORT, COL_HY), HY_SHORT ** -0.5),
        'hy_short_b': bias(ly + (COL_HY,)),
        'hy_ffn_w1': nrm(ly + (HY_EMB, HY_FFN), HY_EMB ** -0.5),
        'hy_ffn_b1': bias(ly + (HY_FFN,)),
        'hy_freq1': gain(ly + (HY_FFN,)),
        'hy_ffn_w2': nrm(ly + (HY_FFN, HY_FFN), HY_FFN ** -0.5),
        'hy_ffn_b2': bias(ly + (HY_FFN,)),
        'hy_freq2': gain(ly + (HY_FFN,)),
        'hy_ffn_w3': nrm(ly + (HY_FFN, HY_ORDER * 2 * HY_WIDTH), HY_FFN ** -0.5),
        'hy_skip': nrm(ly + (HY_ORDER, HY_WIDTH), 0.5),
        'hy_w_out': nrm(ly + (HY_WIDTH, d), HY_WIDTH ** -0.5),
        'w_mix_out': nrm(ly + (d, d), d ** -0.5 * DN_BETA),
        'ln_mix_g': gain(ly + (d,)),
        'ln_mix_b': bias(ly + (d,)),
        'xa_wq': nrm(ly + (d, d), d ** -0.5),
        'xa_wk': nrm(ly + (d, d), d ** -0.5),
        'xa_wv': nrm(ly + (d, d), d ** -0.5 * DN_BETA),
        'xa_wo': nrm(ly + (d, d), d ** -0.5 * DN_BETA),
        'ln_xa_g': gain(ly + (d,)),
        'ln_xa_b': bias(ly + (d,)),
        'moe_w_router': nrm(ly + (d, N_EXPERTS), d ** -0.5),
        'moe_w_gate': nrm(ly + (N_EXPERTS, d, D_FF_EXPERT), d ** -0.5),
        'moe_w_up': nrm(ly + (N_EXPERTS, d, D_FF_EXPERT), d ** -0.5),
        'moe_w_down': nrm(ly + (N_EXPERTS, D_FF_EXPERT, d), D_FF_EXPERT ** -0.5 * DN_BETA),
        'ln_moe_g': gain(ly + (d,)),
        'ln_moe_b': bias(ly + (d,)),
    }


def reference(x, mem, ln_in_g, ln_in_b, w_in, b_gate, conf_dw_w, conf_dw_b, conf_ln_g, conf_ln_b,
              conf_w_out, hy_short_w, hy_short_b, hy_ffn_w1, hy_ffn_b1, hy_freq1, hy_ffn_w2,
              hy_ffn_b2, hy_freq2, hy_ffn_w3, hy_skip, hy_w_out, w_mix_out, ln_mix_g, ln_mix_b,
              xa_wq, xa_wk, xa_wv, xa_wo, ln_xa_g, ln_xa_b, moe_w_router, moe_w_gate, moe_w_up,
              moe_w_down, ln_moe_g, ln_moe_b):
    x = layer_norm(x, ln_in_g, ln_in_b)
    feat, t = hyena_positional_features(x.shape[1])
    for i in range(DEPTH):
        kern = hyena_kernels(feat, t, hy_ffn_w1[i], hy_ffn_b1[i], hy_freq1[i], hy_ffn_w2[i],
                             hy_ffn_b2[i], hy_freq2[i], hy_ffn_w3[i])
        mixed = parallel_mixer(x, w_in[i], b_gate[i], conf_dw_w[i], conf_dw_b[i], conf_ln_g[i],
                               conf_ln_b[i], conf_w_out[i], hy_short_w[i], hy_short_b[i], kern,
                               hy_skip[i], hy_w_out[i], w_mix_out[i])
        x = layer_norm(DN_ALPHA * x + mixed, ln_mix_g[i], ln_mix_b[i])
        xa = memory_cross_attention(x, mem, xa_wq[i], xa_wk[i], xa_wv[i], xa_wo[i])
        x = layer_norm(DN_ALPHA * x + xa, ln_xa_g[i], ln_xa_b[i])
        moe = expert_choice_moe(x, moe_w_router[i], moe_w_gate[i], moe_w_up[i], moe_w_down[i])
        x = layer_norm(DN_ALPHA * x + moe, ln_moe_g[i], ln_moe_b[i])
    return x
```

```python
import math
import numpy as np
import concourse.bass as bass
import concourse.mybir as mybir
from concourse.bass_utils import run_bass_kernel_spmd
from contextlib import ExitStack

F32 = mybir.dt.float32
BF16 = mybir.dt.bfloat16
U32 = mybir.dt.uint32
I32 = mybir.dt.int32
AF = mybir.ActivationFunctionType
ALU = mybir.AluOpType

D = 1024
L = 8192
NT = L // 128
NMEM = 256
CW = 512
HW_ = 512
COLS = 4608
NE = 16
CAP = 1024
DFF = 2048
EPS = 1e-5
ALPHA = 2.0 ** 0.25
NFFT = 16384
GC = 16
SAME_ENGINE_SYNC = True
STOP_AFTER = None
DEBUG_OUT = ()


class Buf:
    __slots__ = ("name", "w", "r", "dsem", "dcnt")

    def __init__(self, name):
        self.name = name
        self.w = None
        self.r = {}
        self.dsem = None
        self.dcnt = 0


class Tile(Buf):
    __slots__ = ("t",)

    def __init__(self, name, t):
        Buf.__init__(self, name)
        self.t = t

    def __getitem__(self, k):
        return self.t[k]


class Sched:
    ROT = 30000

    def __init__(self, nc, es):
        self.nc = nc
        self.es = es
        self.E = {"pe": nc.tensor, "act": nc.scalar, "dve": nc.vector, "pool": nc.gpsimd, "sp": nc.sync}
        self.csem = {}
        self.ccnt = {}
        self.waited = {k: {} for k in self.E}
        self.sems = []
        self.free_dsems = []
        self.latest = {}
        self.same_engine_sync = SAME_ENGINE_SYNC
        self.ninstr = {k: 0 for k in self.E}
        for k in ("pe", "act", "dve", "pool"):
            self._new_csem(k)

    def _alloc_sem(self, name):
        s = self.es.enter_context(self.nc.semaphore(name))
        self.sems.append(s)
        return s

    def _new_csem(self, k):
        self.csem[k] = self._alloc_sem("c_%s_%d" % (k, len(self.sems)))
        self.ccnt[k] = 0

    def _wait(self, eng, ev):
        if ev is None:
            return
        sem, val = ev
        if (not self.same_engine_sync) and eng in self.csem and sem is self.csem[eng]:
            return
        w = self.waited[eng]
        key = id(sem)
        if w.get(key, 0) >= val:
            return
        self.E[eng].wait_ge(sem, val)
        w[key] = val

    def _deps(self, eng, reads, writes):
        for b in reads:
            if b.w is not None:
                if eng == "pe" and b.w[0] is self.csem.get("pe") and False:
                    continue
                self._wait(eng, b.w)
        for b in writes:
            if b.w is not None:
                if eng == "pe" and b.w[0] is self.csem["pe"]:
                    pass
                else:
                    self._wait(eng, b.w)
            for sid, ev in b.r.items():
                if eng == "pe" and ev[0] is self.csem["pe"]:
                    continue
                self._wait(eng, ev)

    def _mark(self, ev, reads, writes):
        self.latest[id(ev[0])] = ev
        for b in reads:
            b.r[id(ev[0])] = ev
        for b in writes:
            b.w = ev
            b.r = {}

    def op(self, eng, fn, reads=(), writes=()):
        if self.ccnt[eng] >= self.ROT:
            self._new_csem(eng)
        self._deps(eng, reads, writes)
        ins = fn(self.E[eng])
        self.ccnt[eng] += 1
        sem = self.csem[eng]
        ins.then_inc(sem, 1)
        self.ninstr[eng] += 1
        self._mark((sem, self.ccnt[eng]), reads, writes)

    def pe(self, fn, r=(), w=()):
        self.op("pe", fn, r, w)

    def act(self, fn, r=(), w=()):
        self.op("act", fn, r, w)

    def dve(self, fn, r=(), w=()):
        self.op("dve", fn, r, w)

    def pool(self, fn, r=(), w=()):
        self.op("pool", fn, r, w)

    def dma(self, q, fn, reads=(), writes=(), owner=None):
        self._deps(q, reads, writes)
        if owner.dsem is None:
            if self.free_dsems:
                owner.dsem, owner.dcnt = self.free_dsems.pop()
            else:
                owner.dsem = self._alloc_sem("d_%s_%d" % (owner.name, len(self.sems)))
                owner.dcnt = 0
        ins = fn(self.E[q])
        owner.dcnt += 16
        ins.then_inc(owner.dsem, 16)
        self.ninstr[q] += 1
        self._mark((owner.dsem, owner.dcnt), reads, writes)

    def release(self, tiles):
        for t in tiles:
            if t.dsem is not None:
                self.free_dsems.append((t.dsem, t.dcnt))
                t.dsem = None

    def barrier(self):
        evs = list(self.latest.values())
        for eng in self.E:
            for ev in evs:
                self._wait(eng, ev)


class Phase:
    def __init__(self, S, name):
        self.S = S
        self.nc = S.nc
        self.name = name
        self.es = ExitStack()
        self.tiles = []
        self.n = 0

    def __enter__(self):
        self.es.__enter__()
        return self

    def __exit__(self, *a):
        self.S.barrier()
        self.S.release(self.tiles)
        return self.es.__exit__(*a)

    def sb(self, name, shape, dt):
        self.n += 1
        t = self.es.enter_context(self.nc.sbuf_tensor("%s_%s_%d" % (self.name, name, self.n), list(shape), dt))
        tl = Tile(name, t)
        self.tiles.append(tl)
        return tl

    def ps(self, name, shape, dt=F32):
        self.n += 1
        t = self.es.enter_context(self.nc.psum_tensor("%s_%s_%d" % (self.name, name, self.n), list(shape), dt))
        tl = Tile(name, t)
        self.tiles.append(tl)
        return tl

    def ring(self, name, shape, dt, n, ps=False):
        return [(self.ps if ps else self.sb)("%s%d" % (name, i), shape, dt) for i in range(n)]


def build_nc():
    nc = bass.Bass("TRN2", target_bir_lowering=False)
    dram = {}

    def din(name, shape, dt=F32):
        dram[name] = nc.dram_tensor(name, list(shape), dt, kind="ExternalInput").ap()
        return dram[name]

    def dscr(name, shape, dt):
        kind = "ExternalOutput" if name in DEBUG_OUT else "Internal"
        dram[name] = nc.dram_tensor(name, list(shape), dt, kind=kind).ap()
        return dram[name]

    x_d = din("x", [L, D])
    mem_d = din("mem", [NMEM, D])
    ln_in_g = din("ln_in_g", [D, 1]); ln_in_b = din("ln_in_b", [D, 1])
    w_in = din("w_in", [D, COLS])
    b_gate = din("b_gate", [2 * D, 1])
    conf_dw_w = din("conf_dw_w", [31, CW]); conf_dw_b = din("conf_dw_b", [CW, 1])
    conf_ln_g = din("conf_ln_g", [CW, 1]); conf_ln_b = din("conf_ln_b", [CW, 1])
    conf_w_out = din("conf_w_out", [CW, D])
    hy_short_w = din("hy_short_w", [3, 3 * HW_]); hy_short_b = din("hy_short_b", [3 * HW_, 1])
    hy_w1 = din("hy_ffn_w1", [33, 64]); hy_b1 = din("hy_ffn_b1", [64, 1]); hy_f1 = din("hy_freq1", [64, 1])
    hy_w2 = din("hy_ffn_w2", [64, 64]); hy_b2 = din("hy_ffn_b2", [64, 1]); hy_f2 = din("hy_freq2", [64, 1])
    hy_w3 = din("hy_ffn_w3", [64, 2048])
    hy_skip = din("hy_skip", [1, 2 * HW_])
    hy_w_out = din("hy_w_out", [HW_, D])
    w_mix = din("w_mix_out", [D, D])
    ln_mix_g = din("ln_mix_g", [1, D]); ln_mix_b = din("ln_mix_b", [1, D])
    xa_wq = din("xa_wq", [D, D]); xa_wk = din("xa_wk", [D, D]); xa_wv = din("xa_wv", [D, D]); xa_wo = din("xa_wo", [D, D])
    ln_xa_g = din("ln_xa_g", [1, D]); ln_xa_b = din("ln_xa_b", [1, D])
    w_router = din("moe_w_router", [D, NE])
    if STOP_AFTER in ("A", "A2", "H", "C", "C1", "E"):
        w_gate = w_up = w_down = None
    else:
        w_gate = din("moe_w_gate", [NE, D, DFF]); w_up = din("moe_w_up", [NE, D, DFF]); w_down = din("moe_w_down", [NE, DFF, D])
    ln_moe_g = din("ln_moe_g", [1, D]); ln_moe_b = din("ln_moe_b", [1, D])
    ln_in_g_row = din("ln_in_g_row", [1, D]); ln_in_b_row = din("ln_in_b_row", [1, D])
    c_F = din("c_F", [128, 256]); c_T = din("c_T", [128, 256])
    c_featT = din("c_featT", [33, NFFT]); c_tts = din("c_tts", [128, 128])
    c_ident = din("c_ident", [128, 128]); c_iota = din("c_iota", [128, CAP])
    c_tidx = din("c_tidx", [128, NT, 2])
    c_blk8 = din("c_blk8", [128, 128])
    out_d = nc.dram_tensor("out", [L, D], F32, kind="ExternalOutput").ap()

    uT_d = dscr("uT_d", [CW, L], BF16)
    hyT_d = dscr("hyT_d", [3 * HW_, L], BF16)
    gT_d = dscr("gT_d", [2 * D, L], BF16)
    ucT_d = dscr("ucT_d", [CW, L], F32)
    hycT_d = dscr("hycT_d", [3 * HW_, L], BF16)
    zT_d = dscr("zT_d", [HW_, L], BF16)
    x2b_d = dscr("x2b_d", [L, D], BF16)
    acc_d = dscr("acc_d", [L, D], F32)
    affT_d = dscr("affT_d", [NE, L], F32)
    dbg_d = dscr("dbg_d", [128, 4096], F32)
    x1_d = dscr("x1_d", [L, D], F32)
    xg_d = dscr("xg_d", [L, D], F32)

    deltas = np.abs(np.linspace(math.log(1e-2) / 1.5, math.log(1e-2) / 0.3, HW_, dtype=np.float32)).astype(np.float64)

    with ExitStack() as es:
        S = Sched(nc, es)
        DB = {k: Buf(k) for k in ("uT", "hyT", "gT", "ucT", "hycT", "zT", "x2b", "acc", "affT", "dbg")}
        dbt = {}

        def dbuf(name, i):
            k = (name, i)
            if k not in dbt:
                dbt[k] = Buf("%s_%s" % (name, i))
            return dbt[k]

        with Phase(S, "G") as G:
            ident = G.sb("ident", [128, 128], F32)
            identb = G.sb("identb", [128, 128], BF16)
            S.dma("sp", lambda q: q.dma_start(out=ident[:], in_=c_ident[:, :]), writes=[ident], owner=ident)
            S.dve(lambda e: e.tensor_copy(identb[:], ident[:]), [ident], [identb])
            ones = G.sb("ones", [128, 128], F32)
            S.dve(lambda e: e.memset(ones[:], 1.0), [], [ones])

            phase_A(S, nc, locals())
            if STOP_AFTER != "A":
                phase_A2(S, nc, locals())
            if STOP_AFTER not in ("A", "A2"):
                phase_H(S, nc, locals())
            if STOP_AFTER not in ("A", "A2", "H"):
                with Phase(S, "G2") as G2:
                    affT = G2.sb("affT", [NE, L], F32)
                    phase_CD(S, nc, locals())
                    if STOP_AFTER not in ("C", "C1"):
                        phase_EF(S, nc, locals())
            if STOP_AFTER not in ("A", "A2", "H", "C", "C1", "E", "F"):
                phase_G(S, nc, locals())
            S.barrier()
        print("instr counts", S.ninstr, "sems", len(S.sems))
    build_nc.in_names = [k for k in dram if k not in ("uT_d", "hyT_d", "gT_d", "ucT_d", "hycT_d", "zT_d", "x2b_d", "acc_d", "affT_d", "dbg_d", "x1_d", "xg_d", "out")]
    return nc


def load_cast(S, P, q_dst, src_ap, shape, name, stage=None, eng="act"):
    dst_tile, dst_ap = q_dst
    st = stage if stage is not None else P.sb(name + "_st", shape, F32)
    S.dma("sp", lambda q: q.dma_start(out=st[:], in_=src_ap), writes=[st], owner=st)
    if eng == "act":
        S.act(lambda e: e.copy(dst_ap, st[:]), [st], [dst_tile])
    elif eng == "pool":
        S.pool(lambda e: e.tensor_copy(dst_ap, st[:]), [st], [dst_tile])
    else:
        S.dve(lambda e: e.tensor_copy(dst_ap, st[:]), [st], [dst_tile])


def ln_stats(S, P, xt, rstd_name="rs"):
    st = P.sb("bnst", [128, 2, 6], F32)
    mv = P.sb("bnmv", [128, 2], F32)
    rs = P.sb(rstd_name, [128, 1], F32)
    return st, mv, rs


def emit_ln_stats(S, xt_tile, x_ap, st, mv, rs):
    for h in range(2):
        S.dve(lambda e, h=h: e.bn_stats(st[:, h, :], x_ap[:, h * 512:(h + 1) * 512]), [xt_tile], [st])
    S.dve(lambda e: e.bn_aggr(mv[:], st[:].rearrange("p a b -> p (a b)")), [st], [mv])
    S.act(lambda e: e.activation(rs[:], mv[:, 1:2], AF.Sqrt, bias=EPS_AP[0][:], scale=1.0), [mv, EPS_AP[1]], [rs])
    S.dve(lambda e: e.reciprocal(rs[:], rs[:]), [rs], [rs])


EPS_AP = [None, None]


def phase_A(S, nc, env):
    x_d = env["x_d"]; w_in = env["w_in"]; ident = env["ident"]
    uT_d = env["uT_d"]; hyT_d = env["hyT_d"]; gT_d = env["gT_d"]; dbuf = env["dbuf"]
    G = env["G"]
    epsT = G.sb("epsT", [128, 1], F32)
    S.dve(lambda e: e.memset(epsT[:], EPS), [], [epsT])
    EPS_AP[0] = epsT; EPS_AP[1] = epsT
    with Phase(S, "A") as P:
        wsb = P.sb("w_in", [128, 8, COLS], BF16)
        stg = P.ring("wst", [128, 1152], F32, 2)
        i = 0
        for dk in range(8):
            for cq in range(4):
                st = stg[i % 2]
                load_cast(S, P, (wsb, wsb[:, dk, cq * 1152:(cq + 1) * 1152]),
                          w_in[dk * 128:(dk + 1) * 128, cq * 1152:(cq + 1) * 1152], None, "w", stage=st,
                          eng=("act" if i % 2 == 0 else "pool"))
                i += 1
        gsc = P.sb("gsc", [128, 8], F32); gbi = P.sb("gbi", [128, 8], F32); bg = P.sb("bg", [128, 16], F32)
        S.dma("sp", lambda q: q.dma_start(out=gsc[:], in_=env["ln_in_g"].rearrange("(k p) o -> p (k o)", p=128), allow_slow_non_contiguous=True), writes=[gsc], owner=gsc)
        S.dma("sp", lambda q: q.dma_start(out=gbi[:], in_=env["ln_in_b"].rearrange("(k p) o -> p (k o)", p=128), allow_slow_non_contiguous=True), writes=[gbi], owner=gbi)
        S.dma("sp", lambda q: q.dma_start(out=bg[:], in_=env["b_gate"].rearrange("(k p) o -> p (k o)", p=128), allow_slow_non_contiguous=True), writes=[bg], owner=bg)
        xts = P.ring("xt", [128, D], F32, 8)
        AG = row_bcast(S, P, "AG", env["ln_in_g_row"], ALPHA); AB = row_bcast(S, P, "AB", env["ln_in_b_row"], ALPHA)
        xgs = P.ring("xgs", [128, D], F32, 2)
        xg_d = env["xg_d"]
        nhs = P.ring("nh", [128, D], F32, 5)
        sts = [ln_stats(S, P, None) for _ in range(2)]
        hT = P.ring("hT", [128, 8, 512], BF16, 2)
        pst = P.ring("pst", [128, 512], F32, 2, ps=True)
        pmm = P.ring("pmm", [128, 512], F32, 4, ps=True)
        sig = P.ring("sig", [128, 512], F32, 2)
        ob = P.ring("ob", [128, 512], BF16, 4)
        xi = 0; oi = 0; pi = 0
        for blk in range(L // 512):
            h = hT[blk % 2]
            nts = []
            if blk == 0:
                for tt in range(4):
                    S.dma("sp", lambda q, tt=tt: q.dma_start(out=xts[tt][:], in_=x_d[tt * 128:(tt + 1) * 128, :]), writes=[xts[tt]], owner=xts[tt])
            if blk + 1 < L // 512:
                for tt in range(4):
                    xn_ = xts[((blk + 1) * 4 + tt) % 8]; tn = (blk + 1) * 512 + tt * 128
                    S.dma("sp", lambda q, xn_=xn_, tn=tn: q.dma_start(out=xn_[:], in_=x_d[tn:tn + 128, :]), writes=[xn_], owner=xn_)
            for tt in range(4):
                t0 = blk * 512 + tt * 128
                xt = xts[xi % 8]; nh = nhs[xi % 5]; st, mv, rs = sts[xi % 2]; xi += 1
                emit_ln_stats(S, xt, xt.t, st, mv, rs)
                S.dve(lambda e, nh=nh, xt=xt, mv=mv, rs=rs: e.tensor_scalar(nh[:], xt[:], mv[:, 0:1], rs[:], op0=ALU.subtract, op1=ALU.mult), [xt, mv, rs], [nh])
                xg = xgs[xi % 2]
                S.dve(lambda e, xg=xg, nh=nh: e.tensor_tensor(xg[:], nh[:], AG[:], op=ALU.mult), [nh, AG], [xg])
                S.dve(lambda e, xg=xg: e.tensor_tensor(xg[:], xg[:], AB[:], op=ALU.add), [xg, AB], [xg])
                S.dma("pool", lambda q, xg=xg, t0=t0: q.dma_start(out=xg_d[t0:t0 + 128, :], in_=xg[:]), reads=[xg], writes=[dbuf("xg", t0)], owner=xg)
                nts.append(nh)
            for dk in range(8):
                pt = pst[dk % 2]
                for tt in range(4):
                    S.pe(lambda e, pt=pt, tt=tt, dk=dk: e.transpose(pt[:, tt * 128:(tt + 1) * 128], nts[tt][:, dk * 128:(dk + 1) * 128], ident[:]), [nts[tt], ident], [pt])
                S.act(lambda e, pt=pt, dk=dk: e.activation(h[:, dk, :], pt[:], AF.Identity, bias=gbi[:, dk:dk + 1], scale=gsc[:, dk:dk + 1]), [pt, gbi, gsc], [h])

            def proj(cc):
                nonlocal pi
                pm = pmm[pi % 4]; pi += 1
                for dk in range(8):
                    S.pe(lambda e, pm=pm, dk=dk, cc=cc: e.matmul(pm[:], wsb[:, dk, cc * 128:(cc + 1) * 128], h[:, dk, :], start=(dk == 0), stop=(dk == 7)), [wsb, h], [pm])
                return pm
            tsl = slice(blk * 512, (blk + 1) * 512)
            for cc in range(4):
                pa = proj(cc); pb = proj(cc + 4)
                sg = sig[cc % 2]; o = ob[oi % 4]; oi += 1
                S.act(lambda e, sg=sg, pb=pb: e.activation(sg[:], pb[:], AF.Sigmoid), [pb], [sg])
                S.dve(lambda e, o=o, pa=pa, sg=sg: e.tensor_tensor(o[:], pa[:], sg[:], op=ALU.mult), [pa, sg], [o])
                S.dma("pool", lambda q, o=o, cc=cc: q.dma_start(out=uT_d[cc * 128:(cc + 1) * 128, tsl], in_=o[:]), reads=[o], writes=[dbuf("uT", cc)], owner=o)
            for cc in range(8, 20):
                pm = proj(cc); o = ob[oi % 4]; oi += 1
                S.act(lambda e, o=o, pm=pm: e.copy(o[:], pm[:]), [pm], [o])
                S.dma("pool", lambda q, o=o, cc=cc: q.dma_start(out=hyT_d[(cc - 8) * 128:(cc - 7) * 128, tsl], in_=o[:]), reads=[o], writes=[dbuf("hyT", cc - 8)], owner=o)
            for cc in range(20, 36):
                pm = proj(cc); o = ob[oi % 4]; oi += 1
                S.act(lambda e, o=o, pm=pm, cc=cc: e.activation(o[:], pm[:], AF.Sigmoid, bias=bg[:, cc - 20:cc - 19], scale=1.0), [pm, bg], [o])
                S.dma("pool", lambda q, o=o, cc=cc: q.dma_start(out=gT_d[(cc - 20) * 128:(cc - 19) * 128, tsl], in_=o[:]), reads=[o], writes=[dbuf("gT", (cc - 20, blk))], owner=o)


def phase_A2(S, nc, env):
    identb = env["identb"]; dbuf = env["dbuf"]
    uT_d = env["uT_d"]; hyT_d = env["hyT_d"]; ucT_d = env["ucT_d"]; hycT_d = env["hycT_d"]
    with Phase(S, "A2") as P:
        rows = P.ring("row", [128, L + 32], BF16, 2)
        for r in rows:
            S.pool(lambda e, r=r: e.memset(r[:, 0:16], 0.0), [], [r])
            S.pool(lambda e, r=r: e.memset(r[:, L + 16:L + 32], 0.0), [], [r])
        wT = P.sb("wT", [128, 4, 31], F32); wT3 = P.sb("wT3", [128, 12, 3], F32)
        cb = P.sb("cb", [128, 4], F32); cb3 = P.sb("cb3", [128, 12], F32)
        for c in range(4):
            S.dma("sp", lambda q, c=c: q.dma_start(out=wT[:, c, :], in_=env["conf_dw_w"][:, c * 128:(c + 1) * 128].rearrange("k p -> p k"), allow_slow_non_contiguous=True), writes=[wT], owner=wT)
        for c in range(12):
            S.dma("sp", lambda q, c=c: q.dma_start(out=wT3[:, c, :], in_=env["hy_short_w"][:, c * 128:(c + 1) * 128].rearrange("k p -> p k"), allow_slow_non_contiguous=True), writes=[wT3], owner=wT3)
        S.dma("sp", lambda q: q.dma_start(out=cb[:], in_=env["conf_dw_b"].rearrange("(c p) o -> p (c o)", p=128), allow_slow_non_contiguous=True), writes=[cb], owner=cb)
        S.dma("sp", lambda q: q.dma_start(out=cb3[:], in_=env["hy_short_b"].rearrange("(c p) o -> p (c o)", p=128), allow_slow_non_contiguous=True), writes=[cb3], owner=cb3)
        dg = P.ring("dg", [128, 31, 128], BF16, 2)
        pc = P.ring("pc", [128, 512], F32, 3, ps=True)
        of = P.ring("of", [128, 512], F32, 3)
        obf = P.ring("obf", [128, 512], BF16, 3)
        pi = 0; ri = 0
        jobs = [("u", c) for c in range(4)] + [("h", c) for c in range(12)]
        for kind, c in jobs:
            row = rows[ri % 2]; dgt = dg[ri % 2]; ri += 1
            K = 31 if kind == "u" else 3
            pad = (K - 1) // 2
            src = uT_d if kind == "u" else hyT_d
            S.dma("sp", lambda q, row=row, src=src, c=c: q.dma_start(out=row[:, 16:16 + L], in_=src[c * 128:(c + 1) * 128, :]),
                  reads=[dbuf("uT" if kind == "u" else "hyT", c)], writes=[row], owner=row)
            wt = wT if kind == "u" else wT3
            for k in range(K):
                S.dve(lambda e, dgt=dgt, k=k, wt=wt, c=c: e.tensor_scalar(dgt[:, k, :], identb[:], wt[:, c, k:k + 1], None, op0=ALU.mult), [identb, wt], [dgt])
            for blk in range(L // 512):
                p = pc[pi % 3]
                for k in range(K):
                    off = 16 + blk * 512 + k - pad
                    S.pe(lambda e, p=p, k=k, off=off, dgt=dgt, row=row: e.matmul(p[:], dgt[:, k, :], row[:, off:off + 512], start=(k == 0), stop=(k == K - 1)), [dgt, row], [p])
                tsl = slice(blk * 512, (blk + 1) * 512)
                if kind == "u":
                    o = of[pi % 3]
                    S.act(lambda e, o=o, p=p, c=c: e.activation(o[:], p[:], AF.Identity, bias=cb[:, c:c + 1], scale=1.0), [p, cb], [o])
                    S.dma("sp", lambda q, o=o, c=c, tsl=tsl: q.dma_start(out=ucT_d[c * 128:(c + 1) * 128, tsl], in_=o[:]), reads=[o], writes=[dbuf("ucT", blk)], owner=o)
                else:
                    o = obf[pi % 3]
                    S.act(lambda e, o=o, p=p, c=c: e.activation(o[:], p[:], AF.Identity, bias=cb3[:, c:c + 1], scale=1.0), [p, cb3], [o])
                    S.dma("sp", lambda q, o=o, c=c, tsl=tsl: q.dma_start(out=hycT_d[c * 128:(c + 1) * 128, tsl], in_=o[:]), reads=[o], writes=[dbuf("hycT", c)], owner=o)
                pi += 1


def phase_H(S, nc, env):
    hycT_d = env["hycT_d"]; zT_d = env["zT_d"]; ones = env["ones"]; deltas = env["deltas"]
    c_F = env["c_F"]; c_T = env["c_T"]; c_featT = env["c_featT"]; c_tts = env["c_tts"]
    INVN = 1.0 / NFFT
    with Phase(S, "H") as P:
        Fst = P.sb("Fst", [128, 256], F32)
        Tst = P.sb("Tst", [128, 256], F32)
        S.dma("sp", lambda q: q.dma_start(out=Fst[:], in_=c_F[:, :]), writes=[Fst], owner=Fst)
        S.dma("sp", lambda q: q.dma_start(out=Tst[:], in_=c_T[:, :]), writes=[Tst], owner=Tst)
        Fb = P.sb("Fb", [128, 256], BF16); FA = P.sb("FA", [128, 256], BF16); FB_ = P.sb("FB", [128, 256], BF16)
        Fin = P.sb("Fin", [128, 128], BF16)
        S.dve(lambda e: e.tensor_copy(Fb[:], Fst[:]), [Fst], [Fb])
        S.dve(lambda e: e.tensor_copy(FA[:, 0:128], Fst[:, 0:128]), [Fst], [FA])
        S.dve(lambda e: e.tensor_scalar(FA[:, 128:256], Fst[:, 128:256], -1.0, None, op0=ALU.mult), [Fst], [FA])
        S.dve(lambda e: e.tensor_copy(FB_[:, 0:128], Fst[:, 128:256]), [Fst], [FB_])
        S.dve(lambda e: e.tensor_copy(FB_[:, 128:256], Fst[:, 0:128]), [Fst], [FB_])
        S.dve(lambda e: e.tensor_scalar(Fin[:], Fst[:, 128:256], -1.0, None, op0=ALU.mult), [Fst], [Fin])
        TT1 = P.sb("TT1", [128, 2, 256], F32); TT2 = P.sb("TT2", [128, 2, 256], F32)
        for i in range(2):
            for hh in range(2):
                S.dve(lambda e, i=i, hh=hh: e.tensor_copy(TT1[:, i, hh * 128:(hh + 1) * 128], Tst[:, 0:128]), [Tst], [TT1])
                S.dve(lambda e, i=i, hh=hh: e.tensor_copy(TT2[:, i, hh * 128:(hh + 1) * 128], Tst[:, 128:256]), [Tst], [TT2])
        NH = 66
        FbH = P.sb("FbH", [128, 2 * NH], BF16)
        S.dve(lambda e: e.tensor_copy(FbH[:, 0:NH], Fst[:, 0:NH]), [Fst], [FbH])
        S.dve(lambda e: e.tensor_copy(FbH[:, NH:2 * NH], Fst[:, 128:128 + NH]), [Fst], [FbH])
        TH1 = P.sb("TH1", [128, 2, 2 * NH], F32); TH2 = P.sb("TH2", [128, 2, 2 * NH], F32)
        TK1 = P.sb("TK1", [128, 2, 2 * NH], F32); TK2 = P.sb("TK2", [128, 2, 2 * NH], F32)
        for i in range(2):
            for hh in range(2):
                S.dve(lambda e, i=i, hh=hh: e.tensor_copy(TH1[:, i, hh * NH:(hh + 1) * NH], Tst[:, 0:NH]), [Tst], [TH1])
                S.dve(lambda e, i=i, hh=hh: e.tensor_copy(TH2[:, i, hh * NH:(hh + 1) * NH], Tst[:, 128:128 + NH]), [Tst], [TH2])
        for src_, dst_ in ((TH1, TK1), (TH2, TK2)):
            S.dve(lambda e, src_=src_, dst_=dst_: e.tensor_scalar(dst_[:], src_[:], 2.0, None, op0=ALU.mult), [src_], [dst_])
            for i in range(2):
                for hh in range(2):
                    b0 = hh * NH
                    S.dve(lambda e, src_=src_, dst_=dst_, i=i, b0=b0: e.tensor_copy(dst_[:, i, b0:b0 + 1], src_[:, i, b0:b0 + 1]), [src_], [dst_])
                    S.dve(lambda e, src_=src_, dst_=dst_, i=i, b0=b0: e.tensor_copy(dst_[:, i, b0 + 64:b0 + 65], src_[:, i, b0 + 64:b0 + 65]), [src_], [dst_])
                    S.dve(lambda e, dst_=dst_, i=i, b0=b0: e.memset(dst_[:, i, b0 + 65:b0 + 66], 0.0), [], [dst_])
        tts = P.sb("tts", [128, 128], F32)
        S.dma("sp", lambda q: q.dma_start(out=tts[:], in_=c_tts[:, :]), writes=[tts], owner=tts)
        e6 = P.sb("e6", [128, 1], F32)
        S.dve(lambda e: e.memset(e6[:], 1e-6), [], [e6])
        H2 = P.sb("H2", [128, NFFT], BF16)
        S.pool(lambda e: e.memset(H2[:], 0.0), [], [H2])
        w1 = P.sb("w1", [33, 64], F32); w2d = P.sb("w2d", [64, 128], F32)
        S.dma("sp", lambda q: q.dma_start(out=w1[:], in_=env["hy_w1"][:, :]), writes=[w1], owner=w1)
        S.dma("sp", lambda q: q.dma_start(out=w2d[:, 0:64], in_=env["hy_w2"][:, :]), writes=[w2d], owner=w2d)
        S.dma("sp", lambda q: q.dma_start(out=w2d[:, 64:128], in_=env["hy_w2"][:, :]), writes=[w2d], owner=w2d)
        fb = P.sb("fb", [128, 4], F32)
        S.dma("sp", lambda q: q.dma_start(out=fb[0:64, 0:1], in_=env["hy_f1"][:, :]), writes=[fb], owner=fb)
        S.dma("sp", lambda q: q.dma_start(out=fb[0:64, 1:2], in_=env["hy_b1"][:, :]), writes=[fb], owner=fb)
        for hh in range(2):
            S.dma("sp", lambda q, hh=hh: q.dma_start(out=fb[hh * 64:(hh + 1) * 64, 2:3], in_=env["hy_f2"][:, :]), writes=[fb], owner=fb)
            S.dma("sp", lambda q, hh=hh: q.dma_start(out=fb[hh * 64:(hh + 1) * 64, 3:4], in_=env["hy_b2"][:, :]), writes=[fb], owner=fb)
        S.dma("sp", lambda q: q.dma_start(out=fb[64:128, 0:1], in_=env["hy_f1"][:, :]), writes=[fb], owner=fb)
        S.dma("sp", lambda q: q.dma_start(out=fb[64:128, 1:2], in_=env["hy_b1"][:, :]), writes=[fb], owner=fb)
        sb_ = P.sb("sb", [128, 4], F32)
        S.dve(lambda e: e.tensor_scalar(sb_[:, 0:1], fb[:, 0:1], 1.0 / 3.0, None, op0=ALU.mult), [fb], [sb_])
        S.dve(lambda e: e.tensor_tensor(sb_[:, 1:2], fb[:, 0:1], fb[:, 1:2], op=ALU.mult), [fb], [sb_])
        S.dve(lambda e: e.tensor_scalar(sb_[:, 1:2], sb_[:, 1:2], 1.0 / 3.0, None, op0=ALU.mult), [sb_], [sb_])
        S.dve(lambda e: e.tensor_scalar(sb_[:, 2:3], fb[:, 2:3], 1.0 / 3.0, None, op0=ALU.mult), [fb], [sb_])
        S.dve(lambda e: e.tensor_tensor(sb_[:, 3:4], fb[:, 2:3], fb[:, 3:4], op=ALU.mult), [fb], [sb_])
        S.dve(lambda e: e.tensor_scalar(sb_[:, 3:4], sb_[:, 3:4], 1.0 / 3.0, None, op0=ALU.mult), [sb_], [sb_])
        bank = P.ring("bk", [128, 512], F32, 8, ps=True)
        PF = Phase(S, "HF"); PF.__enter__()
        fts = PF.ring("ft", [33, 512], F32, 2)
        s1 = PF.ring("s1", [128, 512], F32, 2); qq = PF.ring("qq", [128, 512], F32, 2); h1 = PF.ring("h1", [64, 512], F32, 2)
        for blk in range(NFFT // 512):
            ft = fts[blk % 2]; s = s1[blk % 2]; q_ = qq[blk % 2]; hh1 = h1[blk % 2]
            pb1 = bank[blk % 2]; pb2 = bank[2 + blk % 2]
            S.dma("sp", lambda q, ft=ft, blk=blk: q.dma_start(out=ft[:], in_=c_featT[:, blk * 512:(blk + 1) * 512]), writes=[ft], owner=ft)
            S.pe(lambda e, pb1=pb1, ft=ft: e.matmul(pb1[0:64, :], w1[:], ft[:], start=True, stop=True), [w1, ft], [pb1])
            S.act(lambda e, s=s, pb1=pb1: e.activation(s[0:64, :], pb1[0:64, :], AF.Sin, bias=sb_[0:64, 1:2], scale=sb_[0:64, 0:1]), [pb1, sb_], [s])
            S.dve(lambda e, s=s, q_=q_: e.tensor_tensor(q_[0:64, :], s[0:64, :], s[0:64, :], op=ALU.mult), [s], [q_])
            S.dve(lambda e, q_=q_: e.tensor_scalar(q_[0:64, :], q_[0:64, :], -4.0, 3.0, op0=ALU.mult, op1=ALU.add), [q_], [q_])
            S.dve(lambda e, s=s, q_=q_, hh1=hh1: e.tensor_tensor(hh1[:], q_[0:64, :], s[0:64, :], op=ALU.mult), [s, q_], [hh1])
            S.pe(lambda e, pb2=pb2, hh1=hh1: e.matmul(pb2[:], w2d[:], hh1[:], start=True, stop=True), [w2d, hh1], [pb2])
            lo = 0 if blk < 16 else 64
            S.act(lambda e, s=s, pb2=pb2, lo=lo: e.activation(s[lo:lo + 64, :], pb2[lo:lo + 64, :], AF.Sin, bias=sb_[lo:lo + 64, 3:4], scale=sb_[lo:lo + 64, 2:3]), [pb2, sb_], [s])
            S.dve(lambda e, s=s, q_=q_, lo=lo: e.tensor_tensor(q_[lo:lo + 64, :], s[lo:lo + 64, :], s[lo:lo + 64, :], op=ALU.mult), [s], [q_])
            S.dve(lambda e, q_=q_, lo=lo: e.tensor_scalar(q_[lo:lo + 64, :], q_[lo:lo + 64, :], -4.0, 3.0, op0=ALU.mult, op1=ALU.add), [q_], [q_])
            S.dve(lambda e, s=s, q_=q_, lo=lo, blk=blk: e.tensor_tensor(H2[lo:lo + 64, blk * 512:(blk + 1) * 512], q_[lo:lo + 64, :], s[lo:lo + 64, :], op=ALU.mult), [s, q_], [H2])
        S.dve(lambda e: e.memset(H2[64:128, L:L + 1], 0.0), [], [H2])
        PF.__exit__(None, None, None)
        w3st = P.sb("w3st", [128, 2, 512], F32)
        w3v = env["hy_w3"].rearrange("m (o d c) -> m o d c", o=2, d=2)
        S.dma("sp", lambda q: q.dma_start(out=w3st[0:64, :, :], in_=w3v[:, :, 0, :]), writes=[w3st], owner=w3st)
        S.dma("sp", lambda q: q.dma_start(out=w3st[64:128, :, :], in_=w3v[:, :, 1, :]), writes=[w3st], owner=w3st)
        W3s = P.sb("W3s", [128, 2, 512], BF16)
        S.dve(lambda e: e.tensor_copy(W3s[:], w3st[:]), [w3st], [W3s])
        skp = P.sb("skp", [1, 2, 512], F32)
        S.dma("sp", lambda q: q.dma_start(out=skp[:], in_=env["hy_skip"].rearrange("o (a c) -> o a c", a=2)), writes=[skp], owner=skp)

        QC = 4
        NW = 3

        class Lane:
            pass

        lanes = []
        for l in range(NW):
            ln = Lane()
            ln.kBre = P.sb("kBre", [128, 2 * QC, NH], BF16); ln.kBim = P.sb("kBim", [128, 2 * QC, NH], BF16)
            ln.Kr = P.sb("Kr", [128, 2 * QC, NH], F32); ln.Ki = P.sb("Ki", [128, 2 * QC, NH], F32)
            ln.q1h = P.ring("q1h", [128, 2, 2 * NH], F32, 2); ln.q2h = P.ring("q2h", [128, 2, 2 * NH], F32, 2)
            ln.q1 = P.ring("q1", [128, 2, 256], F32, 2); ln.q2 = P.ring("q2", [128, 2, 256], F32, 2)
            ln.t = [P.sb("t%d" % i, [128, QC * NH], F32) for i in range(4)]
            ln.Uv = P.sb("Uv", [64, QC, 128], BF16); ln.G1 = P.sb("G1", [64, QC, 128], BF16); ln.G2 = P.sb("G2", [64, QC, 128], BF16)
            ln.Bre = P.sb("Bre", [128, QC, NH], BF16); ln.Bim = P.sb("Bim", [128, QC, NH], BF16)
            ln.Yre = P.sb("Yre", [128, QC, NH], BF16); ln.Yim = P.sb("Yim", [128, QC, NH], BF16)
            ln.Cre = P.sb("Cre", [128, QC, 128], BF16); ln.Cim = P.sb("Cim", [128, QC, 128], BF16)
            ln.Z1 = P.sb("Z1", [64, QC, 128], BF16); ln.Z2 = P.sb("Z2", [64, QC, 128], BF16)
            ln.pa = bank[2 * l]; ln.px = (bank[2 * l], bank[2 * l + 1])
            ln.qi = 0
            lanes.append(ln)
        pk = bank[6]; py = bank[7]
        Dg = P.sb("Dg", [128, GC, 128], F32)
        kt = P.sb("kt", [128, 2, GC, 128], F32)
        kbR = P.ring("kb", [128, 2, GC, 128], BF16, 2)
        junk = P.sb("junk", [128, 128], F32)
        ssq = P.sb("ssq", [128, 2 * GC], F32); scl = P.sb("scl", [128, 2 * GC], F32)

        def kblock_stages(g):
            c0 = g * GC
            kb = kbR[g % 2]
            st = []

            def k_dec():
                for c in range(GC):
                    S.act(lambda e, c=c: e.activation(Dg[:, c, :], tts[:], AF.Exp, scale=-float(deltas[c0 + c])), [tts], [Dg])
            st.append(k_dec)

            def k_gen(r):
                def f():
                    for j in range(16):
                        n2 = r * 16 + j
                        S.pe(lambda e, j=j, n2=n2: e.matmul(pk[:, j * 32:(j + 1) * 32], H2[:, n2:NFFT:128], W3s[:, :, c0:c0 + GC], start=True, stop=True), [H2, W3s], [pk])
                    pkv = pk[:].rearrange("p (n o c) -> p o c n", n=16, o=2)
                    for o in range(2):
                        S.dve(lambda e, o=o: e.tensor_tensor(kt[:, o, :, r * 16:(r + 1) * 16], pkv[:, o, :, :], Dg[:, :, r * 16:(r + 1) * 16], op=ALU.mult), [pk, Dg], [kt])
                return f
            for r in range(8):
                st.append(k_gen(r))

            def k_sq():
                for o in range(2):
                    for c in range(GC):
                        m = o * GC + c
                        S.act(lambda e, o=o, c=c, m=m: e.activation(junk[:], kt[:, o, c, :], AF.Square, accum_out=ssq[:, m:m + 1]), [kt], [junk, ssq])
            st.append(k_sq)

            def k_tot():
                S.pe(lambda e: e.matmul(pk[:, 0:2 * GC], ones[:], ssq[:], start=True, stop=True), [ones, ssq], [pk])
                S.act(lambda e: e.activation(scl[:], pk[:, 0:2 * GC], AF.Sqrt, bias=e6[:], scale=1.0), [pk, e6], [scl])
                S.dve(lambda e: e.reciprocal(scl[:], scl[:]), [scl], [scl])
            st.append(k_tot)

            def k_scale():
                for o in range(2):
                    for c in range(GC):
                        m = o * GC + c
                        S.act(lambda e, o=o, c=c, m=m: e.activation(kb[:, o, c, :], kt[:, o, c, :], AF.Copy, scale=scl[:, m:m + 1]), [kt, scl], [kb])
                S.dve(lambda e: e.tensor_tensor(kb[0:1, :, :, 0], kb[0:1, :, :, 0], skp[0:1, :, c0:c0 + GC], op=ALU.add), [kb, skp], [kb])
            st.append(k_scale)
            return st

        def twiddle_f(ln, dre, dim, m0, kern):
            pa = ln.pa
            q1 = ln.q1h[ln.qi % 2]; q2 = ln.q2h[ln.qi % 2]; ln.qi += 1
            pav = pa[:].rearrange("p (a b) -> p a b", a=2)[:, :, 0:2 * NH]
            T1 = TK1 if kern else TH1; T2 = TK2 if kern else TH2
            S.dve(lambda e: e.tensor_tensor(q1[:], pav, T1[:], op=ALU.mult), [pa, T1], [q1])
            S.dve(lambda e: e.tensor_tensor(q2[:], pav, T2[:], op=ALU.mult), [pa, T2], [q2])
            S.pool(lambda e: e.tensor_tensor(dre[:, m0:m0 + 2, :], q1[:, :, 0:NH], q2[:, :, NH:2 * NH], op=ALU.subtract), [q1, q2], [dre])
            S.pool(lambda e: e.tensor_tensor(dim[:, m0:m0 + 2, :], q2[:, :, 0:NH], q1[:, :, NH:2 * NH], op=ALU.add), [q1, q2], [dim])

        def twiddle_i(ln, dre, dim, m0):
            pa = ln.pa
            q1 = ln.q1[ln.qi % 2]; q2 = ln.q2[ln.qi % 2]; ln.qi += 1
            pav = pa[0:NH, :].rearrange("p (a b) -> p a b", a=2)
            S.dve(lambda e: e.tensor_tensor(q1[0:NH], pav, TT1[0:NH], op=ALU.mult), [pa, TT1], [q1])
            S.dve(lambda e: e.tensor_tensor(q2[0:NH], pav, TT2[0:NH], op=ALU.mult), [pa, TT2], [q2])
            S.pool(lambda e: e.tensor_tensor(dre[0:NH, m0:m0 + 2, :], q1[0:NH, :, 0:128], q2[0:NH, :, 128:256], op=ALU.add), [q1, q2], [dre])
            S.pool(lambda e: e.tensor_tensor(dim[0:NH, m0:m0 + 2, :], q1[0:NH, :, 128:256], q2[0:NH, :, 0:128], op=ALU.subtract), [q1, q2], [dim])

        def stage3(ln, bre, bim, m0):
            pxr, pxi = ln.px
            W4 = 4 * NH
            rr = bre[:, m0:m0 + 4, :]; ri = bim[:, m0:m0 + 4, :]
            S.pe(lambda e: e.matmul(pxr[:, 0:W4], Fb[:, 0:128], rr, start=True, stop=False), [Fb, bre], [pxr])
            S.pe(lambda e: e.matmul(pxr[:, 0:W4], Fin[:], ri, start=False, stop=True), [Fin, bim], [pxr])
            S.pe(lambda e: e.matmul(pxi[:, 0:W4], Fb[:, 0:128], ri, start=True, stop=False), [Fb, bim], [pxi])
            S.pe(lambda e: e.matmul(pxi[:, 0:W4], Fb[:, 128:256], rr, start=False, stop=True), [Fb, bre], [pxi])
            return pxr, pxi

        def item_stages(ln, it):
            c0 = it * QC
            st = []

            def s_load():
                for tl, r0 in ((ln.Uv, 2 * HW_ + c0), (ln.G1, c0), (ln.G2, HW_ + c0)):
                    S.dma("sp", lambda q, tl=tl, r0=r0: q.dma_start(out=tl[:], in_=hycT_d[r0:r0 + QC, :].rearrange("c (a b) -> a c b", b=128)), writes=[tl], owner=tl)
            st.append(s_load)
            kb = kbR[(c0 // GC) % 2]
            cl = c0 % GC

            def s_ks1(o):
                def f():
                    for p_ in range(QC // 2):
                        for i in range(2):
                            c = 2 * p_ + i
                            S.pe(lambda e, c=c, i=i: e.matmul(ln.pa[:, i * 256:i * 256 + 2 * NH], kb[:, o, cl + c, :], FbH[:], start=True, stop=True), [kb, FbH], [ln.pa])
                        twiddle_f(ln, ln.kBre, ln.kBim, o * QC + 2 * p_, True)
                return f
            st.append(s_ks1(0)); st.append(s_ks1(1))

            def s_ks3(o):
                def f():
                    pxr, pxi = stage3(ln, ln.kBre, ln.kBim, o * QC)
                    S.act(lambda e: e.activation(ln.Kr[:, o * QC:(o + 1) * QC, :].rearrange("p a b -> p (a b)"), pxr[:, 0:QC * NH], AF.Copy, scale=INVN), [pxr], [ln.Kr])
                    S.act(lambda e: e.activation(ln.Ki[:, o * QC:(o + 1) * QC, :].rearrange("p a b -> p (a b)"), pxi[:, 0:QC * NH], AF.Copy, scale=INVN), [pxi], [ln.Ki])
                return f
            st.append(s_ks3(0)); st.append(s_ks3(1))

            def conv_stages(o, U, Gt, Zt, last):
                def c1():
                    for p_ in range(QC // 2):
                        for i in range(2):
                            c = 2 * p_ + i
                            S.pe(lambda e, c=c, i=i: e.matmul(ln.pa[:, i * 256:i * 256 + 2 * NH], U[:, c, :], FbH[0:64, :], start=True, stop=True), [U, FbH], [ln.pa])
                        twiddle_f(ln, ln.Bre, ln.Bim, 2 * p_, False)

                def c2():
                    pxr, pxi = stage3(ln, ln.Bre, ln.Bim, 0)
                    t1, t2, t3, t4 = ln.t
                    kr = ln.Kr[:, o * QC:(o + 1) * QC, :].rearrange("p a b -> p (a b)"); ki = ln.Ki[:, o * QC:(o + 1) * QC, :].rearrange("p a b -> p (a b)")
                    W4 = QC * NH
                    S.dve(lambda e: e.tensor_tensor(t1[:], pxr[:, 0:W4], kr, op=ALU.mult), [pxr, ln.Kr], [t1])
                    S.dve(lambda e: e.tensor_tensor(t2[:], pxi[:, 0:W4], ki, op=ALU.mult), [pxi, ln.Ki], [t2])
                    S.dve(lambda e: e.tensor_tensor(t3[:], pxr[:, 0:W4], ki, op=ALU.mult), [pxr, ln.Ki], [t3])
                    S.dve(lambda e: e.tensor_tensor(t4[:], pxi[:, 0:W4], kr, op=ALU.mult), [pxi, ln.Kr], [t4])
                    S.pool(lambda e: e.tensor_tensor(ln.Yre[:].rearrange("p a b -> p (a b)"), t1[:], t2[:], op=ALU.subtract), [t1, t2], [ln.Yre])
                    S.pool(lambda e: e.tensor_tensor(ln.Yim[:].rearrange("p a b -> p (a b)"), t3[:], t4[:], op=ALU.add), [t3, t4], [ln.Yim])

                def c3():
                    for p_ in range(QC // 2):
                        for i in range(2):
                            c = 2 * p_ + i
                            S.pe(lambda e, c=c, i=i: e.matmul(ln.pa[0:NH, i * 256:(i + 1) * 256], ln.Yre[:, c, :], FA[:], start=True, stop=False), [ln.Yre, FA], [ln.pa])
                            S.pe(lambda e, c=c, i=i: e.matmul(ln.pa[0:NH, i * 256:(i + 1) * 256], ln.Yim[:, c, :], FB_[:], start=False, stop=True), [ln.Yim, FB_], [ln.pa])
                        twiddle_i(ln, ln.Cre, ln.Cim, 2 * p_)

                def c4():
                    S.pe(lambda e: e.matmul(py[0:64, :], Fb[0:NH, 0:64], ln.Cre[0:NH].rearrange("p a b -> p (a b)"), start=True, stop=False), [Fb, ln.Cre], [py])
                    S.pe(lambda e: e.matmul(py[0:64, :], Fb[0:NH, 128:192], ln.Cim[0:NH].rearrange("p a b -> p (a b)"), start=False, stop=True), [Fb, ln.Cim], [py])
                    S.dve(lambda e: e.tensor_tensor(Zt[:].rearrange("p a b -> p (a b)"), py[0:64, :], Gt[:].rearrange("p a b -> p (a b)"), op=ALU.mult), [py, Gt], [Zt])
                    if last:
                        S.dma("sp", lambda q: q.dma_start(out=zT_d[c0:c0 + QC, :].rearrange("c (a b) -> a c b", b=128), in_=Zt[:]), reads=[Zt], writes=[env["dbuf"]("zT", it)], owner=Zt)
                return [c1, c2, c3, c4]
            st += conv_stages(0, ln.Uv, ln.G1, ln.Z1, False)
            st += conv_stages(1, ln.Z1, ln.G2, ln.Z2, True)
            return st

        nitems = HW_ // QC
        ipg = GC // QC
        ngrp = HW_ // GC
        for f_ in kblock_stages(0):
            f_()
        kq = []
        next_g = 1
        for i0_ in range(0, nitems, NW):
            idxs = [i for i in range(i0_, min(i0_ + NW, nitems))]
            gmax = idxs[-1] // ipg
            while next_g <= gmax:
                kq += [(next_g, f_) for f_ in kblock_stages(next_g)]
                next_g += 1
            while kq and kq[0][0] <= gmax:
                kq.pop(0)[1]()
            if not kq and next_g < ngrp and next_g <= gmax + 1:
                kq += [(next_g, f_) for f_ in kblock_stages(next_g)]
                next_g += 1
            sts_ = [item_stages(lanes[l], idxs[l]) for l in range(len(idxs))]
            for k in range(len(sts_[0])):
                for l in range(len(idxs)):
                    sts_[l][k]()
                if kq:
                    kq.pop(0)[1]()
        while kq:
            kq.pop(0)[1]()


def load_w_bf16(S, dst, src_ap, kchunks, ncols, stg, cnt):
    for k in range(kchunks):
        for c0 in range(0, ncols, 1024):
            st = stg[cnt[0] % len(stg)]
            w = min(1024, ncols - c0)
            S.dma("sp", lambda q, st=st, k=k, c0=c0, w=w: q.dma_start(out=st[:, 0:w], in_=src_ap[k * 128:(k + 1) * 128, c0:c0 + w]), writes=[st], owner=st)
            if cnt[0] % 2 == 0:
                S.act(lambda e, st=st, k=k, c0=c0, w=w: e.copy(dst[:, k, c0:c0 + w], st[:, 0:w]), [st], [dst])
            else:
                S.pool(lambda e, st=st, k=k, c0=c0, w=w: e.tensor_copy(dst[:, k, c0:c0 + w], st[:, 0:w]), [st], [dst])
            cnt[0] += 1


def row_bcast(S, P, name, src_row, scale=None):
    t = P.sb(name, [128, D], F32)
    S.dma("sp", lambda q: q.dma_start(out=t[:], in_=src_row.broadcast_to([128, D])), writes=[t], owner=t)
    if scale is not None:
        S.act(lambda e: e.mul(t[:], t[:], float(scale)), [t], [t])
    return t


def col_chunks(S, P, name, src_col, k):
    t = P.sb(name, [128, k], F32)
    S.dma("sp", lambda q: q.dma_start(out=t[:], in_=src_col.rearrange("(k p) o -> p (k o)", p=128), allow_slow_non_contiguous=True), writes=[t], owner=t)
    return t


def phase_CD(S, nc, env):
    phase_C(S, nc, env)
    if STOP_AFTER != "C1":
        phase_D(S, nc, env)


def phase_C(S, nc, env):
    ones = env["ones"]; x_d = env["x_d"]; ucT_d = env["ucT_d"]; zT_d = env["zT_d"]; gT_d = env["gT_d"]; x1_d = env["x1_d"]
    dbuf = env["dbuf"]
    with Phase(S, "C") as P:
        stg = P.ring("stg", [128, 1024], F32, 2); cnt = [0]
        cwo = P.sb("cwo", [128, 4, D], BF16); hwo = P.sb("hwo", [128, 4, D], BF16); wmx = P.sb("wmx", [128, 8, D], BF16)
        load_w_bf16(S, cwo, env["conf_w_out"], 4, D, stg, cnt)
        load_w_bf16(S, hwo, env["hy_w_out"], 4, D, stg, cnt)
        load_w_bf16(S, wmx, env["w_mix"], 8, D, stg, cnt)
        cg = col_chunks(S, P, "cg", env["conf_ln_g"], 4); cbb = col_chunks(S, P, "cbb", env["conf_ln_b"], 4)
        MG = row_bcast(S, P, "MG", env["ln_mix_g"]); MB = row_bcast(S, P, "MB", env["ln_mix_b"])
        xg_d = env["xg_d"]
        uc = P.sb("uc", [128, 4, 512], F32); sq = P.sb("sq", [128, 4, 512], F32)
        zt = P.sb("zt", [128, 4, 512], BF16); ua = P.sb("ua", [128, 4, 512], BF16)
        mean = P.sb("mean", [128, 512], F32); var = P.sb("var", [128, 512], F32); rstd = P.sb("rstd", [128, 512], F32)
        dd = P.ring("dd", [128, 512], F32, 2)
        gch = P.ring("gch", [128, 2, 512], BF16, 2)
        m1 = P.ring("m1", [128, 512], F32, 2); m2 = P.ring("m2", [128, 512], F32, 2)
        mgR = P.ring("mg", [128, 8, 512], BF16, 2)
        xgs = P.ring("xg", [128, D], F32, 3)
        ss = P.ring("s", [128, D], F32, 2); x1s = P.ring("x1", [128, D], F32, 2)
        sts = [ln_stats(S, P, None) for _ in range(3)]
        bank = P.ring("bk", [128, 512], F32, 8, ps=True)
        cnts = {"ti": 0, "si": 0}

        def front(blk):
            tsl = slice(blk * 512, (blk + 1) * 512)
            mg = mgR[blk % 2]
            st = []

            def f0():
                S.dma("sp", lambda q: q.dma_start(out=uc[:], in_=ucT_d[:, tsl].rearrange("(k p) t -> p k t", p=128)), writes=[uc], owner=uc)
                S.dma("sp", lambda q: q.dma_start(out=zt[:], in_=zT_d[:, tsl].rearrange("(k p) t -> p k t", p=128)), writes=[zt], owner=zt)
                S.act(lambda e: e.activation(sq[:], uc[:], AF.Square), [uc], [sq])
            st.append(f0)

            def f1():
                pS1 = bank[0]; pS2 = bank[1]
                for k in range(4):
                    S.pe(lambda e, k=k: e.matmul(pS1[:], ones[:], uc[:, k, :], start=(k == 0), stop=(k == 3)), [ones, uc], [pS1])
                for k in range(4):
                    S.pe(lambda e, k=k: e.matmul(pS2[:], ones[:], sq[:, k, :], start=(k == 0), stop=(k == 3)), [ones, sq], [pS2])
                S.act(lambda e: e.mul(mean[:], pS1[:], 1.0 / CW), [pS1], [mean])
                S.dve(lambda e: e.tensor_tensor(var[:], mean[:], mean[:], op=ALU.mult), [mean], [var])
                S.dve(lambda e: e.scalar_tensor_tensor(var[:], pS2[:], 1.0 / CW, var[:], op0=ALU.mult, op1=ALU.subtract), [pS2, var], [var])
                S.act(lambda e: e.activation(rstd[:], var[:], AF.Sqrt, bias=EPS_AP[0][:], scale=1.0), [var, EPS_AP[0]], [rstd])
                S.dve(lambda e: e.reciprocal(rstd[:], rstd[:]), [rstd], [rstd])
            st.append(f1)

            def f2(k):
                def f():
                    d_ = dd[k % 2]
                    S.dve(lambda e: e.tensor_tensor(d_[:], uc[:, k, :], mean[:], op=ALU.subtract), [uc, mean], [d_])
                    S.dve(lambda e: e.tensor_tensor(d_[:], d_[:], rstd[:], op=ALU.mult), [d_, rstd], [d_])
                    S.act(lambda e: e.activation(ua[:, k, :], d_[:], AF.Silu, bias=cbb[:, k:k + 1], scale=cg[:, k:k + 1]), [d_, cbb, cg], [ua])
                return f
            for k in range(4):
                st.append(f2(k))

            def f3(dc):
                def f():
                    pya = bank[2 + (dc % 2) * 2]; pyb = bank[3 + (dc % 2) * 2]
                    g_ = gch[dc % 2]; a1 = m1[dc % 2]; a2 = m2[dc % 2]
                    S.dma("sp", lambda q: q.dma_start(out=g_[:, 0, :], in_=gT_d[dc * 128:(dc + 1) * 128, tsl]), writes=[g_], owner=g_)
                    S.dma("sp", lambda q: q.dma_start(out=g_[:, 1, :], in_=gT_d[D + dc * 128:D + (dc + 1) * 128, tsl]), writes=[g_], owner=g_)
                    for k in range(4):
                        S.pe(lambda e, k=k: e.matmul(pya[:], cwo[:, k, dc * 128:(dc + 1) * 128], ua[:, k, :], start=(k == 0), stop=(k == 3)), [cwo, ua], [pya])
                    for k in range(4):
                        S.pe(lambda e, k=k: e.matmul(pyb[:], hwo[:, k, dc * 128:(dc + 1) * 128], zt[:, k, :], start=(k == 0), stop=(k == 3)), [hwo, zt], [pyb])
                    S.dve(lambda e: e.tensor_tensor(a1[:], pya[:], g_[:, 0, :], op=ALU.mult), [pya, g_], [a1])
                    S.dve(lambda e: e.tensor_tensor(a2[:], pyb[:], g_[:, 1, :], op=ALU.mult), [pyb, g_], [a2])
                    S.pool(lambda e: e.tensor_tensor(mg[:, dc, :], a1[:], a2[:], op=ALU.add), [a1, a2], [mg])
                return f
            for dc in range(8):
                st.append(f3(dc))
            return st

        def back(blk):
            mg = mgR[blk % 2]
            st = []
            for tt in range(4):
                t0 = blk * 512 + tt * 128
                ti = cnts["ti"]; cnts["ti"] += 1
                xg = xgs[ti % 3]; s_ = ss[ti % 2]; x1 = x1s[ti % 2]

                def ba(xg=xg, t0=t0):
                    S.dma("sp", lambda q: q.dma_start(out=xg[:], in_=xg_d[t0:t0 + 128, :]), reads=[dbuf("xg", t0)], writes=[xg], owner=xg)
                st.append(ba)

                def bb(xg=xg, s_=s_, x1=x1, tt=tt, t0=t0):
                    for half in range(2):
                        pm = bank[6 + half]
                        for k in range(8):
                            S.pe(lambda e, pm=pm, k=k, half=half: e.matmul(pm[:], mg[:, k, tt * 128:(tt + 1) * 128], wmx[:, k, half * 512:(half + 1) * 512], start=(k == 0), stop=(k == 7)), [mg, wmx], [pm])
                        S.dve(lambda e, pm=pm, half=half: e.tensor_tensor(s_[:, half * 512:(half + 1) * 512], pm[:], xg[:, half * 512:(half + 1) * 512], op=ALU.add), [pm, xg], [s_])
                    st_, mv, rs = sts[cnts["si"] % 3]; cnts["si"] += 1
                    emit_ln_stats(S, s_, s_.t, st_, mv, rs)
                    S.dve(lambda e: e.tensor_scalar(x1[:], s_[:], mv[:, 0:1], rs[:], op0=ALU.subtract, op1=ALU.mult), [s_, mv, rs], [x1])
                    S.dve(lambda e: e.tensor_tensor(x1[:], x1[:], MG[:], op=ALU.mult), [x1, MG], [x1])
                    S.pool(lambda e: e.tensor_tensor(x1[:], x1[:], MB[:], op=ALU.add), [x1, MB], [x1])
                    S.dma("sp", lambda q: q.dma_start(out=x1_d[t0:t0 + 128, :], in_=x1[:]), reads=[x1], writes=[dbuf("x1", t0)], owner=x1)
                st.append(bb)
            return st

        nblk = L // 512
        import os
        if os.environ.get("SEQC", "0") == "1":
            for blk in range(nblk):
                for f_ in front(blk):
                    f_()
                for f_ in back(blk):
                    f_()
            nblk = 0
        else:
            for f_ in front(0):
                f_()
        for blk in range(nblk):
            bs = back(blk)
            fs = front(blk + 1) if blk + 1 < nblk else []
            n = max(len(bs), len(fs))
            bi = 0; fi = 0
            for k in range(n):
                while bi < len(bs) and bi * n <= k * len(bs):
                    bs[bi](); bi += 1
                while fi < len(fs) and fi * n <= k * len(fs):
                    fs[fi](); fi += 1
            while bi < len(bs):
                bs[bi](); bi += 1
            while fi < len(fs):
                fs[fi](); fi += 1


def phase_D(S, nc, env):
    ident = env["ident"]; identb = env["identb"]; x1_d = env["x1_d"]; mem_d = env["mem_d"]; ones = env["ones"]
    acc_d = env["acc_d"]; x2b_d = env["x2b_d"]; affT = env["affT"]; dbuf = env["dbuf"]
    with Phase(S, "D") as P:
        wq = P.sb("wq", [128, 8, D], BF16); wo = P.sb("wo", [128, 8, D], BF16)
        kT = P.sb("kT", [128, 8, NMEM], BF16); V = P.sb("V", [128, 2, D], BF16)
        wr = P.sb("wr", [128, 8, NE], F32)
        S.dma("sp", lambda q: q.dma_start(out=wr[:], in_=env["w_router"].rearrange("(k p) e -> p k e", p=128)), writes=[wr], owner=wr)
        bank = P.ring("bk", [128, 512], F32, 7, ps=True)
        ppT = P.ps("ppT", [128, 8, 128], BF16)
        XG = row_bcast(S, P, "XG", env["ln_xa_g"]); XB = row_bcast(S, P, "XB", env["ln_xa_b"])
        PW = Phase(S, "DW"); PW.__enter__()
        stg = PW.ring("stg", [128, 1024], F32, 2); cnt = [0]
        with Phase(S, "DK") as PK:
            wk = PK.sb("wk", [128, 8, D], BF16); wv = PK.sb("wv", [128, 8, D], BF16)
            load_w_bf16(S, wk, env["xa_wk"], 8, D, stg, cnt)
            load_w_bf16(S, wv, env["xa_wv"], 8, D, stg, cnt)
            memT = PK.sb("memT", [128, 8, NMEM], BF16)
            mt = PK.ring("mt", [128, D], F32, 2)
            for mc in range(2):
                m_ = mt[mc]
                S.dma("sp", lambda q, m_=m_, mc=mc: q.dma_start(out=m_[:], in_=mem_d[mc * 128:(mc + 1) * 128, :]), writes=[m_], owner=m_)
                for half in range(2):
                    pt = bank[half]
                    for j in range(4):
                        dk = half * 4 + j
                        S.pe(lambda e, pt=pt, j=j, dk=dk, m_=m_: e.transpose(pt[:, j * 128:(j + 1) * 128], m_[:, dk * 128:(dk + 1) * 128], ident[:]), [m_, ident], [pt])
                    S.act(lambda e, pt=pt, half=half, mc=mc: e.copy(memT[:, half * 4:half * 4 + 4, mc * 128:(mc + 1) * 128], pt[:].rearrange("p (a b) -> p a b", a=4)), [pt], [memT])
            for hc in range(8):
                pk_ = bank[2 + hc % 2]
                for k in range(8):
                    S.pe(lambda e, pk_=pk_, k=k, hc=hc: e.matmul(pk_[:, 0:NMEM], wk[:, k, hc * 128:(hc + 1) * 128], memT[:, k, :], start=(k == 0), stop=(k == 7)), [wk, memT], [pk_])
                S.act(lambda e, pk_=pk_, hc=hc: e.copy(kT[:, hc, :], pk_[:, 0:NMEM]), [pk_], [kT])
            for mc in range(2):
                for half in range(2):
                    pv = bank[4 + half]
                    for k in range(8):
                        S.pe(lambda e, pv=pv, k=k, mc=mc, half=half: e.matmul(pv[:], memT[:, k, mc * 128:(mc + 1) * 128], wv[:, k, half * 512:(half + 1) * 512], start=(k == 0), stop=(k == 7)), [memT, wv], [pv])
                    S.act(lambda e, pv=pv, mc=mc, half=half: e.copy(V[:, mc, half * 512:(half + 1) * 512], pv[:]), [pv], [V])
        load_w_bf16(S, wq, env["xa_wq"], 8, D, stg, cnt)
        load_w_bf16(S, wo, env["xa_wo"], 8, D, stg, cnt)
        PW.__exit__(None, None, None)
        x1t = P.ring("x1t", [128, D], F32, 4)
        xr = P.ring("xr", [128, D], F32, 2)
        x1T = P.sb("x1T", [128, 8, 512], BF16); qT = P.sb("qT", [128, 8, 512], BF16)
        pf = P.ring("pf", [128, 4, NMEM], F32, 2); pn = P.ring("pn", [128, 4, NMEM], BF16, 2)
        pT = P.sb("pT", [128, 8, 512], BF16); oTR = P.ring("oT", [128, 8, 512], BF16, 2)
        mx = P.ring("mx", [128, 4], F32, 2); sm = P.ring("sm", [128, 4], F32, 2)
        s2 = P.ring("s2", [128, D], F32, 2); x2 = P.ring("x2", [128, D], F32, 2)
        accs = P.ring("accs", [128, D], F32, 2); x2b = P.ring("x2b", [128, D], BF16, 2)
        x2T = P.sb("x2T", [128, 8, 512], F32)
        ex = P.sb("ex", [NE, 512], F32); rsum = P.sb("rsum", [NE, 512], F32)
        sts = [ln_stats(S, P, None) for _ in range(2)]
        cn = {"si": 0, "ti": 0}

        def front(blk):
            oT = oTR[blk % 2]
            st = []

            def d0():
                for tt in range(4):
                    t0 = blk * 512 + tt * 128
                    xt = x1t[tt]
                    S.dma("sp", lambda q, xt=xt, t0=t0: q.dma_start(out=xt[:], in_=x1_d[t0:t0 + 128, :]), reads=[dbuf("x1", t0)], writes=[xt], owner=xt)
            st.append(d0)

            def d1(dk0):
                def f():
                    for dk in range(dk0, dk0 + 4):
                        pt = bank[dk % 2]
                        for tt in range(4):
                            S.pe(lambda e, pt=pt, tt=tt, dk=dk: e.transpose(pt[:, tt * 128:(tt + 1) * 128], x1t[tt][:, dk * 128:(dk + 1) * 128], ident[:]), [x1t[tt], ident], [pt])
                        S.act(lambda e, pt=pt, dk=dk: e.copy(x1T[:, dk, :], pt[:]), [pt], [x1T])
                return f
            st.append(d1(0)); st.append(d1(4))

            def d2(h0):
                def f():
                    for hc in range(h0, h0 + 4):
                        pq = bank[2 + hc % 2]
                        for k in range(8):
                            S.pe(lambda e, pq=pq, k=k, hc=hc: e.matmul(pq[:], wq[:, k, hc * 128:(hc + 1) * 128], x1T[:, k, :], start=(k == 0), stop=(k == 7)), [wq, x1T], [pq])
                        S.act(lambda e, pq=pq, hc=hc: e.mul(qT[:, hc, :], pq[:], 1.0 / 16.0), [pq], [qT])
                return f
            st.append(d2(0)); st.append(d2(4))

            def d3(tt):
                def f():
                    p_f = pf[tt % 2]; p_n = pn[tt % 2]; mx_ = mx[tt % 2]; sm_ = sm[tt % 2]
                    for hp in range(2):
                        psc = bank[4 + hp]
                        for hh in range(2):
                            h = hp * 2 + hh
                            for k in range(2):
                                S.pe(lambda e, psc=psc, hh=hh, h=h, k=k: e.matmul(psc[:, hh * 256:(hh + 1) * 256], qT[:, 2 * h + k, tt * 128:(tt + 1) * 128], kT[:, 2 * h + k, :], start=(k == 0), stop=(k == 1)), [qT, kT], [psc])
                        S.dve(lambda e, psc=psc, hp=hp: e.tensor_reduce(mx_[:, hp * 2:hp * 2 + 2], psc[:].rearrange("p (a b) -> p a b", a=2), axis=mybir.AxisListType.X, op=ALU.max, negate=True), [psc], [mx_])
                        for hh in range(2):
                            h = hp * 2 + hh
                            S.act(lambda e, psc=psc, hh=hh, h=h: e.activation(p_f[:, h, :], psc[:, hh * 256:(hh + 1) * 256], AF.Exp, bias=mx_[:, h:h + 1], scale=1.0, accum_out=sm_[:, h:h + 1]), [psc, mx_], [p_f, sm_])
                    S.dve(lambda e: e.reciprocal(sm_[:], sm_[:]), [sm_], [sm_])
                    for h in range(4):
                        S.dve(lambda e, h=h: e.tensor_scalar(p_n[:, h, :], p_f[:, h, :], sm_[:, h:h + 1], None, op0=ALU.mult), [p_f, sm_], [p_n])
                    for h in range(4):
                        for mc in range(2):
                            S.pe(lambda e, h=h, mc=mc: e.transpose(ppT[:, h * 2 + mc, :], p_n[:, h, mc * 128:(mc + 1) * 128], identb[:]), [p_n, identb], [ppT])
                    S.act(lambda e: e.copy(pT[:, :, tt * 128:(tt + 1) * 128], ppT[:]), [ppT], [pT])
                return f
            for tt in range(4):
                st.append(d3(tt))

            def d4(h0):
                def f():
                    for hc in range(h0, h0 + 4):
                        po = bank[2 + hc % 2]; h = hc // 2
                        for mc in range(2):
                            S.pe(lambda e, po=po, mc=mc, hc=hc, h=h: e.matmul(po[:], V[:, mc, hc * 128:(hc + 1) * 128], pT[:, h * 2 + mc, :], start=(mc == 0), stop=(mc == 1)), [V, pT], [po])
                        S.act(lambda e, po=po, hc=hc: e.copy(oT[:, hc, :], po[:]), [po], [oT])
                return f
            st.append(d4(0)); st.append(d4(4))
            return st

        def back(blk):
            oT = oTR[blk % 2]
            st = []
            for tt in range(4):
                t0 = blk * 512 + tt * 128
                ti = cn["ti"]; cn["ti"] += 1
                s_ = s2[ti % 2]; x2_ = x2[ti % 2]; ac = accs[ti % 2]; xb = x2b[ti % 2]; xr_ = xr[ti % 2]

                def b0(tt=tt, t0=t0, s_=s_, x2_=x2_, ac=ac, xb=xb, xr_=xr_):
                    S.dma("sp", lambda q: q.dma_start(out=xr_[:], in_=x1_d[t0:t0 + 128, :]), reads=[dbuf("x1", t0)], writes=[xr_], owner=xr_)
                    for half in range(2):
                        px = bank[half]
                        for k in range(8):
                            S.pe(lambda e, px=px, k=k, half=half: e.matmul(px[:], oT[:, k, tt * 128:(tt + 1) * 128], wo[:, k, half * 512:(half + 1) * 512], start=(k == 0), stop=(k == 7)), [oT, wo], [px])
                        S.dve(lambda e, px=px, half=half: e.scalar_tensor_tensor(s_[:, half * 512:(half + 1) * 512], xr_[:, half * 512:(half + 1) * 512], ALPHA, px[:], op0=ALU.mult, op1=ALU.add), [px, xr_], [s_])
                    st_, mv, rs = sts[cn["si"] % 2]; cn["si"] += 1
                    emit_ln_stats(S, s_, s_.t, st_, mv, rs)
                    S.dve(lambda e: e.tensor_scalar(x2_[:], s_[:], mv[:, 0:1], rs[:], op0=ALU.subtract, op1=ALU.mult), [s_, mv, rs], [x2_])
                    S.dve(lambda e: e.tensor_tensor(x2_[:], x2_[:], XG[:], op=ALU.mult), [x2_, XG], [x2_])
                    S.dve(lambda e: e.tensor_tensor(x2_[:], x2_[:], XB[:], op=ALU.add), [x2_, XB], [x2_])
                    S.act(lambda e: e.mul(ac[:], x2_[:], ALPHA), [x2_], [ac])
                    S.act(lambda e: e.copy(xb[:], x2_[:]), [x2_], [xb])
                    S.dma("pool", lambda q: q.dma_start(out=acc_d[t0:t0 + 128, :], in_=ac[:]), reads=[ac], writes=[dbuf("acc", t0)], owner=ac)
                    S.dma("pool", lambda q: q.dma_start(out=x2b_d[t0:t0 + 128, :], in_=xb[:]), reads=[xb], writes=[dbuf("x2b", t0)], owner=xb)
                st.append(b0)

                def b1(tt=tt, x2_=x2_):
                    for half in range(2):
                        pt = bank[2 + half]
                        for j in range(4):
                            dk = half * 4 + j
                            S.pe(lambda e, pt=pt, j=j, dk=dk: e.transpose(pt[:, j * 128:(j + 1) * 128], x2_[:, dk * 128:(dk + 1) * 128], ident[:]), [x2_, ident], [pt])
                        S.act(lambda e, pt=pt, half=half: e.copy(x2T[:, half * 4:half * 4 + 4, tt * 128:(tt + 1) * 128], pt[:].rearrange("p (a b) -> p a b", a=4)), [pt], [x2T])
                st.append(b1)

            def b2():
                pl = bank[6]
                for k in range(8):
                    S.pe(lambda e, k=k: e.matmul(pl[0:NE, :], wr[:, k, :], x2T[:, k, :], start=(k == 0), stop=(k == 7)), [wr, x2T], [pl])
                S.act(lambda e: e.activation(ex[:], pl[0:NE, :], AF.Exp), [pl], [ex])
                S.pe(lambda e: e.matmul(pl[0:NE, :], ones[0:NE, 0:NE], ex[:], start=True, stop=True), [ones, ex], [pl])
                S.dve(lambda e: e.reciprocal(rsum[:], pl[0:NE, :]), [pl], [rsum])
                S.dve(lambda e: e.tensor_tensor(affT[:, blk * 512:(blk + 1) * 512], ex[:], rsum[:], op=ALU.mult), [ex, rsum], [affT])
            st.append(b2)
            return st

        nblk = L // 512
        for f_ in front(0):
            f_()
        for blk in range(nblk):
            bs = back(blk)
            fs = front(blk + 1) if blk + 1 < nblk else []
            n = max(len(bs), len(fs))
            bi = 0; fi = 0
            for k in range(n):
                while bi < len(bs) and bi * n <= k * len(bs):
                    bs[bi](); bi += 1
                while fi < len(fs) and fi * n <= k * len(fs):
                    fs[fi](); fi += 1
            while bi < len(bs):
                bs[bi](); bi += 1
            while fi < len(fs):
                fs[fi](); fi += 1
        S.dma("sp", lambda q: q.dma_start(out=env["affT_d"][:, :], in_=affT[:]), reads=[affT], writes=[dbuf("affT", 0)], owner=affT)


def phase_EF(S, nc, env):
    affT = env["affT"]; ident = env["ident"]; identb = env["identb"]
    x2b_d = env["x2b_d"]; acc_d = env["acc_d"]
    with Phase(S, "EF") as PX:
        idxT = PX.sb("idxT", [128, NE, 8], U32)
        gate = PX.sb("gate", [128, NE, 8], F32)
        phase_E(S, nc, env, idxT, gate)
        if STOP_AFTER == "E":
            return
        phase_F(S, nc, env, idxT, gate)


def phase_E(S, nc, env, idxT, gate):
    affT = env["affT"]; ident = env["ident"]
    with Phase(S, "E") as P:
        bank = P.ring("bk", [128, 512], F32, 6, ps=True)
        a128 = P.sb("a128", [128, 1024], F32)
        S.dma("sp", lambda q: q.dma_start(out=a128[:], in_=env["affT_d"].rearrange("e (s t) -> (e s) t", s=8)), writes=[a128], owner=a128)
        blk8 = P.sb("blk8", [128, 128], F32)
        S.dma("sp", lambda q: q.dma_start(out=blk8[:], in_=env["c_blk8"][:, :]), writes=[blk8], owner=blk8)
        jk8 = P.sb("jk8", [128, 1024], BF16)
        lo8 = P.sb("lo8", [128, 2], F32); hi8 = P.sb("hi8", [128, 2], F32); mid8 = P.sb("mid8", [128, 2], F32)
        cnt8 = P.sb("cnt8", [128, 2], F32); ge8 = P.sb("ge8", [128, 2], F32); d8 = P.sb("d8", [128, 2], F32)
        lo = P.sb("lo", [NE, 1], F32)
        msk = P.sb("msk", [NE, L], F32); cum = P.sb("cum", [NE, L], F32)
        one16 = P.sb("one16", [NE, L], BF16)
        S.pool(lambda e: e.memset(one16[:], 1.0), [], [one16])
        S.dve(lambda e: e.memset(lo8[:], 0.0), [], [lo8])
        S.dve(lambda e: e.memset(hi8[:], 1.0), [], [hi8])
        pcn = bank[5]
        for it in range(34):
            S.dve(lambda e: e.tensor_tensor(mid8[:], lo8[:], hi8[:], op=ALU.add), [lo8, hi8], [mid8])
            S.dve(lambda e: e.tensor_scalar(mid8[:], mid8[:], 0.5, None, op0=ALU.mult), [mid8], [mid8])
            S.dve(lambda e: e.tensor_scalar(jk8[:], a128[:], mid8[:, 0:1], None, op0=ALU.is_ge, op1=ALU.add, accum_out=cnt8[:, 0:1]), [a128, mid8], [jk8, cnt8])
            S.dve(lambda e: e.tensor_copy(cnt8[:, 1:2], cnt8[:, 0:1]), [cnt8], [cnt8])
            S.pe(lambda e: e.matmul(pcn[:, 0:2], blk8[:], cnt8[:], start=True, stop=True), [blk8, cnt8], [pcn])
            S.dve(lambda e: e.tensor_scalar(ge8[:], pcn[:, 0:2], CAP - 0.5, None, op0=ALU.is_ge), [pcn], [ge8])
            S.dve(lambda e: e.tensor_tensor(d8[:], mid8[:], lo8[:], op=ALU.subtract), [mid8, lo8], [d8])
            S.dve(lambda e: e.scalar_tensor_tensor(lo8[:], d8[:], ge8[:, 0:1], lo8[:], op0=ALU.mult, op1=ALU.add), [d8, ge8, lo8], [lo8])
            S.dve(lambda e: e.tensor_tensor(d8[:], hi8[:], mid8[:], op=ALU.subtract), [hi8, mid8], [d8])
            S.dve(lambda e: e.scalar_tensor_tensor(hi8[:], d8[:], ge8[:, 0:1], mid8[:], op0=ALU.mult, op1=ALU.add), [d8, ge8, mid8], [hi8])
        lrow = P.sb("lrow", [2, 128], F32)
        S.pe(lambda e: e.transpose(pcn[0:2, 0:128], lo8[:], ident[:]), [lo8, ident], [pcn])
        S.dve(lambda e: e.tensor_copy(lrow[:], pcn[0:2, 0:128]), [pcn], [lrow])
        S.pe(lambda e: e.transpose(pcn[0:NE, 256:257], lrow[0:1, 0:128:8], ident[0:1, 0:1]), [lrow, ident], [pcn])
        S.dve(lambda e: e.tensor_copy(lo[:], pcn[0:NE, 256:257]), [pcn], [lo])
        S.dve(lambda e: e.tensor_scalar(msk[:], affT[:], lo[:, 0:1], None, op0=ALU.is_ge), [affT, lo], [msk])
        S.dve(lambda e: e.tensor_tensor_scan(cum[:], one16[:], msk[:], 0.0, op0=ALU.mult, op1=ALU.add), [one16, msk], [cum])
        S.dve(lambda e: e.tensor_tensor(cum[:], cum[:], msk[:], op=ALU.mult), [cum, msk], [cum])
        S.dve(lambda e: e.tensor_scalar(cum[:], cum[:], -1.0, None, op0=ALU.add), [cum], [cum])
        keyT = P.sb("keyT", [128, NT, NE], F32); afT = P.sb("afT", [128, NT, NE], F32)
        for src, dst in ((cum, keyT), (affT, afT)):
            for hb in range(2):
                pb = bank[hb]
                for j in range(32):
                    tl = hb * 32 + j
                    S.pe(lambda e, pb=pb, j=j, tl=tl, src=src: e.transpose(pb[:, j * NE:(j + 1) * NE], src[:, tl * 128:(tl + 1) * 128], ident[0:NE, 0:NE]), [src, ident], [pb])
                S.act(lambda e, pb=pb, hb=hb, dst=dst: e.copy(dst[:, hb * 32:(hb + 1) * 32, :].rearrange("p a b -> p (a b)"), pb[:]), [pb], [dst])
        vals = P.sb("vals", [128, NT, NE, 5], BF16)
        tidx = P.sb("tidx", [128, NT, 2], F32)
        S.dma("sp", lambda q: q.dma_start(out=tidx[:], in_=env["c_tidx"][:, :, :]), writes=[tidx], owner=tidx)
        for e_ in range(NE):
            S.dve(lambda e, e_=e_: e.tensor_copy(vals[:, :, e_, 0:2], tidx[:]), [tidx], [vals])
        r1 = P.sb("r1", [128, NT, NE], F32); hb16 = P.sb("hb16", [128, NT, NE], BF16)
        S.dve(lambda e: e.tensor_copy(hb16[:], afT[:]), [afT], [hb16])
        S.dve(lambda e: e.tensor_copy(vals[:, :, :, 2], hb16[:]), [hb16], [vals])
        S.dve(lambda e: e.tensor_tensor(r1[:], afT[:], hb16[:], op=ALU.subtract), [afT, hb16], [r1])
        S.dve(lambda e: e.tensor_copy(hb16[:], r1[:]), [r1], [hb16])
        S.dve(lambda e: e.tensor_copy(vals[:, :, :, 3], hb16[:]), [hb16], [vals])
        S.dve(lambda e: e.tensor_tensor(r1[:], r1[:], hb16[:], op=ALU.subtract), [r1, hb16], [r1])
        S.dve(lambda e: e.tensor_copy(vals[:, :, :, 4], r1[:]), [r1], [vals])
        iota32 = P.sb("iota32", [128, CAP], F32)
        S.dma("sp", lambda q: q.dma_start(out=iota32[:], in_=env["c_iota"][:, :]), writes=[iota32], owner=iota32)
        iota = P.sb("iota", [128, CAP], mybir.dt.float16)
        S.dve(lambda e: e.tensor_copy(iota[:], iota32[:]), [iota32], [iota])
        zl = P.sb("zl", [128, 128], BF16); zr = P.sb("zr", [128, 512], BF16)
        S.dve(lambda e: e.memset(zl[:], 0.0), [], [zl])
        S.dve(lambda e: e.memset(zr[:], 0.0), [], [zr])
        accA = bank[2]; accB = bank[3]
        for a_ in (accA, accB):
            S.pe(lambda e, a_=a_: e.matmul(a_[:], zl[:], zr[:], start=True, stop=False, skip_group_check=True), [zl, zr], [a_])
        O = P.ring("O", [128, CAP], BF16, 3)
        oi = 0
        for tl in range(NT):
            for e_ in range(NE):
                o_ = O[oi % 3]; oi += 1
                S.dve(lambda e, o_=o_, tl=tl, e_=e_: e.tensor_scalar(o_[:], iota[:], keyT[:, tl, e_:e_ + 1], None, op0=ALU.is_equal), [iota, keyT], [o_])
                a_ = accA if e_ < 8 else accB
                for sc in range(8):
                    col = ((e_ % 8) * 8 + sc) * 5
                    S.pe(lambda e, a_=a_, o_=o_, sc=sc, col=col, tl=tl, e_=e_: e.matmul(a_[:, col:col + 5], o_[:, sc * 128:(sc + 1) * 128], vals[:, tl, e_, :], start=False, stop=(tl == NT - 1), skip_group_check=True), [o_, vals], [a_])
        res = P.sb("res", [128, NE, 8, 5], F32)
        S.act(lambda e: e.copy(res[:, 0:8, :, :].rearrange("p a b c -> p (a b c)"), accA[:, 0:320]), [accA], [res])
        S.act(lambda e: e.copy(res[:, 8:16, :, :].rearrange("p a b c -> p (a b c)"), accB[:, 0:320]), [accB], [res])
        idf = P.sb("idf", [128, NE, 8], F32)
        S.dve(lambda e: e.scalar_tensor_tensor(idf[:], res[:, :, :, 0], 128.0, res[:, :, :, 1], op0=ALU.mult, op1=ALU.add), [res], [idf])
        S.dve(lambda e: e.tensor_copy(idxT[:], idf[:]), [idf], [idxT])
        S.dve(lambda e: e.tensor_tensor(gate[:], res[:, :, :, 2], res[:, :, :, 3], op=ALU.add), [res], [gate])
        S.dve(lambda e: e.tensor_tensor(gate[:], gate[:], res[:, :, :, 4], op=ALU.add), [gate, res], [gate])


def phase_F(S, nc, env, idxT, gate):
    identb = env["identb"]; x2b_d = env["x2b_d"]; acc_d = env["acc_d"]
    w_gate = env["w_gate"]; w_up = env["w_up"]; w_down = env["w_down"]
    accbuf = env["dbuf"]("accsc", 0)
    FG = 256
    NFG = DFF // FG
    with Phase(S, "F") as P:
        xg = P.sb("xg", [128, 8, D], BF16)
        xgT = P.ring("xgT", [128, 8, CAP], BF16, 2)
        hT = P.sb("hT", [128, 16, CAP], BF16)
        Wd = P.sb("Wd", [128, 16, D], BF16)
        Wg = P.ring("Wg", [128, 8, FG], BF16, 2); Wu = P.ring("Wu", [128, 8, FG], BF16, 2)
        stg = P.ring("stg", [128, 8 * FG], F32, 3)
        yo = P.ring("yo", [128, D], F32, 2)
        sg = P.ring("sg", [128, 512], F32, 2)
        bank = P.ring("bk", [128, 512], F32, 7, ps=True)
        ptr = P.ps("ptr", [128, 8, 128], BF16)
        sc_ = [0]

        def wload(dst, dst_ap, src_ap, a, b):
            st = stg[sc_[0] % 3]; sc_[0] += 1
            S.dma("sp", lambda q: q.dma_start(out=st[:].rearrange("p (a b) -> p a b", a=a), in_=src_ap), writes=[st], owner=st)
            S.act(lambda e: e.copy(dst_ap, st[:].rearrange("p (a b) -> p a b", a=a)), [st], [dst])

        def load_group(e_, fg):
            wg_ = Wg[fg % 2]; wu_ = Wu[fg % 2]
            wload(wg_, wg_[:], w_gate[e_, :, fg * FG:(fg + 1) * FG].rearrange("(k p) f -> p k f", p=128), 8, FG)
            wload(wu_, wu_[:], w_up[e_, :, fg * FG:(fg + 1) * FG].rearrange("(k p) f -> p k f", p=128), 8, FG)
            wload(Wd, Wd[:, fg * 2:fg * 2 + 2, :], w_down[e_, fg * FG:(fg + 1) * FG, :].rearrange("(k p) d -> p k d", p=128), 2, D)

        def gather(e_):
            for sc in range(8):
                S.dma("pool", lambda q, sc=sc: q.indirect_dma_start(out=xg[:, sc, :], out_offset=None, in_=x2b_d[:, :], in_offset=bass.IndirectOffsetOnAxis(ap=idxT[:, e_, sc:sc + 1], axis=0)), reads=[idxT], writes=[xg], owner=xg)

        def transposes(e_):
            xt_ = xgT[e_ % 2]
            for sc in range(8):
                for dk in range(8):
                    S.pe(lambda e, sc=sc, dk=dk: e.transpose(ptr[:, dk, :], xg[:, sc, dk * 128:(dk + 1) * 128], identb[:]), [xg, identb], [ptr])
                S.act(lambda e, sc=sc: e.copy(xt_[:, :, sc * 128:(sc + 1) * 128], ptr[:]), [ptr], [xt_])

        yi = 0; pi = 0
        gather(0)
        transposes(0)
        load_group(0, 0)
        for e_ in range(NE):
            xt_ = xgT[e_ % 2]
            if e_ + 1 < NE:
                gather(e_ + 1)
            for fg in range(NFG):
                wg_ = Wg[fg % 2]; wu_ = Wu[fg % 2]
                if fg + 1 < NFG:
                    load_group(e_, fg + 1)
                for fk in range(FG // 128):
                    fkg = fg * (FG // 128) + fk
                    for sh in range(2):
                        pg = bank[(pi % 2) * 2]; pu = bank[(pi % 2) * 2 + 1]; s_ = sg[pi % 2]; pi += 1
                        for dk in range(8):
                            S.pe(lambda e, pg=pg, dk=dk, fk=fk, sh=sh, wg_=wg_: e.matmul(pg[:], wg_[:, dk, fk * 128:(fk + 1) * 128], xt_[:, dk, sh * 512:(sh + 1) * 512], start=(dk == 0), stop=(dk == 7)), [wg_, xt_], [pg])
                        for dk in range(8):
                            S.pe(lambda e, pu=pu, dk=dk, fk=fk, sh=sh, wu_=wu_: e.matmul(pu[:], wu_[:, dk, fk * 128:(fk + 1) * 128], xt_[:, dk, sh * 512:(sh + 1) * 512], start=(dk == 0), stop=(dk == 7)), [wu_, xt_], [pu])
                        S.act(lambda e, s_=s_, pg=pg: e.activation(s_[:], pg[:], AF.Silu), [pg], [s_])
                        S.dve(lambda e, s_=s_, pu=pu, fkg=fkg, sh=sh: e.tensor_tensor(hT[:, fkg, sh * 512:(sh + 1) * 512], s_[:], pu[:], op=ALU.mult), [s_, pu], [hT])
            for sc in range(8):
                y_ = yo[yi % 2]; yi += 1
                for dh in range(2):
                    py = bank[4 + dh]
                    for fk in range(16):
                        S.pe(lambda e, py=py, fk=fk, sc=sc, dh=dh: e.matmul(py[:], hT[:, fk, sc * 128:(sc + 1) * 128], Wd[:, fk, dh * 512:(dh + 1) * 512], start=(fk == 0), stop=(fk == 15)), [hT, Wd], [py])
                    S.dve(lambda e, py=py, y_=y_, dh=dh, sc=sc: e.tensor_scalar(y_[:, dh * 512:(dh + 1) * 512], py[:], gate[:, e_, sc:sc + 1], None, op0=ALU.mult), [py, gate], [y_])
                S.dma("pool", lambda q, y_=y_, sc=sc: q.indirect_dma_start(out=acc_d[:, :], out_offset=bass.IndirectOffsetOnAxis(ap=idxT[:, e_, sc:sc + 1], axis=0), in_=y_[:], in_offset=None, compute_op=ALU.add), reads=[y_, idxT], writes=[accbuf], owner=y_)
            if e_ + 1 < NE:
                transposes(e_ + 1)
                load_group(e_ + 1, 0)


def phase_G(S, nc, env):
    acc_d = env["acc_d"]; out_d = env["out_d"]
    with Phase(S, "Z") as P:
        OG = row_bcast(S, P, "OG", env["ln_moe_g"]); OB = row_bcast(S, P, "OB", env["ln_moe_b"])
        at = P.ring("at", [128, D], F32, 4); ot = P.ring("ot", [128, D], F32, 4)
        sts = [ln_stats(S, P, None) for _ in range(4)]
        for tl in range(NT):
            a_ = at[tl % 4]; o_ = ot[tl % 4]; st, mv, rs = sts[tl % 4]
            S.dma("sp", lambda q, a_=a_, tl=tl: q.dma_start(out=a_[:], in_=acc_d[tl * 128:(tl + 1) * 128, :]), writes=[a_], owner=a_)
            emit_ln_stats(S, a_, a_.t, st, mv, rs)
            S.dve(lambda e, o_=o_, a_=a_, mv=mv, rs=rs: e.tensor_scalar(o_[:], a_[:], mv[:, 0:1], rs[:], op0=ALU.subtract, op1=ALU.mult), [a_, mv, rs], [o_])
            S.dve(lambda e, o_=o_: e.tensor_tensor(o_[:], o_[:], OG[:], op=ALU.mult), [o_, OG], [o_])
            S.pool(lambda e, o_=o_: e.tensor_tensor(o_[:], o_[:], OB[:], op=ALU.add), [o_, OB], [o_])
            S.dma("sp", lambda q, o_=o_, tl=tl: q.dma_start(out=out_d[tl * 128:(tl + 1) * 128, :], in_=o_[:]), reads=[o_], writes=[env["dbuf"]("out", tl)], owner=o_)


def host_consts():
    a = np.arange(128, dtype=np.float64)
    th = 2.0 * np.pi * np.outer(a, a) / 128.0
    Fm = np.concatenate([np.cos(th), -np.sin(th)], axis=1).astype(np.float32)
    th2 = 2.0 * np.pi * np.outer(a, a) / NFFT
    Tm = np.concatenate([np.cos(th2), -np.sin(th2)], axis=1).astype(np.float32)
    n = np.arange(L, dtype=np.float32)
    t = n / np.float32(L - 1)
    bands = 16
    f = np.linspace(1e-4, bands - 1, bands, dtype=np.float32)
    ang = (np.float32(2.0 * math.pi) * n / np.float32(L))[:, None] * f[None, :]
    feat = np.concatenate([t[:, None], np.cos(ang), -np.sin(ang)], axis=-1).astype(np.float32)
    featTS = np.zeros((NFFT, 33), np.float32)
    featTS[:L] = feat
    featTS[L + 1:] = feat[1:][::-1]
    tts = np.zeros(NFFT, np.float32)
    tts[:L] = t
    tts[L + 1:] = t[1:][::-1]
    tidx = np.zeros((128, NT, 2), np.float32)
    tidx[:, :, 0] = np.arange(NT, dtype=np.float32)[None, :]
    tidx[:, :, 1] = np.arange(128, dtype=np.float32)[:, None]
    return {
        "c_F": Fm, "c_T": Tm, "c_featT": np.ascontiguousarray(featTS.T), "c_tts": tts.reshape(128, 128).copy(),
        "c_ident": np.eye(128, dtype=np.float32),
        "c_iota": np.tile(np.arange(CAP, dtype=np.float32)[None, :], (128, 1)),
        "c_tidx": tidx,
        "c_blk8": np.kron(np.eye(16, dtype=np.float32), np.ones((8, 8), np.float32)),
    }


def make_in_maps(inputs, ncores=8):
    c = host_consts()
    f = lambda k: np.ascontiguousarray(np.asarray(inputs[k], dtype=np.float32))
    shared = {
        "ln_in_g": f("ln_in_g").reshape(D, 1), "ln_in_b": f("ln_in_b").reshape(D, 1),
        "ln_in_g_row": f("ln_in_g").reshape(1, D), "ln_in_b_row": f("ln_in_b").reshape(1, D),
        "w_in": f("w_in")[0], "b_gate": f("b_gate")[0].reshape(2 * D, 1),
        "conf_dw_w": f("conf_dw_w")[0], "conf_dw_b": f("conf_dw_b")[0].reshape(CW, 1),
        "conf_ln_g": f("conf_ln_g")[0].reshape(CW, 1), "conf_ln_b": f("conf_ln_b")[0].reshape(CW, 1),
        "conf_w_out": f("conf_w_out")[0],
        "hy_short_w": f("hy_short_w")[0], "hy_short_b": f("hy_short_b")[0].reshape(3 * HW_, 1),
        "hy_ffn_w1": f("hy_ffn_w1")[0], "hy_ffn_b1": f("hy_ffn_b1")[0].reshape(64, 1), "hy_freq1": f("hy_freq1")[0].reshape(64, 1),
        "hy_ffn_w2": f("hy_ffn_w2")[0], "hy_ffn_b2": f("hy_ffn_b2")[0].reshape(64, 1), "hy_freq2": f("hy_freq2")[0].reshape(64, 1),
        "hy_ffn_w3": f("hy_ffn_w3")[0], "hy_skip": f("hy_skip")[0].reshape(1, 2 * HW_),
        "hy_w_out": f("hy_w_out")[0], "w_mix_out": f("w_mix_out")[0],
        "ln_mix_g": f("ln_mix_g")[0].reshape(1, D), "ln_mix_b": f("ln_mix_b")[0].reshape(1, D),
        "xa_wq": f("xa_wq")[0], "xa_wk": f("xa_wk")[0], "xa_wv": f("xa_wv")[0], "xa_wo": f("xa_wo")[0],
        "ln_xa_g": f("ln_xa_g")[0].reshape(1, D), "ln_xa_b": f("ln_xa_b")[0].reshape(1, D),
        "moe_w_router": f("moe_w_router")[0],
        "moe_w_gate": f("moe_w_gate")[0], "moe_w_up": f("moe_w_up")[0], "moe_w_down": f("moe_w_down")[0],
        "ln_moe_g": f("ln_moe_g")[0].reshape(1, D), "ln_moe_b": f("ln_moe_b")[0].reshape(1, D),
    }
    shared.update(c)
    x = f("x"); mem = f("mem")
    maps = []
    for r in range(ncores):
        m = dict(shared)
        m["x"] = x[r % 4]
        m["mem"] = mem[r % 4]
        maps.append(m)
    return maps


def kernel(**inputs):
    nc = build_nc()
    maps = make_in_maps(inputs)
    res = run_bass_kernel_spmd(nc, maps, core_ids=list(range(8)))
    out = np.stack([np.asarray(res.results[r]["out"], dtype=np.float32) for r in range(4)], axis=0)
    return out
```

```python
import math
import numpy as np
import concourse.bass as bass
import concourse.mybir as mybir
from concourse.bass_utils import run_bass_kernel_spmd
from contextlib import ExitStack

F32 = mybir.dt.float32
BF16 = mybir.dt.bfloat16
U32 = mybir.dt.uint32
I32 = mybir.dt.int32
AF = mybir.ActivationFunctionType
ALU = mybir.AluOpType

D = 1024
L = 8192
NT = L // 128
NMEM = 256
CW = 512
HW_ = 512
COLS = 4608
NE = 16
CAP = 1024
DFF = 2048
EPS = 1e-5
ALPHA = 2.0 ** 0.25
NFFT = 16384
GC = 16
SAME_ENGINE_SYNC = True
STOP_AFTER = None
DEBUG_OUT = ()


class Buf:
    __slots__ = ("name", "w", "r", "dsem", "dcnt")

    def __init__(self, name):
        self.name = name
        self.w = None
        self.r = {}
        self.dsem = None
        self.dcnt = 0


class Tile(Buf):
    __slots__ = ("t",)

    def __init__(self, name, t):
        Buf.__init__(self, name)
        self.t = t

    def __getitem__(self, k):
        return self.t[k]


class Sched:
    ROT = 30000

    def __init__(self, nc, es):
        self.nc = nc
        self.es = es
        self.E = {"pe": nc.tensor, "act": nc.scalar, "dve": nc.vector, "pool": nc.gpsimd, "sp": nc.sync}
        self.csem = {}
        self.ccnt = {}
        self.waited = {k: {} for k in self.E}
        self.sems = []
        self.free_dsems = []
        self.latest = {}
        self.same_engine_sync = SAME_ENGINE_SYNC
        self.ninstr = {k: 0 for k in self.E}
        for k in ("pe", "act", "dve", "pool"):
            self._new_csem(k)

    def _alloc_sem(self, name):
        s = self.es.enter_context(self.nc.semaphore(name))
        self.sems.append(s)
        return s

    def _new_csem(self, k):
        self.csem[k] = self._alloc_sem("c_%s_%d" % (k, len(self.sems)))
        self.ccnt[k] = 0

    def _wait(self, eng, ev):
        if ev is None:
            return
        sem, val = ev
        if (not self.same_engine_sync) and eng in self.csem and sem is self.csem[eng]:
            return
        w = self.waited[eng]
        key = id(sem)
        if w.get(key, 0) >= val:
            return
        self.E[eng].wait_ge(sem, val)
        w[key] = val

    def _deps(self, eng, reads, writes):
        for b in reads:
            if b.w is not None:
                if eng == "pe" and b.w[0] is self.csem.get("pe") and False:
                    continue
                self._wait(eng, b.w)
        for b in writes:
            if b.w is not None:
                if eng == "pe" and b.w[0] is self.csem["pe"]:
                    pass
                else:
                    self._wait(eng, b.w)
            for sid, ev in b.r.items():
                if eng == "pe" and ev[0] is self.csem["pe"]:
                    continue
                self._wait(eng, ev)

    def _mark(self, ev, reads, writes):
        self.latest[id(ev[0])] = ev
        for b in reads:
            b.r[id(ev[0])] = ev
        for b in writes:
            b.w = ev
            b.r = {}

    def op(self, eng, fn, reads=(), writes=()):
        if self.ccnt[eng] >= self.ROT:
            self._new_csem(eng)
        self._deps(eng, reads, writes)
        ins = fn(self.E[eng])
        self.ccnt[eng] += 1
        sem = self.csem[eng]
        ins.then_inc(sem, 1)
        self.ninstr[eng] += 1
        self._mark((sem, self.ccnt[eng]), reads, writes)

    def pe(self, fn, r=(), w=()):
        self.op("pe", fn, r, w)

    def act(self, fn, r=(), w=()):
        self.op("act", fn, r, w)

    def dve(self, fn, r=(), w=()):
        self.op("dve", fn, r, w)

    def pool(self, fn, r=(), w=()):
        self.op("pool", fn, r, w)

    def dma(self, q, fn, reads=(), writes=(), owner=None):
        self._deps(q, reads, writes)
        if owner.dsem is None:
            if self.free_dsems:
                owner.dsem, owner.dcnt = self.free_dsems.pop()
            else:
                owner.dsem = self._alloc_sem("d_%s_%d" % (owner.name, len(self.sems)))
                owner.dcnt = 0
        ins = fn(self.E[q])
        owner.dcnt += 16
        ins.then_inc(owner.dsem, 16)
        self.ninstr[q] += 1
        self._mark((owner.dsem, owner.dcnt), reads, writes)

    def release(self, tiles):
        for t in tiles:
            if t.dsem is not None:
                self.free_dsems.append((t.dsem, t.dcnt))
                t.dsem = None

    def barrier(self):
        evs = list(self.latest.values())
        for eng in self.E:
            for ev in evs:
                self._wait(eng, ev)


class Phase:
    def __init__(self, S, name):
        self.S = S
        self.nc = S.nc
        self.name = name
        self.es = ExitStack()
        self.tiles = []
        self.n = 0

    def __enter__(self):
        self.es.__enter__()
        return self

    def __exit__(self, *a):
        self.S.barrier()
        self.S.release(self.tiles)
        return self.es.__exit__(*a)

    def sb(self, name, shape, dt):
        self.n += 1
        t = self.es.enter_context(self.nc.sbuf_tensor("%s_%s_%d" % (self.name, name, self.n), list(shape), dt))
        tl = Tile(name, t)
        self.tiles.append(tl)
        return tl

    def ps(self, name, shape, dt=F32):
        self.n += 1
        t = self.es.enter_context(self.nc.psum_tensor("%s_%s_%d" % (self.name, name, self.n), list(shape), dt))
        tl = Tile(name, t)
        self.tiles.append(tl)
        return tl

    def ring(self, name, shape, dt, n, ps=False):
        return [(self.ps if ps else self.sb)("%s%d" % (name, i), shape, dt) for i in range(n)]


def build_nc():
    nc = bass.Bass("TRN2", target_bir_lowering=False)
    dram = {}

    def din(name, shape, dt=F32):
        dram[name] = nc.dram_tensor(name, list(shape), dt, kind="ExternalInput").ap()
        return dram[name]

    def dscr(name, shape, dt):
        kind = "ExternalOutput" if name in DEBUG_OUT else "Internal"
        dram[name] = nc.dram_tensor(name, list(shape), dt, kind=kind).ap()
        return dram[name]

    x_d = din("x", [L, D])
    mem_d = din("mem", [NMEM, D])
    ln_in_g = din("ln_in_g", [D, 1]); ln_in_b = din("ln_in_b", [D, 1])
    w_in = din("w_in", [D, COLS])
    b_gate = din("b_gate", [2 * D, 1])
    conf_dw_w = din("conf_dw_w", [31, CW]); conf_dw_b = din("conf_dw_b", [CW, 1])
    conf_ln_g = din("conf_ln_g", [CW, 1]); conf_ln_b = din("conf_ln_b", [CW, 1])
    conf_w_out = din("conf_w_out", [CW, D])
    hy_short_w = din("hy_short_w", [3, 3 * HW_]); hy_short_b = din("hy_short_b", [3 * HW_, 1])
    hy_w1 = din("hy_ffn_w1", [33, 64]); hy_b1 = din("hy_ffn_b1", [64, 1]); hy_f1 = din("hy_freq1", [64, 1])
    hy_w2 = din("hy_ffn_w2", [64, 64]); hy_b2 = din("hy_ffn_b2", [64, 1]); hy_f2 = din("hy_freq2", [64, 1])
    hy_w3 = din("hy_ffn_w3", [64, 2048])
    hy_skip = din("hy_skip", [1, 2 * HW_])
    hy_w_out = din("hy_w_out", [HW_, D])
    w_mix = din("w_mix_out", [D, D])
    ln_mix_g = din("ln_mix_g", [1, D]); ln_mix_b = din("ln_mix_b", [1, D])
    xa_wq = din("xa_wq", [D, D]); xa_wk = din("xa_wk", [D, D]); xa_wv = din("xa_wv", [D, D]); xa_wo = din("xa_wo", [D, D])
    ln_xa_g = din("ln_xa_g", [1, D]); ln_xa_b = din("ln_xa_b", [1, D])
    w_router = din("moe_w_router", [D, NE])
    if STOP_AFTER in ("A", "A2", "H", "C", "C1", "E"):
        w_gate = w_up = w_down = None
    else:
        w_gate = din("moe_w_gate", [NE, D, DFF]); w_up = din("moe_w_up", [NE, D, DFF]); w_down = din("moe_w_down", [NE, DFF, D])
    ln_moe_g = din("ln_moe_g", [1, D]); ln_moe_b = din("ln_moe_b", [1, D])
    ln_in_g_row = din("ln_in_g_row", [1, D]); ln_in_b_row = din("ln_in_b_row", [1, D])
    c_F = din("c_F", [128, 256]); c_T = din("c_T", [128, 256])
    c_featT = din("c_featT", [33, NFFT]); c_tts = din("c_tts", [128, 128])
    c_ident = din("c_ident", [128, 128]); c_iota = din("c_iota", [128, CAP])
    c_tidx = din("c_tidx", [128, NT, 2])
    c_blk8 = din("c_blk8", [128, 128])
    out_d = nc.dram_tensor("out", [L, D], F32, kind="ExternalOutput").ap()

    uT_d = dscr("uT_d", [CW, L], BF16)
    hyT_d = dscr("hyT_d", [3 * HW_, L], BF16)
    gT_d = dscr("gT_d", [2 * D, L], BF16)
    ucT_d = dscr("ucT_d", [CW, L], F32)
    hycT_d = dscr("hycT_d", [3 * HW_, L], BF16)
    zT_d = dscr("zT_d", [HW_, L], BF16)
    x2b_d = dscr("x2b_d", [L, D], BF16)
    acc_d = dscr("acc_d", [L, D], F32)
    affT_d = dscr("affT_d", [NE, L], F32)
    dbg_d = dscr("dbg_d", [128, 4096], F32)
    x1_d = dscr("x1_d", [L, D], F32)
    xg_d = dscr("xg_d", [L, D], F32)

    deltas = np.abs(np.linspace(math.log(1e-2) / 1.5, math.log(1e-2) / 0.3, HW_, dtype=np.float32)).astype(np.float64)

    with ExitStack() as es:
        S = Sched(nc, es)
        DB = {k: Buf(k) for k in ("uT", "hyT", "gT", "ucT", "hycT", "zT", "x2b", "acc", "affT", "dbg")}
        dbt = {}

        def dbuf(name, i):
            k = (name, i)
            if k not in dbt:
                dbt[k] = Buf("%s_%s" % (name, i))
            return dbt[k]

        with Phase(S, "G") as G:
            ident = G.sb("ident", [128, 128], F32)
            identb = G.sb("identb", [128, 128], BF16)
            S.dma("sp", lambda q: q.dma_start(out=ident[:], in_=c_ident[:, :]), writes=[ident], owner=ident)
            S.dve(lambda e: e.tensor_copy(identb[:], ident[:]), [ident], [identb])
            ones = G.sb("ones", [128, 128], F32)
            S.dve(lambda e: e.memset(ones[:], 1.0), [], [ones])

            phase_A(S, nc, locals())
            if STOP_AFTER != "A":
                phase_A2(S, nc, locals())
            if STOP_AFTER not in ("A", "A2"):
                phase_H(S, nc, locals())
            if STOP_AFTER not in ("A", "A2", "H"):
                with Phase(S, "G2") as G2:
                    affT = G2.sb("affT", [NE, L], F32)
                    phase_CD(S, nc, locals())
                    if STOP_AFTER not in ("C", "C1"):
                        phase_EF(S, nc, locals())
            if STOP_AFTER not in ("A", "A2", "H", "C", "C1", "E", "F"):
                phase_G(S, nc, locals())
            S.barrier()
        print("instr counts", S.ninstr, "sems", len(S.sems))
    build_nc.in_names = [k for k in dram if k not in ("uT_d", "hyT_d", "gT_d", "ucT_d", "hycT_d", "zT_d", "x2b_d", "acc_d", "affT_d", "dbg_d", "x1_d", "xg_d", "out")]
    return nc


def load_cast(S, P, q_dst, src_ap, shape, name, stage=None, eng="act", q="sp"):
    dst_tile, dst_ap = q_dst
    st = stage if stage is not None else P.sb(name + "_st", shape, F32)
    S.dma(q, lambda q_: q_.dma_start(out=st[:], in_=src_ap), writes=[st], owner=st)
    if eng == "act":
        S.act(lambda e: e.copy(dst_ap, st[:]), [st], [dst_tile])
    elif eng == "pool":
        S.pool(lambda e: e.tensor_copy(dst_ap, st[:]), [st], [dst_tile])
    else:
        S.dve(lambda e: e.tensor_copy(dst_ap, st[:]), [st], [dst_tile])


def ln_stats(S, P, xt, rstd_name="rs"):
    st = P.sb("bnst", [128, 2, 6], F32)
    mv = P.sb("bnmv", [128, 2], F32)
    rs = P.sb(rstd_name, [128, 1], F32)
    return st, mv, rs


def emit_ln_stats(S, xt_tile, x_ap, st, mv, rs):
    for h in range(2):
        S.dve(lambda e, h=h: e.bn_stats(st[:, h, :], x_ap[:, h * 512:(h + 1) * 512]), [xt_tile], [st])
    S.dve(lambda e: e.bn_aggr(mv[:], st[:].rearrange("p a b -> p (a b)")), [st], [mv])
    S.act(lambda e: e.activation(rs[:], mv[:, 1:2], AF.Sqrt, bias=EPS_AP[0][:], scale=1.0), [mv, EPS_AP[1]], [rs])
    S.dve(lambda e: e.reciprocal(rs[:], rs[:]), [rs], [rs])


EPS_AP = [None, None]


def phase_A(S, nc, env):
    x_d = env["x_d"]; w_in = env["w_in"]; ident = env["ident"]
    uT_d = env["uT_d"]; hyT_d = env["hyT_d"]; gT_d = env["gT_d"]; dbuf = env["dbuf"]
    G = env["G"]
    epsT = G.sb("epsT", [128, 1], F32)
    S.dve(lambda e: e.memset(epsT[:], EPS), [], [epsT])
    EPS_AP[0] = epsT; EPS_AP[1] = epsT
    with Phase(S, "A") as P:
        wsb = P.sb("w_in", [128, 8, COLS], BF16)
        stg = P.ring("wst", [128, 1152], F32, 4)
        i = 0
        for dk in range(8):
            for cq in range(4):
                st = stg[i % 4]
                load_cast(S, P, (wsb, wsb[:, dk, cq * 1152:(cq + 1) * 1152]),
                          w_in[dk * 128:(dk + 1) * 128, cq * 1152:(cq + 1) * 1152], None, "w", stage=st,
                          eng=("act" if i % 2 == 0 else "dve"), q=("sp" if i % 2 == 0 else "pool"))
                i += 1
        gsc = P.sb("gsc", [128, 8], F32); gbi = P.sb("gbi", [128, 8], F32); bg = P.sb("bg", [128, 16], F32)
        S.dma("sp", lambda q: q.dma_start(out=gsc[:], in_=env["ln_in_g"].rearrange("(k p) o -> p (k o)", p=128), allow_slow_non_contiguous=True), writes=[gsc], owner=gsc)
        S.dma("sp", lambda q: q.dma_start(out=gbi[:], in_=env["ln_in_b"].rearrange("(k p) o -> p (k o)", p=128), allow_slow_non_contiguous=True), writes=[gbi], owner=gbi)
        S.dma("sp", lambda q: q.dma_start(out=bg[:], in_=env["b_gate"].rearrange("(k p) o -> p (k o)", p=128), allow_slow_non_contiguous=True), writes=[bg], owner=bg)
        xts = P.ring("xt", [128, D], F32, 8)
        AG = row_bcast(S, P, "AG", env["ln_in_g_row"], ALPHA); AB = row_bcast(S, P, "AB", env["ln_in_b_row"], ALPHA)
        xgs = P.ring("xgs", [128, D], F32, 2)
        xg_d = env["xg_d"]
        nhs = P.ring("nh", [128, D], F32, 5)
        sts = [ln_stats(S, P, None) for _ in range(2)]
        hT = P.ring("hT", [128, 8, 512], BF16, 2)
        pst = P.ring("pst", [128, 512], F32, 2, ps=True)
        pmm = P.ring("pmm", [128, 512], F32, 4, ps=True)
        sig = P.ring("sig", [128, 512], F32, 2)
        ob = P.ring("ob", [128, 512], BF16, 4)
        xi = 0; oi = 0; pi = 0
        for blk in range(L // 512):
            h = hT[blk % 2]
            nts = []
            if blk == 0:
                for tt in range(4):
                    S.dma("sp", lambda q, tt=tt: q.dma_start(out=xts[tt][:], in_=x_d[tt * 128:(tt + 1) * 128, :]), writes=[xts[tt]], owner=xts[tt])
            if blk + 1 < L // 512:
                for tt in range(4):
                    xn_ = xts[((blk + 1) * 4 + tt) % 8]; tn = (blk + 1) * 512 + tt * 128
                    S.dma("sp", lambda q, xn_=xn_, tn=tn: q.dma_start(out=xn_[:], in_=x_d[tn:tn + 128, :]), writes=[xn_], owner=xn_)
            for tt in range(4):
                t0 = blk * 512 + tt * 128
                xt = xts[xi % 8]; nh = nhs[xi % 5]; st, mv, rs = sts[xi % 2]; xi += 1
                emit_ln_stats(S, xt, xt.t, st, mv, rs)
                S.dve(lambda e, nh=nh, xt=xt, mv=mv, rs=rs: e.tensor_scalar(nh[:], xt[:], mv[:, 0:1], rs[:], op0=ALU.subtract, op1=ALU.mult), [xt, mv, rs], [nh])
                xg = xgs[xi % 2]
                S.dve(lambda e, xg=xg, nh=nh: e.tensor_tensor(xg[:], nh[:], AG[:], op=ALU.mult), [nh, AG], [xg])
                S.dve(lambda e, xg=xg: e.tensor_tensor(xg[:], xg[:], AB[:], op=ALU.add), [xg, AB], [xg])
                S.dma("pool", lambda q, xg=xg, t0=t0: q.dma_start(out=xg_d[t0:t0 + 128, :], in_=xg[:]), reads=[xg], writes=[dbuf("xg", t0)], owner=xg)
                nts.append(nh)
            for dk in range(8):
                pt = pst[dk % 2]
                for tt in range(4):
                    S.pe(lambda e, pt=pt, tt=tt, dk=dk: e.transpose(pt[:, tt * 128:(tt + 1) * 128], nts[tt][:, dk * 128:(dk + 1) * 128], ident[:]), [nts[tt], ident], [pt])
                S.act(lambda e, pt=pt, dk=dk: e.activation(h[:, dk, :], pt[:], AF.Identity, bias=gbi[:, dk:dk + 1], scale=gsc[:, dk:dk + 1]), [pt, gbi, gsc], [h])

            def proj(cc):
                nonlocal pi
                pm = pmm[pi % 4]; pi += 1
                for dk in range(8):
                    S.pe(lambda e, pm=pm, dk=dk, cc=cc: e.matmul(pm[:], wsb[:, dk, cc * 128:(cc + 1) * 128], h[:, dk, :], start=(dk == 0), stop=(dk == 7)), [wsb, h], [pm])
                return pm
            tsl = slice(blk * 512, (blk + 1) * 512)
            for cc in range(4):
                pa = proj(cc); pb = proj(cc + 4)
                sg = sig[cc % 2]; o = ob[oi % 4]; oi += 1
                S.act(lambda e, sg=sg, pb=pb: e.activation(sg[:], pb[:], AF.Sigmoid), [pb], [sg])
                S.dve(lambda e, o=o, pa=pa, sg=sg: e.tensor_tensor(o[:], pa[:], sg[:], op=ALU.mult), [pa, sg], [o])
                S.dma("pool", lambda q, o=o, cc=cc: q.dma_start(out=uT_d[cc * 128:(cc + 1) * 128, tsl], in_=o[:]), reads=[o], writes=[dbuf("uT", cc)], owner=o)
            for cc in range(8, 20):
                pm = proj(cc); o = ob[oi % 4]; oi += 1
                S.act(lambda e, o=o, pm=pm: e.copy(o[:], pm[:]), [pm], [o])
                S.dma("pool", lambda q, o=o, cc=cc: q.dma_start(out=hyT_d[(cc - 8) * 128:(cc - 7) * 128, tsl], in_=o[:]), reads=[o], writes=[dbuf("hyT", cc - 8)], owner=o)
            for cc in range(20, 36):
                pm = proj(cc); o = ob[oi % 4]; oi += 1
                S.act(lambda e, o=o, pm=pm, cc=cc: e.activation(o[:], pm[:], AF.Sigmoid, bias=bg[:, cc - 20:cc - 19], scale=1.0), [pm, bg], [o])
                S.dma("pool", lambda q, o=o, cc=cc: q.dma_start(out=gT_d[(cc - 20) * 128:(cc - 19) * 128, tsl], in_=o[:]), reads=[o], writes=[dbuf("gT", (cc - 20, blk))], owner=o)


def phase_A2(S, nc, env):
    identb = env["identb"]; dbuf = env["dbuf"]
    uT_d = env["uT_d"]; hyT_d = env["hyT_d"]; ucT_d = env["ucT_d"]; hycT_d = env["hycT_d"]
    with Phase(S, "A2") as P:
        rows = P.ring("row", [128, L + 32], BF16, 2)
        for r in rows:
            S.pool(lambda e, r=r: e.memset(r[:, 0:16], 0.0), [], [r])
            S.pool(lambda e, r=r: e.memset(r[:, L + 16:L + 32], 0.0), [], [r])
        wT = P.sb("wT", [128, 4, 31], F32); wT3 = P.sb("wT3", [128, 12, 3], F32)
        cb = P.sb("cb", [128, 4], F32); cb3 = P.sb("cb3", [128, 12], F32)
        for c in range(4):
            S.dma("sp", lambda q, c=c: q.dma_start(out=wT[:, c, :], in_=env["conf_dw_w"][:, c * 128:(c + 1) * 128].rearrange("k p -> p k"), allow_slow_non_contiguous=True), writes=[wT], owner=wT)
        for c in range(12):
            S.dma("sp", lambda q, c=c: q.dma_start(out=wT3[:, c, :], in_=env["hy_short_w"][:, c * 128:(c + 1) * 128].rearrange("k p -> p k"), allow_slow_non_contiguous=True), writes=[wT3], owner=wT3)
        S.dma("sp", lambda q: q.dma_start(out=cb[:], in_=env["conf_dw_b"].rearrange("(c p) o -> p (c o)", p=128), allow_slow_non_contiguous=True), writes=[cb], owner=cb)
        S.dma("sp", lambda q: q.dma_start(out=cb3[:], in_=env["hy_short_b"].rearrange("(c p) o -> p (c o)", p=128), allow_slow_non_contiguous=True), writes=[cb3], owner=cb3)
        dg = P.ring("dg", [128, 31, 128], BF16, 2)
        pc = P.ring("pc", [128, 512], F32, 3, ps=True)
        of = P.ring("of", [128, 512], F32, 3)
        obf = P.ring("obf", [128, 512], BF16, 3)
        pi = 0; ri = 0
        jobs = [("u", c) for c in range(4)] + [("h", c) for c in range(12)]
        for kind, c in jobs:
            row = rows[ri % 2]; dgt = dg[ri % 2]; ri += 1
            K = 31 if kind == "u" else 3
            pad = (K - 1) // 2
            src = uT_d if kind == "u" else hyT_d
            S.dma("sp", lambda q, row=row, src=src, c=c: q.dma_start(out=row[:, 16:16 + L], in_=src[c * 128:(c + 1) * 128, :]),
                  reads=[dbuf("uT" if kind == "u" else "hyT", c)], writes=[row], owner=row)
            wt = wT if kind == "u" else wT3
            for k in range(K):
                S.dve(lambda e, dgt=dgt, k=k, wt=wt, c=c: e.tensor_scalar(dgt[:, k, :], identb[:], wt[:, c, k:k + 1], None, op0=ALU.mult), [identb, wt], [dgt])
            for blk in range(L // 512):
                p = pc[pi % 3]
                for k in range(K):
                    off = 16 + blk * 512 + k - pad
                    S.pe(lambda e, p=p, k=k, off=off, dgt=dgt, row=row: e.matmul(p[:], dgt[:, k, :], row[:, off:off + 512], start=(k == 0), stop=(k == K - 1)), [dgt, row], [p])
                tsl = slice(blk * 512, (blk + 1) * 512)
                if kind == "u":
                    o = of[pi % 3]
                    S.act(lambda e, o=o, p=p, c=c: e.activation(o[:], p[:], AF.Identity, bias=cb[:, c:c + 1], scale=1.0), [p, cb], [o])
                    S.dma("sp", lambda q, o=o, c=c, tsl=tsl: q.dma_start(out=ucT_d[c * 128:(c + 1) * 128, tsl], in_=o[:]), reads=[o], writes=[dbuf("ucT", blk)], owner=o)
                else:
                    o = obf[pi % 3]
                    S.act(lambda e, o=o, p=p, c=c: e.activation(o[:], p[:], AF.Identity, bias=cb3[:, c:c + 1], scale=1.0), [p, cb3], [o])
                    S.dma("sp", lambda q, o=o, c=c, tsl=tsl: q.dma_start(out=hycT_d[c * 128:(c + 1) * 128, tsl], in_=o[:]), reads=[o], writes=[dbuf("hycT", c)], owner=o)
                pi += 1


def phase_H(S, nc, env):
    hycT_d = env["hycT_d"]; zT_d = env["zT_d"]; ones = env["ones"]; deltas = env["deltas"]
    c_F = env["c_F"]; c_T = env["c_T"]; c_featT = env["c_featT"]; c_tts = env["c_tts"]
    INVN = 1.0 / NFFT
    with Phase(S, "H") as P:
        Fst = P.sb("Fst", [128, 256], F32)
        Tst = P.sb("Tst", [128, 256], F32)
        S.dma("sp", lambda q: q.dma_start(out=Fst[:], in_=c_F[:, :]), writes=[Fst], owner=Fst)
        S.dma("sp", lambda q: q.dma_start(out=Tst[:], in_=c_T[:, :]), writes=[Tst], owner=Tst)
        Fb = P.sb("Fb", [128, 256], BF16); FA = P.sb("FA", [128, 256], BF16); FB_ = P.sb("FB", [128, 256], BF16)
        Fin = P.sb("Fin", [128, 128], BF16)
        S.dve(lambda e: e.tensor_copy(Fb[:], Fst[:]), [Fst], [Fb])
        S.dve(lambda e: e.tensor_copy(FA[:, 0:128], Fst[:, 0:128]), [Fst], [FA])
        S.dve(lambda e: e.tensor_scalar(FA[:, 128:256], Fst[:, 128:256], -1.0, None, op0=ALU.mult), [Fst], [FA])
        S.dve(lambda e: e.tensor_copy(FB_[:, 0:128], Fst[:, 128:256]), [Fst], [FB_])
        S.dve(lambda e: e.tensor_copy(FB_[:, 128:256], Fst[:, 0:128]), [Fst], [FB_])
        S.dve(lambda e: e.tensor_scalar(Fin[:], Fst[:, 128:256], -1.0, None, op0=ALU.mult), [Fst], [Fin])
        TT1 = P.sb("TT1", [128, 2, 256], F32); TT2 = P.sb("TT2", [128, 2, 256], F32)
        for i in range(2):
            for hh in range(2):
                S.dve(lambda e, i=i, hh=hh: e.tensor_copy(TT1[:, i, hh * 128:(hh + 1) * 128], Tst[:, 0:128]), [Tst], [TT1])
                S.dve(lambda e, i=i, hh=hh: e.tensor_copy(TT2[:, i, hh * 128:(hh + 1) * 128], Tst[:, 128:256]), [Tst], [TT2])
        NH = 66
        FbH = P.sb("FbH", [128, 2 * NH], BF16)
        S.dve(lambda e: e.tensor_copy(FbH[:, 0:NH], Fst[:, 0:NH]), [Fst], [FbH])
        S.dve(lambda e: e.tensor_copy(FbH[:, NH:2 * NH], Fst[:, 128:128 + NH]), [Fst], [FbH])
        TH1 = P.sb("TH1", [128, 2, 2 * NH], F32); TH2 = P.sb("TH2", [128, 2, 2 * NH], F32)
        TK1 = P.sb("TK1", [128, 2, 2 * NH], F32); TK2 = P.sb("TK2", [128, 2, 2 * NH], F32)
        for i in range(2):
            for hh in range(2):
                S.dve(lambda e, i=i, hh=hh: e.tensor_copy(TH1[:, i, hh * NH:(hh + 1) * NH], Tst[:, 0:NH]), [Tst], [TH1])
                S.dve(lambda e, i=i, hh=hh: e.tensor_copy(TH2[:, i, hh * NH:(hh + 1) * NH], Tst[:, 128:128 + NH]), [Tst], [TH2])
        for src_, dst_ in ((TH1, TK1), (TH2, TK2)):
            S.dve(lambda e, src_=src_, dst_=dst_: e.tensor_scalar(dst_[:], src_[:], 2.0, None, op0=ALU.mult), [src_], [dst_])
            for i in range(2):
                for hh in range(2):
                    b0 = hh * NH
                    S.dve(lambda e, src_=src_, dst_=dst_, i=i, b0=b0: e.tensor_copy(dst_[:, i, b0:b0 + 1], src_[:, i, b0:b0 + 1]), [src_], [dst_])
                    S.dve(lambda e, src_=src_, dst_=dst_, i=i, b0=b0: e.tensor_copy(dst_[:, i, b0 + 64:b0 + 65], src_[:, i, b0 + 64:b0 + 65]), [src_], [dst_])
                    S.dve(lambda e, dst_=dst_, i=i, b0=b0: e.memset(dst_[:, i, b0 + 65:b0 + 66], 0.0), [], [dst_])
        tts = P.sb("tts", [128, 128], F32)
        S.dma("sp", lambda q: q.dma_start(out=tts[:], in_=c_tts[:, :]), writes=[tts], owner=tts)
        e6 = P.sb("e6", [128, 1], F32)
        S.dve(lambda e: e.memset(e6[:], 1e-6), [], [e6])
        H2 = P.sb("H2", [128, NFFT], BF16)
        S.pool(lambda e: e.memset(H2[:], 0.0), [], [H2])
        w1 = P.sb("w1", [33, 64], F32); w2d = P.sb("w2d", [64, 128], F32)
        S.dma("sp", lambda q: q.dma_start(out=w1[:], in_=env["hy_w1"][:, :]), writes=[w1], owner=w1)
        S.dma("sp", lambda q: q.dma_start(out=w2d[:, 0:64], in_=env["hy_w2"][:, :]), writes=[w2d], owner=w2d)
        S.dma("sp", lambda q: q.dma_start(out=w2d[:, 64:128], in_=env["hy_w2"][:, :]), writes=[w2d], owner=w2d)
        fb = P.sb("fb", [128, 4], F32)
        S.dma("sp", lambda q: q.dma_start(out=fb[0:64, 0:1], in_=env["hy_f1"][:, :]), writes=[fb], owner=fb)
        S.dma("sp", lambda q: q.dma_start(out=fb[0:64, 1:2], in_=env["hy_b1"][:, :]), writes=[fb], owner=fb)
        for hh in range(2):
            S.dma("sp", lambda q, hh=hh: q.dma_start(out=fb[hh * 64:(hh + 1) * 64, 2:3], in_=env["hy_f2"][:, :]), writes=[fb], owner=fb)
            S.dma("sp", lambda q, hh=hh: q.dma_start(out=fb[hh * 64:(hh + 1) * 64, 3:4], in_=env["hy_b2"][:, :]), writes=[fb], owner=fb)
        S.dma("sp", lambda q: q.dma_start(out=fb[64:128, 0:1], in_=env["hy_f1"][:, :]), writes=[fb], owner=fb)
        S.dma("sp", lambda q: q.dma_start(out=fb[64:128, 1:2], in_=env["hy_b1"][:, :]), writes=[fb], owner=fb)
        sb_ = P.sb("sb", [128, 4], F32)
        S.dve(lambda e: e.tensor_scalar(sb_[:, 0:1], fb[:, 0:1], 1.0 / 3.0, None, op0=ALU.mult), [fb], [sb_])
        S.dve(lambda e: e.tensor_tensor(sb_[:, 1:2], fb[:, 0:1], fb[:, 1:2], op=ALU.mult), [fb], [sb_])
        S.dve(lambda e: e.tensor_scalar(sb_[:, 1:2], sb_[:, 1:2], 1.0 / 3.0, None, op0=ALU.mult), [sb_], [sb_])
        S.dve(lambda e: e.tensor_scalar(sb_[:, 2:3], fb[:, 2:3], 1.0 / 3.0, None, op0=ALU.mult), [fb], [sb_])
        S.dve(lambda e: e.tensor_tensor(sb_[:, 3:4], fb[:, 2:3], fb[:, 3:4], op=ALU.mult), [fb], [sb_])
        S.dve(lambda e: e.tensor_scalar(sb_[:, 3:4], sb_[:, 3:4], 1.0 / 3.0, None, op0=ALU.mult), [sb_], [sb_])
        bank = P.ring("bk", [128, 512], F32, 8, ps=True)
        PF = Phase(S, "HF"); PF.__enter__()
        fts = PF.ring("ft", [33, 512], F32, 2)
        s1 = PF.ring("s1", [128, 512], F32, 2); qq = PF.ring("qq", [128, 512], F32, 2); h1 = PF.ring("h1", [64, 512], F32, 2)
        for blk in range(NFFT // 512):
            ft = fts[blk % 2]; s = s1[blk % 2]; q_ = qq[blk % 2]; hh1 = h1[blk % 2]
            pb1 = bank[blk % 2]; pb2 = bank[2 + blk % 2]
            S.dma("sp", lambda q, ft=ft, blk=blk: q.dma_start(out=ft[:], in_=c_featT[:, blk * 512:(blk + 1) * 512]), writes=[ft], owner=ft)
            S.pe(lambda e, pb1=pb1, ft=ft: e.matmul(pb1[0:64, :], w1[:], ft[:], start=True, stop=True), [w1, ft], [pb1])
            S.act(lambda e, s=s, pb1=pb1: e.activation(s[0:64, :], pb1[0:64, :], AF.Sin, bias=sb_[0:64, 1:2], scale=sb_[0:64, 0:1]), [pb1, sb_], [s])
            S.dve(lambda e, s=s, q_=q_: e.tensor_tensor(q_[0:64, :], s[0:64, :], s[0:64, :], op=ALU.mult), [s], [q_])
            S.dve(lambda e, q_=q_: e.tensor_scalar(q_[0:64, :], q_[0:64, :], -4.0, 3.0, op0=ALU.mult, op1=ALU.add), [q_], [q_])
            S.dve(lambda e, s=s, q_=q_, hh1=hh1: e.tensor_tensor(hh1[:], q_[0:64, :], s[0:64, :], op=ALU.mult), [s, q_], [hh1])
            S.pe(lambda e, pb2=pb2, hh1=hh1: e.matmul(pb2[:], w2d[:], hh1[:], start=True, stop=True), [w2d, hh1], [pb2])
            lo = 0 if blk < 16 else 64
            S.act(lambda e, s=s, pb2=pb2, lo=lo: e.activation(s[lo:lo + 64, :], pb2[lo:lo + 64, :], AF.Sin, bias=sb_[lo:lo + 64, 3:4], scale=sb_[lo:lo + 64, 2:3]), [pb2, sb_], [s])
            S.dve(lambda e, s=s, q_=q_, lo=lo: e.tensor_tensor(q_[lo:lo + 64, :], s[lo:lo + 64, :], s[lo:lo + 64, :], op=ALU.mult), [s], [q_])
            S.dve(lambda e, q_=q_, lo=lo: e.tensor_scalar(q_[lo:lo + 64, :], q_[lo:lo + 64, :], -4.0, 3.0, op0=ALU.mult, op1=ALU.add), [q_], [q_])
            S.dve(lambda e, s=s, q_=q_, lo=lo, blk=blk: e.tensor_tensor(H2[lo:lo + 64, blk * 512:(blk + 1) * 512], q_[lo:lo + 64, :], s[lo:lo + 64, :], op=ALU.mult), [s, q_], [H2])
        S.dve(lambda e: e.memset(H2[64:128, L:L + 1], 0.0), [], [H2])
        PF.__exit__(None, None, None)
        w3st = P.sb("w3st", [128, 2, 512], F32)
        w3v = env["hy_w3"].rearrange("m (o d c) -> m o d c", o=2, d=2)
        S.dma("sp", lambda q: q.dma_start(out=w3st[0:64, :, :], in_=w3v[:, :, 0, :]), writes=[w3st], owner=w3st)
        S.dma("sp", lambda q: q.dma_start(out=w3st[64:128, :, :], in_=w3v[:, :, 1, :]), writes=[w3st], owner=w3st)
        W3s = P.sb("W3s", [128, 2, 512], BF16)
        S.dve(lambda e: e.tensor_copy(W3s[:], w3st[:]), [w3st], [W3s])
        skp = P.sb("skp", [1, 2, 512], F32)
        S.dma("sp", lambda q: q.dma_start(out=skp[:], in_=env["hy_skip"].rearrange("o (a c) -> o a c", a=2)), writes=[skp], owner=skp)

        QC = 4
        NW = 3

        class Lane:
            pass

        lanes = []
        for l in range(NW):
            ln = Lane()
            ln.kBre = P.sb("kBre", [128, 2 * QC, NH], BF16); ln.kBim = P.sb("kBim", [128, 2 * QC, NH], BF16)
            ln.Kr = P.sb("Kr", [128, 2 * QC, NH], F32); ln.Ki = P.sb("Ki", [128, 2 * QC, NH], F32)
            ln.q1h = P.ring("q1h", [128, 2, 2 * NH], F32, 2); ln.q2h = P.ring("q2h", [128, 2, 2 * NH], F32, 2)
            ln.q1 = P.ring("q1", [128, 2, 256], F32, 2); ln.q2 = P.ring("q2", [128, 2, 256], F32, 2)
            ln.t = [P.sb("t%d" % i, [128, QC * NH], F32) for i in range(4)]
            ln.Uv = P.sb("Uv", [64, QC, 128], BF16); ln.G1 = P.sb("G1", [64, QC, 128], BF16); ln.G2 = P.sb("G2", [64, QC, 128], BF16)
            ln.Bre = P.sb("Bre", [128, QC, NH], BF16); ln.Bim = P.sb("Bim", [128, QC, NH], BF16)
            ln.Yre = P.sb("Yre", [128, QC, NH], BF16); ln.Yim = P.sb("Yim", [128, QC, NH], BF16)
            ln.Cre = P.sb("Cre", [128, QC, 128], BF16); ln.Cim = P.sb("Cim", [128, QC, 128], BF16)
            ln.Z1 = P.sb("Z1", [64, QC, 128], BF16); ln.Z2 = P.sb("Z2", [64, QC, 128], BF16)
            ln.pa = bank[2 * l]; ln.px = (bank[2 * l], bank[2 * l + 1])
            ln.qi = 0
            lanes.append(ln)
        pk = bank[6]; py = bank[7]
        Dg = P.sb("Dg", [128, GC, 128], F32)
        kt = P.sb("kt", [128, 2, GC, 128], F32)
        kbR = P.ring("kb", [128, 2, GC, 128], BF16, 2)
        junk = P.sb("junk", [128, 128], F32)
        ssq = P.sb("ssq", [128, 2 * GC], F32); scl = P.sb("scl", [128, 2 * GC], F32)

        def kblock_stages(g):
            c0 = g * GC
            kb = kbR[g % 2]
            st = []

            def k_dec():
                for c in range(GC):
                    S.act(lambda e, c=c: e.activation(Dg[:, c, :], tts[:], AF.Exp, scale=-float(deltas[c0 + c])), [tts], [Dg])
            st.append(k_dec)

            def k_gen(r):
                def f():
                    for j in range(16):
                        n2 = r * 16 + j
                        S.pe(lambda e, j=j, n2=n2: e.matmul(pk[:, j * 32:(j + 1) * 32], H2[:, n2:NFFT:128], W3s[:, :, c0:c0 + GC], start=True, stop=True), [H2, W3s], [pk])
                    pkv = pk[:].rearrange("p (n o c) -> p o c n", n=16, o=2)
                    for o in range(2):
                        S.dve(lambda e, o=o: e.tensor_tensor(kt[:, o, :, r * 16:(r + 1) * 16], pkv[:, o, :, :], Dg[:, :, r * 16:(r + 1) * 16], op=ALU.mult), [pk, Dg], [kt])
                return f
            for r in range(8):
                st.append(k_gen(r))

            def k_sq():
                for o in range(2):
                    for c in range(GC):
                        m = o * GC + c
                        S.act(lambda e, o=o, c=c, m=m: e.activation(junk[:], kt[:, o, c, :], AF.Square, accum_out=ssq[:, m:m + 1]), [kt], [junk, ssq])
            st.append(k_sq)

            def k_tot():
                S.pe(lambda e: e.matmul(pk[:, 0:2 * GC], ones[:], ssq[:], start=True, stop=True), [ones, ssq], [pk])
                S.act(lambda e: e.activation(scl[:], pk[:, 0:2 * GC], AF.Sqrt, bias=e6[:], scale=1.0), [pk, e6], [scl])
                S.dve(lambda e: e.reciprocal(scl[:], scl[:]), [scl], [scl])
            st.append(k_tot)

            def k_scale():
                for o in range(2):
                    for c in range(GC):
                        m = o * GC + c
                        S.act(lambda e, o=o, c=c, m=m: e.activation(kb[:, o, c, :], kt[:, o, c, :], AF.Copy, scale=scl[:, m:m + 1]), [kt, scl], [kb])
                S.dve(lambda e: e.tensor_tensor(kb[0:1, :, :, 0], kb[0:1, :, :, 0], skp[0:1, :, c0:c0 + GC], op=ALU.add), [kb, skp], [kb])
            st.append(k_scale)
            return st

        def twiddle_f(ln, dre, dim, m0, kern):
            pa = ln.pa
            q1 = ln.q1h[ln.qi % 2]; q2 = ln.q2h[ln.qi % 2]; ln.qi += 1
            pav = pa[:].rearrange("p (a b) -> p a b", a=2)[:, :, 0:2 * NH]
            T1 = TK1 if kern else TH1; T2 = TK2 if kern else TH2
            S.dve(lambda e: e.tensor_tensor(q1[:], pav, T1[:], op=ALU.mult), [pa, T1], [q1])
            S.dve(lambda e: e.tensor_tensor(q2[:], pav, T2[:], op=ALU.mult), [pa, T2], [q2])
            S.pool(lambda e: e.tensor_tensor(dre[:, m0:m0 + 2, :], q1[:, :, 0:NH], q2[:, :, NH:2 * NH], op=ALU.subtract), [q1, q2], [dre])
            S.pool(lambda e: e.tensor_tensor(dim[:, m0:m0 + 2, :], q2[:, :, 0:NH], q1[:, :, NH:2 * NH], op=ALU.add), [q1, q2], [dim])

        def twiddle_i(ln, dre, dim, m0):
            pa = ln.pa
            q1 = ln.q1[ln.qi % 2]; q2 = ln.q2[ln.qi % 2]; ln.qi += 1
            pav = pa[0:NH, :].rearrange("p (a b) -> p a b", a=2)
            S.dve(lambda e: e.tensor_tensor(q1[0:NH], pav, TT1[0:NH], op=ALU.mult), [pa, TT1], [q1])
            S.dve(lambda e: e.tensor_tensor(q2[0:NH], pav, TT2[0:NH], op=ALU.mult), [pa, TT2], [q2])
            S.pool(lambda e: e.tensor_tensor(dre[0:NH, m0:m0 + 2, :], q1[0:NH, :, 0:128], q2[0:NH, :, 128:256], op=ALU.add), [q1, q2], [dre])
            S.pool(lambda e: e.tensor_tensor(dim[0:NH, m0:m0 + 2, :], q1[0:NH, :, 128:256], q2[0:NH, :, 0:128], op=ALU.subtract), [q1, q2], [dim])

        def stage3(ln, bre, bim, m0):
            pxr, pxi = ln.px
            W4 = 4 * NH
            rr = bre[:, m0:m0 + 4, :]; ri = bim[:, m0:m0 + 4, :]
            S.pe(lambda e: e.matmul(pxr[:, 0:W4], Fb[:, 0:128], rr, start=True, stop=False), [Fb, bre], [pxr])
            S.pe(lambda e: e.matmul(pxr[:, 0:W4], Fin[:], ri, start=False, stop=True), [Fin, bim], [pxr])
            S.pe(lambda e: e.matmul(pxi[:, 0:W4], Fb[:, 0:128], ri, start=True, stop=False), [Fb, bim], [pxi])
            S.pe(lambda e: e.matmul(pxi[:, 0:W4], Fb[:, 128:256], rr, start=False, stop=True), [Fb, bre], [pxi])
            return pxr, pxi

        def item_stages(ln, it):
            c0 = it * QC
            st = []

            def s_load():
                for tl, r0 in ((ln.Uv, 2 * HW_ + c0), (ln.G1, c0), (ln.G2, HW_ + c0)):
                    S.dma("sp", lambda q, tl=tl, r0=r0: q.dma_start(out=tl[:], in_=hycT_d[r0:r0 + QC, :].rearrange("c (a b) -> a c b", b=128)), writes=[tl], owner=tl)
            st.append(s_load)
            kb = kbR[(c0 // GC) % 2]
            cl = c0 % GC

            def s_ks1(o):
                def f():
                    for p_ in range(QC // 2):
                        for i in range(2):
                            c = 2 * p_ + i
                            S.pe(lambda e, c=c, i=i: e.matmul(ln.pa[:, i * 256:i * 256 + 2 * NH], kb[:, o, cl + c, :], FbH[:], start=True, stop=True), [kb, FbH], [ln.pa])
                        twiddle_f(ln, ln.kBre, ln.kBim, o * QC + 2 * p_, True)
                return f
            st.append(s_ks1(0)); st.append(s_ks1(1))

            def s_ks3(o):
                def f():
                    pxr, pxi = stage3(ln, ln.kBre, ln.kBim, o * QC)
                    S.act(lambda e: e.activation(ln.Kr[:, o * QC:(o + 1) * QC, :].rearrange("p a b -> p (a b)"), pxr[:, 0:QC * NH], AF.Copy, scale=INVN), [pxr], [ln.Kr])
                    S.act(lambda e: e.activation(ln.Ki[:, o * QC:(o + 1) * QC, :].rearrange("p a b -> p (a b)"), pxi[:, 0:QC * NH], AF.Copy, scale=INVN), [pxi], [ln.Ki])
                return f
            st.append(s_ks3(0)); st.append(s_ks3(1))

            def conv_stages(o, U, Gt, Zt, last):
                def c1():
                    for p_ in range(QC // 2):
                        for i in range(2):
                            c = 2 * p_ + i
                            S.pe(lambda e, c=c, i=i: e.matmul(ln.pa[:, i * 256:i * 256 + 2 * NH], U[:, c, :], FbH[0:64, :], start=True, stop=True), [U, FbH], [ln.pa])
                        twiddle_f(ln, ln.Bre, ln.Bim, 2 * p_, False)

                def c2():
                    pxr, pxi = stage3(ln, ln.Bre, ln.Bim, 0)
                    t1, t2, t3, t4 = ln.t
                    kr = ln.Kr[:, o * QC:(o + 1) * QC, :].rearrange("p a b -> p (a b)"); ki = ln.Ki[:, o * QC:(o + 1) * QC, :].rearrange("p a b -> p (a b)")
                    W4 = QC * NH
                    S.dve(lambda e: e.tensor_tensor(t1[:], pxr[:, 0:W4], kr, op=ALU.mult), [pxr, ln.Kr], [t1])
                    S.dve(lambda e: e.tensor_tensor(t2[:], pxi[:, 0:W4], ki, op=ALU.mult), [pxi, ln.Ki], [t2])
                    S.dve(lambda e: e.tensor_tensor(t3[:], pxr[:, 0:W4], ki, op=ALU.mult), [pxr, ln.Ki], [t3])
                    S.dve(lambda e: e.tensor_tensor(t4[:], pxi[:, 0:W4], kr, op=ALU.mult), [pxi, ln.Kr], [t4])
                    S.pool(lambda e: e.tensor_tensor(ln.Yre[:].rearrange("p a b -> p (a b)"), t1[:], t2[:], op=ALU.subtract), [t1, t2], [ln.Yre])
                    S.pool(lambda e: e.tensor_tensor(ln.Yim[:].rearrange("p a b -> p (a b)"), t3[:], t4[:], op=ALU.add), [t3, t4], [ln.Yim])

                def c3():
                    for p_ in range(QC // 2):
                        for i in range(2):
                            c = 2 * p_ + i
                            S.pe(lambda e, c=c, i=i: e.matmul(ln.pa[0:NH, i * 256:(i + 1) * 256], ln.Yre[:, c, :], FA[:], start=True, stop=False), [ln.Yre, FA], [ln.pa])
                            S.pe(lambda e, c=c, i=i: e.matmul(ln.pa[0:NH, i * 256:(i + 1) * 256], ln.Yim[:, c, :], FB_[:], start=False, stop=True), [ln.Yim, FB_], [ln.pa])
                        twiddle_i(ln, ln.Cre, ln.Cim, 2 * p_)

                def c4():
                    S.pe(lambda e: e.matmul(py[0:64, :], Fb[0:NH, 0:64], ln.Cre[0:NH].rearrange("p a b -> p (a b)"), start=True, stop=False), [Fb, ln.Cre], [py])
                    S.pe(lambda e: e.matmul(py[0:64, :], Fb[0:NH, 128:192], ln.Cim[0:NH].rearrange("p a b -> p (a b)"), start=False, stop=True), [Fb, ln.Cim], [py])
                    S.dve(lambda e: e.tensor_tensor(Zt[:].rearrange("p a b -> p (a b)"), py[0:64, :], Gt[:].rearrange("p a b -> p (a b)"), op=ALU.mult), [py, Gt], [Zt])
                    if last:
                        S.dma("sp", lambda q: q.dma_start(out=zT_d[c0:c0 + QC, :].rearrange("c (a b) -> a c b", b=128), in_=Zt[:]), reads=[Zt], writes=[env["dbuf"]("zT", it)], owner=Zt)
                return [c1, c2, c3, c4]
            st += conv_stages(0, ln.Uv, ln.G1, ln.Z1, False)
            st += conv_stages(1, ln.Z1, ln.G2, ln.Z2, True)
            return st

        nitems = HW_ // QC
        ipg = GC // QC
        ngrp = HW_ // GC
        for f_ in kblock_stages(0):
            f_()
        kq = []
        next_g = 1
        for i0_ in range(0, nitems, NW):
            idxs = [i for i in range(i0_, min(i0_ + NW, nitems))]
            gmax = idxs[-1] // ipg
            while next_g <= gmax:
                kq += [(next_g, f_) for f_ in kblock_stages(next_g)]
                next_g += 1
            while kq and kq[0][0] <= gmax:
                kq.pop(0)[1]()
            if not kq and next_g < ngrp and next_g <= gmax + 1:
                kq += [(next_g, f_) for f_ in kblock_stages(next_g)]
                next_g += 1
            sts_ = [item_stages(lanes[l], idxs[l]) for l in range(len(idxs))]
            for k in range(len(sts_[0])):
                for l in range(len(idxs)):
                    sts_[l][k]()
                if kq:
                    kq.pop(0)[1]()
        while kq:
            kq.pop(0)[1]()


def load_w_bf16(S, dst, src_ap, kchunks, ncols, stg, cnt):
    for k in range(kchunks):
        for c0 in range(0, ncols, 1024):
            st = stg[cnt[0] % len(stg)]
            w = min(1024, ncols - c0)
            S.dma("sp" if cnt[0] % 2 == 0 else "pool", lambda q, st=st, k=k, c0=c0, w=w: q.dma_start(out=st[:, 0:w], in_=src_ap[k * 128:(k + 1) * 128, c0:c0 + w]), writes=[st], owner=st)
            if cnt[0] % 2 == 0:
                S.act(lambda e, st=st, k=k, c0=c0, w=w: e.copy(dst[:, k, c0:c0 + w], st[:, 0:w]), [st], [dst])
            else:
                S.dve(lambda e, st=st, k=k, c0=c0, w=w: e.tensor_copy(dst[:, k, c0:c0 + w], st[:, 0:w]), [st], [dst])
            cnt[0] += 1


def row_bcast(S, P, name, src_row, scale=None):
    t = P.sb(name, [128, D], F32)
    S.dma("sp", lambda q: q.dma_start(out=t[:], in_=src_row.broadcast_to([128, D])), writes=[t], owner=t)
    if scale is not None:
        S.act(lambda e: e.mul(t[:], t[:], float(scale)), [t], [t])
    return t


def col_chunks(S, P, name, src_col, k):
    t = P.sb(name, [128, k], F32)
    S.dma("sp", lambda q: q.dma_start(out=t[:], in_=src_col.rearrange("(k p) o -> p (k o)", p=128), allow_slow_non_contiguous=True), writes=[t], owner=t)
    return t


def phase_CD(S, nc, env):
    phase_C(S, nc, env)
    if STOP_AFTER != "C1":
        phase_D(S, nc, env)


def phase_C(S, nc, env):
    ones = env["ones"]; x_d = env["x_d"]; ucT_d = env["ucT_d"]; zT_d = env["zT_d"]; gT_d = env["gT_d"]; x1_d = env["x1_d"]
    dbuf = env["dbuf"]
    with Phase(S, "C") as P:
        stg = P.ring("stg", [128, 1024], F32, 4); cnt = [0]
        cwo = P.sb("cwo", [128, 4, D], BF16); hwo = P.sb("hwo", [128, 4, D], BF16); wmx = P.sb("wmx", [128, 8, D], BF16)
        load_w_bf16(S, cwo, env["conf_w_out"], 4, D, stg, cnt)
        load_w_bf16(S, hwo, env["hy_w_out"], 4, D, stg, cnt)
        load_w_bf16(S, wmx, env["w_mix"], 8, D, stg, cnt)
        cg = col_chunks(S, P, "cg", env["conf_ln_g"], 4); cbb = col_chunks(S, P, "cbb", env["conf_ln_b"], 4)
        MG = row_bcast(S, P, "MG", env["ln_mix_g"]); MB = row_bcast(S, P, "MB", env["ln_mix_b"])
        xg_d = env["xg_d"]
        uc = P.sb("uc", [128, 4, 512], F32); sq = P.sb("sq", [128, 4, 512], F32)
        zt = P.sb("zt", [128, 4, 512], BF16); ua = P.sb("ua", [128, 4, 512], BF16)
        mean = P.sb("mean", [128, 512], F32); var = P.sb("var", [128, 512], F32); rstd = P.sb("rstd", [128, 512], F32)
        dd = P.ring("dd", [128, 512], F32, 2)
        gch = P.ring("gch", [128, 2, 512], BF16, 2)
        m1 = P.ring("m1", [128, 512], F32, 2); m2 = P.ring("m2", [128, 512], F32, 2)
        mgR = P.ring("mg", [128, 8, 512], BF16, 2)
        xgs = P.ring("xg", [128, D], F32, 3)
        ss = P.ring("s", [128, D], F32, 2); x1s = P.ring("x1", [128, D], F32, 2)
        sts = [ln_stats(S, P, None) for _ in range(3)]
        bank = P.ring("bk", [128, 512], F32, 8, ps=True)
        cnts = {"ti": 0, "si": 0}

        def front(blk):
            tsl = slice(blk * 512, (blk + 1) * 512)
            mg = mgR[blk % 2]
            st = []

            def f0():
                S.dma("sp", lambda q: q.dma_start(out=uc[:], in_=ucT_d[:, tsl].rearrange("(k p) t -> p k t", p=128)), writes=[uc], owner=uc)
                S.dma("sp", lambda q: q.dma_start(out=zt[:], in_=zT_d[:, tsl].rearrange("(k p) t -> p k t", p=128)), writes=[zt], owner=zt)
                S.act(lambda e: e.activation(sq[:], uc[:], AF.Square), [uc], [sq])
            st.append(f0)

            def f1():
                pS1 = bank[0]; pS2 = bank[1]
                for k in range(4):
                    S.pe(lambda e, k=k: e.matmul(pS1[:], ones[:], uc[:, k, :], start=(k == 0), stop=(k == 3)), [ones, uc], [pS1])
                for k in range(4):
                    S.pe(lambda e, k=k: e.matmul(pS2[:], ones[:], sq[:, k, :], start=(k == 0), stop=(k == 3)), [ones, sq], [pS2])
                S.act(lambda e: e.mul(mean[:], pS1[:], 1.0 / CW), [pS1], [mean])
                S.dve(lambda e: e.tensor_tensor(var[:], mean[:], mean[:], op=ALU.mult), [mean], [var])
                S.dve(lambda e: e.scalar_tensor_tensor(var[:], pS2[:], 1.0 / CW, var[:], op0=ALU.mult, op1=ALU.subtract), [pS2, var], [var])
                S.act(lambda e: e.activation(rstd[:], var[:], AF.Sqrt, bias=EPS_AP[0][:], scale=1.0), [var, EPS_AP[0]], [rstd])
                S.dve(lambda e: e.reciprocal(rstd[:], rstd[:]), [rstd], [rstd])
            st.append(f1)

            def f2(k):
                def f():
                    d_ = dd[k % 2]
                    S.dve(lambda e: e.tensor_tensor(d_[:], uc[:, k, :], mean[:], op=ALU.subtract), [uc, mean], [d_])
                    S.dve(lambda e: e.tensor_tensor(d_[:], d_[:], rstd[:], op=ALU.mult), [d_, rstd], [d_])
                    S.act(lambda e: e.activation(ua[:, k, :], d_[:], AF.Silu, bias=cbb[:, k:k + 1], scale=cg[:, k:k + 1]), [d_, cbb, cg], [ua])
                return f
            for k in range(4):
                st.append(f2(k))

            def f3(dc):
                def f():
                    pya = bank[2 + (dc % 2) * 2]; pyb = bank[3 + (dc % 2) * 2]
                    g_ = gch[dc % 2]; a1 = m1[dc % 2]; a2 = m2[dc % 2]
                    S.dma("sp", lambda q: q.dma_start(out=g_[:, 0, :], in_=gT_d[dc * 128:(dc + 1) * 128, tsl]), writes=[g_], owner=g_)
                    S.dma("sp", lambda q: q.dma_start(out=g_[:, 1, :], in_=gT_d[D + dc * 128:D + (dc + 1) * 128, tsl]), writes=[g_], owner=g_)
                    for k in range(4):
                        S.pe(lambda e, k=k: e.matmul(pya[:], cwo[:, k, dc * 128:(dc + 1) * 128], ua[:, k, :], start=(k == 0), stop=(k == 3)), [cwo, ua], [pya])
                    for k in range(4):
                        S.pe(lambda e, k=k: e.matmul(pyb[:], hwo[:, k, dc * 128:(dc + 1) * 128], zt[:, k, :], start=(k == 0), stop=(k == 3)), [hwo, zt], [pyb])
                    S.dve(lambda e: e.tensor_tensor(a1[:], pya[:], g_[:, 0, :], op=ALU.mult), [pya, g_], [a1])
                    S.dve(lambda e: e.tensor_tensor(a2[:], pyb[:], g_[:, 1, :], op=ALU.mult), [pyb, g_], [a2])
                    S.pool(lambda e: e.tensor_tensor(mg[:, dc, :], a1[:], a2[:], op=ALU.add), [a1, a2], [mg])
                return f
            for dc in range(8):
                st.append(f3(dc))
            return st

        def back(blk):
            mg = mgR[blk % 2]
            st = []
            for tt in range(4):
                t0 = blk * 512 + tt * 128
                ti = cnts["ti"]; cnts["ti"] += 1
                xg = xgs[ti % 3]; s_ = ss[ti % 2]; x1 = x1s[ti % 2]

                def ba(xg=xg, t0=t0):
                    S.dma("sp", lambda q: q.dma_start(out=xg[:], in_=xg_d[t0:t0 + 128, :]), reads=[dbuf("xg", t0)], writes=[xg], owner=xg)
                st.append(ba)

                def bb(xg=xg, s_=s_, x1=x1, tt=tt, t0=t0):
                    for half in range(2):
                        pm = bank[6 + half]
                        for k in range(8):
                            S.pe(lambda e, pm=pm, k=k, half=half: e.matmul(pm[:], mg[:, k, tt * 128:(tt + 1) * 128], wmx[:, k, half * 512:(half + 1) * 512], start=(k == 0), stop=(k == 7)), [mg, wmx], [pm])
                        S.dve(lambda e, pm=pm, half=half: e.tensor_tensor(s_[:, half * 512:(half + 1) * 512], pm[:], xg[:, half * 512:(half + 1) * 512], op=ALU.add), [pm, xg], [s_])
                    st_, mv, rs = sts[cnts["si"] % 3]; cnts["si"] += 1
                    emit_ln_stats(S, s_, s_.t, st_, mv, rs)
                    S.dve(lambda e: e.tensor_scalar(x1[:], s_[:], mv[:, 0:1], rs[:], op0=ALU.subtract, op1=ALU.mult), [s_, mv, rs], [x1])
                    S.dve(lambda e: e.tensor_tensor(x1[:], x1[:], MG[:], op=ALU.mult), [x1, MG], [x1])
                    S.pool(lambda e: e.tensor_tensor(x1[:], x1[:], MB[:], op=ALU.add), [x1, MB], [x1])
                    S.dma("sp", lambda q: q.dma_start(out=x1_d[t0:t0 + 128, :], in_=x1[:]), reads=[x1], writes=[dbuf("x1", t0)], owner=x1)
                st.append(bb)
            return st

        nblk = L // 512
        import os
        if os.environ.get("SEQC", "0") == "1":
            for blk in range(nblk):
                for f_ in front(blk):
                    f_()
                for f_ in back(blk):
                    f_()
            nblk = 0
        else:
            for f_ in front(0):
                f_()
        for blk in range(nblk):
            bs = back(blk)
            fs = front(blk + 1) if blk + 1 < nblk else []
            n = max(len(bs), len(fs))
            bi = 0; fi = 0
            for k in range(n):
                while bi < len(bs) and bi * n <= k * len(bs):
                    bs[bi](); bi += 1
                while fi < len(fs) and fi * n <= k * len(fs):
                    fs[fi](); fi += 1
            while bi < len(bs):
                bs[bi](); bi += 1
            while fi < len(fs):
                fs[fi](); fi += 1


def phase_D(S, nc, env):
    ident = env["ident"]; identb = env["identb"]; x1_d = env["x1_d"]; mem_d = env["mem_d"]; ones = env["ones"]
    acc_d = env["acc_d"]; x2b_d = env["x2b_d"]; affT = env["affT"]; dbuf = env["dbuf"]
    with Phase(S, "D") as P:
        wq = P.sb("wq", [128, 8, D], BF16); wo = P.sb("wo", [128, 8, D], BF16)
        kT = P.sb("kT", [128, 8, NMEM], BF16); V = P.sb("V", [128, 2, D], BF16)
        wr = P.sb("wr", [128, 8, NE], F32)
        S.dma("sp", lambda q: q.dma_start(out=wr[:], in_=env["w_router"].rearrange("(k p) e -> p k e", p=128)), writes=[wr], owner=wr)
        bank = P.ring("bk", [128, 512], F32, 7, ps=True)
        ppT = P.ps("ppT", [128, 8, 128], BF16)
        XG = row_bcast(S, P, "XG", env["ln_xa_g"]); XB = row_bcast(S, P, "XB", env["ln_xa_b"])
        PW = Phase(S, "DW"); PW.__enter__()
        stg = PW.ring("stg", [128, 1024], F32, 4); cnt = [0]
        with Phase(S, "DK") as PK:
            wk = PK.sb("wk", [128, 8, D], BF16); wv = PK.sb("wv", [128, 8, D], BF16)
            load_w_bf16(S, wk, env["xa_wk"], 8, D, stg, cnt)
            load_w_bf16(S, wv, env["xa_wv"], 8, D, stg, cnt)
            memT = PK.sb("memT", [128, 8, NMEM], BF16)
            mt = PK.ring("mt", [128, D], F32, 2)
            for mc in range(2):
                m_ = mt[mc]
                S.dma("sp", lambda q, m_=m_, mc=mc: q.dma_start(out=m_[:], in_=mem_d[mc * 128:(mc + 1) * 128, :]), writes=[m_], owner=m_)
                for half in range(2):
                    pt = bank[half]
                    for j in range(4):
                        dk = half * 4 + j
                        S.pe(lambda e, pt=pt, j=j, dk=dk, m_=m_: e.transpose(pt[:, j * 128:(j + 1) * 128], m_[:, dk * 128:(dk + 1) * 128], ident[:]), [m_, ident], [pt])
                    S.act(lambda e, pt=pt, half=half, mc=mc: e.copy(memT[:, half * 4:half * 4 + 4, mc * 128:(mc + 1) * 128], pt[:].rearrange("p (a b) -> p a b", a=4)), [pt], [memT])
            for hc in range(8):
                pk_ = bank[2 + hc % 2]
                for k in range(8):
                    S.pe(lambda e, pk_=pk_, k=k, hc=hc: e.matmul(pk_[:, 0:NMEM], wk[:, k, hc * 128:(hc + 1) * 128], memT[:, k, :], start=(k == 0), stop=(k == 7)), [wk, memT], [pk_])
                S.act(lambda e, pk_=pk_, hc=hc: e.copy(kT[:, hc, :], pk_[:, 0:NMEM]), [pk_], [kT])
            for mc in range(2):
                for half in range(2):
                    pv = bank[4 + half]
                    for k in range(8):
                        S.pe(lambda e, pv=pv, k=k, mc=mc, half=half: e.matmul(pv[:], memT[:, k, mc * 128:(mc + 1) * 128], wv[:, k, half * 512:(half + 1) * 512], start=(k == 0), stop=(k == 7)), [memT, wv], [pv])
                    S.act(lambda e, pv=pv, mc=mc, half=half: e.copy(V[:, mc, half * 512:(half + 1) * 512], pv[:]), [pv], [V])
        load_w_bf16(S, wq, env["xa_wq"], 8, D, stg, cnt)
        load_w_bf16(S, wo, env["xa_wo"], 8, D, stg, cnt)
        PW.__exit__(None, None, None)
        x1t = P.ring("x1t", [128, D], F32, 4)
        xr = P.ring("xr", [128, D], F32, 2)
        x1T = P.sb("x1T", [128, 8, 512], BF16); qT = P.sb("qT", [128, 8, 512], BF16)
        pf = P.ring("pf", [128, 4, NMEM], F32, 2); pn = P.ring("pn", [128, 4, NMEM], BF16, 2)
        pT = P.sb("pT", [128, 8, 512], BF16); oTR = P.ring("oT", [128, 8, 512], BF16, 2)
        mx = P.ring("mx", [128, 4], F32, 2); sm = P.ring("sm", [128, 4], F32, 2)
        s2 = P.ring("s2", [128, D], F32, 2); x2 = P.ring("x2", [128, D], F32, 2)
        accs = P.ring("accs", [128, D], F32, 2); x2b = P.ring("x2b", [128, D], BF16, 2)
        x2T = P.sb("x2T", [128, 8, 512], F32)
        ex = P.sb("ex", [NE, 512], F32); rsum = P.sb("rsum", [NE, 512], F32)
        sts = [ln_stats(S, P, None) for _ in range(2)]
        cn = {"si": 0, "ti": 0}

        def front(blk):
            oT = oTR[blk % 2]
            st = []

            def d0():
                for tt in range(4):
                    t0 = blk * 512 + tt * 128
                    xt = x1t[tt]
                    S.dma("sp", lambda q, xt=xt, t0=t0: q.dma_start(out=xt[:], in_=x1_d[t0:t0 + 128, :]), reads=[dbuf("x1", t0)], writes=[xt], owner=xt)
            st.append(d0)

            def d1(dk0):
                def f():
                    for dk in range(dk0, dk0 + 4):
                        pt = bank[dk % 2]
                        for tt in range(4):
                            S.pe(lambda e, pt=pt, tt=tt, dk=dk: e.transpose(pt[:, tt * 128:(tt + 1) * 128], x1t[tt][:, dk * 128:(dk + 1) * 128], ident[:]), [x1t[tt], ident], [pt])
                        S.act(lambda e, pt=pt, dk=dk: e.copy(x1T[:, dk, :], pt[:]), [pt], [x1T])
                return f
            st.append(d1(0)); st.append(d1(4))

            def d2(h0):
                def f():
                    for hc in range(h0, h0 + 4):
                        pq = bank[2 + hc % 2]
                        for k in range(8):
                            S.pe(lambda e, pq=pq, k=k, hc=hc: e.matmul(pq[:], wq[:, k, hc * 128:(hc + 1) * 128], x1T[:, k, :], start=(k == 0), stop=(k == 7)), [wq, x1T], [pq])
                        S.act(lambda e, pq=pq, hc=hc: e.mul(qT[:, hc, :], pq[:], 1.0 / 16.0), [pq], [qT])
                return f
            st.append(d2(0)); st.append(d2(4))

            def d3(tt):
                def f():
                    p_f = pf[tt % 2]; p_n = pn[tt % 2]; mx_ = mx[tt % 2]; sm_ = sm[tt % 2]
                    for hp in range(2):
                        psc = bank[4 + hp]
                        for hh in range(2):
                            h = hp * 2 + hh
                            for k in range(2):
                                S.pe(lambda e, psc=psc, hh=hh, h=h, k=k: e.matmul(psc[:, hh * 256:(hh + 1) * 256], qT[:, 2 * h + k, tt * 128:(tt + 1) * 128], kT[:, 2 * h + k, :], start=(k == 0), stop=(k == 1)), [qT, kT], [psc])
                        S.dve(lambda e, psc=psc, hp=hp: e.tensor_reduce(mx_[:, hp * 2:hp * 2 + 2], psc[:].rearrange("p (a b) -> p a b", a=2), axis=mybir.AxisListType.X, op=ALU.max, negate=True), [psc], [mx_])
                        for hh in range(2):
                            h = hp * 2 + hh
                            S.act(lambda e, psc=psc, hh=hh, h=h: e.activation(p_f[:, h, :], psc[:, hh * 256:(hh + 1) * 256], AF.Exp, bias=mx_[:, h:h + 1], scale=1.0, accum_out=sm_[:, h:h + 1]), [psc, mx_], [p_f, sm_])
                    S.dve(lambda e: e.reciprocal(sm_[:], sm_[:]), [sm_], [sm_])
                    for h in range(4):
                        S.dve(lambda e, h=h: e.tensor_scalar(p_n[:, h, :], p_f[:, h, :], sm_[:, h:h + 1], None, op0=ALU.mult), [p_f, sm_], [p_n])
                    for h in range(4):
                        for mc in range(2):
                            S.pe(lambda e, h=h, mc=mc: e.transpose(ppT[:, h * 2 + mc, :], p_n[:, h, mc * 128:(mc + 1) * 128], identb[:]), [p_n, identb], [ppT])
                    S.act(lambda e: e.copy(pT[:, :, tt * 128:(tt + 1) * 128], ppT[:]), [ppT], [pT])
                return f
            for tt in range(4):
                st.append(d3(tt))

            def d4(h0):
                def f():
                    for hc in range(h0, h0 + 4):
                        po = bank[2 + hc % 2]; h = hc // 2
                        for mc in range(2):
                            S.pe(lambda e, po=po, mc=mc, hc=hc, h=h: e.matmul(po[:], V[:, mc, hc * 128:(hc + 1) * 128], pT[:, h * 2 + mc, :], start=(mc == 0), stop=(mc == 1)), [V, pT], [po])
                        S.act(lambda e, po=po, hc=hc: e.copy(oT[:, hc, :], po[:]), [po], [oT])
                return f
            st.append(d4(0)); st.append(d4(4))
            return st

        def back(blk):
            oT = oTR[blk % 2]
            st = []
            for tt in range(4):
                t0 = blk * 512 + tt * 128
                ti = cn["ti"]; cn["ti"] += 1
                s_ = s2[ti % 2]; x2_ = x2[ti % 2]; ac = accs[ti % 2]; xb = x2b[ti % 2]; xr_ = xr[ti % 2]

                def b0(tt=tt, t0=t0, s_=s_, x2_=x2_, ac=ac, xb=xb, xr_=xr_):
                    S.dma("sp", lambda q: q.dma_start(out=xr_[:], in_=x1_d[t0:t0 + 128, :]), reads=[dbuf("x1", t0)], writes=[xr_], owner=xr_)
                    for half in range(2):
                        px = bank[half]
                        for k in range(8):
                            S.pe(lambda e, px=px, k=k, half=half: e.matmul(px[:], oT[:, k, tt * 128:(tt + 1) * 128], wo[:, k, half * 512:(half + 1) * 512], start=(k == 0), stop=(k == 7)), [oT, wo], [px])
                        S.dve(lambda e, px=px, half=half: e.scalar_tensor_tensor(s_[:, half * 512:(half + 1) * 512], xr_[:, half * 512:(half + 1) * 512], ALPHA, px[:], op0=ALU.mult, op1=ALU.add), [px, xr_], [s_])
                    st_, mv, rs = sts[cn["si"] % 2]; cn["si"] += 1
                    emit_ln_stats(S, s_, s_.t, st_, mv, rs)
                    S.dve(lambda e: e.tensor_scalar(x2_[:], s_[:], mv[:, 0:1], rs[:], op0=ALU.subtract, op1=ALU.mult), [s_, mv, rs], [x2_])
                    S.dve(lambda e: e.tensor_tensor(x2_[:], x2_[:], XG[:], op=ALU.mult), [x2_, XG], [x2_])
                    S.dve(lambda e: e.tensor_tensor(x2_[:], x2_[:], XB[:], op=ALU.add), [x2_, XB], [x2_])
                    S.act(lambda e: e.mul(ac[:], x2_[:], ALPHA), [x2_], [ac])
                    S.act(lambda e: e.copy(xb[:], x2_[:]), [x2_], [xb])
                    S.dma("pool", lambda q: q.dma_start(out=acc_d[t0:t0 + 128, :], in_=ac[:]), reads=[ac], writes=[dbuf("acc", t0)], owner=ac)
                    S.dma("pool", lambda q: q.dma_start(out=x2b_d[t0:t0 + 128, :], in_=xb[:]), reads=[xb], writes=[dbuf("x2b", t0)], owner=xb)
                st.append(b0)

                def b1(tt=tt, x2_=x2_):
                    for half in range(2):
                        pt = bank[2 + half]
                        for j in range(4):
                            dk = half * 4 + j
                            S.pe(lambda e, pt=pt, j=j, dk=dk: e.transpose(pt[:, j * 128:(j + 1) * 128], x2_[:, dk * 128:(dk + 1) * 128], ident[:]), [x2_, ident], [pt])
                        S.act(lambda e, pt=pt, half=half: e.copy(x2T[:, half * 4:half * 4 + 4, tt * 128:(tt + 1) * 128], pt[:].rearrange("p (a b) -> p a b", a=4)), [pt], [x2T])
                st.append(b1)

            def b2():
                pl = bank[6]
                for k in range(8):
                    S.pe(lambda e, k=k: e.matmul(pl[0:NE, :], wr[:, k, :], x2T[:, k, :], start=(k == 0), stop=(k == 7)), [wr, x2T], [pl])
                S.act(lambda e: e.activation(ex[:], pl[0:NE, :], AF.Exp), [pl], [ex])
                S.pe(lambda e: e.matmul(pl[0:NE, :], ones[0:NE, 0:NE], ex[:], start=True, stop=True), [ones, ex], [pl])
                S.dve(lambda e: e.reciprocal(rsum[:], pl[0:NE, :]), [pl], [rsum])
                S.dve(lambda e: e.tensor_tensor(affT[:, blk * 512:(blk + 1) * 512], ex[:], rsum[:], op=ALU.mult), [ex, rsum], [affT])
            st.append(b2)
            return st

        nblk = L // 512
        for f_ in front(0):
            f_()
        for blk in range(nblk):
            bs = back(blk)
            fs = front(blk + 1) if blk + 1 < nblk else []
            n = max(len(bs), len(fs))
            bi = 0; fi = 0
            for k in range(n):
                while bi < len(bs) and bi * n <= k * len(bs):
                    bs[bi](); bi += 1
                while fi < len(fs) and fi * n <= k * len(fs):
                    fs[fi](); fi += 1
            while bi < len(bs):
                bs[bi](); bi += 1
            while fi < len(fs):
                fs[fi](); fi += 1
        S.dma("sp", lambda q: q.dma_start(out=env["affT_d"][:, :], in_=affT[:]), reads=[affT], writes=[dbuf("affT", 0)], owner=affT)


def phase_EF(S, nc, env):
    affT = env["affT"]; ident = env["ident"]; identb = env["identb"]
    x2b_d = env["x2b_d"]; acc_d = env["acc_d"]
    with Phase(S, "EF") as PX:
        idxT = PX.sb("idxT", [128, NE, 8], U32)
        gate = PX.sb("gate", [128, NE, 8], F32)
        phase_E(S, nc, env, idxT, gate)
        if STOP_AFTER == "E":
            return
        phase_F(S, nc, env, idxT, gate)


def phase_E(S, nc, env, idxT, gate):
    affT = env["affT"]; ident = env["ident"]
    with Phase(S, "E") as P:
        bank = P.ring("bk", [128, 512], F32, 6, ps=True)
        a128 = P.sb("a128", [128, 1024], F32)
        S.dma("sp", lambda q: q.dma_start(out=a128[:], in_=env["affT_d"].rearrange("e (s t) -> (e s) t", s=8)), writes=[a128], owner=a128)
        blk8 = P.sb("blk8", [128, 128], F32)
        S.dma("sp", lambda q: q.dma_start(out=blk8[:], in_=env["c_blk8"][:, :]), writes=[blk8], owner=blk8)
        jk8 = P.sb("jk8", [128, 1024], BF16)
        lo8 = P.sb("lo8", [128, 2], F32); hi8 = P.sb("hi8", [128, 2], F32); mid8 = P.sb("mid8", [128, 2], F32)
        cnt8 = P.sb("cnt8", [128, 2], F32); ge8 = P.sb("ge8", [128, 2], F32); d8 = P.sb("d8", [128, 2], F32)
        lo = P.sb("lo", [NE, 1], F32)
        msk = P.sb("msk", [NE, L], F32); cum = P.sb("cum", [NE, L], F32)
        one16 = P.sb("one16", [NE, L], BF16)
        S.pool(lambda e: e.memset(one16[:], 1.0), [], [one16])
        S.dve(lambda e: e.memset(lo8[:], 0.0), [], [lo8])
        S.dve(lambda e: e.memset(hi8[:], 1.0), [], [hi8])
        pcn = bank[5]
        for it in range(34):
            S.dve(lambda e: e.tensor_tensor(mid8[:], lo8[:], hi8[:], op=ALU.add), [lo8, hi8], [mid8])
            S.dve(lambda e: e.tensor_scalar(mid8[:], mid8[:], 0.5, None, op0=ALU.mult), [mid8], [mid8])
            S.dve(lambda e: e.tensor_scalar(jk8[:], a128[:], mid8[:, 0:1], None, op0=ALU.is_ge, op1=ALU.add, accum_out=cnt8[:, 0:1]), [a128, mid8], [jk8, cnt8])
            S.dve(lambda e: e.tensor_copy(cnt8[:, 1:2], cnt8[:, 0:1]), [cnt8], [cnt8])
            S.pe(lambda e: e.matmul(pcn[:, 0:2], blk8[:], cnt8[:], start=True, stop=True), [blk8, cnt8], [pcn])
            S.dve(lambda e: e.tensor_scalar(ge8[:], pcn[:, 0:2], CAP - 0.5, None, op0=ALU.is_ge), [pcn], [ge8])
            S.dve(lambda e: e.tensor_tensor(d8[:], mid8[:], lo8[:], op=ALU.subtract), [mid8, lo8], [d8])
            S.dve(lambda e: e.scalar_tensor_tensor(lo8[:], d8[:], ge8[:, 0:1], lo8[:], op0=ALU.mult, op1=ALU.add), [d8, ge8, lo8], [lo8])
            S.dve(lambda e: e.tensor_tensor(d8[:], hi8[:], mid8[:], op=ALU.subtract), [hi8, mid8], [d8])
            S.dve(lambda e: e.scalar_tensor_tensor(hi8[:], d8[:], ge8[:, 0:1], mid8[:], op0=ALU.mult, op1=ALU.add), [d8, ge8, mid8], [hi8])
        lrow = P.sb("lrow", [2, 128], F32)
        S.pe(lambda e: e.transpose(pcn[0:2, 0:128], lo8[:], ident[:]), [lo8, ident], [pcn])
        S.dve(lambda e: e.tensor_copy(lrow[:], pcn[0:2, 0:128]), [pcn], [lrow])
        S.pe(lambda e: e.transpose(pcn[0:NE, 256:257], lrow[0:1, 0:128:8], ident[0:1, 0:1]), [lrow, ident], [pcn])
        S.dve(lambda e: e.tensor_copy(lo[:], pcn[0:NE, 256:257]), [pcn], [lo])
        S.dve(lambda e: e.tensor_scalar(msk[:], affT[:], lo[:, 0:1], None, op0=ALU.is_ge), [affT, lo], [msk])
        S.dve(lambda e: e.tensor_tensor_scan(cum[:], one16[:], msk[:], 0.0, op0=ALU.mult, op1=ALU.add), [one16, msk], [cum])
        S.dve(lambda e: e.tensor_tensor(cum[:], cum[:], msk[:], op=ALU.mult), [cum, msk], [cum])
        S.dve(lambda e: e.tensor_scalar(cum[:], cum[:], -1.0, None, op0=ALU.add), [cum], [cum])
        keyT = P.sb("keyT", [128, NT, NE], F32); afT = P.sb("afT", [128, NT, NE], F32)
        for src, dst in ((cum, keyT), (affT, afT)):
            for hb in range(2):
                pb = bank[hb]
                for j in range(32):
                    tl = hb * 32 + j
                    S.pe(lambda e, pb=pb, j=j, tl=tl, src=src: e.transpose(pb[:, j * NE:(j + 1) * NE], src[:, tl * 128:(tl + 1) * 128], ident[0:NE, 0:NE]), [src, ident], [pb])
                S.act(lambda e, pb=pb, hb=hb, dst=dst: e.copy(dst[:, hb * 32:(hb + 1) * 32, :].rearrange("p a b -> p (a b)"), pb[:]), [pb], [dst])
        vals = P.sb("vals", [128, NT, NE, 5], BF16)
        tidx = P.sb("tidx", [128, NT, 2], F32)
        S.dma("sp", lambda q: q.dma_start(out=tidx[:], in_=env["c_tidx"][:, :, :]), writes=[tidx], owner=tidx)
        for e_ in range(NE):
            S.dve(lambda e, e_=e_: e.tensor_copy(vals[:, :, e_, 0:2], tidx[:]), [tidx], [vals])
        r1 = P.sb("r1", [128, NT, NE], F32); hb16 = P.sb("hb16", [128, NT, NE], BF16)
        S.dve(lambda e: e.tensor_copy(hb16[:], afT[:]), [afT], [hb16])
        S.dve(lambda e: e.tensor_copy(vals[:, :, :, 2], hb16[:]), [hb16], [vals])
        S.dve(lambda e: e.tensor_tensor(r1[:], afT[:], hb16[:], op=ALU.subtract), [afT, hb16], [r1])
        S.dve(lambda e: e.tensor_copy(hb16[:], r1[:]), [r1], [hb16])
        S.dve(lambda e: e.tensor_copy(vals[:, :, :, 3], hb16[:]), [hb16], [vals])
        S.dve(lambda e: e.tensor_tensor(r1[:], r1[:], hb16[:], op=ALU.subtract), [r1, hb16], [r1])
        S.dve(lambda e: e.tensor_copy(vals[:, :, :, 4], r1[:]), [r1], [vals])
        iota32 = P.sb("iota32", [128, CAP], F32)
        S.dma("sp", lambda q: q.dma_start(out=iota32[:], in_=env["c_iota"][:, :]), writes=[iota32], owner=iota32)
        iota = P.sb("iota", [128, CAP], mybir.dt.float16)
        S.dve(lambda e: e.tensor_copy(iota[:], iota32[:]), [iota32], [iota])
        zl = P.sb("zl", [128, 128], BF16); zr = P.sb("zr", [128, 512], BF16)
        S.dve(lambda e: e.memset(zl[:], 0.0), [], [zl])
        S.dve(lambda e: e.memset(zr[:], 0.0), [], [zr])
        accA = bank[2]; accB = bank[3]
        for a_ in (accA, accB):
            S.pe(lambda e, a_=a_: e.matmul(a_[:], zl[:], zr[:], start=True, stop=False, skip_group_check=True), [zl, zr], [a_])
        O = P.ring("O", [128, CAP], BF16, 3)
        oi = 0
        for tl in range(NT):
            for e_ in range(NE):
                o_ = O[oi % 3]; oi += 1
                S.dve(lambda e, o_=o_, tl=tl, e_=e_: e.tensor_scalar(o_[:], iota[:], keyT[:, tl, e_:e_ + 1], None, op0=ALU.is_equal), [iota, keyT], [o_])
                a_ = accA if e_ < 8 else accB
                for sc in range(8):
                    col = ((e_ % 8) * 8 + sc) * 5
                    S.pe(lambda e, a_=a_, o_=o_, sc=sc, col=col, tl=tl, e_=e_: e.matmul(a_[:, col:col + 5], o_[:, sc * 128:(sc + 1) * 128], vals[:, tl, e_, :], start=False, stop=(tl == NT - 1), skip_group_check=True), [o_, vals], [a_])
        res = P.sb("res", [128, NE, 8, 5], F32)
        S.act(lambda e: e.copy(res[:, 0:8, :, :].rearrange("p a b c -> p (a b c)"), accA[:, 0:320]), [accA], [res])
        S.act(lambda e: e.copy(res[:, 8:16, :, :].rearrange("p a b c -> p (a b c)"), accB[:, 0:320]), [accB], [res])
        idf = P.sb("idf", [128, NE, 8], F32)
        S.dve(lambda e: e.scalar_tensor_tensor(idf[:], res[:, :, :, 0], 128.0, res[:, :, :, 1], op0=ALU.mult, op1=ALU.add), [res], [idf])
        S.dve(lambda e: e.tensor_copy(idxT[:], idf[:]), [idf], [idxT])
        S.dve(lambda e: e.tensor_tensor(gate[:], res[:, :, :, 2], res[:, :, :, 3], op=ALU.add), [res], [gate])
        S.dve(lambda e: e.tensor_tensor(gate[:], gate[:], res[:, :, :, 4], op=ALU.add), [gate, res], [gate])


def phase_F(S, nc, env, idxT, gate):
    identb = env["identb"]; x2b_d = env["x2b_d"]; acc_d = env["acc_d"]
    w_gate = env["w_gate"]; w_up = env["w_up"]; w_down = env["w_down"]
    accbuf = env["dbuf"]("accsc", 0)
    FG = 256
    NFG = DFF // FG
    with Phase(S, "F") as P:
        xg = P.sb("xg", [128, 8, D], BF16)
        xgT = P.ring("xgT", [128, 8, CAP], BF16, 2)
        hT = P.sb("hT", [128, 16, CAP], BF16)
        Wd = P.sb("Wd", [128, 16, D], BF16)
        Wg = P.ring("Wg", [128, 8, FG], BF16, 2); Wu = P.ring("Wu", [128, 8, FG], BF16, 2)
        stg = P.ring("stg", [128, 8 * FG], F32, 3)
        yo = P.ring("yo", [128, D], F32, 2)
        sg = P.ring("sg", [128, 512], F32, 2)
        bank = P.ring("bk", [128, 512], F32, 7, ps=True)
        ptr = P.ps("ptr", [128, 8, 128], BF16)
        sc_ = [0]

        def wload(dst, dst_ap, src_ap, a, b):
            st = stg[sc_[0] % 3]; sc_[0] += 1
            S.dma("sp", lambda q: q.dma_start(out=st[:].rearrange("p (a b) -> p a b", a=a), in_=src_ap), writes=[st], owner=st)
            S.act(lambda e: e.copy(dst_ap, st[:].rearrange("p (a b) -> p a b", a=a)), [st], [dst])

        def load_group(e_, fg):
            wg_ = Wg[fg % 2]; wu_ = Wu[fg % 2]
            wload(wg_, wg_[:], w_gate[e_, :, fg * FG:(fg + 1) * FG].rearrange("(k p) f -> p k f", p=128), 8, FG)
            wload(wu_, wu_[:], w_up[e_, :, fg * FG:(fg + 1) * FG].rearrange("(k p) f -> p k f", p=128), 8, FG)
            wload(Wd, Wd[:, fg * 2:fg * 2 + 2, :], w_down[e_, fg * FG:(fg + 1) * FG, :].rearrange("(k p) d -> p k d", p=128), 2, D)

        def gather(e_):
            for sc in range(8):
                S.dma("pool", lambda q, sc=sc: q.indirect_dma_start(out=xg[:, sc, :], out_offset=None, in_=x2b_d[:, :], in_offset=bass.IndirectOffsetOnAxis(ap=idxT[:, e_, sc:sc + 1], axis=0)), reads=[idxT], writes=[xg], owner=xg)

        def transposes(e_):
            xt_ = xgT[e_ % 2]
            for sc in range(8):
                for dk in range(8):
                    S.pe(lambda e, sc=sc, dk=dk: e.transpose(ptr[:, dk, :], xg[:, sc, dk * 128:(dk + 1) * 128], identb[:]), [xg, identb], [ptr])
                S.act(lambda e, sc=sc: e.copy(xt_[:, :, sc * 128:(sc + 1) * 128], ptr[:]), [ptr], [xt_])

        yi = 0; pi = 0
        gather(0)
        transposes(0)
        load_group(0, 0)
        for e_ in range(NE):
            xt_ = xgT[e_ % 2]
            if e_ + 1 < NE:
                gather(e_ + 1)
            for fg in range(NFG):
                wg_ = Wg[fg % 2]; wu_ = Wu[fg % 2]
                if fg + 1 < NFG:
                    load_group(e_, fg + 1)
                for fk in range(FG // 128):
                    fkg = fg * (FG // 128) + fk
                    for sh in range(2):
                        pg = bank[(pi % 2) * 2]; pu = bank[(pi % 2) * 2 + 1]; s_ = sg[pi % 2]; pi += 1
                        for dk in range(8):
                            S.pe(lambda e, pg=pg, dk=dk, fk=fk, sh=sh, wg_=wg_: e.matmul(pg[:], wg_[:, dk, fk * 128:(fk + 1) * 128], xt_[:, dk, sh * 512:(sh + 1) * 512], start=(dk == 0), stop=(dk == 7)), [wg_, xt_], [pg])
                        for dk in range(8):
                            S.pe(lambda e, pu=pu, dk=dk, fk=fk, sh=sh, wu_=wu_: e.matmul(pu[:], wu_[:, dk, fk * 128:(fk + 1) * 128], xt_[:, dk, sh * 512:(sh + 1) * 512], start=(dk == 0), stop=(dk == 7)), [wu_, xt_], [pu])
                        S.act(lambda e, s_=s_, pg=pg: e.activation(s_[:], pg[:], AF.Silu), [pg], [s_])
                        S.dve(lambda e, s_=s_, pu=pu, fkg=fkg, sh=sh: e.tensor_tensor(hT[:, fkg, sh * 512:(sh + 1) * 512], s_[:], pu[:], op=ALU.mult), [s_, pu], [hT])
            for sc in range(8):
                y_ = yo[yi % 2]; yi += 1
                for dh in range(2):
                    py = bank[4 + dh]
                    for fk in range(16):
                        S.pe(lambda e, py=py, fk=fk, sc=sc, dh=dh: e.matmul(py[:], hT[:, fk, sc * 128:(sc + 1) * 128], Wd[:, fk, dh * 512:(dh + 1) * 512], start=(fk == 0), stop=(fk == 15)), [hT, Wd], [py])
                    S.dve(lambda e, py=py, y_=y_, dh=dh, sc=sc: e.tensor_scalar(y_[:, dh * 512:(dh + 1) * 512], py[:], gate[:, e_, sc:sc + 1], None, op0=ALU.mult), [py, gate], [y_])
                S.dma("pool", lambda q, y_=y_, sc=sc: q.indirect_dma_start(out=acc_d[:, :], out_offset=bass.IndirectOffsetOnAxis(ap=idxT[:, e_, sc:sc + 1], axis=0), in_=y_[:], in_offset=None, compute_op=ALU.add), reads=[y_, idxT], writes=[accbuf], owner=y_)
            if e_ + 1 < NE:
                transposes(e_ + 1)
                load_group(e_ + 1, 0)


def phase_G(S, nc, env):
    acc_d = env["acc_d"]; out_d = env["out_d"]
    with Phase(S, "Z") as P:
        OG = row_bcast(S, P, "OG", env["ln_moe_g"]); OB = row_bcast(S, P, "OB", env["ln_moe_b"])
        at = P.ring("at", [128, D], F32, 4); ot = P.ring("ot", [128, D], F32, 4)
        sts = [ln_stats(S, P, None) for _ in range(4)]
        for tl in range(NT):
            a_ = at[tl % 4]; o_ = ot[tl % 4]; st, mv, rs = sts[tl % 4]
            S.dma("sp", lambda q, a_=a_, tl=tl: q.dma_start(out=a_[:], in_=acc_d[tl * 128:(tl + 1) * 128, :]), writes=[a_], owner=a_)
            emit_ln_stats(S, a_, a_.t, st, mv, rs)
            S.dve(lambda e, o_=o_, a_=a_, mv=mv, rs=rs: e.tensor_scalar(o_[:], a_[:], mv[:, 0:1], rs[:], op0=ALU.subtract, op1=ALU.mult), [a_, mv, rs], [o_])
            S.dve(lambda e, o_=o_: e.tensor_tensor(o_[:], o_[:], OG[:], op=ALU.mult), [o_, OG], [o_])
            S.pool(lambda e, o_=o_: e.tensor_tensor(o_[:], o_[:], OB[:], op=ALU.add), [o_, OB], [o_])
            S.dma("sp", lambda q, o_=o_, tl=tl: q.dma_start(out=out_d[tl * 128:(tl + 1) * 128, :], in_=o_[:]), reads=[o_], writes=[env["dbuf"]("out", tl)], owner=o_)


def host_consts():
    a = np.arange(128, dtype=np.float64)
    th = 2.0 * np.pi * np.outer(a, a) / 128.0
    Fm = np.concatenate([np.cos(th), -np.sin(th)], axis=1).astype(np.float32)
    th2 = 2.0 * np.pi * np.outer(a, a) / NFFT
    Tm = np.concatenate([np.cos(th2), -np.sin(th2)], axis=1).astype(np.float32)
    n = np.arange(L, dtype=np.float32)
    t = n / np.float32(L - 1)
    bands = 16
    f = np.linspace(1e-4, bands - 1, bands, dtype=np.float32)
    ang = (np.float32(2.0 * math.pi) * n / np.float32(L))[:, None] * f[None, :]
    feat = np.concatenate([t[:, None], np.cos(ang), -np.sin(ang)], axis=-1).astype(np.float32)
    featTS = np.zeros((NFFT, 33), np.float32)
    featTS[:L] = feat
    featTS[L + 1:] = feat[1:][::-1]
    tts = np.zeros(NFFT, np.float32)
    tts[:L] = t
    tts[L + 1:] = t[1:][::-1]
    tidx = np.zeros((128, NT, 2), np.float32)
    tidx[:, :, 0] = np.arange(NT, dtype=np.float32)[None, :]
    tidx[:, :, 1] = np.arange(128, dtype=np.float32)[:, None]
    return {
        "c_F": Fm, "c_T": Tm, "c_featT": np.ascontiguousarray(featTS.T), "c_tts": tts.reshape(128, 128).copy(),
        "c_ident": np.eye(128, dtype=np.float32),
        "c_iota": np.tile(np.arange(CAP, dtype=np.float32)[None, :], (128, 1)),
        "c_tidx": tidx,
        "c_blk8": np.kron(np.eye(16, dtype=np.float32), np.ones((8, 8), np.float32)),
    }


def make_in_maps(inputs, ncores=8):
    c = host_consts()
    f = lambda k: np.ascontiguousarray(np.asarray(inputs[k], dtype=np.float32))
    shared = {
        "ln_in_g": f("ln_in_g").reshape(D, 1), "ln_in_b": f("ln_in_b").reshape(D, 1),
        "ln_in_g_row": f("ln_in_g").reshape(1, D), "ln_in_b_row": f("ln_in_b").reshape(1, D),
        "w_in": f("w_in")[0], "b_gate": f("b_gate")[0].reshape(2 * D, 1),
        "conf_dw_w": f("conf_dw_w")[0], "conf_dw_b": f("conf_dw_b")[0].reshape(CW, 1),
        "conf_ln_g": f("conf_ln_g")[0].reshape(CW, 1), "conf_ln_b": f("conf_ln_b")[0].reshape(CW, 1),
        "conf_w_out": f("conf_w_out")[0],
        "hy_short_w": f("hy_short_w")[0], "hy_short_b": f("hy_short_b")[0].reshape(3 * HW_, 1),
        "hy_ffn_w1": f("hy_ffn_w1")[0], "hy_ffn_b1": f("hy_ffn_b1")[0].reshape(64, 1), "hy_freq1": f("hy_freq1")[0].reshape(64, 1),
        "hy_ffn_w2": f("hy_ffn_w2")[0], "hy_ffn_b2": f("hy_ffn_b2")[0].reshape(64, 1), "hy_freq2": f("hy_freq2")[0].reshape(64, 1),
        "hy_ffn_w3": f("hy_ffn_w3")[0], "hy_skip": f("hy_skip")[0].reshape(1, 2 * HW_),
        "hy_w_out": f("hy_w_out")[0], "w_mix_out": f("w_mix_out")[0],
        "ln_mix_g": f("ln_mix_g")[0].reshape(1, D), "ln_mix_b": f("ln_mix_b")[0].reshape(1, D),
        "xa_wq": f("xa_wq")[0], "xa_wk": f("xa_wk")[0], "xa_wv": f("xa_wv")[0], "xa_wo": f("xa_wo")[0],
        "ln_xa_g": f("ln_xa_g")[0].reshape(1, D), "ln_xa_b": f("ln_xa_b")[0].reshape(1, D),
        "moe_w_router": f("moe_w_router")[0],
        "moe_w_gate": f("moe_w_gate")[0], "moe_w_up": f("moe_w_up")[0], "moe_w_down": f("moe_w_down")[0],
        "ln_moe_g": f("ln_moe_g")[0].reshape(1, D), "ln_moe_b": f("ln_moe_b")[0].reshape(1, D),
    }
    shared.update(c)
    x = f("x"); mem = f("mem")
    maps = []
    for r in range(ncores):
        m = dict(shared)
        m["x"] = x[r % 4]
        m["mem"] = mem[r % 4]
        maps.append(m)
    return maps


def kernel(**inputs):
    nc = build_nc()
    maps = make_in_maps(inputs)
    res = run_bass_kernel_spmd(nc, maps, core_ids=list(range(8)))
    out = np.stack([np.asarray(res.results[r]["out"], dtype=np.float32) for r in range(4)], axis=0)
    return out
```

```python
import math
import numpy as np
import concourse.bass as bass
import concourse.mybir as mybir
from concourse.bass_utils import run_bass_kernel_spmd
from contextlib import ExitStack

F32 = mybir.dt.float32
BF16 = mybir.dt.bfloat16
U32 = mybir.dt.uint32
I32 = mybir.dt.int32
AF = mybir.ActivationFunctionType
ALU = mybir.AluOpType

D = 1024
L = 8192
NT = L // 128
NMEM = 256
CW = 512
HW_ = 512
COLS = 4608
NE = 16
CAP = 1024
DFF = 2048
EPS = 1e-5
ALPHA = 2.0 ** 0.25
NFFT = 16384
GC = 16
SAME_ENGINE_SYNC = True
STOP_AFTER = None
DEBUG_OUT = ()


class Buf:
    __slots__ = ("name", "w", "r", "dsem", "dcnt")

    def __init__(self, name):
        self.name = name
        self.w = None
        self.r = {}
        self.dsem = None
        self.dcnt = 0


class Tile(Buf):
    __slots__ = ("t",)

    def __init__(self, name, t):
        Buf.__init__(self, name)
        self.t = t

    def __getitem__(self, k):
        return self.t[k]


class Sched:
    ROT = 30000

    def __init__(self, nc, es):
        self.nc = nc
        self.es = es
        self.E = {"pe": nc.tensor, "act": nc.scalar, "dve": nc.vector, "pool": nc.gpsimd, "sp": nc.sync}
        self.csem = {}
        self.ccnt = {}
        self.waited = {k: {} for k in self.E}
        self.sems = []
        self.free_dsems = []
        self.latest = {}
        self.same_engine_sync = SAME_ENGINE_SYNC
        self.ninstr = {k: 0 for k in self.E}
        for k in ("pe", "act", "dve", "pool"):
            self._new_csem(k)

    def _alloc_sem(self, name):
        s = self.es.enter_context(self.nc.semaphore(name))
        self.sems.append(s)
        return s

    def _new_csem(self, k):
        self.csem[k] = self._alloc_sem("c_%s_%d" % (k, len(self.sems)))
        self.ccnt[k] = 0

    def _wait(self, eng, ev):
        if ev is None:
            return
        sem, val = ev
        if (not self.same_engine_sync) and eng in self.csem and sem is self.csem[eng]:
            return
        w = self.waited[eng]
        key = id(sem)
        if w.get(key, 0) >= val:
            return
        self.E[eng].wait_ge(sem, val)
        w[key] = val

    def _deps(self, eng, reads, writes):
        for b in reads:
            if b.w is not None:
                if eng == "pe" and b.w[0] is self.csem.get("pe") and False:
                    continue
                self._wait(eng, b.w)
        for b in writes:
            if b.w is not None:
                if eng == "pe" and b.w[0] is self.csem["pe"]:
                    pass
                else:
                    self._wait(eng, b.w)
            for sid, ev in b.r.items():
                if eng == "pe" and ev[0] is self.csem["pe"]:
                    continue
                self._wait(eng, ev)

    def _mark(self, ev, reads, writes):
        self.latest[id(ev[0])] = ev
        for b in reads:
            b.r[id(ev[0])] = ev
        for b in writes:
            b.w = ev
            b.r = {}

    def op(self, eng, fn, reads=(), writes=()):
        if self.ccnt[eng] >= self.ROT:
            self._new_csem(eng)
        self._deps(eng, reads, writes)
        ins = fn(self.E[eng])
        self.ccnt[eng] += 1
        sem = self.csem[eng]
        ins.then_inc(sem, 1)
        self.ninstr[eng] += 1
        self._mark((sem, self.ccnt[eng]), reads, writes)

    def pe(self, fn, r=(), w=()):
        self.op("pe", fn, r, w)

    def act(self, fn, r=(), w=()):
        self.op("act", fn, r, w)

    def dve(self, fn, r=(), w=()):
        self.op("dve", fn, r, w)

    def pool(self, fn, r=(), w=()):
        self.op("pool", fn, r, w)

    def dma(self, q, fn, reads=(), writes=(), owner=None):
        self._deps(q, reads, writes)
        if owner.dsem is None:
            if self.free_dsems:
                owner.dsem, owner.dcnt = self.free_dsems.pop()
            else:
                owner.dsem = self._alloc_sem("d_%s_%d" % (owner.name, len(self.sems)))
                owner.dcnt = 0
        ins = fn(self.E[q])
        owner.dcnt += 16
        ins.then_inc(owner.dsem, 16)
        self.ninstr[q] += 1
        self._mark((owner.dsem, owner.dcnt), reads, writes)

    def release(self, tiles):
        for t in tiles:
            if t.dsem is not None:
                self.free_dsems.append((t.dsem, t.dcnt))
                t.dsem = None

    def barrier(self):
        evs = list(self.latest.values())
        for eng in self.E:
            for ev in evs:
                self._wait(eng, ev)


class Phase:
    def __init__(self, S, name):
        self.S = S
        self.nc = S.nc
        self.name = name
        self.es = ExitStack()
        self.tiles = []
        self.n = 0

    def __enter__(self):
        self.es.__enter__()
        return self

    def __exit__(self, *a):
        self.S.barrier()
        self.S.release(self.tiles)
        return self.es.__exit__(*a)

    def sb(self, name, shape, dt):
        self.n += 1
        t = self.es.enter_context(self.nc.sbuf_tensor("%s_%s_%d" % (self.name, name, self.n), list(shape), dt))
        tl = Tile(name, t)
        self.tiles.append(tl)
        return tl

    def ps(self, name, shape, dt=F32):
        self.n += 1
        t = self.es.enter_context(self.nc.psum_tensor("%s_%s_%d" % (self.name, name, self.n), list(shape), dt))
        tl = Tile(name, t)
        self.tiles.append(tl)
        return tl

    def ring(self, name, shape, dt, n, ps=False):
        return [(self.ps if ps else self.sb)("%s%d" % (name, i), shape, dt) for i in range(n)]


def build_nc():
    nc = bass.Bass("TRN2", target_bir_lowering=False)
    dram = {}

    def din(name, shape, dt=F32):
        dram[name] = nc.dram_tensor(name, list(shape), dt, kind="ExternalInput").ap()
        return dram[name]

    def dscr(name, shape, dt):
        kind = "ExternalOutput" if name in DEBUG_OUT else "Internal"
        dram[name] = nc.dram_tensor(name, list(shape), dt, kind=kind).ap()
        return dram[name]

    x_d = din("x", [L, D])
    mem_d = din("mem", [NMEM, D])
    ln_in_g = din("ln_in_g", [D, 1]); ln_in_b = din("ln_in_b", [D, 1])
    w_in = din("w_in", [D, COLS])
    b_gate = din("b_gate", [2 * D, 1])
    conf_dw_w = din("conf_dw_w", [31, CW]); conf_dw_b = din("conf_dw_b", [CW, 1])
    conf_ln_g = din("conf_ln_g", [CW, 1]); conf_ln_b = din("conf_ln_b", [CW, 1])
    conf_w_out = din("conf_w_out", [CW, D])
    hy_short_w = din("hy_short_w", [3, 3 * HW_]); hy_short_b = din("hy_short_b", [3 * HW_, 1])
    hy_w1 = din("hy_ffn_w1", [33, 64]); hy_b1 = din("hy_ffn_b1", [64, 1]); hy_f1 = din("hy_freq1", [64, 1])
    hy_w2 = din("hy_ffn_w2", [64, 64]); hy_b2 = din("hy_ffn_b2", [64, 1]); hy_f2 = din("hy_freq2", [64, 1])
    hy_w3 = din("hy_ffn_w3", [64, 2048])
    hy_skip = din("hy_skip", [1, 2 * HW_])
    hy_w_out = din("hy_w_out", [HW_, D])
    w_mix = din("w_mix_out", [D, D])
    ln_mix_g = din("ln_mix_g", [1, D]); ln_mix_b = din("ln_mix_b", [1, D])
    xa_wq = din("xa_wq", [D, D]); xa_wk = din("xa_wk", [D, D]); xa_wv = din("xa_wv", [D, D]); xa_wo = din("xa_wo", [D, D])
    ln_xa_g = din("ln_xa_g", [1, D]); ln_xa_b = din("ln_xa_b", [1, D])
    w_router = din("moe_w_router", [D, NE])
    if STOP_AFTER in ("A", "A2", "H", "C", "C1", "E"):
        w_gate = w_up = w_down = None
    else:
        w_gate = din("moe_w_gate", [NE, D, DFF]); w_up = din("moe_w_up", [NE, D, DFF]); w_down = din("moe_w_down", [NE, DFF, D])
    ln_moe_g = din("ln_moe_g", [1, D]); ln_moe_b = din("ln_moe_b", [1, D])
    ln_in_g_row = din("ln_in_g_row", [1, D]); ln_in_b_row = din("ln_in_b_row", [1, D])
    c_F = din("c_F", [128, 256]); c_T = din("c_T", [128, 256])
    c_featT = din("c_featT", [33, NFFT]); c_tts = din("c_tts", [128, 128])
    c_ident = din("c_ident", [128, 128]); c_iota = din("c_iota", [128, CAP])
    c_tidx = din("c_tidx", [128, NT, 2])
    c_blk8 = din("c_blk8", [128, 128])
    out_d = nc.dram_tensor("out", [L, D], F32, kind="ExternalOutput").ap()

    uT_d = dscr("uT_d", [CW, L], BF16)
    hyT_d = dscr("hyT_d", [3 * HW_, L], BF16)
    gT_d = dscr("gT_d", [2 * D, L], BF16)
    ucT_d = dscr("ucT_d", [CW, L], F32)
    hycT_d = dscr("hycT_d", [3 * HW_, L], BF16)
    zT_d = dscr("zT_d", [HW_, L], BF16)
    x2b_d = dscr("x2b_d", [L, D], BF16)
    acc_d = dscr("acc_d", [L, D], F32)
    affT_d = dscr("affT_d", [NE, L], F32)
    dbg_d = dscr("dbg_d", [128, 4096], F32)
    x1_d = dscr("x1_d", [L, D], F32)
    xg_d = dscr("xg_d", [L, D], F32)

    deltas = np.abs(np.linspace(math.log(1e-2) / 1.5, math.log(1e-2) / 0.3, HW_, dtype=np.float32)).astype(np.float64)

    with ExitStack() as es:
        S = Sched(nc, es)
        DB = {k: Buf(k) for k in ("uT", "hyT", "gT", "ucT", "hycT", "zT", "x2b", "acc", "affT", "dbg")}
        dbt = {}

        def dbuf(name, i):
            k = (name, i)
            if k not in dbt:
                dbt[k] = Buf("%s_%s" % (name, i))
            return dbt[k]

        with Phase(S, "G") as G:
            ident = G.sb("ident", [128, 128], F32)
            identb = G.sb("identb", [128, 128], BF16)
            S.dma("sp", lambda q: q.dma_start(out=ident[:], in_=c_ident[:, :]), writes=[ident], owner=ident)
            S.dve(lambda e: e.tensor_copy(identb[:], ident[:]), [ident], [identb])
            ones = G.sb("ones", [128, 128], F32)
            S.dve(lambda e: e.memset(ones[:], 1.0), [], [ones])

            phase_A(S, nc, locals())
            if STOP_AFTER != "A":
                phase_A2(S, nc, locals())
            if STOP_AFTER not in ("A", "A2"):
                phase_H(S, nc, locals())
            if STOP_AFTER not in ("A", "A2", "H"):
                with Phase(S, "G2") as G2:
                    affT = G2.sb("affT", [NE, L], F32)
                    phase_CD(S, nc, locals())
                    if STOP_AFTER not in ("C", "C1"):
                        phase_EF(S, nc, locals())
            if STOP_AFTER not in ("A", "A2", "H", "C", "C1", "E", "F"):
                phase_G(S, nc, locals())
            S.barrier()
        print("instr counts", S.ninstr, "sems", len(S.sems))
    build_nc.in_names = [k for k in dram if k not in ("uT_d", "hyT_d", "gT_d", "ucT_d", "hycT_d", "zT_d", "x2b_d", "acc_d", "affT_d", "dbg_d", "x1_d", "xg_d", "out")]
    return nc


def load_cast(S, P, q_dst, src_ap, shape, name, stage=None, eng="act", q="sp"):
    dst_tile, dst_ap = q_dst
    st = stage if stage is not None else P.sb(name + "_st", shape, F32)
    S.dma(q, lambda q_: q_.dma_start(out=st[:], in_=src_ap), writes=[st], owner=st)
    if eng == "act":
        S.act(lambda e: e.copy(dst_ap, st[:]), [st], [dst_tile])
    elif eng == "pool":
        S.pool(lambda e: e.tensor_copy(dst_ap, st[:]), [st], [dst_tile])
    else:
        S.dve(lambda e: e.tensor_copy(dst_ap, st[:]), [st], [dst_tile])


def ln_stats(S, P, xt, rstd_name="rs"):
    st = P.sb("bnst", [128, 2, 6], F32)
    mv = P.sb("bnmv", [128, 2], F32)
    rs = P.sb(rstd_name, [128, 1], F32)
    return st, mv, rs


def emit_ln_stats(S, xt_tile, x_ap, st, mv, rs):
    for h in range(2):
        S.dve(lambda e, h=h: e.bn_stats(st[:, h, :], x_ap[:, h * 512:(h + 1) * 512]), [xt_tile], [st])
    S.dve(lambda e: e.bn_aggr(mv[:], st[:].rearrange("p a b -> p (a b)")), [st], [mv])
    S.act(lambda e: e.activation(rs[:], mv[:, 1:2], AF.Sqrt, bias=EPS_AP[0][:], scale=1.0), [mv, EPS_AP[1]], [rs])
    S.dve(lambda e: e.reciprocal(rs[:], rs[:]), [rs], [rs])


EPS_AP = [None, None]


def phase_A(S, nc, env):
    x_d = env["x_d"]; w_in = env["w_in"]; ident = env["ident"]
    uT_d = env["uT_d"]; hyT_d = env["hyT_d"]; gT_d = env["gT_d"]; dbuf = env["dbuf"]
    G = env["G"]
    epsT = G.sb("epsT", [128, 1], F32)
    S.dve(lambda e: e.memset(epsT[:], EPS), [], [epsT])
    EPS_AP[0] = epsT; EPS_AP[1] = epsT
    with Phase(S, "A") as P:
        wsb = P.sb("w_in", [128, 8, COLS], BF16)
        stg = P.ring("wst", [128, 1152], F32, 4)
        i = 0
        for dk in range(8):
            for cq in range(4):
                st = stg[i % 4]
                load_cast(S, P, (wsb, wsb[:, dk, cq * 1152:(cq + 1) * 1152]),
                          w_in[dk * 128:(dk + 1) * 128, cq * 1152:(cq + 1) * 1152], None, "w", stage=st,
                          eng=("act" if i % 2 == 0 else "dve"), q=("sp" if i % 2 == 0 else "pool"))
                i += 1
        gsc = P.sb("gsc", [128, 8], F32); gbi = P.sb("gbi", [128, 8], F32); bg = P.sb("bg", [128, 16], F32)
        S.dma("sp", lambda q: q.dma_start(out=gsc[:], in_=env["ln_in_g"].rearrange("(k p) o -> p (k o)", p=128), allow_slow_non_contiguous=True), writes=[gsc], owner=gsc)
        S.dma("sp", lambda q: q.dma_start(out=gbi[:], in_=env["ln_in_b"].rearrange("(k p) o -> p (k o)", p=128), allow_slow_non_contiguous=True), writes=[gbi], owner=gbi)
        S.dma("sp", lambda q: q.dma_start(out=bg[:], in_=env["b_gate"].rearrange("(k p) o -> p (k o)", p=128), allow_slow_non_contiguous=True), writes=[bg], owner=bg)
        xts = P.ring("xt", [128, D], F32, 8)
        AG = row_bcast(S, P, "AG", env["ln_in_g_row"], ALPHA); AB = row_bcast(S, P, "AB", env["ln_in_b_row"], ALPHA)
        xgs = P.ring("xgs", [128, D], F32, 2)
        xg_d = env["xg_d"]
        nhs = P.ring("nh", [128, D], F32, 5)
        sts = [ln_stats(S, P, None) for _ in range(2)]
        hT = P.ring("hT", [128, 8, 512], BF16, 2)
        pst = P.ring("pst", [128, 512], F32, 2, ps=True)
        pmm = P.ring("pmm", [128, 512], F32, 4, ps=True)
        sig = P.ring("sig", [128, 512], F32, 2)
        ob = P.ring("ob", [128, 512], BF16, 4)
        xi = 0; oi = 0; pi = 0
        for blk in range(L // 512):
            h = hT[blk % 2]
            nts = []
            if blk == 0:
                for tt in range(4):
                    S.dma("sp", lambda q, tt=tt: q.dma_start(out=xts[tt][:], in_=x_d[tt * 128:(tt + 1) * 128, :]), writes=[xts[tt]], owner=xts[tt])
            if blk + 1 < L // 512:
                for tt in range(4):
                    xn_ = xts[((blk + 1) * 4 + tt) % 8]; tn = (blk + 1) * 512 + tt * 128
                    S.dma("sp", lambda q, xn_=xn_, tn=tn: q.dma_start(out=xn_[:], in_=x_d[tn:tn + 128, :]), writes=[xn_], owner=xn_)
            for tt in range(4):
                t0 = blk * 512 + tt * 128
                xt = xts[xi % 8]; nh = nhs[xi % 5]; st, mv, rs = sts[xi % 2]; xi += 1
                emit_ln_stats(S, xt, xt.t, st, mv, rs)
                S.dve(lambda e, nh=nh, xt=xt, mv=mv, rs=rs: e.tensor_scalar(nh[:], xt[:], mv[:, 0:1], rs[:], op0=ALU.subtract, op1=ALU.mult), [xt, mv, rs], [nh])
                xg = xgs[xi % 2]
                S.dve(lambda e, xg=xg, nh=nh: e.tensor_tensor(xg[:], nh[:], AG[:], op=ALU.mult), [nh, AG], [xg])
                S.dve(lambda e, xg=xg: e.tensor_tensor(xg[:], xg[:], AB[:], op=ALU.add), [xg, AB], [xg])
                S.dma("pool", lambda q, xg=xg, t0=t0: q.dma_start(out=xg_d[t0:t0 + 128, :], in_=xg[:]), reads=[xg], writes=[dbuf("xg", t0)], owner=xg)
                nts.append(nh)
            for dk in range(8):
                pt = pst[dk % 2]
                for tt in range(4):
                    S.pe(lambda e, pt=pt, tt=tt, dk=dk: e.transpose(pt[:, tt * 128:(tt + 1) * 128], nts[tt][:, dk * 128:(dk + 1) * 128], ident[:]), [nts[tt], ident], [pt])
                S.act(lambda e, pt=pt, dk=dk: e.activation(h[:, dk, :], pt[:], AF.Identity, bias=gbi[:, dk:dk + 1], scale=gsc[:, dk:dk + 1]), [pt, gbi, gsc], [h])

            def proj(cc):
                nonlocal pi
                pm = pmm[pi % 4]; pi += 1
                for dk in range(8):
                    S.pe(lambda e, pm=pm, dk=dk, cc=cc: e.matmul(pm[:], wsb[:, dk, cc * 128:(cc + 1) * 128], h[:, dk, :], start=(dk == 0), stop=(dk == 7)), [wsb, h], [pm])
                return pm
            tsl = slice(blk * 512, (blk + 1) * 512)
            for cc in range(4):
                pa = proj(cc); pb = proj(cc + 4)
                sg = sig[cc % 2]; o = ob[oi % 4]; oi += 1
                S.act(lambda e, sg=sg, pb=pb: e.activation(sg[:], pb[:], AF.Sigmoid), [pb], [sg])
                S.dve(lambda e, o=o, pa=pa, sg=sg: e.tensor_tensor(o[:], pa[:], sg[:], op=ALU.mult), [pa, sg], [o])
                S.dma("pool", lambda q, o=o, cc=cc: q.dma_start(out=uT_d[cc * 128:(cc + 1) * 128, tsl], in_=o[:]), reads=[o], writes=[dbuf("uT", cc)], owner=o)
            for cc in range(8, 20):
                pm = proj(cc); o = ob[oi % 4]; oi += 1
                S.act(lambda e, o=o, pm=pm: e.copy(o[:], pm[:]), [pm], [o])
                S.dma("pool", lambda q, o=o, cc=cc: q.dma_start(out=hyT_d[(cc - 8) * 128:(cc - 7) * 128, tsl], in_=o[:]), reads=[o], writes=[dbuf("hyT", cc - 8)], owner=o)
            for cc in range(20, 36):
                pm = proj(cc); o = ob[oi % 4]; oi += 1
                S.act(lambda e, o=o, pm=pm, cc=cc: e.activation(o[:], pm[:], AF.Sigmoid, bias=bg[:, cc - 20:cc - 19], scale=1.0), [pm, bg], [o])
                S.dma("pool", lambda q, o=o, cc=cc: q.dma_start(out=gT_d[(cc - 20) * 128:(cc - 19) * 128, tsl], in_=o[:]), reads=[o], writes=[dbuf("gT", (cc - 20, blk))], owner=o)


def phase_A2(S, nc, env):
    identb = env["identb"]; dbuf = env["dbuf"]
    uT_d = env["uT_d"]; hyT_d = env["hyT_d"]; ucT_d = env["ucT_d"]; hycT_d = env["hycT_d"]
    with Phase(S, "A2") as P:
        rows = P.ring("row", [128, L + 32], BF16, 2)
        for r in rows:
            S.pool(lambda e, r=r: e.memset(r[:, 0:16], 0.0), [], [r])
            S.pool(lambda e, r=r: e.memset(r[:, L + 16:L + 32], 0.0), [], [r])
        wT = P.sb("wT", [128, 4, 31], F32); wT3 = P.sb("wT3", [128, 12, 3], F32)
        cb = P.sb("cb", [128, 4], F32); cb3 = P.sb("cb3", [128, 12], F32)
        for c in range(4):
            S.dma("sp", lambda q, c=c: q.dma_start(out=wT[:, c, :], in_=env["conf_dw_w"][:, c * 128:(c + 1) * 128].rearrange("k p -> p k"), allow_slow_non_contiguous=True), writes=[wT], owner=wT)
        for c in range(12):
            S.dma("sp", lambda q, c=c: q.dma_start(out=wT3[:, c, :], in_=env["hy_short_w"][:, c * 128:(c + 1) * 128].rearrange("k p -> p k"), allow_slow_non_contiguous=True), writes=[wT3], owner=wT3)
        S.dma("sp", lambda q: q.dma_start(out=cb[:], in_=env["conf_dw_b"].rearrange("(c p) o -> p (c o)", p=128), allow_slow_non_contiguous=True), writes=[cb], owner=cb)
        S.dma("sp", lambda q: q.dma_start(out=cb3[:], in_=env["hy_short_b"].rearrange("(c p) o -> p (c o)", p=128), allow_slow_non_contiguous=True), writes=[cb3], owner=cb3)
        dg = P.ring("dg", [128, 31, 128], BF16, 2)
        pc = P.ring("pc", [128, 512], F32, 3, ps=True)
        of = P.ring("of", [128, 512], F32, 3)
        obf = P.ring("obf", [128, 512], BF16, 3)
        pi = 0; ri = 0
        jobs = [("u", c) for c in range(4)] + [("h", c) for c in range(12)]
        def prep(j):
            kind, c = jobs[j]
            row = rows[j % 2]; dgt = dg[j % 2]
            K = 31 if kind == "u" else 3
            src = uT_d if kind == "u" else hyT_d
            S.dma("sp", lambda q: q.dma_start(out=row[:, 16:16 + L], in_=src[c * 128:(c + 1) * 128, :]),
                  reads=[dbuf("uT" if kind == "u" else "hyT", c)], writes=[row], owner=row)
            wt = wT if kind == "u" else wT3
            for k in range(K):
                S.dve(lambda e, k=k: e.tensor_scalar(dgt[:, k, :], identb[:], wt[:, c, k:k + 1], None, op0=ALU.mult), [identb, wt], [dgt])
        prep(0)
        for j, (kind, c) in enumerate(jobs):
            row = rows[j % 2]; dgt = dg[j % 2]
            K = 31 if kind == "u" else 3
            pad = (K - 1) // 2
            if j + 1 < len(jobs):
                prep(j + 1)
            for blk in range(L // 512):
                p = pc[pi % 3]
                for k in range(K):
                    off = 16 + blk * 512 + k - pad
                    S.pe(lambda e, p=p, k=k, off=off, dgt=dgt, row=row: e.matmul(p[:], dgt[:, k, :], row[:, off:off + 512], start=(k == 0), stop=(k == K - 1)), [dgt, row], [p])
                tsl = slice(blk * 512, (blk + 1) * 512)
                if kind == "u":
                    o = of[pi % 3]
                    S.act(lambda e, o=o, p=p, c=c: e.activation(o[:], p[:], AF.Identity, bias=cb[:, c:c + 1], scale=1.0), [p, cb], [o])
                    S.dma("sp", lambda q, o=o, c=c, tsl=tsl: q.dma_start(out=ucT_d[c * 128:(c + 1) * 128, tsl], in_=o[:]), reads=[o], writes=[dbuf("ucT", blk)], owner=o)
                else:
                    o = obf[pi % 3]
                    S.act(lambda e, o=o, p=p, c=c: e.activation(o[:], p[:], AF.Identity, bias=cb3[:, c:c + 1], scale=1.0), [p, cb3], [o])
                    S.dma("sp", lambda q, o=o, c=c, tsl=tsl: q.dma_start(out=hycT_d[c * 128:(c + 1) * 128, tsl], in_=o[:]), reads=[o], writes=[dbuf("hycT", c)], owner=o)
                pi += 1


def phase_H(S, nc, env):
    hycT_d = env["hycT_d"]; zT_d = env["zT_d"]; ones = env["ones"]; deltas = env["deltas"]
    c_F = env["c_F"]; c_T = env["c_T"]; c_featT = env["c_featT"]; c_tts = env["c_tts"]
    INVN = 1.0 / NFFT
    with Phase(S, "H") as P:
        Fst = P.sb("Fst", [128, 256], F32)
        Tst = P.sb("Tst", [128, 256], F32)
        S.dma("sp", lambda q: q.dma_start(out=Fst[:], in_=c_F[:, :]), writes=[Fst], owner=Fst)
        S.dma("sp", lambda q: q.dma_start(out=Tst[:], in_=c_T[:, :]), writes=[Tst], owner=Tst)
        Fb = P.sb("Fb", [128, 256], BF16); FA = P.sb("FA", [128, 256], BF16); FB_ = P.sb("FB", [128, 256], BF16)
        Fin = P.sb("Fin", [128, 128], BF16)
        S.dve(lambda e: e.tensor_copy(Fb[:], Fst[:]), [Fst], [Fb])
        S.dve(lambda e: e.tensor_copy(FA[:, 0:128], Fst[:, 0:128]), [Fst], [FA])
        S.dve(lambda e: e.tensor_scalar(FA[:, 128:256], Fst[:, 128:256], -1.0, None, op0=ALU.mult), [Fst], [FA])
        S.dve(lambda e: e.tensor_copy(FB_[:, 0:128], Fst[:, 128:256]), [Fst], [FB_])
        S.dve(lambda e: e.tensor_copy(FB_[:, 128:256], Fst[:, 0:128]), [Fst], [FB_])
        S.dve(lambda e: e.tensor_scalar(Fin[:], Fst[:, 128:256], -1.0, None, op0=ALU.mult), [Fst], [Fin])
        TT1 = P.sb("TT1", [128, 2, 256], F32); TT2 = P.sb("TT2", [128, 2, 256], F32)
        for i in range(2):
            for hh in range(2):
                S.dve(lambda e, i=i, hh=hh: e.tensor_copy(TT1[:, i, hh * 128:(hh + 1) * 128], Tst[:, 0:128]), [Tst], [TT1])
                S.dve(lambda e, i=i, hh=hh: e.tensor_copy(TT2[:, i, hh * 128:(hh + 1) * 128], Tst[:, 128:256]), [Tst], [TT2])
        NH = 66
        FbH = P.sb("FbH", [128, 2 * NH], BF16)
        S.dve(lambda e: e.tensor_copy(FbH[:, 0:NH], Fst[:, 0:NH]), [Fst], [FbH])
        S.dve(lambda e: e.tensor_copy(FbH[:, NH:2 * NH], Fst[:, 128:128 + NH]), [Fst], [FbH])
        TH1 = P.sb("TH1", [128, 2, 2 * NH], F32); TH2 = P.sb("TH2", [128, 2, 2 * NH], F32)
        TK1 = P.sb("TK1", [128, 2, 2 * NH], F32); TK2 = P.sb("TK2", [128, 2, 2 * NH], F32)
        for i in range(2):
            for hh in range(2):
                S.dve(lambda e, i=i, hh=hh: e.tensor_copy(TH1[:, i, hh * NH:(hh + 1) * NH], Tst[:, 0:NH]), [Tst], [TH1])
                S.dve(lambda e, i=i, hh=hh: e.tensor_copy(TH2[:, i, hh * NH:(hh + 1) * NH], Tst[:, 128:128 + NH]), [Tst], [TH2])
        for src_, dst_ in ((TH1, TK1), (TH2, TK2)):
            S.dve(lambda e, src_=src_, dst_=dst_: e.tensor_scalar(dst_[:], src_[:], 2.0, None, op0=ALU.mult), [src_], [dst_])
            for i in range(2):
                for hh in range(2):
                    b0 = hh * NH
                    S.dve(lambda e, src_=src_, dst_=dst_, i=i, b0=b0: e.tensor_copy(dst_[:, i, b0:b0 + 1], src_[:, i, b0:b0 + 1]), [src_], [dst_])
                    S.dve(lambda e, src_=src_, dst_=dst_, i=i, b0=b0: e.tensor_copy(dst_[:, i, b0 + 64:b0 + 65], src_[:, i, b0 + 64:b0 + 65]), [src_], [dst_])
                    S.dve(lambda e, dst_=dst_, i=i, b0=b0: e.memset(dst_[:, i, b0 + 65:b0 + 66], 0.0), [], [dst_])
        tts = P.sb("tts", [128, 128], F32)
        S.dma("sp", lambda q: q.dma_start(out=tts[:], in_=c_tts[:, :]), writes=[tts], owner=tts)
        e6 = P.sb("e6", [128, 1], F32)
        S.dve(lambda e: e.memset(e6[:], 1e-6), [], [e6])
        H2 = P.sb("H2", [128, NFFT], BF16)
        S.pool(lambda e: e.memset(H2[:], 0.0), [], [H2])
        w1 = P.sb("w1", [33, 64], F32); w2d = P.sb("w2d", [64, 128], F32)
        S.dma("sp", lambda q: q.dma_start(out=w1[:], in_=env["hy_w1"][:, :]), writes=[w1], owner=w1)
        S.dma("sp", lambda q: q.dma_start(out=w2d[:, 0:64], in_=env["hy_w2"][:, :]), writes=[w2d], owner=w2d)
        S.dma("sp", lambda q: q.dma_start(out=w2d[:, 64:128], in_=env["hy_w2"][:, :]), writes=[w2d], owner=w2d)
        fb = P.sb("fb", [128, 4], F32)
        S.dma("sp", lambda q: q.dma_start(out=fb[0:64, 0:1], in_=env["hy_f1"][:, :]), writes=[fb], owner=fb)
        S.dma("sp", lambda q: q.dma_start(out=fb[0:64, 1:2], in_=env["hy_b1"][:, :]), writes=[fb], owner=fb)
        for hh in range(2):
            S.dma("sp", lambda q, hh=hh: q.dma_start(out=fb[hh * 64:(hh + 1) * 64, 2:3], in_=env["hy_f2"][:, :]), writes=[fb], owner=fb)
            S.dma("sp", lambda q, hh=hh: q.dma_start(out=fb[hh * 64:(hh + 1) * 64, 3:4], in_=env["hy_b2"][:, :]), writes=[fb], owner=fb)
        S.dma("sp", lambda q: q.dma_start(out=fb[64:128, 0:1], in_=env["hy_f1"][:, :]), writes=[fb], owner=fb)
        S.dma("sp", lambda q: q.dma_start(out=fb[64:128, 1:2], in_=env["hy_b1"][:, :]), writes=[fb], owner=fb)
        sb_ = P.sb("sb", [128, 4], F32)
        S.dve(lambda e: e.tensor_scalar(sb_[:, 0:1], fb[:, 0:1], 1.0 / 3.0, None, op0=ALU.mult), [fb], [sb_])
        S.dve(lambda e: e.tensor_tensor(sb_[:, 1:2], fb[:, 0:1], fb[:, 1:2], op=ALU.mult), [fb], [sb_])
        S.dve(lambda e: e.tensor_scalar(sb_[:, 1:2], sb_[:, 1:2], 1.0 / 3.0, None, op0=ALU.mult), [sb_], [sb_])
        S.dve(lambda e: e.tensor_scalar(sb_[:, 2:3], fb[:, 2:3], 1.0 / 3.0, None, op0=ALU.mult), [fb], [sb_])
        S.dve(lambda e: e.tensor_tensor(sb_[:, 3:4], fb[:, 2:3], fb[:, 3:4], op=ALU.mult), [fb], [sb_])
        S.dve(lambda e: e.tensor_scalar(sb_[:, 3:4], sb_[:, 3:4], 1.0 / 3.0, None, op0=ALU.mult), [sb_], [sb_])
        bank = P.ring("bk", [128, 512], F32, 8, ps=True)
        PF = Phase(S, "HF"); PF.__enter__()
        fts = PF.ring("ft", [33, 512], F32, 2)
        s1 = PF.ring("s1", [128, 512], F32, 2); qq = PF.ring("qq", [128, 512], F32, 2); h1 = PF.ring("h1", [64, 512], F32, 2)
        for blk in range(NFFT // 512):
            ft = fts[blk % 2]; s = s1[blk % 2]; q_ = qq[blk % 2]; hh1 = h1[blk % 2]
            pb1 = bank[blk % 2]; pb2 = bank[2 + blk % 2]
            S.dma("sp", lambda q, ft=ft, blk=blk: q.dma_start(out=ft[:], in_=c_featT[:, blk * 512:(blk + 1) * 512]), writes=[ft], owner=ft)
            S.pe(lambda e, pb1=pb1, ft=ft: e.matmul(pb1[0:64, :], w1[:], ft[:], start=True, stop=True), [w1, ft], [pb1])
            S.act(lambda e, s=s, pb1=pb1: e.activation(s[0:64, :], pb1[0:64, :], AF.Sin, bias=sb_[0:64, 1:2], scale=sb_[0:64, 0:1]), [pb1, sb_], [s])
            S.dve(lambda e, s=s, q_=q_: e.tensor_tensor(q_[0:64, :], s[0:64, :], s[0:64, :], op=ALU.mult), [s], [q_])
            S.dve(lambda e, q_=q_: e.tensor_scalar(q_[0:64, :], q_[0:64, :], -4.0, 3.0, op0=ALU.mult, op1=ALU.add), [q_], [q_])
            S.dve(lambda e, s=s, q_=q_, hh1=hh1: e.tensor_tensor(hh1[:], q_[0:64, :], s[0:64, :], op=ALU.mult), [s, q_], [hh1])
            S.pe(lambda e, pb2=pb2, hh1=hh1: e.matmul(pb2[:], w2d[:], hh1[:], start=True, stop=True), [w2d, hh1], [pb2])
            lo = 0 if blk < 16 else 64
            S.act(lambda e, s=s, pb2=pb2, lo=lo: e.activation(s[lo:lo + 64, :], pb2[lo:lo + 64, :], AF.Sin, bias=sb_[lo:lo + 64, 3:4], scale=sb_[lo:lo + 64, 2:3]), [pb2, sb_], [s])
            S.dve(lambda e, s=s, q_=q_, lo=lo: e.tensor_tensor(q_[lo:lo + 64, :], s[lo:lo + 64, :], s[lo:lo + 64, :], op=ALU.mult), [s], [q_])
            S.dve(lambda e, q_=q_, lo=lo: e.tensor_scalar(q_[lo:lo + 64, :], q_[lo:lo + 64, :], -4.0, 3.0, op0=ALU.mult, op1=ALU.add), [q_], [q_])
            S.dve(lambda e, s=s, q_=q_, lo=lo, blk=blk: e.tensor_tensor(H2[lo:lo + 64, blk * 512:(blk + 1) * 512], q_[lo:lo + 64, :], s[lo:lo + 64, :], op=ALU.mult), [s, q_], [H2])
        S.dve(lambda e: e.memset(H2[64:128, L:L + 1], 0.0), [], [H2])
        PF.__exit__(None, None, None)
        w3st = P.sb("w3st", [128, 2, 512], F32)
        w3v = env["hy_w3"].rearrange("m (o d c) -> m o d c", o=2, d=2)
        S.dma("sp", lambda q: q.dma_start(out=w3st[0:64, :, :], in_=w3v[:, :, 0, :]), writes=[w3st], owner=w3st)
        S.dma("sp", lambda q: q.dma_start(out=w3st[64:128, :, :], in_=w3v[:, :, 1, :]), writes=[w3st], owner=w3st)
        W3s = P.sb("W3s", [128, 2, 512], BF16)
        S.dve(lambda e: e.tensor_copy(W3s[:], w3st[:]), [w3st], [W3s])
        skp = P.sb("skp", [1, 2, 512], F32)
        S.dma("sp", lambda q: q.dma_start(out=skp[:], in_=env["hy_skip"].rearrange("o (a c) -> o a c", a=2)), writes=[skp], owner=skp)

        QC = 4
        NW = 3

        class Lane:
            pass

        lanes = []
        for l in range(NW):
            ln = Lane()
            ln.kBre = P.sb("kBre", [128, 2 * QC, NH], BF16); ln.kBim = P.sb("kBim", [128, 2 * QC, NH], BF16)
            ln.Kr = P.sb("Kr", [128, 2 * QC, NH], F32); ln.Ki = P.sb("Ki", [128, 2 * QC, NH], F32)
            ln.q1h = P.ring("q1h", [128, 2, 2 * NH], F32, 2); ln.q2h = P.ring("q2h", [128, 2, 2 * NH], F32, 2)
            ln.q1 = P.ring("q1", [128, 2, 256], F32, 2); ln.q2 = P.ring("q2", [128, 2, 256], F32, 2)
            ln.t = [P.sb("t%d" % i, [128, QC * NH], F32) for i in range(4)]
            ln.Uv = P.sb("Uv", [64, QC, 128], BF16); ln.G1 = P.sb("G1", [64, QC, 128], BF16); ln.G2 = P.sb("G2", [64, QC, 128], BF16)
            ln.Bre = P.sb("Bre", [128, QC, NH], BF16); ln.Bim = P.sb("Bim", [128, QC, NH], BF16)
            ln.Yre = P.sb("Yre", [128, QC, NH], BF16); ln.Yim = P.sb("Yim", [128, QC, NH], BF16)
            ln.Cre = P.sb("Cre", [128, QC, 128], BF16); ln.Cim = P.sb("Cim", [128, QC, 128], BF16)
            ln.Z1 = P.sb("Z1", [64, QC, 128], BF16); ln.Z2 = P.sb("Z2", [64, QC, 128], BF16)
            ln.pa = bank[2 * l]; ln.px = (bank[2 * l], bank[2 * l + 1])
            ln.qi = 0
            lanes.append(ln)
        pk = bank[6]; py = bank[7]
        Dg = P.sb("Dg", [128, GC, 128], F32)
        kt = P.sb("kt", [128, 2, GC, 128], F32)
        kbR = P.ring("kb", [128, 2, GC, 128], BF16, 2)
        junk = P.sb("junk", [128, 128], F32)
        ssq = P.sb("ssq", [128, 2 * GC], F32); scl = P.sb("scl", [128, 2 * GC], F32)

        def kblock_stages(g):
            c0 = g * GC
            kb = kbR[g % 2]
            st = []

            def k_dec():
                for c in range(GC):
                    S.act(lambda e, c=c: e.activation(Dg[:, c, :], tts[:], AF.Exp, scale=-float(deltas[c0 + c])), [tts], [Dg])
            st.append(k_dec)

            def k_gen(r):
                def f():
                    for j in range(16):
                        n2 = r * 16 + j
                        S.pe(lambda e, j=j, n2=n2: e.matmul(pk[:, j * 32:(j + 1) * 32], H2[:, n2:NFFT:128], W3s[:, :, c0:c0 + GC], start=True, stop=True), [H2, W3s], [pk])
                    pkv = pk[:].rearrange("p (n o c) -> p o c n", n=16, o=2)
                    for o in range(2):
                        S.dve(lambda e, o=o: e.tensor_tensor(kt[:, o, :, r * 16:(r + 1) * 16], pkv[:, o, :, :], Dg[:, :, r * 16:(r + 1) * 16], op=ALU.mult), [pk, Dg], [kt])
                return f
            for r in range(8):
                st.append(k_gen(r))

            def k_sq():
                for o in range(2):
                    for c in range(GC):
                        m = o * GC + c
                        S.act(lambda e, o=o, c=c, m=m: e.activation(junk[:], kt[:, o, c, :], AF.Square, accum_out=ssq[:, m:m + 1]), [kt], [junk, ssq])
            st.append(k_sq)

            def k_tot():
                S.pe(lambda e: e.matmul(pk[:, 0:2 * GC], ones[:], ssq[:], start=True, stop=True), [ones, ssq], [pk])
                S.act(lambda e: e.activation(scl[:], pk[:, 0:2 * GC], AF.Sqrt, bias=e6[:], scale=1.0), [pk, e6], [scl])
                S.dve(lambda e: e.reciprocal(scl[:], scl[:]), [scl], [scl])
            st.append(k_tot)

            def k_scale():
                for o in range(2):
                    for c in range(GC):
                        m = o * GC + c
                        S.act(lambda e, o=o, c=c, m=m: e.activation(kb[:, o, c, :], kt[:, o, c, :], AF.Copy, scale=scl[:, m:m + 1]), [kt, scl], [kb])
                S.dve(lambda e: e.tensor_tensor(kb[0:1, :, :, 0], kb[0:1, :, :, 0], skp[0:1, :, c0:c0 + GC], op=ALU.add), [kb, skp], [kb])
            st.append(k_scale)
            return st

        def twiddle_f(ln, dre, dim, m0, kern):
            pa = ln.pa
            q1 = ln.q1h[ln.qi % 2]; q2 = ln.q2h[ln.qi % 2]; ln.qi += 1
            pav = pa[:].rearrange("p (a b) -> p a b", a=2)[:, :, 0:2 * NH]
            T1 = TK1 if kern else TH1; T2 = TK2 if kern else TH2
            S.dve(lambda e: e.tensor_tensor(q1[:], pav, T1[:], op=ALU.mult), [pa, T1], [q1])
            S.dve(lambda e: e.tensor_tensor(q2[:], pav, T2[:], op=ALU.mult), [pa, T2], [q2])
            S.pool(lambda e: e.tensor_tensor(dre[:, m0:m0 + 2, :], q1[:, :, 0:NH], q2[:, :, NH:2 * NH], op=ALU.subtract), [q1, q2], [dre])
            S.pool(lambda e: e.tensor_tensor(dim[:, m0:m0 + 2, :], q2[:, :, 0:NH], q1[:, :, NH:2 * NH], op=ALU.add), [q1, q2], [dim])

        def twiddle_i(ln, dre, dim, m0):
            pa = ln.pa
            q1 = ln.q1[ln.qi % 2]; q2 = ln.q2[ln.qi % 2]; ln.qi += 1
            pav = pa[0:NH, :].rearrange("p (a b) -> p a b", a=2)
            S.dve(lambda e: e.tensor_tensor(q1[0:NH], pav, TT1[0:NH], op=ALU.mult), [pa, TT1], [q1])
            S.dve(lambda e: e.tensor_tensor(q2[0:NH], pav, TT2[0:NH], op=ALU.mult), [pa, TT2], [q2])
            S.pool(lambda e: e.tensor_tensor(dre[0:NH, m0:m0 + 2, :], q1[0:NH, :, 0:128], q2[0:NH, :, 128:256], op=ALU.add), [q1, q2], [dre])
            S.pool(lambda e: e.tensor_tensor(dim[0:NH, m0:m0 + 2, :], q1[0:NH, :, 128:256], q2[0:NH, :, 0:128], op=ALU.subtract), [q1, q2], [dim])

        def stage3(ln, bre, bim, m0):
            pxr, pxi = ln.px
            W4 = 4 * NH
            rr = bre[:, m0:m0 + 4, :]; ri = bim[:, m0:m0 + 4, :]
            S.pe(lambda e: e.matmul(pxr[:, 0:W4], Fb[:, 0:128], rr, start=True, stop=False), [Fb, bre], [pxr])
            S.pe(lambda e: e.matmul(pxr[:, 0:W4], Fin[:], ri, start=False, stop=True), [Fin, bim], [pxr])
            S.pe(lambda e: e.matmul(pxi[:, 0:W4], Fb[:, 0:128], ri, start=True, stop=False), [Fb, bim], [pxi])
            S.pe(lambda e: e.matmul(pxi[:, 0:W4], Fb[:, 128:256], rr, start=False, stop=True), [Fb, bre], [pxi])
            return pxr, pxi

        def item_stages(ln, it):
            c0 = it * QC
            st = []

            def s_load():
                for tl, r0 in ((ln.Uv, 2 * HW_ + c0), (ln.G1, c0), (ln.G2, HW_ + c0)):
                    S.dma("sp", lambda q, tl=tl, r0=r0: q.dma_start(out=tl[:], in_=hycT_d[r0:r0 + QC, :].rearrange("c (a b) -> a c b", b=128)), writes=[tl], owner=tl)
            st.append(s_load)
            kb = kbR[(c0 // GC) % 2]
            cl = c0 % GC

            def s_ks1(o):
                def f():
                    for p_ in range(QC // 2):
                        for i in range(2):
                            c = 2 * p_ + i
                            S.pe(lambda e, c=c, i=i: e.matmul(ln.pa[:, i * 256:i * 256 + 2 * NH], kb[:, o, cl + c, :], FbH[:], start=True, stop=True), [kb, FbH], [ln.pa])
                        twiddle_f(ln, ln.kBre, ln.kBim, o * QC + 2 * p_, True)
                return f
            st.append(s_ks1(0)); st.append(s_ks1(1))

            def s_ks3(o):
                def f():
                    pxr, pxi = stage3(ln, ln.kBre, ln.kBim, o * QC)
                    S.act(lambda e: e.activation(ln.Kr[:, o * QC:(o + 1) * QC, :].rearrange("p a b -> p (a b)"), pxr[:, 0:QC * NH], AF.Copy, scale=INVN), [pxr], [ln.Kr])
                    S.act(lambda e: e.activation(ln.Ki[:, o * QC:(o + 1) * QC, :].rearrange("p a b -> p (a b)"), pxi[:, 0:QC * NH], AF.Copy, scale=INVN), [pxi], [ln.Ki])
                return f
            st.append(s_ks3(0)); st.append(s_ks3(1))

            def conv_stages(o, U, Gt, Zt, last):
                def c1():
                    for p_ in range(QC // 2):
                        for i in range(2):
                            c = 2 * p_ + i
                            S.pe(lambda e, c=c, i=i: e.matmul(ln.pa[:, i * 256:i * 256 + 2 * NH], U[:, c, :], FbH[0:64, :], start=True, stop=True), [U, FbH], [ln.pa])
                        twiddle_f(ln, ln.Bre, ln.Bim, 2 * p_, False)

                def c2():
                    pxr, pxi = stage3(ln, ln.Bre, ln.Bim, 0)
                    t1, t2, t3, t4 = ln.t
                    kr = ln.Kr[:, o * QC:(o + 1) * QC, :].rearrange("p a b -> p (a b)"); ki = ln.Ki[:, o * QC:(o + 1) * QC, :].rearrange("p a b -> p (a b)")
                    W4 = QC * NH
                    S.dve(lambda e: e.tensor_tensor(t1[:], pxr[:, 0:W4], kr, op=ALU.mult), [pxr, ln.Kr], [t1])
                    S.dve(lambda e: e.tensor_tensor(t2[:], pxi[:, 0:W4], ki, op=ALU.mult), [pxi, ln.Ki], [t2])
                    S.dve(lambda e: e.tensor_tensor(t3[:], pxr[:, 0:W4], ki, op=ALU.mult), [pxr, ln.Ki], [t3])
                    S.dve(lambda e: e.tensor_tensor(t4[:], pxi[:, 0:W4], kr, op=ALU.mult), [pxi, ln.Kr], [t4])
                    S.pool(lambda e: e.tensor_tensor(ln.Yre[:].rearrange("p a b -> p (a b)"), t1[:], t2[:], op=ALU.subtract), [t1, t2], [ln.Yre])
                    S.pool(lambda e: e.tensor_tensor(ln.Yim[:].rearrange("p a b -> p (a b)"), t3[:], t4[:], op=ALU.add), [t3, t4], [ln.Yim])

                def c3():
                    for p_ in range(QC // 2):
                        for i in range(2):
                            c = 2 * p_ + i
                            S.pe(lambda e, c=c, i=i: e.matmul(ln.pa[0:NH, i * 256:(i + 1) * 256], ln.Yre[:, c, :], FA[:], start=True, stop=False), [ln.Yre, FA], [ln.pa])
                            S.pe(lambda e, c=c, i=i: e.matmul(ln.pa[0:NH, i * 256:(i + 1) * 256], ln.Yim[:, c, :], FB_[:], start=False, stop=True), [ln.Yim, FB_], [ln.pa])
                        twiddle_i(ln, ln.Cre, ln.Cim, 2 * p_)

                def c4():
                    S.pe(lambda e: e.matmul(py[0:64, :], Fb[0:NH, 0:64], ln.Cre[0:NH].rearrange("p a b -> p (a b)"), start=True, stop=False), [Fb, ln.Cre], [py])
                    S.pe(lambda e: e.matmul(py[0:64, :], Fb[0:NH, 128:192], ln.Cim[0:NH].rearrange("p a b -> p (a b)"), start=False, stop=True), [Fb, ln.Cim], [py])
                    S.dve(lambda e: e.tensor_tensor(Zt[:].rearrange("p a b -> p (a b)"), py[0:64, :], Gt[:].rearrange("p a b -> p (a b)"), op=ALU.mult), [py, Gt], [Zt])
                    if last:
                        S.dma("sp", lambda q: q.dma_start(out=zT_d[c0:c0 + QC, :].rearrange("c (a b) -> a c b", b=128), in_=Zt[:]), reads=[Zt], writes=[env["dbuf"]("zT", it)], owner=Zt)
                return [c1, c2, c3, c4]
            st += conv_stages(0, ln.Uv, ln.G1, ln.Z1, False)
            st += conv_stages(1, ln.Z1, ln.G2, ln.Z2, True)
            return st

        nitems = HW_ // QC
        ipg = GC // QC
        ngrp = HW_ // GC
        for f_ in kblock_stages(0):
            f_()
        kq = []
        next_g = 1
        for i0_ in range(0, nitems, NW):
            idxs = [i for i in range(i0_, min(i0_ + NW, nitems))]
            gmax = idxs[-1] // ipg
            while next_g <= gmax:
                kq += [(next_g, f_) for f_ in kblock_stages(next_g)]
                next_g += 1
            while kq and kq[0][0] <= gmax:
                kq.pop(0)[1]()
            if not kq and next_g < ngrp and next_g <= gmax + 1:
                kq += [(next_g, f_) for f_ in kblock_stages(next_g)]
                next_g += 1
            sts_ = [item_stages(lanes[l], idxs[l]) for l in range(len(idxs))]
            for k in range(len(sts_[0])):
                for l in range(len(idxs)):
                    sts_[l][k]()
                if kq:
                    kq.pop(0)[1]()
        while kq:
            kq.pop(0)[1]()


def load_w_bf16(S, dst, src_ap, kchunks, ncols, stg, cnt):
    for k in range(kchunks):
        for c0 in range(0, ncols, 1024):
            st = stg[cnt[0] % len(stg)]
            w = min(1024, ncols - c0)
            S.dma("sp" if cnt[0] % 2 == 0 else "pool", lambda q, st=st, k=k, c0=c0, w=w: q.dma_start(out=st[:, 0:w], in_=src_ap[k * 128:(k + 1) * 128, c0:c0 + w]), writes=[st], owner=st)
            if cnt[0] % 2 == 0:
                S.act(lambda e, st=st, k=k, c0=c0, w=w: e.copy(dst[:, k, c0:c0 + w], st[:, 0:w]), [st], [dst])
            else:
                S.dve(lambda e, st=st, k=k, c0=c0, w=w: e.tensor_copy(dst[:, k, c0:c0 + w], st[:, 0:w]), [st], [dst])
            cnt[0] += 1


def row_bcast(S, P, name, src_row, scale=None):
    t = P.sb(name, [128, D], F32)
    S.dma("sp", lambda q: q.dma_start(out=t[:], in_=src_row.broadcast_to([128, D])), writes=[t], owner=t)
    if scale is not None:
        S.act(lambda e: e.mul(t[:], t[:], float(scale)), [t], [t])
    return t


def col_chunks(S, P, name, src_col, k):
    t = P.sb(name, [128, k], F32)
    S.dma("sp", lambda q: q.dma_start(out=t[:], in_=src_col.rearrange("(k p) o -> p (k o)", p=128), allow_slow_non_contiguous=True), writes=[t], owner=t)
    return t


def phase_CD(S, nc, env):
    phase_C(S, nc, env)
    if STOP_AFTER != "C1":
        phase_D(S, nc, env)


def phase_C(S, nc, env):
    ones = env["ones"]; x_d = env["x_d"]; ucT_d = env["ucT_d"]; zT_d = env["zT_d"]; gT_d = env["gT_d"]; x1_d = env["x1_d"]
    dbuf = env["dbuf"]
    with Phase(S, "C") as P:
        stg = P.ring("stg", [128, 1024], F32, 4); cnt = [0]
        cwo = P.sb("cwo", [128, 4, D], BF16); hwo = P.sb("hwo", [128, 4, D], BF16); wmx = P.sb("wmx", [128, 8, D], BF16)
        load_w_bf16(S, cwo, env["conf_w_out"], 4, D, stg, cnt)
        load_w_bf16(S, hwo, env["hy_w_out"], 4, D, stg, cnt)
        load_w_bf16(S, wmx, env["w_mix"], 8, D, stg, cnt)
        cg = col_chunks(S, P, "cg", env["conf_ln_g"], 4); cbb = col_chunks(S, P, "cbb", env["conf_ln_b"], 4)
        MG = row_bcast(S, P, "MG", env["ln_mix_g"]); MB = row_bcast(S, P, "MB", env["ln_mix_b"])
        xg_d = env["xg_d"]
        uc = P.sb("uc", [128, 4, 512], F32); sq = P.sb("sq", [128, 4, 512], F32)
        zt = P.sb("zt", [128, 4, 512], BF16); ua = P.sb("ua", [128, 4, 512], BF16)
        mean = P.sb("mean", [128, 512], F32); var = P.sb("var", [128, 512], F32); rstd = P.sb("rstd", [128, 512], F32)
        dd = P.ring("dd", [128, 512], F32, 2)
        gch = P.ring("gch", [128, 2, 512], BF16, 2)
        m1 = P.ring("m1", [128, 512], F32, 2); m2 = P.ring("m2", [128, 512], F32, 2)
        mgR = P.ring("mg", [128, 8, 512], BF16, 2)
        xgs = P.ring("xg", [128, D], F32, 3)
        ss = P.ring("s", [128, D], F32, 2); x1s = P.ring("x1", [128, D], F32, 2)
        sts = [ln_stats(S, P, None) for _ in range(3)]
        bank = P.ring("bk", [128, 512], F32, 8, ps=True)
        cnts = {"ti": 0, "si": 0}

        def front(blk):
            tsl = slice(blk * 512, (blk + 1) * 512)
            mg = mgR[blk % 2]
            st = []

            def f0():
                S.dma("sp", lambda q: q.dma_start(out=uc[:], in_=ucT_d[:, tsl].rearrange("(k p) t -> p k t", p=128)), writes=[uc], owner=uc)
                S.dma("sp", lambda q: q.dma_start(out=zt[:], in_=zT_d[:, tsl].rearrange("(k p) t -> p k t", p=128)), writes=[zt], owner=zt)
                S.act(lambda e: e.activation(sq[:], uc[:], AF.Square), [uc], [sq])
            st.append(f0)

            def f1():
                pS1 = bank[0]; pS2 = bank[1]
                for k in range(4):
                    S.pe(lambda e, k=k: e.matmul(pS1[:], ones[:], uc[:, k, :], start=(k == 0), stop=(k == 3)), [ones, uc], [pS1])
                for k in range(4):
                    S.pe(lambda e, k=k: e.matmul(pS2[:], ones[:], sq[:, k, :], start=(k == 0), stop=(k == 3)), [ones, sq], [pS2])
                S.act(lambda e: e.mul(mean[:], pS1[:], 1.0 / CW), [pS1], [mean])
                S.dve(lambda e: e.tensor_tensor(var[:], mean[:], mean[:], op=ALU.mult), [mean], [var])
                S.dve(lambda e: e.scalar_tensor_tensor(var[:], pS2[:], 1.0 / CW, var[:], op0=ALU.mult, op1=ALU.subtract), [pS2, var], [var])
                S.act(lambda e: e.activation(rstd[:], var[:], AF.Sqrt, bias=EPS_AP[0][:], scale=1.0), [var, EPS_AP[0]], [rstd])
                S.dve(lambda e: e.reciprocal(rstd[:], rstd[:]), [rstd], [rstd])
            st.append(f1)

            def f2(k):
                def f():
                    d_ = dd[k % 2]
                    S.dve(lambda e: e.tensor_tensor(d_[:], uc[:, k, :], mean[:], op=ALU.subtract), [uc, mean], [d_])
                    S.dve(lambda e: e.tensor_tensor(d_[:], d_[:], rstd[:], op=ALU.mult), [d_, rstd], [d_])
                    S.act(lambda e: e.activation(ua[:, k, :], d_[:], AF.Silu, bias=cbb[:, k:k + 1], scale=cg[:, k:k + 1]), [d_, cbb, cg], [ua])
                return f
            for k in range(4):
                st.append(f2(k))

            def f3(dc):
                def f():
                    pya = bank[2 + (dc % 2) * 2]; pyb = bank[3 + (dc % 2) * 2]
                    g_ = gch[dc % 2]; a1 = m1[dc % 2]; a2 = m2[dc % 2]
                    S.dma("sp", lambda q: q.dma_start(out=g_[:, 0, :], in_=gT_d[dc * 128:(dc + 1) * 128, tsl]), writes=[g_], owner=g_)
                    S.dma("sp", lambda q: q.dma_start(out=g_[:, 1, :], in_=gT_d[D + dc * 128:D + (dc + 1) * 128, tsl]), writes=[g_], owner=g_)
                    for k in range(4):
                        S.pe(lambda e, k=k: e.matmul(pya[:], cwo[:, k, dc * 128:(dc + 1) * 128], ua[:, k, :], start=(k == 0), stop=(k == 3)), [cwo, ua], [pya])
                    for k in range(4):
                        S.pe(lambda e, k=k: e.matmul(pyb[:], hwo[:, k, dc * 128:(dc + 1) * 128], zt[:, k, :], start=(k == 0), stop=(k == 3)), [hwo, zt], [pyb])
                    S.dve(lambda e: e.tensor_tensor(a1[:], pya[:], g_[:, 0, :], op=ALU.mult), [pya, g_], [a1])
                    S.dve(lambda e: e.tensor_tensor(a2[:], pyb[:], g_[:, 1, :], op=ALU.mult), [pyb, g_], [a2])
                    S.pool(lambda e: e.tensor_tensor(mg[:, dc, :], a1[:], a2[:], op=ALU.add), [a1, a2], [mg])
                return f
            for dc in range(8):
                st.append(f3(dc))
            return st

        def back(blk):
            mg = mgR[blk % 2]
            st = []
            for tt in range(4):
                t0 = blk * 512 + tt * 128
                ti = cnts["ti"]; cnts["ti"] += 1
                xg = xgs[ti % 3]; s_ = ss[ti % 2]; x1 = x1s[ti % 2]

                def ba(xg=xg, t0=t0):
                    S.dma("sp", lambda q: q.dma_start(out=xg[:], in_=xg_d[t0:t0 + 128, :]), reads=[dbuf("xg", t0)], writes=[xg], owner=xg)
                st.append(ba)

                def bb(xg=xg, s_=s_, x1=x1, tt=tt, t0=t0):
                    for half in range(2):
                        pm = bank[6 + half]
                        for k in range(8):
                            S.pe(lambda e, pm=pm, k=k, half=half: e.matmul(pm[:], mg[:, k, tt * 128:(tt + 1) * 128], wmx[:, k, half * 512:(half + 1) * 512], start=(k == 0), stop=(k == 7)), [mg, wmx], [pm])
                        S.dve(lambda e, pm=pm, half=half: e.tensor_tensor(s_[:, half * 512:(half + 1) * 512], pm[:], xg[:, half * 512:(half + 1) * 512], op=ALU.add), [pm, xg], [s_])
                    st_, mv, rs = sts[cnts["si"] % 3]; cnts["si"] += 1
                    emit_ln_stats(S, s_, s_.t, st_, mv, rs)
                    S.dve(lambda e: e.tensor_scalar(x1[:], s_[:], mv[:, 0:1], rs[:], op0=ALU.subtract, op1=ALU.mult), [s_, mv, rs], [x1])
                    S.dve(lambda e: e.tensor_tensor(x1[:], x1[:], MG[:], op=ALU.mult), [x1, MG], [x1])
                    S.pool(lambda e: e.tensor_tensor(x1[:], x1[:], MB[:], op=ALU.add), [x1, MB], [x1])
                    S.dma("pool", lambda q: q.dma_start(out=x1_d[t0:t0 + 128, :], in_=x1[:]), reads=[x1], writes=[dbuf("x1", t0)], owner=x1)
                st.append(bb)
            return st

        nblk = L // 512
        import os
        if os.environ.get("SEQC", "0") == "1":
            for blk in range(nblk):
                for f_ in front(blk):
                    f_()
                for f_ in back(blk):
                    f_()
            nblk = 0
        else:
            for f_ in front(0):
                f_()
        for blk in range(nblk):
            bs = back(blk)
            fs = front(blk + 1) if blk + 1 < nblk else []
            n = max(len(bs), len(fs))
            bi = 0; fi = 0
            for k in range(n):
                while bi < len(bs) and bi * n <= k * len(bs):
                    bs[bi](); bi += 1
                while fi < len(fs) and fi * n <= k * len(fs):
                    fs[fi](); fi += 1
            while bi < len(bs):
                bs[bi](); bi += 1
            while fi < len(fs):
                fs[fi](); fi += 1


def phase_D(S, nc, env):
    ident = env["ident"]; identb = env["identb"]; x1_d = env["x1_d"]; mem_d = env["mem_d"]; ones = env["ones"]
    acc_d = env["acc_d"]; x2b_d = env["x2b_d"]; affT = env["affT"]; dbuf = env["dbuf"]
    with Phase(S, "D") as P:
        wq = P.sb("wq", [128, 8, D], BF16); wo = P.sb("wo", [128, 8, D], BF16)
        kT = P.sb("kT", [128, 8, NMEM], BF16); V = P.sb("V", [128, 2, D], BF16)
        wr = P.sb("wr", [128, 8, NE], F32)
        S.dma("sp", lambda q: q.dma_start(out=wr[:], in_=env["w_router"].rearrange("(k p) e -> p k e", p=128)), writes=[wr], owner=wr)
        bank = P.ring("bk", [128, 512], F32, 7, ps=True)
        ppT = P.ps("ppT", [128, 8, 128], BF16)
        XG = row_bcast(S, P, "XG", env["ln_xa_g"]); XB = row_bcast(S, P, "XB", env["ln_xa_b"])
        PW = Phase(S, "DW"); PW.__enter__()
        stg = PW.ring("stg", [128, 1024], F32, 4); cnt = [0]
        with Phase(S, "DK") as PK:
            wk = PK.sb("wk", [128, 8, D], BF16); wv = PK.sb("wv", [128, 8, D], BF16)
            load_w_bf16(S, wk, env["xa_wk"], 8, D, stg, cnt)
            load_w_bf16(S, wv, env["xa_wv"], 8, D, stg, cnt)
            memT = PK.sb("memT", [128, 8, NMEM], BF16)
            mt = PK.ring("mt", [128, D], F32, 2)
            for mc in range(2):
                m_ = mt[mc]
                S.dma("sp", lambda q, m_=m_, mc=mc: q.dma_start(out=m_[:], in_=mem_d[mc * 128:(mc + 1) * 128, :]), writes=[m_], owner=m_)
                for half in range(2):
                    pt = bank[half]
                    for j in range(4):
                        dk = half * 4 + j
                        S.pe(lambda e, pt=pt, j=j, dk=dk, m_=m_: e.transpose(pt[:, j * 128:(j + 1) * 128], m_[:, dk * 128:(dk + 1) * 128], ident[:]), [m_, ident], [pt])
                    S.act(lambda e, pt=pt, half=half, mc=mc: e.copy(memT[:, half * 4:half * 4 + 4, mc * 128:(mc + 1) * 128], pt[:].rearrange("p (a b) -> p a b", a=4)), [pt], [memT])
            for hc in range(8):
                pk_ = bank[2 + hc % 2]
                for k in range(8):
                    S.pe(lambda e, pk_=pk_, k=k, hc=hc: e.matmul(pk_[:, 0:NMEM], wk[:, k, hc * 128:(hc + 1) * 128], memT[:, k, :], start=(k == 0), stop=(k == 7)), [wk, memT], [pk_])
                S.act(lambda e, pk_=pk_, hc=hc: e.copy(kT[:, hc, :], pk_[:, 0:NMEM]), [pk_], [kT])
            for mc in range(2):
                for half in range(2):
                    pv = bank[4 + half]
                    for k in range(8):
                        S.pe(lambda e, pv=pv, k=k, mc=mc, half=half: e.matmul(pv[:], memT[:, k, mc * 128:(mc + 1) * 128], wv[:, k, half * 512:(half + 1) * 512], start=(k == 0), stop=(k == 7)), [memT, wv], [pv])
                    S.act(lambda e, pv=pv, mc=mc, half=half: e.copy(V[:, mc, half * 512:(half + 1) * 512], pv[:]), [pv], [V])
        load_w_bf16(S, wq, env["xa_wq"], 8, D, stg, cnt)
        load_w_bf16(S, wo, env["xa_wo"], 8, D, stg, cnt)
        PW.__exit__(None, None, None)
        x1t = P.ring("x1t", [128, D], F32, 4)
        xr = P.ring("xr", [128, D], F32, 2)
        x1T = P.sb("x1T", [128, 8, 512], BF16); qT = P.sb("qT", [128, 8, 512], BF16)
        pf = P.ring("pf", [128, 4, NMEM], F32, 2); pn = P.ring("pn", [128, 4, NMEM], BF16, 2)
        pT = P.sb("pT", [128, 8, 512], BF16); oTR = P.ring("oT", [128, 8, 512], BF16, 2)
        mx = P.ring("mx", [128, 4], F32, 2); sm = P.ring("sm", [128, 4], F32, 2)
        s2 = P.ring("s2", [128, D], F32, 2); x2 = P.ring("x2", [128, D], F32, 2)
        accs = P.ring("accs", [128, D], F32, 2); x2b = P.ring("x2b", [128, D], BF16, 2)
        x2T = P.sb("x2T", [128, 8, 512], F32)
        ex = P.sb("ex", [NE, 512], F32); rsum = P.sb("rsum", [NE, 512], F32)
        sts = [ln_stats(S, P, None) for _ in range(2)]
        cn = {"si": 0, "ti": 0}

        def front(blk):
            oT = oTR[blk % 2]
            st = []

            def d0():
                for tt in range(4):
                    t0 = blk * 512 + tt * 128
                    xt = x1t[tt]
                    S.dma("sp", lambda q, xt=xt, t0=t0: q.dma_start(out=xt[:], in_=x1_d[t0:t0 + 128, :]), reads=[dbuf("x1", t0)], writes=[xt], owner=xt)
            st.append(d0)

            def d1(dk0):
                def f():
                    for dk in range(dk0, dk0 + 4):
                        pt = bank[dk % 2]
                        for tt in range(4):
                            S.pe(lambda e, pt=pt, tt=tt, dk=dk: e.transpose(pt[:, tt * 128:(tt + 1) * 128], x1t[tt][:, dk * 128:(dk + 1) * 128], ident[:]), [x1t[tt], ident], [pt])
                        S.act(lambda e, pt=pt, dk=dk: e.copy(x1T[:, dk, :], pt[:]), [pt], [x1T])
                return f
            st.append(d1(0)); st.append(d1(4))

            def d2(h0):
                def f():
                    for hc in range(h0, h0 + 4):
                        pq = bank[2 + hc % 2]
                        for k in range(8):
                            S.pe(lambda e, pq=pq, k=k, hc=hc: e.matmul(pq[:], wq[:, k, hc * 128:(hc + 1) * 128], x1T[:, k, :], start=(k == 0), stop=(k == 7)), [wq, x1T], [pq])
                        S.act(lambda e, pq=pq, hc=hc: e.mul(qT[:, hc, :], pq[:], 1.0 / 16.0), [pq], [qT])
                return f
            st.append(d2(0)); st.append(d2(4))

            def d3(tt):
                def f():
                    p_f = pf[tt % 2]; p_n = pn[tt % 2]; mx_ = mx[tt % 2]; sm_ = sm[tt % 2]
                    for hp in range(2):
                        psc = bank[4 + hp]
                        for hh in range(2):
                            h = hp * 2 + hh
                            for k in range(2):
                                S.pe(lambda e, psc=psc, hh=hh, h=h, k=k: e.matmul(psc[:, hh * 256:(hh + 1) * 256], qT[:, 2 * h + k, tt * 128:(tt + 1) * 128], kT[:, 2 * h + k, :], start=(k == 0), stop=(k == 1)), [qT, kT], [psc])
                        S.dve(lambda e, psc=psc, hp=hp: e.tensor_reduce(mx_[:, hp * 2:hp * 2 + 2], psc[:].rearrange("p (a b) -> p a b", a=2), axis=mybir.AxisListType.X, op=ALU.max, negate=True), [psc], [mx_])
                        for hh in range(2):
                            h = hp * 2 + hh
                            S.act(lambda e, psc=psc, hh=hh, h=h: e.activation(p_f[:, h, :], psc[:, hh * 256:(hh + 1) * 256], AF.Exp, bias=mx_[:, h:h + 1], scale=1.0, accum_out=sm_[:, h:h + 1]), [psc, mx_], [p_f, sm_])
                    S.dve(lambda e: e.reciprocal(sm_[:], sm_[:]), [sm_], [sm_])
                    for h in range(4):
                        S.dve(lambda e, h=h: e.tensor_scalar(p_n[:, h, :], p_f[:, h, :], sm_[:, h:h + 1], None, op0=ALU.mult), [p_f, sm_], [p_n])
                    for h in range(4):
                        for mc in range(2):
                            S.pe(lambda e, h=h, mc=mc: e.transpose(ppT[:, h * 2 + mc, :], p_n[:, h, mc * 128:(mc + 1) * 128], identb[:]), [p_n, identb], [ppT])
                    S.act(lambda e: e.copy(pT[:, :, tt * 128:(tt + 1) * 128], ppT[:]), [ppT], [pT])
                return f
            for tt in range(4):
                st.append(d3(tt))

            def d4(h0):
                def f():
                    for hc in range(h0, h0 + 4):
                        po = bank[2 + hc % 2]; h = hc // 2
                        for mc in range(2):
                            S.pe(lambda e, po=po, mc=mc, hc=hc, h=h: e.matmul(po[:], V[:, mc, hc * 128:(hc + 1) * 128], pT[:, h * 2 + mc, :], start=(mc == 0), stop=(mc == 1)), [V, pT], [po])
                        S.act(lambda e, po=po, hc=hc: e.copy(oT[:, hc, :], po[:]), [po], [oT])
                return f
            st.append(d4(0)); st.append(d4(4))
            return st

        def back(blk):
            oT = oTR[blk % 2]
            st = []
            for tt in range(4):
                t0 = blk * 512 + tt * 128
                ti = cn["ti"]; cn["ti"] += 1
                s_ = s2[ti % 2]; x2_ = x2[ti % 2]; ac = accs[ti % 2]; xb = x2b[ti % 2]; xr_ = xr[ti % 2]

                def b0(tt=tt, t0=t0, s_=s_, x2_=x2_, ac=ac, xb=xb, xr_=xr_):
                    S.dma("sp", lambda q: q.dma_start(out=xr_[:], in_=x1_d[t0:t0 + 128, :]), reads=[dbuf("x1", t0)], writes=[xr_], owner=xr_)
                    for half in range(2):
                        px = bank[half]
                        for k in range(8):
                            S.pe(lambda e, px=px, k=k, half=half: e.matmul(px[:], oT[:, k, tt * 128:(tt + 1) * 128], wo[:, k, half * 512:(half + 1) * 512], start=(k == 0), stop=(k == 7)), [oT, wo], [px])
                        S.dve(lambda e, px=px, half=half: e.scalar_tensor_tensor(s_[:, half * 512:(half + 1) * 512], xr_[:, half * 512:(half + 1) * 512], ALPHA, px[:], op0=ALU.mult, op1=ALU.add), [px, xr_], [s_])
                    st_, mv, rs = sts[cn["si"] % 2]; cn["si"] += 1
                    emit_ln_stats(S, s_, s_.t, st_, mv, rs)
                    S.dve(lambda e: e.tensor_scalar(x2_[:], s_[:], mv[:, 0:1], rs[:], op0=ALU.subtract, op1=ALU.mult), [s_, mv, rs], [x2_])
                    S.dve(lambda e: e.tensor_tensor(x2_[:], x2_[:], XG[:], op=ALU.mult), [x2_, XG], [x2_])
                    S.dve(lambda e: e.tensor_tensor(x2_[:], x2_[:], XB[:], op=ALU.add), [x2_, XB], [x2_])
                    S.act(lambda e: e.mul(ac[:], x2_[:], ALPHA), [x2_], [ac])
                    S.act(lambda e: e.copy(xb[:], x2_[:]), [x2_], [xb])
                    S.dma("pool", lambda q: q.dma_start(out=acc_d[t0:t0 + 128, :], in_=ac[:]), reads=[ac], writes=[dbuf("acc", t0)], owner=ac)
                    S.dma("pool", lambda q: q.dma_start(out=x2b_d[t0:t0 + 128, :], in_=xb[:]), reads=[xb], writes=[dbuf("x2b", t0)], owner=xb)
                st.append(b0)

                def b1(tt=tt, x2_=x2_):
                    for half in range(2):
                        pt = bank[2 + half]
                        for j in range(4):
                            dk = half * 4 + j
                            S.pe(lambda e, pt=pt, j=j, dk=dk: e.transpose(pt[:, j * 128:(j + 1) * 128], x2_[:, dk * 128:(dk + 1) * 128], ident[:]), [x2_, ident], [pt])
                        S.act(lambda e, pt=pt, half=half: e.copy(x2T[:, half * 4:half * 4 + 4, tt * 128:(tt + 1) * 128], pt[:].rearrange("p (a b) -> p a b", a=4)), [pt], [x2T])
                st.append(b1)

            def b2():
                pl = bank[6]
                for k in range(8):
                    S.pe(lambda e, k=k: e.matmul(pl[0:NE, :], wr[:, k, :], x2T[:, k, :], start=(k == 0), stop=(k == 7)), [wr, x2T], [pl])
                S.act(lambda e: e.activation(ex[:], pl[0:NE, :], AF.Exp), [pl], [ex])
                S.pe(lambda e: e.matmul(pl[0:NE, :], ones[0:NE, 0:NE], ex[:], start=True, stop=True), [ones, ex], [pl])
                S.dve(lambda e: e.reciprocal(rsum[:], pl[0:NE, :]), [pl], [rsum])
                S.dve(lambda e: e.tensor_tensor(affT[:, blk * 512:(blk + 1) * 512], ex[:], rsum[:], op=ALU.mult), [ex, rsum], [affT])
            st.append(b2)
            return st

        nblk = L // 512
        for f_ in front(0):
            f_()
        for blk in range(nblk):
            bs = back(blk)
            fs = front(blk + 1) if blk + 1 < nblk else []
            n = max(len(bs), len(fs))
            bi = 0; fi = 0
            for k in range(n):
                while bi < len(bs) and bi * n <= k * len(bs):
                    bs[bi](); bi += 1
                while fi < len(fs) and fi * n <= k * len(fs):
                    fs[fi](); fi += 1
            while bi < len(bs):
                bs[bi](); bi += 1
            while fi < len(fs):
                fs[fi](); fi += 1
        S.dma("sp", lambda q: q.dma_start(out=env["affT_d"][:, :], in_=affT[:]), reads=[affT], writes=[dbuf("affT", 0)], owner=affT)


def phase_EF(S, nc, env):
    affT = env["affT"]; ident = env["ident"]; identb = env["identb"]
    x2b_d = env["x2b_d"]; acc_d = env["acc_d"]
    with Phase(S, "EF") as PX:
        idxT = PX.sb("idxT", [128, NE, 8], U32)
        gate = PX.sb("gate", [128, NE, 8], F32)
        phase_E(S, nc, env, idxT, gate)
        if STOP_AFTER == "E":
            return
        phase_F(S, nc, env, idxT, gate)


def phase_E(S, nc, env, idxT, gate):
    affT = env["affT"]; ident = env["ident"]
    with Phase(S, "E") as P:
        bank = P.ring("bk", [128, 512], F32, 6, ps=True)
        a128 = P.sb("a128", [128, 1024], F32)
        S.dma("sp", lambda q: q.dma_start(out=a128[:], in_=env["affT_d"].rearrange("e (s t) -> (e s) t", s=8)), writes=[a128], owner=a128)
        blk8 = P.sb("blk8", [128, 128], F32)
        S.dma("sp", lambda q: q.dma_start(out=blk8[:], in_=env["c_blk8"][:, :]), writes=[blk8], owner=blk8)
        jk8 = P.sb("jk8", [128, 1024], BF16)
        lo8 = P.sb("lo8", [128, 2], F32); hi8 = P.sb("hi8", [128, 2], F32); mid8 = P.sb("mid8", [128, 2], F32)
        cnt8 = P.sb("cnt8", [128, 2], F32); ge8 = P.sb("ge8", [128, 2], F32); d8 = P.sb("d8", [128, 2], F32)
        lo = P.sb("lo", [NE, 1], F32)
        msk = P.sb("msk", [NE, L], F32); cum = P.sb("cum", [NE, L], F32)
        one16 = P.sb("one16", [NE, L], BF16)
        S.pool(lambda e: e.memset(one16[:], 1.0), [], [one16])
        S.dve(lambda e: e.memset(lo8[:], 0.0), [], [lo8])
        S.dve(lambda e: e.memset(hi8[:], 1.0), [], [hi8])
        pcn = bank[5]
        for it in range(34):
            S.dve(lambda e: e.tensor_tensor(mid8[:], lo8[:], hi8[:], op=ALU.add), [lo8, hi8], [mid8])
            S.dve(lambda e: e.tensor_scalar(mid8[:], mid8[:], 0.5, None, op0=ALU.mult), [mid8], [mid8])
            S.dve(lambda e: e.tensor_scalar(jk8[:], a128[:], mid8[:, 0:1], None, op0=ALU.is_ge, op1=ALU.add, accum_out=cnt8[:, 0:1]), [a128, mid8], [jk8, cnt8])
            S.dve(lambda e: e.tensor_copy(cnt8[:, 1:2], cnt8[:, 0:1]), [cnt8], [cnt8])
            S.pe(lambda e: e.matmul(pcn[:, 0:2], blk8[:], cnt8[:], start=True, stop=True), [blk8, cnt8], [pcn])
            S.dve(lambda e: e.tensor_scalar(ge8[:], pcn[:, 0:2], CAP - 0.5, None, op0=ALU.is_ge), [pcn], [ge8])
            S.dve(lambda e: e.tensor_tensor(d8[:], mid8[:], lo8[:], op=ALU.subtract), [mid8, lo8], [d8])
            S.dve(lambda e: e.scalar_tensor_tensor(lo8[:], d8[:], ge8[:, 0:1], lo8[:], op0=ALU.mult, op1=ALU.add), [d8, ge8, lo8], [lo8])
            S.dve(lambda e: e.tensor_tensor(d8[:], hi8[:], mid8[:], op=ALU.subtract), [hi8, mid8], [d8])
            S.dve(lambda e: e.scalar_tensor_tensor(hi8[:], d8[:], ge8[:, 0:1], mid8[:], op0=ALU.mult, op1=ALU.add), [d8, ge8, mid8], [hi8])
        lrow = P.sb("lrow", [2, 128], F32)
        S.pe(lambda e: e.transpose(pcn[0:2, 0:128], lo8[:], ident[:]), [lo8, ident], [pcn])
        S.dve(lambda e: e.tensor_copy(lrow[:], pcn[0:2, 0:128]), [pcn], [lrow])
        S.pe(lambda e: e.transpose(pcn[0:NE, 256:257], lrow[0:1, 0:128:8], ident[0:1, 0:1]), [lrow, ident], [pcn])
        S.dve(lambda e: e.tensor_copy(lo[:], pcn[0:NE, 256:257]), [pcn], [lo])
        S.dve(lambda e: e.tensor_scalar(msk[:], affT[:], lo[:, 0:1], None, op0=ALU.is_ge), [affT, lo], [msk])
        S.dve(lambda e: e.tensor_tensor_scan(cum[:], one16[:], msk[:], 0.0, op0=ALU.mult, op1=ALU.add), [one16, msk], [cum])
        S.dve(lambda e: e.tensor_tensor(cum[:], cum[:], msk[:], op=ALU.mult), [cum, msk], [cum])
        S.dve(lambda e: e.tensor_scalar(cum[:], cum[:], -1.0, None, op0=ALU.add), [cum], [cum])
        keyT = P.sb("keyT", [128, NT, NE], F32); afT = P.sb("afT", [128, NT, NE], F32)
        for src, dst in ((cum, keyT), (affT, afT)):
            for hb in range(2):
                pb = bank[hb]
                for j in range(32):
                    tl = hb * 32 + j
                    S.pe(lambda e, pb=pb, j=j, tl=tl, src=src: e.transpose(pb[:, j * NE:(j + 1) * NE], src[:, tl * 128:(tl + 1) * 128], ident[0:NE, 0:NE]), [src, ident], [pb])
                S.act(lambda e, pb=pb, hb=hb, dst=dst: e.copy(dst[:, hb * 32:(hb + 1) * 32, :].rearrange("p a b -> p (a b)"), pb[:]), [pb], [dst])
        vals = P.sb("vals", [128, NT, NE, 5], BF16)
        tidx = P.sb("tidx", [128, NT, 2], F32)
        S.dma("sp", lambda q: q.dma_start(out=tidx[:], in_=env["c_tidx"][:, :, :]), writes=[tidx], owner=tidx)
        for e_ in range(NE):
            S.dve(lambda e, e_=e_: e.tensor_copy(vals[:, :, e_, 0:2], tidx[:]), [tidx], [vals])
        r1 = P.sb("r1", [128, NT, NE], F32); hb16 = P.sb("hb16", [128, NT, NE], BF16)
        S.dve(lambda e: e.tensor_copy(hb16[:], afT[:]), [afT], [hb16])
        S.dve(lambda e: e.tensor_copy(vals[:, :, :, 2], hb16[:]), [hb16], [vals])
        S.dve(lambda e: e.tensor_tensor(r1[:], afT[:], hb16[:], op=ALU.subtract), [afT, hb16], [r1])
        S.dve(lambda e: e.tensor_copy(hb16[:], r1[:]), [r1], [hb16])
        S.dve(lambda e: e.tensor_copy(vals[:, :, :, 3], hb16[:]), [hb16], [vals])
        S.dve(lambda e: e.tensor_tensor(r1[:], r1[:], hb16[:], op=ALU.subtract), [r1, hb16], [r1])
        S.dve(lambda e: e.tensor_copy(vals[:, :, :, 4], r1[:]), [r1], [vals])
        iota32 = P.sb("iota32", [128, CAP], F32)
        S.dma("sp", lambda q: q.dma_start(out=iota32[:], in_=env["c_iota"][:, :]), writes=[iota32], owner=iota32)
        iota = P.sb("iota", [128, CAP], mybir.dt.float16)
        S.dve(lambda e: e.tensor_copy(iota[:], iota32[:]), [iota32], [iota])
        zl = P.sb("zl", [128, 128], BF16); zr = P.sb("zr", [128, 512], BF16)
        S.dve(lambda e: e.memset(zl[:], 0.0), [], [zl])
        S.dve(lambda e: e.memset(zr[:], 0.0), [], [zr])
        accA = bank[2]; accB = bank[3]
        for a_ in (accA, accB):
            S.pe(lambda e, a_=a_: e.matmul(a_[:], zl[:], zr[:], start=True, stop=False, skip_group_check=True), [zl, zr], [a_])
        O = P.ring("O", [128, CAP], BF16, 3)
        oi = 0
        for tl in range(NT):
            for e_ in range(NE):
                o_ = O[oi % 3]; oi += 1
                S.dve(lambda e, o_=o_, tl=tl, e_=e_: e.tensor_scalar(o_[:], iota[:], keyT[:, tl, e_:e_ + 1], None, op0=ALU.is_equal), [iota, keyT], [o_])
                a_ = accA if e_ < 8 else accB
                for sc in range(8):
                    col = ((e_ % 8) * 8 + sc) * 5
                    S.pe(lambda e, a_=a_, o_=o_, sc=sc, col=col, tl=tl, e_=e_: e.matmul(a_[:, col:col + 5], o_[:, sc * 128:(sc + 1) * 128], vals[:, tl, e_, :], start=False, stop=(tl == NT - 1), skip_group_check=True), [o_, vals], [a_])
        res = P.sb("res", [128, NE, 8, 5], F32)
        S.act(lambda e: e.copy(res[:, 0:8, :, :].rearrange("p a b c -> p (a b c)"), accA[:, 0:320]), [accA], [res])
        S.act(lambda e: e.copy(res[:, 8:16, :, :].rearrange("p a b c -> p (a b c)"), accB[:, 0:320]), [accB], [res])
        idf = P.sb("idf", [128, NE, 8], F32)
        S.dve(lambda e: e.scalar_tensor_tensor(idf[:], res[:, :, :, 0], 128.0, res[:, :, :, 1], op0=ALU.mult, op1=ALU.add), [res], [idf])
        S.dve(lambda e: e.tensor_copy(idxT[:], idf[:]), [idf], [idxT])
        S.dve(lambda e: e.tensor_tensor(gate[:], res[:, :, :, 2], res[:, :, :, 3], op=ALU.add), [res], [gate])
        S.dve(lambda e: e.tensor_tensor(gate[:], gate[:], res[:, :, :, 4], op=ALU.add), [gate, res], [gate])


def phase_F(S, nc, env, idxT, gate):
    identb = env["identb"]; x2b_d = env["x2b_d"]; acc_d = env["acc_d"]
    w_gate = env["w_gate"]; w_up = env["w_up"]; w_down = env["w_down"]
    accbuf = env["dbuf"]("accsc", 0)
    FG = 256
    NFG = DFF // FG
    with Phase(S, "F") as P:
        xg = P.sb("xg", [128, 8, D], BF16)
        xgT = P.ring("xgT", [128, 8, CAP], BF16, 2)
        hT = P.sb("hT", [128, 16, CAP], BF16)
        Wd = P.sb("Wd", [128, 16, D], BF16)
        Wg = P.ring("Wg", [128, 8, FG], BF16, 2); Wu = P.ring("Wu", [128, 8, FG], BF16, 2)
        stg = P.ring("stg", [128, 8 * FG], F32, 3)
        yo = P.ring("yo", [128, D], F32, 2)
        sg = P.ring("sg", [128, 512], F32, 2)
        bank = P.ring("bk", [128, 512], F32, 7, ps=True)
        ptr = P.ps("ptr", [128, 8, 128], BF16)
        sc_ = [0]

        def wload(dst, dst_ap, src_ap, a, b):
            st = stg[sc_[0] % 3]; sc_[0] += 1
            S.dma("sp", lambda q: q.dma_start(out=st[:].rearrange("p (a b) -> p a b", a=a), in_=src_ap), writes=[st], owner=st)
            S.act(lambda e: e.copy(dst_ap, st[:].rearrange("p (a b) -> p a b", a=a)), [st], [dst])

        def load_group(e_, fg):
            wg_ = Wg[fg % 2]; wu_ = Wu[fg % 2]
            wload(wg_, wg_[:], w_gate[e_, :, fg * FG:(fg + 1) * FG].rearrange("(k p) f -> p k f", p=128), 8, FG)
            wload(wu_, wu_[:], w_up[e_, :, fg * FG:(fg + 1) * FG].rearrange("(k p) f -> p k f", p=128), 8, FG)
            wload(Wd, Wd[:, fg * 2:fg * 2 + 2, :], w_down[e_, fg * FG:(fg + 1) * FG, :].rearrange("(k p) d -> p k d", p=128), 2, D)

        def gather(e_):
            for sc in range(8):
                S.dma("pool", lambda q, sc=sc: q.indirect_dma_start(out=xg[:, sc, :], out_offset=None, in_=x2b_d[:, :], in_offset=bass.IndirectOffsetOnAxis(ap=idxT[:, e_, sc:sc + 1], axis=0)), reads=[idxT], writes=[xg], owner=xg)

        def transposes(e_):
            xt_ = xgT[e_ % 2]
            for sc in range(8):
                for dk in range(8):
                    S.pe(lambda e, sc=sc, dk=dk: e.transpose(ptr[:, dk, :], xg[:, sc, dk * 128:(dk + 1) * 128], identb[:]), [xg, identb], [ptr])
                S.act(lambda e, sc=sc: e.copy(xt_[:, :, sc * 128:(sc + 1) * 128], ptr[:]), [ptr], [xt_])

        yi = 0; pi = 0
        gather(0)
        transposes(0)
        load_group(0, 0)
        for e_ in range(NE):
            xt_ = xgT[e_ % 2]
            if e_ + 1 < NE:
                gather(e_ + 1)
            for fg in range(NFG):
                wg_ = Wg[fg % 2]; wu_ = Wu[fg % 2]
                if fg + 1 < NFG:
                    load_group(e_, fg + 1)
                for fk in range(FG // 128):
                    fkg = fg * (FG // 128) + fk
                    for sh in range(2):
                        pg = bank[(pi % 2) * 2]; pu = bank[(pi % 2) * 2 + 1]; s_ = sg[pi % 2]; pi += 1
                        for dk in range(8):
                            S.pe(lambda e, pg=pg, dk=dk, fk=fk, sh=sh, wg_=wg_: e.matmul(pg[:], wg_[:, dk, fk * 128:(fk + 1) * 128], xt_[:, dk, sh * 512:(sh + 1) * 512], start=(dk == 0), stop=(dk == 7)), [wg_, xt_], [pg])
                        for dk in range(8):
                            S.pe(lambda e, pu=pu, dk=dk, fk=fk, sh=sh, wu_=wu_: e.matmul(pu[:], wu_[:, dk, fk * 128:(fk + 1) * 128], xt_[:, dk, sh * 512:(sh + 1) * 512], start=(dk == 0), stop=(dk == 7)), [wu_, xt_], [pu])
                        S.act(lambda e, s_=s_, pg=pg: e.activation(s_[:], pg[:], AF.Silu), [pg], [s_])
                        S.dve(lambda e, s_=s_, pu=pu, fkg=fkg, sh=sh: e.tensor_tensor(hT[:, fkg, sh * 512:(sh + 1) * 512], s_[:], pu[:], op=ALU.mult), [s_, pu], [hT])
            for sc in range(8):
                y_ = yo[yi % 2]; yi += 1
                for dh in range(2):
                    py = bank[4 + dh]
                    for fk in range(16):
                        S.pe(lambda e, py=py, fk=fk, sc=sc, dh=dh: e.matmul(py[:], hT[:, fk, sc * 128:(sc + 1) * 128], Wd[:, fk, dh * 512:(dh + 1) * 512], start=(fk == 0), stop=(fk == 15)), [hT, Wd], [py])
                    S.dve(lambda e, py=py, y_=y_, dh=dh, sc=sc: e.tensor_scalar(y_[:, dh * 512:(dh + 1) * 512], py[:], gate[:, e_, sc:sc + 1], None, op0=ALU.mult), [py, gate], [y_])
                S.dma("pool", lambda q, y_=y_, sc=sc: q.indirect_dma_start(out=acc_d[:, :], out_offset=bass.IndirectOffsetOnAxis(ap=idxT[:, e_, sc:sc + 1], axis=0), in_=y_[:], in_offset=None, compute_op=ALU.add), reads=[y_, idxT], writes=[accbuf], owner=y_)
            if e_ + 1 < NE:
                transposes(e_ + 1)
                load_group(e_ + 1, 0)


def phase_G(S, nc, env):
    acc_d = env["acc_d"]; out_d = env["out_d"]
    with Phase(S, "Z") as P:
        OG = row_bcast(S, P, "OG", env["ln_moe_g"]); OB = row_bcast(S, P, "OB", env["ln_moe_b"])
        at = P.ring("at", [128, D], F32, 4); ot = P.ring("ot", [128, D], F32, 4)
        sts = [ln_stats(S, P, None) for _ in range(4)]
        for tl in range(NT):
            a_ = at[tl % 4]; o_ = ot[tl % 4]; st, mv, rs = sts[tl % 4]
            S.dma("sp", lambda q, a_=a_, tl=tl: q.dma_start(out=a_[:], in_=acc_d[tl * 128:(tl + 1) * 128, :]), writes=[a_], owner=a_)
            emit_ln_stats(S, a_, a_.t, st, mv, rs)
            S.dve(lambda e, o_=o_, a_=a_, mv=mv, rs=rs: e.tensor_scalar(o_[:], a_[:], mv[:, 0:1], rs[:], op0=ALU.subtract, op1=ALU.mult), [a_, mv, rs], [o_])
            S.dve(lambda e, o_=o_: e.tensor_tensor(o_[:], o_[:], OG[:], op=ALU.mult), [o_, OG], [o_])
            S.pool(lambda e, o_=o_: e.tensor_tensor(o_[:], o_[:], OB[:], op=ALU.add), [o_, OB], [o_])
            S.dma("pool", lambda q, o_=o_, tl=tl: q.dma_start(out=out_d[tl * 128:(tl + 1) * 128, :], in_=o_[:]), reads=[o_], writes=[env["dbuf"]("out", tl)], owner=o_)


def host_consts():
    a = np.arange(128, dtype=np.float64)
    th = 2.0 * np.pi * np.outer(a, a) / 128.0
    Fm = np.concatenate([np.cos(th), -np.sin(th)], axis=1).astype(np.float32)
    th2 = 2.0 * np.pi * np.outer(a, a) / NFFT
    Tm = np.concatenate([np.cos(th2), -np.sin(th2)], axis=1).astype(np.float32)
    n = np.arange(L, dtype=np.float32)
    t = n / np.float32(L - 1)
    bands = 16
    f = np.linspace(1e-4, bands - 1, bands, dtype=np.float32)
    ang = (np.float32(2.0 * math.pi) * n / np.float32(L))[:, None] * f[None, :]
    feat = np.concatenate([t[:, None], np.cos(ang), -np.sin(ang)], axis=-1).astype(np.float32)
    featTS = np.zeros((NFFT, 33), np.float32)
    featTS[:L] = feat
    featTS[L + 1:] = feat[1:][::-1]
    tts = np.zeros(NFFT, np.float32)
    tts[:L] = t
    tts[L + 1:] = t[1:][::-1]
    tidx = np.zeros((128, NT, 2), np.float32)
    tidx[:, :, 0] = np.arange(NT, dtype=np.float32)[None, :]
    tidx[:, :, 1] = np.arange(128, dtype=np.float32)[:, None]
    return {
        "c_F": Fm, "c_T": Tm, "c_featT": np.ascontiguousarray(featTS.T), "c_tts": tts.reshape(128, 128).copy(),
        "c_ident": np.eye(128, dtype=np.float32),
        "c_iota": np.tile(np.arange(CAP, dtype=np.float32)[None, :], (128, 1)),
        "c_tidx": tidx,
        "c_blk8": np.kron(np.eye(16, dtype=np.float32), np.ones((8, 8), np.float32)),
    }


def make_in_maps(inputs, ncores=8):
    c = host_consts()
    f = lambda k: np.ascontiguousarray(np.asarray(inputs[k], dtype=np.float32))
    shared = {
        "ln_in_g": f("ln_in_g").reshape(D, 1), "ln_in_b": f("ln_in_b").reshape(D, 1),
        "ln_in_g_row": f("ln_in_g").reshape(1, D), "ln_in_b_row": f("ln_in_b").reshape(1, D),
        "w_in": f("w_in")[0], "b_gate": f("b_gate")[0].reshape(2 * D, 1),
        "conf_dw_w": f("conf_dw_w")[0], "conf_dw_b": f("conf_dw_b")[0].reshape(CW, 1),
        "conf_ln_g": f("conf_ln_g")[0].reshape(CW, 1), "conf_ln_b": f("conf_ln_b")[0].reshape(CW, 1),
        "conf_w_out": f("conf_w_out")[0],
        "hy_short_w": f("hy_short_w")[0], "hy_short_b": f("hy_short_b")[0].reshape(3 * HW_, 1),
        "hy_ffn_w1": f("hy_ffn_w1")[0], "hy_ffn_b1": f("hy_ffn_b1")[0].reshape(64, 1), "hy_freq1": f("hy_freq1")[0].reshape(64, 1),
        "hy_ffn_w2": f("hy_ffn_w2")[0], "hy_ffn_b2": f("hy_ffn_b2")[0].reshape(64, 1), "hy_freq2": f("hy_freq2")[0].reshape(64, 1),
        "hy_ffn_w3": f("hy_ffn_w3")[0], "hy_skip": f("hy_skip")[0].reshape(1, 2 * HW_),
        "hy_w_out": f("hy_w_out")[0], "w_mix_out": f("w_mix_out")[0],
        "ln_mix_g": f("ln_mix_g")[0].reshape(1, D), "ln_mix_b": f("ln_mix_b")[0].reshape(1, D),
        "xa_wq": f("xa_wq")[0], "xa_wk": f("xa_wk")[0], "xa_wv": f("xa_wv")[0], "xa_wo": f("xa_wo")[0],
        "ln_xa_g": f("ln_xa_g")[0].reshape(1, D), "ln_xa_b": f("ln_xa_b")[0].reshape(1, D),
        "moe_w_router": f("moe_w_router")[0],
        "moe_w_gate": f("moe_w_gate")[0], "moe_w_up": f("moe_w_up")[0], "moe_w_down": f("moe_w_down")[0],
        "ln_moe_g": f("ln_moe_g")[0].reshape(1, D), "ln_moe_b": f("ln_moe_b")[0].reshape(1, D),
    }
    shared.update(c)
    x = f("x"); mem = f("mem")
    maps = []
    for r in range(ncores):
        m = dict(shared)
        m["x"] = x[r % 4]
        m["mem"] = mem[r % 4]
        maps.append(m)
    return maps


def kernel(**inputs):
    nc = build_nc()
    maps = make_in_maps(inputs)
    res = run_bass_kernel_spmd(nc, maps, core_ids=list(range(8)))
    out = np.stack([np.asarray(res.results[r]["out"], dtype=np.float32) for r in range(4)], axis=0)
    return out
```

```python
import math
import numpy as np
import concourse.bass as bass
import concourse.mybir as mybir
from concourse.bass_utils import run_bass_kernel_spmd
from contextlib import ExitStack

F32 = mybir.dt.float32
BF16 = mybir.dt.bfloat16
U32 = mybir.dt.uint32
I32 = mybir.dt.int32
AF = mybir.ActivationFunctionType
ALU = mybir.AluOpType

D = 1024
L = 8192
NT = L // 128
NMEM = 256
CW = 512
HW_ = 512
COLS = 4608
NE = 16
CAP = 1024
DFF = 2048
EPS = 1e-5
ALPHA = 2.0 ** 0.25
NFFT = 16384
GC = 16
SAME_ENGINE_SYNC = True
STOP_AFTER = None
DEBUG_OUT = ()


class Buf:
    __slots__ = ("name", "w", "r", "dsem", "dcnt")

    def __init__(self, name):
        self.name = name
        self.w = None
        self.r = {}
        self.dsem = None
        self.dcnt = 0


class Tile(Buf):
    __slots__ = ("t",)

    def __init__(self, name, t):
        Buf.__init__(self, name)
        self.t = t

    def __getitem__(self, k):
        return self.t[k]


class Sched:
    ROT = 30000

    def __init__(self, nc, es):
        self.nc = nc
        self.es = es
        self.E = {"pe": nc.tensor, "act": nc.scalar, "dve": nc.vector, "pool": nc.gpsimd, "sp": nc.sync}
        self.csem = {}
        self.ccnt = {}
        self.waited = {k: {} for k in self.E}
        self.sems = []
        self.free_dsems = []
        self.latest = {}
        self.same_engine_sync = SAME_ENGINE_SYNC
        self.ninstr = {k: 0 for k in self.E}
        for k in ("pe", "act", "dve", "pool"):
            self._new_csem(k)

    def _alloc_sem(self, name):
        s = self.es.enter_context(self.nc.semaphore(name))
        self.sems.append(s)
        return s

    def _new_csem(self, k):
        self.csem[k] = self._alloc_sem("c_%s_%d" % (k, len(self.sems)))
        self.ccnt[k] = 0

    def _wait(self, eng, ev):
        if ev is None:
            return
        sem, val = ev
        if (not self.same_engine_sync) and eng in self.csem and sem is self.csem[eng]:
            return
        w = self.waited[eng]
        key = id(sem)
        if w.get(key, 0) >= val:
            return
        self.E[eng].wait_ge(sem, val)
        w[key] = val

    def _deps(self, eng, reads, writes):
        for b in reads:
            if b.w is not None:
                if eng == "pe" and b.w[0] is self.csem.get("pe") and False:
                    continue
                self._wait(eng, b.w)
        for b in writes:
            if b.w is not None:
                if eng == "pe" and b.w[0] is self.csem["pe"]:
                    pass
                else:
                    self._wait(eng, b.w)
            for sid, ev in b.r.items():
                if eng == "pe" and ev[0] is self.csem["pe"]:
                    continue
                self._wait(eng, ev)

    def _mark(self, ev, reads, writes):
        self.latest[id(ev[0])] = ev
        for b in reads:
            b.r[id(ev[0])] = ev
        for b in writes:
            b.w = ev
            b.r = {}

    def op(self, eng, fn, reads=(), writes=()):
        if self.ccnt[eng] >= self.ROT:
            self._new_csem(eng)
        self._deps(eng, reads, writes)
        ins = fn(self.E[eng])
        self.ccnt[eng] += 1
        sem = self.csem[eng]
        ins.then_inc(sem, 1)
        self.ninstr[eng] += 1
        self._mark((sem, self.ccnt[eng]), reads, writes)

    def pe(self, fn, r=(), w=()):
        self.op("pe", fn, r, w)

    def act(self, fn, r=(), w=()):
        self.op("act", fn, r, w)

    def dve(self, fn, r=(), w=()):
        self.op("dve", fn, r, w)

    def pool(self, fn, r=(), w=()):
        self.op("pool", fn, r, w)

    def dma(self, q, fn, reads=(), writes=(), owner=None):
        self._deps(q, reads, writes)
        if owner.dsem is None:
            if self.free_dsems:
                owner.dsem, owner.dcnt = self.free_dsems.pop()
            else:
                owner.dsem = self._alloc_sem("d_%s_%d" % (owner.name, len(self.sems)))
                owner.dcnt = 0
        ins = fn(self.E[q])
        owner.dcnt += 16
        ins.then_inc(owner.dsem, 16)
        self.ninstr[q] += 1
        self._mark((owner.dsem, owner.dcnt), reads, writes)

    def release(self, tiles):
        for t in tiles:
            if t.dsem is not None:
                self.free_dsems.append((t.dsem, t.dcnt))
                t.dsem = None

    def barrier(self):
        evs = list(self.latest.values())
        for eng in self.E:
            for ev in evs:
                self._wait(eng, ev)


class Phase:
    def __init__(self, S, name):
        self.S = S
        self.nc = S.nc
        self.name = name
        self.es = ExitStack()
        self.tiles = []
        self.n = 0

    def __enter__(self):
        self.es.__enter__()
        return self

    def __exit__(self, *a):
        self.S.barrier()
        self.S.release(self.tiles)
        return self.es.__exit__(*a)

    def sb(self, name, shape, dt):
        self.n += 1
        t = self.es.enter_context(self.nc.sbuf_tensor("%s_%s_%d" % (self.name, name, self.n), list(shape), dt))
        tl = Tile(name, t)
        self.tiles.append(tl)
        return tl

    def ps(self, name, shape, dt=F32):
        self.n += 1
        t = self.es.enter_context(self.nc.psum_tensor("%s_%s_%d" % (self.name, name, self.n), list(shape), dt))
        tl = Tile(name, t)
        self.tiles.append(tl)
        return tl

    def ring(self, name, shape, dt, n, ps=False):
        return [(self.ps if ps else self.sb)("%s%d" % (name, i), shape, dt) for i in range(n)]


def build_nc():
    nc = bass.Bass("TRN2", target_bir_lowering=False)
    dram = {}

    def din(name, shape, dt=F32):
        dram[name] = nc.dram_tensor(name, list(shape), dt, kind="ExternalInput").ap()
        return dram[name]

    def dscr(name, shape, dt):
        kind = "ExternalOutput" if name in DEBUG_OUT else "Internal"
        dram[name] = nc.dram_tensor(name, list(shape), dt, kind=kind).ap()
        return dram[name]

    x_d = din("x", [L, D])
    mem_d = din("mem", [NMEM, D])
    ln_in_g = din("ln_in_g", [D, 1]); ln_in_b = din("ln_in_b", [D, 1])
    w_in = din("w_in", [D, COLS])
    b_gate = din("b_gate", [2 * D, 1])
    conf_dw_w = din("conf_dw_w", [31, CW]); conf_dw_b = din("conf_dw_b", [CW, 1])
    conf_ln_g = din("conf_ln_g", [CW, 1]); conf_ln_b = din("conf_ln_b", [CW, 1])
    conf_w_out = din("conf_w_out", [CW, D])
    hy_short_w = din("hy_short_w", [3, 3 * HW_]); hy_short_b = din("hy_short_b", [3 * HW_, 1])
    hy_w1 = din("hy_ffn_w1", [33, 64]); hy_b1 = din("hy_ffn_b1", [64, 1]); hy_f1 = din("hy_freq1", [64, 1])
    hy_w2 = din("hy_ffn_w2", [64, 64]); hy_b2 = din("hy_ffn_b2", [64, 1]); hy_f2 = din("hy_freq2", [64, 1])
    hy_w3 = din("hy_ffn_w3", [64, 2048])
    hy_skip = din("hy_skip", [1, 2 * HW_])
    hy_w_out = din("hy_w_out", [HW_, D])
    w_mix = din("w_mix_out", [D, D])
    ln_mix_g = din("ln_mix_g", [1, D]); ln_mix_b = din("ln_mix_b", [1, D])
    xa_wq = din("xa_wq", [D, D]); xa_wk = din("xa_wk", [D, D]); xa_wv = din("xa_wv", [D, D]); xa_wo = din("xa_wo", [D, D])
    ln_xa_g = din("ln_xa_g", [1, D]); ln_xa_b = din("ln_xa_b", [1, D])
    w_router = din("moe_w_router", [D, NE])
    if STOP_AFTER in ("A", "A2", "H", "C", "C1", "E"):
        w_gate = w_up = w_down = None
    else:
        w_gate = din("moe_w_gate", [NE, D, DFF]); w_up = din("moe_w_up", [NE, D, DFF]); w_down = din("moe_w_down", [NE, DFF, D])
    ln_moe_g = din("ln_moe_g", [1, D]); ln_moe_b = din("ln_moe_b", [1, D])
    ln_in_g_row = din("ln_in_g_row", [1, D]); ln_in_b_row = din("ln_in_b_row", [1, D])
    c_F = din("c_F", [128, 256]); c_T = din("c_T", [128, 256])
    c_featT = din("c_featT", [33, NFFT]); c_tts = din("c_tts", [128, 128])
    c_ident = din("c_ident", [128, 128]); c_iota = din("c_iota", [128, CAP])
    c_tidx = din("c_tidx", [128, NT, 2])
    c_blk8 = din("c_blk8", [128, 128])
    out_d = nc.dram_tensor("out", [L, D], F32, kind="ExternalOutput").ap()

    uT_d = dscr("uT_d", [CW, L], BF16)
    hyT_d = dscr("hyT_d", [3 * HW_, L], BF16)
    gT_d = dscr("gT_d", [2 * D, L], BF16)
    ucT_d = dscr("ucT_d", [CW, L], F32)
    hycT_d = dscr("hycT_d", [3 * HW_, L], BF16)
    zT_d = dscr("zT_d", [HW_, L], BF16)
    x2b_d = dscr("x2b_d", [L, D], BF16)
    acc_d = dscr("acc_d", [L, D], F32)
    affT_d = dscr("affT_d", [NE, L], F32)
    dbg_d = dscr("dbg_d", [128, 4096], F32)
    x1_d = dscr("x1_d", [L, D], F32)
    xg_d = dscr("xg_d", [L, D], F32)

    deltas = np.abs(np.linspace(math.log(1e-2) / 1.5, math.log(1e-2) / 0.3, HW_, dtype=np.float32)).astype(np.float64)

    with ExitStack() as es:
        S = Sched(nc, es)
        DB = {k: Buf(k) for k in ("uT", "hyT", "gT", "ucT", "hycT", "zT", "x2b", "acc", "affT", "dbg")}
        dbt = {}

        def dbuf(name, i):
            k = (name, i)
            if k not in dbt:
                dbt[k] = Buf("%s_%s" % (name, i))
            return dbt[k]

        with Phase(S, "G") as G:
            ident = G.sb("ident", [128, 128], F32)
            identb = G.sb("identb", [128, 128], BF16)
            S.dma("sp", lambda q: q.dma_start(out=ident[:], in_=c_ident[:, :]), writes=[ident], owner=ident)
            S.dve(lambda e: e.tensor_copy(identb[:], ident[:]), [ident], [identb])
            ones = G.sb("ones", [128, 128], F32)
            S.dve(lambda e: e.memset(ones[:], 1.0), [], [ones])

            phase_A(S, nc, locals())
            if STOP_AFTER != "A":
                phase_A2(S, nc, locals())
            if STOP_AFTER not in ("A", "A2"):
                phase_H(S, nc, locals())
            if STOP_AFTER not in ("A", "A2", "H"):
                with Phase(S, "G2") as G2:
                    affT = G2.sb("affT", [NE, L], F32)
                    phase_CD(S, nc, locals())
                    if STOP_AFTER not in ("C", "C1"):
                        phase_EF(S, nc, locals())
            if STOP_AFTER not in ("A", "A2", "H", "C", "C1", "E", "F"):
                phase_G(S, nc, locals())
            S.barrier()
        print("instr counts", S.ninstr, "sems", len(S.sems))
    build_nc.in_names = [k for k in dram if k not in ("uT_d", "hyT_d", "gT_d", "ucT_d", "hycT_d", "zT_d", "x2b_d", "acc_d", "affT_d", "dbg_d", "x1_d", "xg_d", "out")]
    return nc


def load_cast(S, P, q_dst, src_ap, shape, name, stage=None, eng="act", q="sp"):
    dst_tile, dst_ap = q_dst
    st = stage if stage is not None else P.sb(name + "_st", shape, F32)
    S.dma(q, lambda q_: q_.dma_start(out=st[:], in_=src_ap), writes=[st], owner=st)
    if eng == "act":
        S.act(lambda e: e.copy(dst_ap, st[:]), [st], [dst_tile])
    elif eng == "pool":
        S.pool(lambda e: e.tensor_copy(dst_ap, st[:]), [st], [dst_tile])
    else:
        S.dve(lambda e: e.tensor_copy(dst_ap, st[:]), [st], [dst_tile])


def ln_stats(S, P, xt, rstd_name="rs"):
    st = P.sb("bnst", [128, 2, 6], F32)
    mv = P.sb("bnmv", [128, 2], F32)
    rs = P.sb(rstd_name, [128, 1], F32)
    return st, mv, rs


def emit_ln_stats(S, xt_tile, x_ap, st, mv, rs):
    for h in range(2):
        S.dve(lambda e, h=h: e.bn_stats(st[:, h, :], x_ap[:, h * 512:(h + 1) * 512]), [xt_tile], [st])
    S.dve(lambda e: e.bn_aggr(mv[:], st[:].rearrange("p a b -> p (a b)")), [st], [mv])
    S.act(lambda e: e.activation(rs[:], mv[:, 1:2], AF.Sqrt, bias=EPS_AP[0][:], scale=1.0), [mv, EPS_AP[1]], [rs])
    S.dve(lambda e: e.reciprocal(rs[:], rs[:]), [rs], [rs])


EPS_AP = [None, None]


def phase_A(S, nc, env):
    x_d = env["x_d"]; w_in = env["w_in"]; ident = env["ident"]
    uT_d = env["uT_d"]; hyT_d = env["hyT_d"]; gT_d = env["gT_d"]; dbuf = env["dbuf"]
    G = env["G"]
    epsT = G.sb("epsT", [128, 1], F32)
    S.dve(lambda e: e.memset(epsT[:], EPS), [], [epsT])
    EPS_AP[0] = epsT; EPS_AP[1] = epsT
    with Phase(S, "A") as P:
        wsb = P.sb("w_in", [128, 8, COLS], BF16)
        stg = P.ring("wst", [128, 1152], F32, 4)
        i = 0
        for dk in range(8):
            for cq in range(4):
                st = stg[i % 4]
                load_cast(S, P, (wsb, wsb[:, dk, cq * 1152:(cq + 1) * 1152]),
                          w_in[dk * 128:(dk + 1) * 128, cq * 1152:(cq + 1) * 1152], None, "w", stage=st,
                          eng=("act" if i % 2 == 0 else "dve"), q=("sp" if i % 2 == 0 else "pool"))
                i += 1
        gsc = P.sb("gsc", [128, 8], F32); gbi = P.sb("gbi", [128, 8], F32); bg = P.sb("bg", [128, 16], F32)
        S.dma("sp", lambda q: q.dma_start(out=gsc[:], in_=env["ln_in_g"].rearrange("(k p) o -> p (k o)", p=128), allow_slow_non_contiguous=True), writes=[gsc], owner=gsc)
        S.dma("sp", lambda q: q.dma_start(out=gbi[:], in_=env["ln_in_b"].rearrange("(k p) o -> p (k o)", p=128), allow_slow_non_contiguous=True), writes=[gbi], owner=gbi)
        S.dma("sp", lambda q: q.dma_start(out=bg[:], in_=env["b_gate"].rearrange("(k p) o -> p (k o)", p=128), allow_slow_non_contiguous=True), writes=[bg], owner=bg)
        xts = P.ring("xt", [128, D], F32, 8)
        AG = row_bcast(S, P, "AG", env["ln_in_g_row"], ALPHA); AB = row_bcast(S, P, "AB", env["ln_in_b_row"], ALPHA)
        xgs = P.ring("xgs", [128, D], F32, 2)
        xg_d = env["xg_d"]
        nhs = P.ring("nh", [128, D], F32, 5)
        sts = [ln_stats(S, P, None) for _ in range(2)]
        hT = P.ring("hT", [128, 8, 512], BF16, 2)
        pst = P.ring("pst", [128, 512], F32, 2, ps=True)
        pmm = P.ring("pmm", [128, 512], F32, 4, ps=True)
        sig = P.ring("sig", [128, 512], F32, 2)
        ob = P.ring("ob", [128, 512], BF16, 4)
        xi = 0; oi = 0; pi = 0
        for blk in range(L // 512):
            h = hT[blk % 2]
            nts = []
            if blk == 0:
                for tt in range(4):
                    S.dma("sp", lambda q, tt=tt: q.dma_start(out=xts[tt][:], in_=x_d[tt * 128:(tt + 1) * 128, :]), writes=[xts[tt]], owner=xts[tt])
            if blk + 1 < L // 512:
                for tt in range(4):
                    xn_ = xts[((blk + 1) * 4 + tt) % 8]; tn = (blk + 1) * 512 + tt * 128
                    S.dma("sp", lambda q, xn_=xn_, tn=tn: q.dma_start(out=xn_[:], in_=x_d[tn:tn + 128, :]), writes=[xn_], owner=xn_)
            for tt in range(4):
                t0 = blk * 512 + tt * 128
                xt = xts[xi % 8]; nh = nhs[xi % 5]; st, mv, rs = sts[xi % 2]; xi += 1
                emit_ln_stats(S, xt, xt.t, st, mv, rs)
                S.dve(lambda e, nh=nh, xt=xt, mv=mv, rs=rs: e.tensor_scalar(nh[:], xt[:], mv[:, 0:1], rs[:], op0=ALU.subtract, op1=ALU.mult), [xt, mv, rs], [nh])
                xg = xgs[xi % 2]
                S.dve(lambda e, xg=xg, nh=nh: e.tensor_tensor(xg[:], nh[:], AG[:], op=ALU.mult), [nh, AG], [xg])
                S.dve(lambda e, xg=xg: e.tensor_tensor(xg[:], xg[:], AB[:], op=ALU.add), [xg, AB], [xg])
                S.dma("pool", lambda q, xg=xg, t0=t0: q.dma_start(out=xg_d[t0:t0 + 128, :], in_=xg[:]), reads=[xg], writes=[dbuf("xg", t0)], owner=xg)
                nts.append(nh)
            for dk in range(8):
                pt = pst[dk % 2]
                for tt in range(4):
                    S.pe(lambda e, pt=pt, tt=tt, dk=dk: e.transpose(pt[:, tt * 128:(tt + 1) * 128], nts[tt][:, dk * 128:(dk + 1) * 128], ident[:]), [nts[tt], ident], [pt])
                S.act(lambda e, pt=pt, dk=dk: e.activation(h[:, dk, :], pt[:], AF.Identity, bias=gbi[:, dk:dk + 1], scale=gsc[:, dk:dk + 1]), [pt, gbi, gsc], [h])

            def proj(cc):
                nonlocal pi
                pm = pmm[pi % 4]; pi += 1
                for dk in range(8):
                    S.pe(lambda e, pm=pm, dk=dk, cc=cc: e.matmul(pm[:], wsb[:, dk, cc * 128:(cc + 1) * 128], h[:, dk, :], start=(dk == 0), stop=(dk == 7)), [wsb, h], [pm])
                return pm
            tsl = slice(blk * 512, (blk + 1) * 512)
            for cc in range(4):
                pa = proj(cc); pb = proj(cc + 4)
                sg = sig[cc % 2]; o = ob[oi % 4]; oi += 1
                S.act(lambda e, sg=sg, pb=pb: e.activation(sg[:], pb[:], AF.Sigmoid), [pb], [sg])
                S.dve(lambda e, o=o, pa=pa, sg=sg: e.tensor_tensor(o[:], pa[:], sg[:], op=ALU.mult), [pa, sg], [o])
                S.dma("pool", lambda q, o=o, cc=cc: q.dma_start(out=uT_d[cc * 128:(cc + 1) * 128, tsl], in_=o[:]), reads=[o], writes=[dbuf("uT", cc)], owner=o)
            for cc in range(8, 20):
                pm = proj(cc); o = ob[oi % 4]; oi += 1
                S.act(lambda e, o=o, pm=pm: e.copy(o[:], pm[:]), [pm], [o])
                S.dma("pool", lambda q, o=o, cc=cc: q.dma_start(out=hyT_d[(cc - 8) * 128:(cc - 7) * 128, tsl], in_=o[:]), reads=[o], writes=[dbuf("hyT", cc - 8)], owner=o)
            for cc in range(20, 36):
                pm = proj(cc); o = ob[oi % 4]; oi += 1
                S.act(lambda e, o=o, pm=pm, cc=cc: e.activation(o[:], pm[:], AF.Sigmoid, bias=bg[:, cc - 20:cc - 19], scale=1.0), [pm, bg], [o])
                S.dma("pool", lambda q, o=o, cc=cc: q.dma_start(out=gT_d[(cc - 20) * 128:(cc - 19) * 128, tsl], in_=o[:]), reads=[o], writes=[dbuf("gT", (cc - 20, blk))], owner=o)


def phase_A2(S, nc, env):
    identb = env["identb"]; dbuf = env["dbuf"]
    uT_d = env["uT_d"]; hyT_d = env["hyT_d"]; ucT_d = env["ucT_d"]; hycT_d = env["hycT_d"]
    with Phase(S, "A2") as P:
        rows = P.ring("row", [128, L + 32], BF16, 2)
        for r in rows:
            S.pool(lambda e, r=r: e.memset(r[:, 0:16], 0.0), [], [r])
            S.pool(lambda e, r=r: e.memset(r[:, L + 16:L + 32], 0.0), [], [r])
        wT = P.sb("wT", [128, 4, 31], F32); wT3 = P.sb("wT3", [128, 12, 3], F32)
        cb = P.sb("cb", [128, 4], F32); cb3 = P.sb("cb3", [128, 12], F32)
        for c in range(4):
            S.dma("sp", lambda q, c=c: q.dma_start(out=wT[:, c, :], in_=env["conf_dw_w"][:, c * 128:(c + 1) * 128].rearrange("k p -> p k"), allow_slow_non_contiguous=True), writes=[wT], owner=wT)
        for c in range(12):
            S.dma("sp", lambda q, c=c: q.dma_start(out=wT3[:, c, :], in_=env["hy_short_w"][:, c * 128:(c + 1) * 128].rearrange("k p -> p k"), allow_slow_non_contiguous=True), writes=[wT3], owner=wT3)
        S.dma("sp", lambda q: q.dma_start(out=cb[:], in_=env["conf_dw_b"].rearrange("(c p) o -> p (c o)", p=128), allow_slow_non_contiguous=True), writes=[cb], owner=cb)
        S.dma("sp", lambda q: q.dma_start(out=cb3[:], in_=env["hy_short_b"].rearrange("(c p) o -> p (c o)", p=128), allow_slow_non_contiguous=True), writes=[cb3], owner=cb3)
        dg = P.ring("dg", [128, 31, 128], BF16, 2)
        pc = P.ring("pc", [128, 512], F32, 3, ps=True)
        of = P.ring("of", [128, 512], F32, 3)
        obf = P.ring("obf", [128, 512], BF16, 3)
        pi = 0; ri = 0
        jobs = [("u", c) for c in range(4)] + [("h", c) for c in range(12)]
        def prep(j):
            kind, c = jobs[j]
            row = rows[j % 2]; dgt = dg[j % 2]
            K = 31 if kind == "u" else 3
            src = uT_d if kind == "u" else hyT_d
            S.dma("sp", lambda q: q.dma_start(out=row[:, 16:16 + L], in_=src[c * 128:(c + 1) * 128, :]),
                  reads=[dbuf("uT" if kind == "u" else "hyT", c)], writes=[row], owner=row)
            wt = wT if kind == "u" else wT3
            for k in range(K):
                S.dve(lambda e, k=k: e.tensor_scalar(dgt[:, k, :], identb[:], wt[:, c, k:k + 1], None, op0=ALU.mult), [identb, wt], [dgt])
        prep(0)
        for j, (kind, c) in enumerate(jobs):
            row = rows[j % 2]; dgt = dg[j % 2]
            K = 31 if kind == "u" else 3
            pad = (K - 1) // 2
            if j + 1 < len(jobs):
                prep(j + 1)
            for blk in range(L // 512):
                p = pc[pi % 3]
                for k in range(K):
                    off = 16 + blk * 512 + k - pad
                    S.pe(lambda e, p=p, k=k, off=off, dgt=dgt, row=row: e.matmul(p[:], dgt[:, k, :], row[:, off:off + 512], start=(k == 0), stop=(k == K - 1)), [dgt, row], [p])
                tsl = slice(blk * 512, (blk + 1) * 512)
                if kind == "u":
                    o = of[pi % 3]
                    S.act(lambda e, o=o, p=p, c=c: e.activation(o[:], p[:], AF.Identity, bias=cb[:, c:c + 1], scale=1.0), [p, cb], [o])
                    S.dma("sp", lambda q, o=o, c=c, tsl=tsl: q.dma_start(out=ucT_d[c * 128:(c + 1) * 128, tsl], in_=o[:]), reads=[o], writes=[dbuf("ucT", blk)], owner=o)
                else:
                    o = obf[pi % 3]
                    S.act(lambda e, o=o, p=p, c=c: e.activation(o[:], p[:], AF.Identity, bias=cb3[:, c:c + 1], scale=1.0), [p, cb3], [o])
                    S.dma("sp", lambda q, o=o, c=c, tsl=tsl: q.dma_start(out=hycT_d[c * 128:(c + 1) * 128, tsl], in_=o[:]), reads=[o], writes=[dbuf("hycT", c)], owner=o)
                pi += 1


def phase_H(S, nc, env):
    hycT_d = env["hycT_d"]; zT_d = env["zT_d"]; ones = env["ones"]; deltas = env["deltas"]
    c_F = env["c_F"]; c_T = env["c_T"]; c_featT = env["c_featT"]; c_tts = env["c_tts"]
    INVN = 1.0 / NFFT
    with Phase(S, "H") as P:
        Fst = P.sb("Fst", [128, 256], F32)
        Tst = P.sb("Tst", [128, 256], F32)
        S.dma("sp", lambda q: q.dma_start(out=Fst[:], in_=c_F[:, :]), writes=[Fst], owner=Fst)
        S.dma("sp", lambda q: q.dma_start(out=Tst[:], in_=c_T[:, :]), writes=[Tst], owner=Tst)
        Fb = P.sb("Fb", [128, 256], BF16); FA = P.sb("FA", [128, 256], BF16); FB_ = P.sb("FB", [128, 256], BF16)
        Fin = P.sb("Fin", [128, 128], BF16)
        S.dve(lambda e: e.tensor_copy(Fb[:], Fst[:]), [Fst], [Fb])
        S.dve(lambda e: e.tensor_copy(FA[:, 0:128], Fst[:, 0:128]), [Fst], [FA])
        S.dve(lambda e: e.tensor_scalar(FA[:, 128:256], Fst[:, 128:256], -1.0, None, op0=ALU.mult), [Fst], [FA])
        S.dve(lambda e: e.tensor_copy(FB_[:, 0:128], Fst[:, 128:256]), [Fst], [FB_])
        S.dve(lambda e: e.tensor_copy(FB_[:, 128:256], Fst[:, 0:128]), [Fst], [FB_])
        S.dve(lambda e: e.tensor_scalar(Fin[:], Fst[:, 128:256], -1.0, None, op0=ALU.mult), [Fst], [Fin])
        TT1 = P.sb("TT1", [128, 2, 256], F32); TT2 = P.sb("TT2", [128, 2, 256], F32)
        for i in range(2):
            for hh in range(2):
                S.dve(lambda e, i=i, hh=hh: e.tensor_copy(TT1[:, i, hh * 128:(hh + 1) * 128], Tst[:, 0:128]), [Tst], [TT1])
                S.dve(lambda e, i=i, hh=hh: e.tensor_copy(TT2[:, i, hh * 128:(hh + 1) * 128], Tst[:, 128:256]), [Tst], [TT2])
        NH = 66
        FbH = P.sb("FbH", [128, 2 * NH], BF16)
        S.dve(lambda e: e.tensor_copy(FbH[:, 0:NH], Fst[:, 0:NH]), [Fst], [FbH])
        S.dve(lambda e: e.tensor_copy(FbH[:, NH:2 * NH], Fst[:, 128:128 + NH]), [Fst], [FbH])
        TH1 = P.sb("TH1", [128, 2, 2 * NH], F32); TH2 = P.sb("TH2", [128, 2, 2 * NH], F32)
        TK1 = P.sb("TK1", [128, 2, 2 * NH], F32); TK2 = P.sb("TK2", [128, 2, 2 * NH], F32)
        for i in range(2):
            for hh in range(2):
                S.dve(lambda e, i=i, hh=hh: e.tensor_copy(TH1[:, i, hh * NH:(hh + 1) * NH], Tst[:, 0:NH]), [Tst], [TH1])
                S.dve(lambda e, i=i, hh=hh: e.tensor_copy(TH2[:, i, hh * NH:(hh + 1) * NH], Tst[:, 128:128 + NH]), [Tst], [TH2])
        for src_, dst_ in ((TH1, TK1), (TH2, TK2)):
            S.dve(lambda e, src_=src_, dst_=dst_: e.tensor_scalar(dst_[:], src_[:], 2.0, None, op0=ALU.mult), [src_], [dst_])
            for i in range(2):
                for hh in range(2):
                    b0 = hh * NH
                    S.dve(lambda e, src_=src_, dst_=dst_, i=i, b0=b0: e.tensor_copy(dst_[:, i, b0:b0 + 1], src_[:, i, b0:b0 + 1]), [src_], [dst_])
                    S.dve(lambda e, src_=src_, dst_=dst_, i=i, b0=b0: e.tensor_copy(dst_[:, i, b0 + 64:b0 + 65], src_[:, i, b0 + 64:b0 + 65]), [src_], [dst_])
                    S.dve(lambda e, dst_=dst_, i=i, b0=b0: e.memset(dst_[:, i, b0 + 65:b0 + 66], 0.0), [], [dst_])
        tts = P.sb("tts", [128, 128], F32)
        S.dma("sp", lambda q: q.dma_start(out=tts[:], in_=c_tts[:, :]), writes=[tts], owner=tts)
        e6 = P.sb("e6", [128, 1], F32)
        S.dve(lambda e: e.memset(e6[:], 1e-6), [], [e6])
        H2 = P.sb("H2", [128, NFFT], BF16)
        S.pool(lambda e: e.memset(H2[:], 0.0), [], [H2])
        w1 = P.sb("w1", [33, 64], F32); w2d = P.sb("w2d", [64, 128], F32)
        S.dma("sp", lambda q: q.dma_start(out=w1[:], in_=env["hy_w1"][:, :]), writes=[w1], owner=w1)
        S.dma("sp", lambda q: q.dma_start(out=w2d[:, 0:64], in_=env["hy_w2"][:, :]), writes=[w2d], owner=w2d)
        S.dma("sp", lambda q: q.dma_start(out=w2d[:, 64:128], in_=env["hy_w2"][:, :]), writes=[w2d], owner=w2d)
        fb = P.sb("fb", [128, 4], F32)
        S.dma("sp", lambda q: q.dma_start(out=fb[0:64, 0:1], in_=env["hy_f1"][:, :]), writes=[fb], owner=fb)
        S.dma("sp", lambda q: q.dma_start(out=fb[0:64, 1:2], in_=env["hy_b1"][:, :]), writes=[fb], owner=fb)
        for hh in range(2):
            S.dma("sp", lambda q, hh=hh: q.dma_start(out=fb[hh * 64:(hh + 1) * 64, 2:3], in_=env["hy_f2"][:, :]), writes=[fb], owner=fb)
            S.dma("sp", lambda q, hh=hh: q.dma_start(out=fb[hh * 64:(hh + 1) * 64, 3:4], in_=env["hy_b2"][:, :]), writes=[fb], owner=fb)
        S.dma("sp", lambda q: q.dma_start(out=fb[64:128, 0:1], in_=env["hy_f1"][:, :]), writes=[fb], owner=fb)
        S.dma("sp", lambda q: q.dma_start(out=fb[64:128, 1:2], in_=env["hy_b1"][:, :]), writes=[fb], owner=fb)
        sb_ = P.sb("sb", [128, 4], F32)
        S.dve(lambda e: e.tensor_scalar(sb_[:, 0:1], fb[:, 0:1], 1.0 / 3.0, None, op0=ALU.mult), [fb], [sb_])
        S.dve(lambda e: e.tensor_tensor(sb_[:, 1:2], fb[:, 0:1], fb[:, 1:2], op=ALU.mult), [fb], [sb_])
        S.dve(lambda e: e.tensor_scalar(sb_[:, 1:2], sb_[:, 1:2], 1.0 / 3.0, None, op0=ALU.mult), [sb_], [sb_])
        S.dve(lambda e: e.tensor_scalar(sb_[:, 2:3], fb[:, 2:3], 1.0 / 3.0, None, op0=ALU.mult), [fb], [sb_])
        S.dve(lambda e: e.tensor_tensor(sb_[:, 3:4], fb[:, 2:3], fb[:, 3:4], op=ALU.mult), [fb], [sb_])
        S.dve(lambda e: e.tensor_scalar(sb_[:, 3:4], sb_[:, 3:4], 1.0 / 3.0, None, op0=ALU.mult), [sb_], [sb_])
        bank = P.ring("bk", [128, 512], F32, 8, ps=True)
        PF = Phase(S, "HF"); PF.__enter__()
        fts = PF.ring("ft", [33, 512], F32, 2)
        s1 = PF.ring("s1", [128, 512], F32, 2); qq = PF.ring("qq", [128, 512], F32, 2); h1 = PF.ring("h1", [64, 512], F32, 2)
        for blk in range(NFFT // 512):
            ft = fts[blk % 2]; s = s1[blk % 2]; q_ = qq[blk % 2]; hh1 = h1[blk % 2]
            pb1 = bank[blk % 2]; pb2 = bank[2 + blk % 2]
            S.dma("sp", lambda q, ft=ft, blk=blk: q.dma_start(out=ft[:], in_=c_featT[:, blk * 512:(blk + 1) * 512]), writes=[ft], owner=ft)
            S.pe(lambda e, pb1=pb1, ft=ft: e.matmul(pb1[0:64, :], w1[:], ft[:], start=True, stop=True), [w1, ft], [pb1])
            S.act(lambda e, s=s, pb1=pb1: e.activation(s[0:64, :], pb1[0:64, :], AF.Sin, bias=sb_[0:64, 1:2], scale=sb_[0:64, 0:1]), [pb1, sb_], [s])
            S.dve(lambda e, s=s, q_=q_: e.tensor_tensor(q_[0:64, :], s[0:64, :], s[0:64, :], op=ALU.mult), [s], [q_])
            S.dve(lambda e, q_=q_: e.tensor_scalar(q_[0:64, :], q_[0:64, :], -4.0, 3.0, op0=ALU.mult, op1=ALU.add), [q_], [q_])
            S.dve(lambda e, s=s, q_=q_, hh1=hh1: e.tensor_tensor(hh1[:], q_[0:64, :], s[0:64, :], op=ALU.mult), [s, q_], [hh1])
            S.pe(lambda e, pb2=pb2, hh1=hh1: e.matmul(pb2[:], w2d[:], hh1[:], start=True, stop=True), [w2d, hh1], [pb2])
            lo = 0 if blk < 16 else 64
            S.act(lambda e, s=s, pb2=pb2, lo=lo: e.activation(s[lo:lo + 64, :], pb2[lo:lo + 64, :], AF.Sin, bias=sb_[lo:lo + 64, 3:4], scale=sb_[lo:lo + 64, 2:3]), [pb2, sb_], [s])
            S.dve(lambda e, s=s, q_=q_, lo=lo: e.tensor_tensor(q_[lo:lo + 64, :], s[lo:lo + 64, :], s[lo:lo + 64, :], op=ALU.mult), [s], [q_])
            S.dve(lambda e, q_=q_, lo=lo: e.tensor_scalar(q_[lo:lo + 64, :], q_[lo:lo + 64, :], -4.0, 3.0, op0=ALU.mult, op1=ALU.add), [q_], [q_])
            S.dve(lambda e, s=s, q_=q_, lo=lo, blk=blk: e.tensor_tensor(H2[lo:lo + 64, blk * 512:(blk + 1) * 512], q_[lo:lo + 64, :], s[lo:lo + 64, :], op=ALU.mult), [s, q_], [H2])
        S.dve(lambda e: e.memset(H2[64:128, L:L + 1], 0.0), [], [H2])
        PF.__exit__(None, None, None)
        w3st = P.sb("w3st", [128, 2, 512], F32)
        w3v = env["hy_w3"].rearrange("m (o d c) -> m o d c", o=2, d=2)
        S.dma("sp", lambda q: q.dma_start(out=w3st[0:64, :, :], in_=w3v[:, :, 0, :]), writes=[w3st], owner=w3st)
        S.dma("sp", lambda q: q.dma_start(out=w3st[64:128, :, :], in_=w3v[:, :, 1, :]), writes=[w3st], owner=w3st)
        W3s = P.sb("W3s", [128, 2, 512], BF16)
        S.dve(lambda e: e.tensor_copy(W3s[:], w3st[:]), [w3st], [W3s])
        skp = P.sb("skp", [1, 2, 512], F32)
        S.dma("sp", lambda q: q.dma_start(out=skp[:], in_=env["hy_skip"].rearrange("o (a c) -> o a c", a=2)), writes=[skp], owner=skp)

        QC = 4
        NW = 3

        class Lane:
            pass

        lanes = []
        for l in range(NW):
            ln = Lane()
            ln.kBre = P.sb("kBre", [128, 2 * QC, NH], BF16); ln.kBim = P.sb("kBim", [128, 2 * QC, NH], BF16)
            ln.Kr = P.sb("Kr", [128, 2 * QC, NH], F32); ln.Ki = P.sb("Ki", [128, 2 * QC, NH], F32)
            ln.q1h = P.ring("q1h", [128, 2, 2 * NH], F32, 2); ln.q2h = P.ring("q2h", [128, 2, 2 * NH], F32, 2)
            ln.q1 = P.ring("q1", [128, 2, 256], F32, 2); ln.q2 = P.ring("q2", [128, 2, 256], F32, 2)
            ln.t = [P.sb("t%d" % i, [128, QC * NH], F32) for i in range(4)]
            ln.Uv = P.sb("Uv", [64, QC, 128], BF16); ln.G1 = P.sb("G1", [64, QC, 128], BF16); ln.G2 = P.sb("G2", [64, QC, 128], BF16)
            ln.Bre = P.sb("Bre", [128, QC, NH], BF16); ln.Bim = P.sb("Bim", [128, QC, NH], BF16)
            ln.Yre = P.sb("Yre", [128, QC, NH], BF16); ln.Yim = P.sb("Yim", [128, QC, NH], BF16)
            ln.Cre = P.sb("Cre", [128, QC, 128], BF16); ln.Cim = P.sb("Cim", [128, QC, 128], BF16)
            ln.Z1 = P.sb("Z1", [64, QC, 128], BF16); ln.Z2 = P.sb("Z2", [64, QC, 128], BF16)
            ln.pa = bank[2 * l]; ln.px = (bank[2 * l], bank[2 * l + 1])
            ln.qi = 0
            lanes.append(ln)
        pk = bank[6]; py = bank[7]
        Dg = P.sb("Dg", [128, GC, 128], F32)
        kt = P.sb("kt", [128, 2, GC, 128], F32)
        kbR = P.ring("kb", [128, 2, GC, 128], BF16, 2)
        junk = P.sb("junk", [128, 128], F32)
        ssq = P.sb("ssq", [128, 2 * GC], F32); scl = P.sb("scl", [128, 2 * GC], F32)

        def kblock_stages(g):
            c0 = g * GC
            kb = kbR[g % 2]
            st = []

            def k_dec():
                for c in range(GC):
                    S.act(lambda e, c=c: e.activation(Dg[:, c, :], tts[:], AF.Exp, scale=-float(deltas[c0 + c])), [tts], [Dg])
            st.append(k_dec)

            def k_gen(r):
                def f():
                    for j in range(16):
                        n2 = r * 16 + j
                        S.pe(lambda e, j=j, n2=n2: e.matmul(pk[:, j * 32:(j + 1) * 32], H2[:, n2:NFFT:128], W3s[:, :, c0:c0 + GC], start=True, stop=True), [H2, W3s], [pk])
                    pkv = pk[:].rearrange("p (n o c) -> p o c n", n=16, o=2)
                    for o in range(2):
                        S.dve(lambda e, o=o: e.tensor_tensor(kt[:, o, :, r * 16:(r + 1) * 16], pkv[:, o, :, :], Dg[:, :, r * 16:(r + 1) * 16], op=ALU.mult), [pk, Dg], [kt])
                return f
            for r in range(8):
                st.append(k_gen(r))

            def k_sq():
                for o in range(2):
                    for c in range(GC):
                        m = o * GC + c
                        S.act(lambda e, o=o, c=c, m=m: e.activation(junk[:], kt[:, o, c, :], AF.Square, accum_out=ssq[:, m:m + 1]), [kt], [junk, ssq])
            st.append(k_sq)

            def k_tot():
                S.pe(lambda e: e.matmul(pk[:, 0:2 * GC], ones[:], ssq[:], start=True, stop=True), [ones, ssq], [pk])
                S.act(lambda e: e.activation(scl[:], pk[:, 0:2 * GC], AF.Sqrt, bias=e6[:], scale=1.0), [pk, e6], [scl])
                S.dve(lambda e: e.reciprocal(scl[:], scl[:]), [scl], [scl])
            st.append(k_tot)

            def k_scale():
                for o in range(2):
                    for c in range(GC):
                        m = o * GC + c
                        S.act(lambda e, o=o, c=c, m=m: e.activation(kb[:, o, c, :], kt[:, o, c, :], AF.Copy, scale=scl[:, m:m + 1]), [kt, scl], [kb])
                S.dve(lambda e: e.tensor_tensor(kb[0:1, :, :, 0], kb[0:1, :, :, 0], skp[0:1, :, c0:c0 + GC], op=ALU.add), [kb, skp], [kb])
            st.append(k_scale)
            return st

        def twiddle_f(ln, dre, dim, m0, kern):
            pa = ln.pa
            q1 = ln.q1h[ln.qi % 2]; q2 = ln.q2h[ln.qi % 2]; ln.qi += 1
            pav = pa[:].rearrange("p (a b) -> p a b", a=2)[:, :, 0:2 * NH]
            T1 = TK1 if kern else TH1; T2 = TK2 if kern else TH2
            S.dve(lambda e: e.tensor_tensor(q1[:], pav, T1[:], op=ALU.mult), [pa, T1], [q1])
            S.dve(lambda e: e.tensor_tensor(q2[:], pav, T2[:], op=ALU.mult), [pa, T2], [q2])
            S.pool(lambda e: e.tensor_tensor(dre[:, m0:m0 + 2, :], q1[:, :, 0:NH], q2[:, :, NH:2 * NH], op=ALU.subtract), [q1, q2], [dre])
            S.pool(lambda e: e.tensor_tensor(dim[:, m0:m0 + 2, :], q2[:, :, 0:NH], q1[:, :, NH:2 * NH], op=ALU.add), [q1, q2], [dim])

        def twiddle_i(ln, dre, dim, m0):
            pa = ln.pa
            q1 = ln.q1[ln.qi % 2]; q2 = ln.q2[ln.qi % 2]; ln.qi += 1
            pav = pa[0:NH, :].rearrange("p (a b) -> p a b", a=2)
            S.dve(lambda e: e.tensor_tensor(q1[0:NH], pav, TT1[0:NH], op=ALU.mult), [pa, TT1], [q1])
            S.dve(lambda e: e.tensor_tensor(q2[0:NH], pav, TT2[0:NH], op=ALU.mult), [pa, TT2], [q2])
            S.pool(lambda e: e.tensor_tensor(dre[0:NH, m0:m0 + 2, :], q1[0:NH, :, 0:128], q2[0:NH, :, 128:256], op=ALU.add), [q1, q2], [dre])
            S.pool(lambda e: e.tensor_tensor(dim[0:NH, m0:m0 + 2, :], q1[0:NH, :, 128:256], q2[0:NH, :, 0:128], op=ALU.subtract), [q1, q2], [dim])

        def stage3(ln, bre, bim, m0):
            pxr, pxi = ln.px
            W4 = 4 * NH
            rr = bre[:, m0:m0 + 4, :]; ri = bim[:, m0:m0 + 4, :]
            S.pe(lambda e: e.matmul(pxr[:, 0:W4], Fb[:, 0:128], rr, start=True, stop=False), [Fb, bre], [pxr])
            S.pe(lambda e: e.matmul(pxr[:, 0:W4], Fin[:], ri, start=False, stop=True), [Fin, bim], [pxr])
            S.pe(lambda e: e.matmul(pxi[:, 0:W4], Fb[:, 0:128], ri, start=True, stop=False), [Fb, bim], [pxi])
            S.pe(lambda e: e.matmul(pxi[:, 0:W4], Fb[:, 128:256], rr, start=False, stop=True), [Fb, bre], [pxi])
            return pxr, pxi

        def item_stages(ln, it):
            c0 = it * QC
            st = []

            def s_load():
                for tl, r0 in ((ln.Uv, 2 * HW_ + c0), (ln.G1, c0), (ln.G2, HW_ + c0)):
                    S.dma("sp", lambda q, tl=tl, r0=r0: q.dma_start(out=tl[:], in_=hycT_d[r0:r0 + QC, :].rearrange("c (a b) -> a c b", b=128)), writes=[tl], owner=tl)
            st.append(s_load)
            kb = kbR[(c0 // GC) % 2]
            cl = c0 % GC

            def s_ks1(o):
                def f():
                    for p_ in range(QC // 2):
                        for i in range(2):
                            c = 2 * p_ + i
                            S.pe(lambda e, c=c, i=i: e.matmul(ln.pa[:, i * 256:i * 256 + 2 * NH], kb[:, o, cl + c, :], FbH[:], start=True, stop=True), [kb, FbH], [ln.pa])
                        twiddle_f(ln, ln.kBre, ln.kBim, o * QC + 2 * p_, True)
                return f
            st.append(s_ks1(0)); st.append(s_ks1(1))

            def s_ks3(o):
                def f():
                    pxr, pxi = stage3(ln, ln.kBre, ln.kBim, o * QC)
                    S.act(lambda e: e.activation(ln.Kr[:, o * QC:(o + 1) * QC, :].rearrange("p a b -> p (a b)"), pxr[:, 0:QC * NH], AF.Copy, scale=INVN), [pxr], [ln.Kr])
                    S.act(lambda e: e.activation(ln.Ki[:, o * QC:(o + 1) * QC, :].rearrange("p a b -> p (a b)"), pxi[:, 0:QC * NH], AF.Copy, scale=INVN), [pxi], [ln.Ki])
                return f
            st.append(s_ks3(0)); st.append(s_ks3(1))

            def conv_stages(o, U, Gt, Zt, last):
                def c1():
                    for p_ in range(QC // 2):
                        for i in range(2):
                            c = 2 * p_ + i
                            S.pe(lambda e, c=c, i=i: e.matmul(ln.pa[:, i * 256:i * 256 + 2 * NH], U[:, c, :], FbH[0:64, :], start=True, stop=True), [U, FbH], [ln.pa])
                        twiddle_f(ln, ln.Bre, ln.Bim, 2 * p_, False)

                def c2():
                    pxr, pxi = stage3(ln, ln.Bre, ln.Bim, 0)
                    t1, t2, t3, t4 = ln.t
                    kr = ln.Kr[:, o * QC:(o + 1) * QC, :].rearrange("p a b -> p (a b)"); ki = ln.Ki[:, o * QC:(o + 1) * QC, :].rearrange("p a b -> p (a b)")
                    W4 = QC * NH
                    S.dve(lambda e: e.tensor_tensor(t1[:], pxr[:, 0:W4], kr, op=ALU.mult), [pxr, ln.Kr], [t1])
                    S.dve(lambda e: e.tensor_tensor(t2[:], pxi[:, 0:W4], ki, op=ALU.mult), [pxi, ln.Ki], [t2])
                    S.dve(lambda e: e.tensor_tensor(t3[:], pxr[:, 0:W4], ki, op=ALU.mult), [pxr, ln.Ki], [t3])
                    S.dve(lambda e: e.tensor_tensor(t4[:], pxi[:, 0:W4], kr, op=ALU.mult), [pxi, ln.Kr], [t4])
                    S.pool(lambda e: e.tensor_tensor(ln.Yre[:].rearrange("p a b -> p (a b)"), t1[:], t2[:], op=ALU.subtract), [t1, t2], [ln.Yre])
                    S.pool(lambda e: e.tensor_tensor(ln.Yim[:].rearrange("p a b -> p (a b)"), t3[:], t4[:], op=ALU.add), [t3, t4], [ln.Yim])

                def c3():
                    for p_ in range(QC // 2):
                        for i in range(2):
                            c = 2 * p_ + i
                            S.pe(lambda e, c=c, i=i: e.matmul(ln.pa[0:NH, i * 256:(i + 1) * 256], ln.Yre[:, c, :], FA[:], start=True, stop=False), [ln.Yre, FA], [ln.pa])
                            S.pe(lambda e, c=c, i=i: e.matmul(ln.pa[0:NH, i * 256:(i + 1) * 256], ln.Yim[:, c, :], FB_[:], start=False, stop=True), [ln.Yim, FB_], [ln.pa])
                        twiddle_i(ln, ln.Cre, ln.Cim, 2 * p_)

                def c4():
                    S.pe(lambda e: e.matmul(py[0:64, :], Fb[0:NH, 0:64], ln.Cre[0:NH].rearrange("p a b -> p (a b)"), start=True, stop=False), [Fb, ln.Cre], [py])
                    S.pe(lambda e: e.matmul(py[0:64, :], Fb[0:NH, 128:192], ln.Cim[0:NH].rearrange("p a b -> p (a b)"), start=False, stop=True), [Fb, ln.Cim], [py])
                    S.dve(lambda e: e.tensor_tensor(Zt[:].rearrange("p a b -> p (a b)"), py[0:64, :], Gt[:].rearrange("p a b -> p (a b)"), op=ALU.mult), [py, Gt], [Zt])
                    if last:
                        S.dma("sp", lambda q: q.dma_start(out=zT_d[c0:c0 + QC, :].rearrange("c (a b) -> a c b", b=128), in_=Zt[:]), reads=[Zt], writes=[env["dbuf"]("zT", it)], owner=Zt)
                return [c1, c2, c3, c4]
            st += conv_stages(0, ln.Uv, ln.G1, ln.Z1, False)
            st += conv_stages(1, ln.Z1, ln.G2, ln.Z2, True)
            return st

        nitems = HW_ // QC
        ipg = GC // QC
        ngrp = HW_ // GC
        for f_ in kblock_stages(0):
            f_()
        kq = []
        next_g = 1
        for i0_ in range(0, nitems, NW):
            idxs = [i for i in range(i0_, min(i0_ + NW, nitems))]
            gmax = idxs[-1] // ipg
            while next_g <= gmax:
                kq += [(next_g, f_) for f_ in kblock_stages(next_g)]
                next_g += 1
            while kq and kq[0][0] <= gmax:
                kq.pop(0)[1]()
            if not kq and next_g < ngrp and next_g <= gmax + 1:
                kq += [(next_g, f_) for f_ in kblock_stages(next_g)]
                next_g += 1
            sts_ = [item_stages(lanes[l], idxs[l]) for l in range(len(idxs))]
            for k in range(len(sts_[0])):
                for l in range(len(idxs)):
                    sts_[l][k]()
                if kq:
                    kq.pop(0)[1]()
        while kq:
            kq.pop(0)[1]()


def load_w_bf16(S, dst, src_ap, kchunks, ncols, stg, cnt):
    for k in range(kchunks):
        for c0 in range(0, ncols, 1024):
            st = stg[cnt[0] % len(stg)]
            w = min(1024, ncols - c0)
            S.dma("sp" if cnt[0] % 2 == 0 else "pool", lambda q, st=st, k=k, c0=c0, w=w: q.dma_start(out=st[:, 0:w], in_=src_ap[k * 128:(k + 1) * 128, c0:c0 + w]), writes=[st], owner=st)
            if cnt[0] % 2 == 0:
                S.act(lambda e, st=st, k=k, c0=c0, w=w: e.copy(dst[:, k, c0:c0 + w], st[:, 0:w]), [st], [dst])
            else:
                S.dve(lambda e, st=st, k=k, c0=c0, w=w: e.tensor_copy(dst[:, k, c0:c0 + w], st[:, 0:w]), [st], [dst])
            cnt[0] += 1


def row_bcast(S, P, name, src_row, scale=None):
    t = P.sb(name, [128, D], F32)
    S.dma("sp", lambda q: q.dma_start(out=t[:], in_=src_row.broadcast_to([128, D])), writes=[t], owner=t)
    if scale is not None:
        S.act(lambda e: e.mul(t[:], t[:], float(scale)), [t], [t])
    return t


def col_chunks(S, P, name, src_col, k):
    t = P.sb(name, [128, k], F32)
    S.dma("sp", lambda q: q.dma_start(out=t[:], in_=src_col.rearrange("(k p) o -> p (k o)", p=128), allow_slow_non_contiguous=True), writes=[t], owner=t)
    return t


def phase_CD(S, nc, env):
    phase_C(S, nc, env)
    if STOP_AFTER != "C1":
        phase_D(S, nc, env)


def phase_C(S, nc, env):
    ones = env["ones"]; x_d = env["x_d"]; ucT_d = env["ucT_d"]; zT_d = env["zT_d"]; gT_d = env["gT_d"]; x1_d = env["x1_d"]
    dbuf = env["dbuf"]
    with Phase(S, "C") as P:
        stg = P.ring("stg", [128, 1024], F32, 4); cnt = [0]
        cwo = P.sb("cwo", [128, 4, D], BF16); hwo = P.sb("hwo", [128, 4, D], BF16); wmx = P.sb("wmx", [128, 8, D], BF16)
        load_w_bf16(S, cwo, env["conf_w_out"], 4, D, stg, cnt)
        load_w_bf16(S, hwo, env["hy_w_out"], 4, D, stg, cnt)
        load_w_bf16(S, wmx, env["w_mix"], 8, D, stg, cnt)
        cg = col_chunks(S, P, "cg", env["conf_ln_g"], 4); cbb = col_chunks(S, P, "cbb", env["conf_ln_b"], 4)
        MG = row_bcast(S, P, "MG", env["ln_mix_g"]); MB = row_bcast(S, P, "MB", env["ln_mix_b"])
        xg_d = env["xg_d"]
        uc = P.sb("uc", [128, 4, 512], F32); sq = P.sb("sq", [128, 4, 512], F32)
        zt = P.sb("zt", [128, 4, 512], BF16); ua = P.sb("ua", [128, 4, 512], BF16)
        mean = P.sb("mean", [128, 512], F32); var = P.sb("var", [128, 512], F32); rstd = P.sb("rstd", [128, 512], F32)
        dd = P.ring("dd", [128, 512], F32, 2)
        gch = P.ring("gch", [128, 2, 512], BF16, 4)
        m1 = P.ring("m1", [128, 512], F32, 2); m2 = P.ring("m2", [128, 512], F32, 2)
        mgR = P.ring("mg", [128, 8, 512], BF16, 2)
        xgs = P.ring("xg", [128, D], F32, 3)
        ss = P.ring("s", [128, D], F32, 2); x1s = P.ring("x1", [128, D], F32, 2)
        sts = [ln_stats(S, P, None) for _ in range(3)]
        bank = P.ring("bk", [128, 512], F32, 8, ps=True)
        cnts = {"ti": 0, "si": 0}

        def front(blk):
            tsl = slice(blk * 512, (blk + 1) * 512)
            mg = mgR[blk % 2]
            st = []

            def f0():
                S.dma("sp", lambda q: q.dma_start(out=uc[:], in_=ucT_d[:, tsl].rearrange("(k p) t -> p k t", p=128)), writes=[uc], owner=uc)
                S.dma("sp", lambda q: q.dma_start(out=zt[:], in_=zT_d[:, tsl].rearrange("(k p) t -> p k t", p=128)), writes=[zt], owner=zt)
                S.act(lambda e: e.activation(sq[:], uc[:], AF.Square), [uc], [sq])
            st.append(f0)

            def f1():
                pS1 = bank[0]; pS2 = bank[1]
                for k in range(4):
                    S.pe(lambda e, k=k: e.matmul(pS1[:], ones[:], uc[:, k, :], start=(k == 0), stop=(k == 3)), [ones, uc], [pS1])
                for k in range(4):
                    S.pe(lambda e, k=k: e.matmul(pS2[:], ones[:], sq[:, k, :], start=(k == 0), stop=(k == 3)), [ones, sq], [pS2])
                S.act(lambda e: e.mul(mean[:], pS1[:], 1.0 / CW), [pS1], [mean])
                S.dve(lambda e: e.tensor_tensor(var[:], mean[:], mean[:], op=ALU.mult), [mean], [var])
                S.dve(lambda e: e.scalar_tensor_tensor(var[:], pS2[:], 1.0 / CW, var[:], op0=ALU.mult, op1=ALU.subtract), [pS2, var], [var])
                S.act(lambda e: e.activation(rstd[:], var[:], AF.Sqrt, bias=EPS_AP[0][:], scale=1.0), [var, EPS_AP[0]], [rstd])
                S.dve(lambda e: e.reciprocal(rstd[:], rstd[:]), [rstd], [rstd])
            st.append(f1)

            def f2(k):
                def f():
                    d_ = dd[k % 2]
                    S.dve(lambda e: e.tensor_tensor(d_[:], uc[:, k, :], mean[:], op=ALU.subtract), [uc, mean], [d_])
                    S.dve(lambda e: e.tensor_tensor(d_[:], d_[:], rstd[:], op=ALU.mult), [d_, rstd], [d_])
                    S.act(lambda e: e.activation(ua[:, k, :], d_[:], AF.Silu, bias=cbb[:, k:k + 1], scale=cg[:, k:k + 1]), [d_, cbb, cg], [ua])
                return f
            for k in range(4):
                st.append(f2(k))

            def f3(dc):
                def f():
                    pya = bank[2 + (dc % 2) * 2]; pyb = bank[3 + (dc % 2) * 2]
                    g_ = gch[dc % 4]; a1 = m1[dc % 2]; a2 = m2[dc % 2]
                    S.dma("sp", lambda q: q.dma_start(out=g_[:, 0, :], in_=gT_d[dc * 128:(dc + 1) * 128, tsl]), writes=[g_], owner=g_)
                    S.dma("sp", lambda q: q.dma_start(out=g_[:, 1, :], in_=gT_d[D + dc * 128:D + (dc + 1) * 128, tsl]), writes=[g_], owner=g_)
                    for k in range(4):
                        S.pe(lambda e, k=k: e.matmul(pya[:], cwo[:, k, dc * 128:(dc + 1) * 128], ua[:, k, :], start=(k == 0), stop=(k == 3)), [cwo, ua], [pya])
                    for k in range(4):
                        S.pe(lambda e, k=k: e.matmul(pyb[:], hwo[:, k, dc * 128:(dc + 1) * 128], zt[:, k, :], start=(k == 0), stop=(k == 3)), [hwo, zt], [pyb])
                    S.dve(lambda e: e.tensor_tensor(a1[:], pya[:], g_[:, 0, :], op=ALU.mult), [pya, g_], [a1])
                    S.dve(lambda e: e.tensor_tensor(a2[:], pyb[:], g_[:, 1, :], op=ALU.mult), [pyb, g_], [a2])
                    S.pool(lambda e: e.tensor_tensor(mg[:, dc, :], a1[:], a2[:], op=ALU.add), [a1, a2], [mg])
                return f
            for dc in range(8):
                st.append(f3(dc))
            return st

        def back(blk):
            mg = mgR[blk % 2]
            st = []
            for tt in range(4):
                t0 = blk * 512 + tt * 128
                ti = cnts["ti"]; cnts["ti"] += 1
                xg = xgs[ti % 3]; s_ = ss[ti % 2]; x1 = x1s[ti % 2]

                def ba(xg=xg, t0=t0):
                    S.dma("sp", lambda q: q.dma_start(out=xg[:], in_=xg_d[t0:t0 + 128, :]), reads=[dbuf("xg", t0)], writes=[xg], owner=xg)
                st.append(ba)

                def bb(xg=xg, s_=s_, x1=x1, tt=tt, t0=t0):
                    for half in range(2):
                        pm = bank[6 + half]
                        for k in range(8):
                            S.pe(lambda e, pm=pm, k=k, half=half: e.matmul(pm[:], mg[:, k, tt * 128:(tt + 1) * 128], wmx[:, k, half * 512:(half + 1) * 512], start=(k == 0), stop=(k == 7)), [mg, wmx], [pm])
                        S.dve(lambda e, pm=pm, half=half: e.tensor_tensor(s_[:, half * 512:(half + 1) * 512], pm[:], xg[:, half * 512:(half + 1) * 512], op=ALU.add), [pm, xg], [s_])
                    st_, mv, rs = sts[cnts["si"] % 3]; cnts["si"] += 1
                    emit_ln_stats(S, s_, s_.t, st_, mv, rs)
                    S.dve(lambda e: e.tensor_scalar(x1[:], s_[:], mv[:, 0:1], rs[:], op0=ALU.subtract, op1=ALU.mult), [s_, mv, rs], [x1])
                    S.dve(lambda e: e.tensor_tensor(x1[:], x1[:], MG[:], op=ALU.mult), [x1, MG], [x1])
                    S.pool(lambda e: e.tensor_tensor(x1[:], x1[:], MB[:], op=ALU.add), [x1, MB], [x1])
                    S.dma("pool", lambda q: q.dma_start(out=x1_d[t0:t0 + 128, :], in_=x1[:]), reads=[x1], writes=[dbuf("x1", t0)], owner=x1)
                st.append(bb)
            return st

        nblk = L // 512
        import os
        if os.environ.get("SEQC", "0") == "1":
            for blk in range(nblk):
                for f_ in front(blk):
                    f_()
                for f_ in back(blk):
                    f_()
            nblk = 0
        else:
            for f_ in front(0):
                f_()
        for blk in range(nblk):
            bs = back(blk)
            fs = front(blk + 1) if blk + 1 < nblk else []
            n = max(len(bs), len(fs))
            bi = 0; fi = 0
            for k in range(n):
                while bi < len(bs) and bi * n <= k * len(bs):
                    bs[bi](); bi += 1
                while fi < len(fs) and fi * n <= k * len(fs):
                    fs[fi](); fi += 1
            while bi < len(bs):
                bs[bi](); bi += 1
            while fi < len(fs):
                fs[fi](); fi += 1


def phase_D(S, nc, env):
    ident = env["ident"]; identb = env["identb"]; x1_d = env["x1_d"]; mem_d = env["mem_d"]; ones = env["ones"]
    acc_d = env["acc_d"]; x2b_d = env["x2b_d"]; affT = env["affT"]; dbuf = env["dbuf"]
    with Phase(S, "D") as P:
        wq = P.sb("wq", [128, 8, D], BF16); wo = P.sb("wo", [128, 8, D], BF16)
        kT = P.sb("kT", [128, 8, NMEM], BF16); V = P.sb("V", [128, 2, D], BF16)
        wr = P.sb("wr", [128, 8, NE], F32)
        S.dma("sp", lambda q: q.dma_start(out=wr[:], in_=env["w_router"].rearrange("(k p) e -> p k e", p=128)), writes=[wr], owner=wr)
        bank = P.ring("bk", [128, 512], F32, 7, ps=True)
        ppT = P.ps("ppT", [128, 8, 128], BF16)
        XG = row_bcast(S, P, "XG", env["ln_xa_g"]); XB = row_bcast(S, P, "XB", env["ln_xa_b"])
        PW = Phase(S, "DW"); PW.__enter__()
        stg = PW.ring("stg", [128, 1024], F32, 4); cnt = [0]
        with Phase(S, "DK") as PK:
            wk = PK.sb("wk", [128, 8, D], BF16); wv = PK.sb("wv", [128, 8, D], BF16)
            load_w_bf16(S, wk, env["xa_wk"], 8, D, stg, cnt)
            load_w_bf16(S, wv, env["xa_wv"], 8, D, stg, cnt)
            memT = PK.sb("memT", [128, 8, NMEM], BF16)
            mt = PK.ring("mt", [128, D], F32, 2)
            for mc in range(2):
                m_ = mt[mc]
                S.dma("sp", lambda q, m_=m_, mc=mc: q.dma_start(out=m_[:], in_=mem_d[mc * 128:(mc + 1) * 128, :]), writes=[m_], owner=m_)
                for half in range(2):
                    pt = bank[half]
                    for j in range(4):
                        dk = half * 4 + j
                        S.pe(lambda e, pt=pt, j=j, dk=dk, m_=m_: e.transpose(pt[:, j * 128:(j + 1) * 128], m_[:, dk * 128:(dk + 1) * 128], ident[:]), [m_, ident], [pt])
                    S.act(lambda e, pt=pt, half=half, mc=mc: e.copy(memT[:, half * 4:half * 4 + 4, mc * 128:(mc + 1) * 128], pt[:].rearrange("p (a b) -> p a b", a=4)), [pt], [memT])
            for hc in range(8):
                pk_ = bank[2 + hc % 2]
                for k in range(8):
                    S.pe(lambda e, pk_=pk_, k=k, hc=hc: e.matmul(pk_[:, 0:NMEM], wk[:, k, hc * 128:(hc + 1) * 128], memT[:, k, :], start=(k == 0), stop=(k == 7)), [wk, memT], [pk_])
                S.act(lambda e, pk_=pk_, hc=hc: e.copy(kT[:, hc, :], pk_[:, 0:NMEM]), [pk_], [kT])
            for mc in range(2):
                for half in range(2):
                    pv = bank[4 + half]
                    for k in range(8):
                        S.pe(lambda e, pv=pv, k=k, mc=mc, half=half: e.matmul(pv[:], memT[:, k, mc * 128:(mc + 1) * 128], wv[:, k, half * 512:(half + 1) * 512], start=(k == 0), stop=(k == 7)), [memT, wv], [pv])
                    S.act(lambda e, pv=pv, mc=mc, half=half: e.copy(V[:, mc, half * 512:(half + 1) * 512], pv[:]), [pv], [V])
        load_w_bf16(S, wq, env["xa_wq"], 8, D, stg, cnt)
        load_w_bf16(S, wo, env["xa_wo"], 8, D, stg, cnt)
        PW.__exit__(None, None, None)
        x1t = P.ring("x1t", [128, D], F32, 4)
        xr = P.ring("xr", [128, D], F32, 2)
        x1T = P.sb("x1T", [128, 8, 512], BF16); qT = P.sb("qT", [128, 8, 512], BF16)
        pf = P.ring("pf", [128, 4, NMEM], F32, 2); pn = P.ring("pn", [128, 4, NMEM], BF16, 2)
        pT = P.sb("pT", [128, 8, 512], BF16); oTR = P.ring("oT", [128, 8, 512], BF16, 2)
        mx = P.ring("mx", [128, 4], F32, 2); sm = P.ring("sm", [128, 4], F32, 2)
        s2 = P.ring("s2", [128, D], F32, 2); x2 = P.ring("x2", [128, D], F32, 2)
        accs = P.ring("accs", [128, D], F32, 2); x2b = P.ring("x2b", [128, D], BF16, 2)
        x2T = P.sb("x2T", [128, 8, 512], F32)
        ex = P.sb("ex", [NE, 512], F32); rsum = P.sb("rsum", [NE, 512], F32)
        sts = [ln_stats(S, P, None) for _ in range(2)]
        cn = {"si": 0, "ti": 0}

        def front(blk):
            oT = oTR[blk % 2]
            st = []

            def d0():
                for tt in range(4):
                    t0 = blk * 512 + tt * 128
                    xt = x1t[tt]
                    S.dma("sp", lambda q, xt=xt, t0=t0: q.dma_start(out=xt[:], in_=x1_d[t0:t0 + 128, :]), reads=[dbuf("x1", t0)], writes=[xt], owner=xt)
            st.append(d0)

            def d1(dk0):
                def f():
                    for dk in range(dk0, dk0 + 4):
                        pt = bank[dk % 2]
                        for tt in range(4):
                            S.pe(lambda e, pt=pt, tt=tt, dk=dk: e.transpose(pt[:, tt * 128:(tt + 1) * 128], x1t[tt][:, dk * 128:(dk + 1) * 128], ident[:]), [x1t[tt], ident], [pt])
                        S.act(lambda e, pt=pt, dk=dk: e.copy(x1T[:, dk, :], pt[:]), [pt], [x1T])
                return f
            st.append(d1(0)); st.append(d1(4))

            def d2(h0):
                def f():
                    for hc in range(h0, h0 + 4):
                        pq = bank[2 + hc % 2]
                        for k in range(8):
                            S.pe(lambda e, pq=pq, k=k, hc=hc: e.matmul(pq[:], wq[:, k, hc * 128:(hc + 1) * 128], x1T[:, k, :], start=(k == 0), stop=(k == 7)), [wq, x1T], [pq])
                        S.act(lambda e, pq=pq, hc=hc: e.mul(qT[:, hc, :], pq[:], 1.0 / 16.0), [pq], [qT])
                return f
            st.append(d2(0)); st.append(d2(4))

            def d3(tt):
                def f():
                    p_f = pf[tt % 2]; p_n = pn[tt % 2]; mx_ = mx[tt % 2]; sm_ = sm[tt % 2]
                    for hp in range(2):
                        psc = bank[4 + hp]
                        for hh in range(2):
                            h = hp * 2 + hh
                            for k in range(2):
                                S.pe(lambda e, psc=psc, hh=hh, h=h, k=k: e.matmul(psc[:, hh * 256:(hh + 1) * 256], qT[:, 2 * h + k, tt * 128:(tt + 1) * 128], kT[:, 2 * h + k, :], start=(k == 0), stop=(k == 1)), [qT, kT], [psc])
                        S.dve(lambda e, psc=psc, hp=hp: e.tensor_reduce(mx_[:, hp * 2:hp * 2 + 2], psc[:].rearrange("p (a b) -> p a b", a=2), axis=mybir.AxisListType.X, op=ALU.max, negate=True), [psc], [mx_])
                        for hh in range(2):
                            h = hp * 2 + hh
                            S.act(lambda e, psc=psc, hh=hh, h=h: e.activation(p_f[:, h, :], psc[:, hh * 256:(hh + 1) * 256], AF.Exp, bias=mx_[:, h:h + 1], scale=1.0, accum_out=sm_[:, h:h + 1]), [psc, mx_], [p_f, sm_])
                    S.dve(lambda e: e.reciprocal(sm_[:], sm_[:]), [sm_], [sm_])
                    for h in range(4):
                        S.dve(lambda e, h=h: e.tensor_scalar(p_n[:, h, :], p_f[:, h, :], sm_[:, h:h + 1], None, op0=ALU.mult), [p_f, sm_], [p_n])
                    for h in range(4):
                        for mc in range(2):
                            S.pe(lambda e, h=h, mc=mc: e.transpose(ppT[:, h * 2 + mc, :], p_n[:, h, mc * 128:(mc + 1) * 128], identb[:]), [p_n, identb], [ppT])
                    S.act(lambda e: e.copy(pT[:, :, tt * 128:(tt + 1) * 128], ppT[:]), [ppT], [pT])
                return f
            for tt in range(4):
                st.append(d3(tt))

            def d4(h0):
                def f():
                    for hc in range(h0, h0 + 4):
                        po = bank[2 + hc % 2]; h = hc // 2
                        for mc in range(2):
                            S.pe(lambda e, po=po, mc=mc, hc=hc, h=h: e.matmul(po[:], V[:, mc, hc * 128:(hc + 1) * 128], pT[:, h * 2 + mc, :], start=(mc == 0), stop=(mc == 1)), [V, pT], [po])
                        S.act(lambda e, po=po, hc=hc: e.copy(oT[:, hc, :], po[:]), [po], [oT])
                return f
            st.append(d4(0)); st.append(d4(4))
            return st

        def back(blk):
            oT = oTR[blk % 2]
            st = []
            for tt in range(4):
                t0 = blk * 512 + tt * 128
                ti = cn["ti"]; cn["ti"] += 1
                s_ = s2[ti % 2]; x2_ = x2[ti % 2]; ac = accs[ti % 2]; xb = x2b[ti % 2]; xr_ = xr[ti % 2]

                def b0(tt=tt, t0=t0, s_=s_, x2_=x2_, ac=ac, xb=xb, xr_=xr_):
                    S.dma("sp", lambda q: q.dma_start(out=xr_[:], in_=x1_d[t0:t0 + 128, :]), reads=[dbuf("x1", t0)], writes=[xr_], owner=xr_)
                    for half in range(2):
                        px = bank[half]
                        for k in range(8):
                            S.pe(lambda e, px=px, k=k, half=half: e.matmul(px[:], oT[:, k, tt * 128:(tt + 1) * 128], wo[:, k, half * 512:(half + 1) * 512], start=(k == 0), stop=(k == 7)), [oT, wo], [px])
                        S.dve(lambda e, px=px, half=half: e.scalar_tensor_tensor(s_[:, half * 512:(half + 1) * 512], xr_[:, half * 512:(half + 1) * 512], ALPHA, px[:], op0=ALU.mult, op1=ALU.add), [px, xr_], [s_])
                    st_, mv, rs = sts[cn["si"] % 2]; cn["si"] += 1
                    emit_ln_stats(S, s_, s_.t, st_, mv, rs)
                    S.dve(lambda e: e.tensor_scalar(x2_[:], s_[:], mv[:, 0:1], rs[:], op0=ALU.subtract, op1=ALU.mult), [s_, mv, rs], [x2_])
                    S.dve(lambda e: e.tensor_tensor(x2_[:], x2_[:], XG[:], op=ALU.mult), [x2_, XG], [x2_])
                    S.dve(lambda e: e.tensor_tensor(x2_[:], x2_[:], XB[:], op=ALU.add), [x2_, XB], [x2_])
                    S.act(lambda e: e.mul(ac[:], x2_[:], ALPHA), [x2_], [ac])
                    S.act(lambda e: e.copy(xb[:], x2_[:]), [x2_], [xb])
                    S.dma("pool", lambda q: q.dma_start(out=acc_d[t0:t0 + 128, :], in_=ac[:]), reads=[ac], writes=[dbuf("acc", t0)], owner=ac)
                    S.dma("pool", lambda q: q.dma_start(out=x2b_d[t0:t0 + 128, :], in_=xb[:]), reads=[xb], writes=[dbuf("x2b", t0)], owner=xb)
                st.append(b0)

                def b1(tt=tt, x2_=x2_):
                    for half in range(2):
                        pt = bank[2 + half]
                        for j in range(4):
                            dk = half * 4 + j
                            S.pe(lambda e, pt=pt, j=j, dk=dk: e.transpose(pt[:, j * 128:(j + 1) * 128], x2_[:, dk * 128:(dk + 1) * 128], ident[:]), [x2_, ident], [pt])
                        S.act(lambda e, pt=pt, half=half: e.copy(x2T[:, half * 4:half * 4 + 4, tt * 128:(tt + 1) * 128], pt[:].rearrange("p (a b) -> p a b", a=4)), [pt], [x2T])
                st.append(b1)

            def b2():
                pl = bank[6]
                for k in range(8):
                    S.pe(lambda e, k=k: e.matmul(pl[0:NE, :], wr[:, k, :], x2T[:, k, :], start=(k == 0), stop=(k == 7)), [wr, x2T], [pl])
                S.act(lambda e: e.activation(ex[:], pl[0:NE, :], AF.Exp), [pl], [ex])
                S.pe(lambda e: e.matmul(pl[0:NE, :], ones[0:NE, 0:NE], ex[:], start=True, stop=True), [ones, ex], [pl])
                S.dve(lambda e: e.reciprocal(rsum[:], pl[0:NE, :]), [pl], [rsum])
                S.dve(lambda e: e.tensor_tensor(affT[:, blk * 512:(blk + 1) * 512], ex[:], rsum[:], op=ALU.mult), [ex, rsum], [affT])
            st.append(b2)
            return st

        nblk = L // 512
        for f_ in front(0):
            f_()
        for blk in range(nblk):
            bs = back(blk)
            fs = front(blk + 1) if blk + 1 < nblk else []
            n = max(len(bs), len(fs))
            bi = 0; fi = 0
            for k in range(n):
                while bi < len(bs) and bi * n <= k * len(bs):
                    bs[bi](); bi += 1
                while fi < len(fs) and fi * n <= k * len(fs):
                    fs[fi](); fi += 1
            while bi < len(bs):
                bs[bi](); bi += 1
            while fi < len(fs):
                fs[fi](); fi += 1
        S.dma("sp", lambda q: q.dma_start(out=env["affT_d"][:, :], in_=affT[:]), reads=[affT], writes=[dbuf("affT", 0)], owner=affT)


def phase_EF(S, nc, env):
    affT = env["affT"]; ident = env["ident"]; identb = env["identb"]
    x2b_d = env["x2b_d"]; acc_d = env["acc_d"]
    with Phase(S, "EF") as PX:
        idxT = PX.sb("idxT", [128, NE, 8], U32)
        gate = PX.sb("gate", [128, NE, 8], F32)
        phase_E(S, nc, env, idxT, gate)
        if STOP_AFTER == "E":
            return
        phase_F(S, nc, env, idxT, gate)


def phase_E(S, nc, env, idxT, gate):
    affT = env["affT"]; ident = env["ident"]
    with Phase(S, "E") as P:
        bank = P.ring("bk", [128, 512], F32, 6, ps=True)
        a128 = P.sb("a128", [128, 1024], F32)
        S.dma("sp", lambda q: q.dma_start(out=a128[:], in_=env["affT_d"].rearrange("e (s t) -> (e s) t", s=8)), writes=[a128], owner=a128)
        blk8 = P.sb("blk8", [128, 128], F32)
        S.dma("sp", lambda q: q.dma_start(out=blk8[:], in_=env["c_blk8"][:, :]), writes=[blk8], owner=blk8)
        jk8 = P.sb("jk8", [128, 1024], BF16)
        lo8 = P.sb("lo8", [128, 2], F32); hi8 = P.sb("hi8", [128, 2], F32); mid8 = P.sb("mid8", [128, 2], F32)
        cnt8 = P.sb("cnt8", [128, 2], F32); ge8 = P.sb("ge8", [128, 2], F32); d8 = P.sb("d8", [128, 2], F32)
        lo = P.sb("lo", [NE, 1], F32)
        msk = P.sb("msk", [NE, L], F32); cum = P.sb("cum", [NE, L], F32)
        one16 = P.sb("one16", [NE, L], BF16)
        S.pool(lambda e: e.memset(one16[:], 1.0), [], [one16])
        S.dve(lambda e: e.memset(lo8[:], 0.0), [], [lo8])
        S.dve(lambda e: e.memset(hi8[:], 1.0), [], [hi8])
        pcn = bank[5]
        for it in range(34):
            S.dve(lambda e: e.tensor_tensor(mid8[:], lo8[:], hi8[:], op=ALU.add), [lo8, hi8], [mid8])
            S.dve(lambda e: e.tensor_scalar(mid8[:], mid8[:], 0.5, None, op0=ALU.mult), [mid8], [mid8])
            S.dve(lambda e: e.tensor_scalar(jk8[:], a128[:], mid8[:, 0:1], None, op0=ALU.is_ge, op1=ALU.add, accum_out=cnt8[:, 0:1]), [a128, mid8], [jk8, cnt8])
            S.dve(lambda e: e.tensor_copy(cnt8[:, 1:2], cnt8[:, 0:1]), [cnt8], [cnt8])
            S.pe(lambda e: e.matmul(pcn[:, 0:2], blk8[:], cnt8[:], start=True, stop=True), [blk8, cnt8], [pcn])
            S.dve(lambda e: e.tensor_scalar(ge8[:], pcn[:, 0:2], CAP - 0.5, None, op0=ALU.is_ge), [pcn], [ge8])
            S.dve(lambda e: e.tensor_tensor(d8[:], mid8[:], lo8[:], op=ALU.subtract), [mid8, lo8], [d8])
            S.dve(lambda e: e.scalar_tensor_tensor(lo8[:], d8[:], ge8[:, 0:1], lo8[:], op0=ALU.mult, op1=ALU.add), [d8, ge8, lo8], [lo8])
            S.dve(lambda e: e.tensor_tensor(d8[:], hi8[:], mid8[:], op=ALU.subtract), [hi8, mid8], [d8])
            S.dve(lambda e: e.scalar_tensor_tensor(hi8[:], d8[:], ge8[:, 0:1], mid8[:], op0=ALU.mult, op1=ALU.add), [d8, ge8, mid8], [hi8])
        lrow = P.sb("lrow", [2, 128], F32)
        S.pe(lambda e: e.transpose(pcn[0:2, 0:128], lo8[:], ident[:]), [lo8, ident], [pcn])
        S.dve(lambda e: e.tensor_copy(lrow[:], pcn[0:2, 0:128]), [pcn], [lrow])
        S.pe(lambda e: e.transpose(pcn[0:NE, 256:257], lrow[0:1, 0:128:8], ident[0:1, 0:1]), [lrow, ident], [pcn])
        S.dve(lambda e: e.tensor_copy(lo[:], pcn[0:NE, 256:257]), [pcn], [lo])
        S.dve(lambda e: e.tensor_scalar(msk[:], affT[:], lo[:, 0:1], None, op0=ALU.is_ge), [affT, lo], [msk])
        S.dve(lambda e: e.tensor_tensor_scan(cum[:], one16[:], msk[:], 0.0, op0=ALU.mult, op1=ALU.add), [one16, msk], [cum])
        S.dve(lambda e: e.tensor_tensor(cum[:], cum[:], msk[:], op=ALU.mult), [cum, msk], [cum])
        S.dve(lambda e: e.tensor_scalar(cum[:], cum[:], -1.0, None, op0=ALU.add), [cum], [cum])
        keyT = P.sb("keyT", [128, NT, NE], F32); afT = P.sb("afT", [128, NT, NE], F32)
        for src, dst in ((cum, keyT), (affT, afT)):
            for hb in range(2):
                pb = bank[hb]
                for j in range(32):
                    tl = hb * 32 + j
                    S.pe(lambda e, pb=pb, j=j, tl=tl, src=src: e.transpose(pb[:, j * NE:(j + 1) * NE], src[:, tl * 128:(tl + 1) * 128], ident[0:NE, 0:NE]), [src, ident], [pb])
                S.act(lambda e, pb=pb, hb=hb, dst=dst: e.copy(dst[:, hb * 32:(hb + 1) * 32, :].rearrange("p a b -> p (a b)"), pb[:]), [pb], [dst])
        vals = P.sb("vals", [128, NT, NE, 5], BF16)
        tidx = P.sb("tidx", [128, NT, 2], F32)
        S.dma("sp", lambda q: q.dma_start(out=tidx[:], in_=env["c_tidx"][:, :, :]), writes=[tidx], owner=tidx)
        for e_ in range(NE):
            S.dve(lambda e, e_=e_: e.tensor_copy(vals[:, :, e_, 0:2], tidx[:]), [tidx], [vals])
        r1 = P.sb("r1", [128, NT, NE], F32); hb16 = P.sb("hb16", [128, NT, NE], BF16)
        S.dve(lambda e: e.tensor_copy(hb16[:], afT[:]), [afT], [hb16])
        S.dve(lambda e: e.tensor_copy(vals[:, :, :, 2], hb16[:]), [hb16], [vals])
        S.dve(lambda e: e.tensor_tensor(r1[:], afT[:], hb16[:], op=ALU.subtract), [afT, hb16], [r1])
        S.dve(lambda e: e.tensor_copy(hb16[:], r1[:]), [r1], [hb16])
        S.dve(lambda e: e.tensor_copy(vals[:, :, :, 3], hb16[:]), [hb16], [vals])
        S.dve(lambda e: e.tensor_tensor(r1[:], r1[:], hb16[:], op=ALU.subtract), [r1, hb16], [r1])
        S.dve(lambda e: e.tensor_copy(vals[:, :, :, 4], r1[:]), [r1], [vals])
        iota32 = P.sb("iota32", [128, CAP], F32)
        S.dma("sp", lambda q: q.dma_start(out=iota32[:], in_=env["c_iota"][:, :]), writes=[iota32], owner=iota32)
        iota = P.sb("iota", [128, CAP], mybir.dt.float16)
        S.dve(lambda e: e.tensor_copy(iota[:], iota32[:]), [iota32], [iota])
        zl = P.sb("zl", [128, 128], BF16); zr = P.sb("zr", [128, 512], BF16)
        S.dve(lambda e: e.memset(zl[:], 0.0), [], [zl])
        S.dve(lambda e: e.memset(zr[:], 0.0), [], [zr])
        accA = bank[2]; accB = bank[3]
        for a_ in (accA, accB):
            S.pe(lambda e, a_=a_: e.matmul(a_[:], zl[:], zr[:], start=True, stop=False, skip_group_check=True), [zl, zr], [a_])
        O = P.ring("O", [128, CAP], BF16, 3)
        oi = 0
        for tl in range(NT):
            for e_ in range(NE):
                o_ = O[oi % 3]; oi += 1
                S.dve(lambda e, o_=o_, tl=tl, e_=e_: e.tensor_scalar(o_[:], iota[:], keyT[:, tl, e_:e_ + 1], None, op0=ALU.is_equal), [iota, keyT], [o_])
                a_ = accA if e_ < 8 else accB
                for sc in range(8):
                    col = ((e_ % 8) * 8 + sc) * 5
                    S.pe(lambda e, a_=a_, o_=o_, sc=sc, col=col, tl=tl, e_=e_: e.matmul(a_[:, col:col + 5], o_[:, sc * 128:(sc + 1) * 128], vals[:, tl, e_, :], start=False, stop=(tl == NT - 1), skip_group_check=True), [o_, vals], [a_])
        res = P.sb("res", [128, NE, 8, 5], F32)
        S.act(lambda e: e.copy(res[:, 0:8, :, :].rearrange("p a b c -> p (a b c)"), accA[:, 0:320]), [accA], [res])
        S.act(lambda e: e.copy(res[:, 8:16, :, :].rearrange("p a b c -> p (a b c)"), accB[:, 0:320]), [accB], [res])
        idf = P.sb("idf", [128, NE, 8], F32)
        S.dve(lambda e: e.scalar_tensor_tensor(idf[:], res[:, :, :, 0], 128.0, res[:, :, :, 1], op0=ALU.mult, op1=ALU.add), [res], [idf])
        S.dve(lambda e: e.tensor_copy(idxT[:], idf[:]), [idf], [idxT])
        S.dve(lambda e: e.tensor_tensor(gate[:], res[:, :, :, 2], res[:, :, :, 3], op=ALU.add), [res], [gate])
        S.dve(lambda e: e.tensor_tensor(gate[:], gate[:], res[:, :, :, 4], op=ALU.add), [gate, res], [gate])


def phase_F(S, nc, env, idxT, gate):
    identb = env["identb"]; x2b_d = env["x2b_d"]; acc_d = env["acc_d"]
    w_gate = env["w_gate"]; w_up = env["w_up"]; w_down = env["w_down"]
    accbuf = env["dbuf"]("accsc", 0)
    FG = 256
    NFG = DFF // FG
    with Phase(S, "F") as P:
        xg = P.sb("xg", [128, 8, D], BF16)
        xgT = P.ring("xgT", [128, 8, CAP], BF16, 2)
        hT = P.sb("hT", [128, 16, CAP], BF16)
        Wd = P.sb("Wd", [128, 16, D], BF16)
        Wg = P.ring("Wg", [128, 8, FG], BF16, 2); Wu = P.ring("Wu", [128, 8, FG], BF16, 2)
        stg = P.ring("stg", [128, 8 * FG], F32, 3)
        yo = P.ring("yo", [128, D], F32, 2)
        sg = P.ring("sg", [128, 512], F32, 2)
        bank = P.ring("bk", [128, 512], F32, 7, ps=True)
        ptr = P.ps("ptr", [128, 8, 128], BF16)
        sc_ = [0]

        def wload(dst, dst_ap, src_ap, a, b):
            st = stg[sc_[0] % 3]; sc_[0] += 1
            S.dma("sp", lambda q: q.dma_start(out=st[:].rearrange("p (a b) -> p a b", a=a), in_=src_ap), writes=[st], owner=st)
            S.act(lambda e: e.copy(dst_ap, st[:].rearrange("p (a b) -> p a b", a=a)), [st], [dst])

        def load_group(e_, fg):
            wg_ = Wg[fg % 2]; wu_ = Wu[fg % 2]
            wload(wg_, wg_[:], w_gate[e_, :, fg * FG:(fg + 1) * FG].rearrange("(k p) f -> p k f", p=128), 8, FG)
            wload(wu_, wu_[:], w_up[e_, :, fg * FG:(fg + 1) * FG].rearrange("(k p) f -> p k f", p=128), 8, FG)
            wload(Wd, Wd[:, fg * 2:fg * 2 + 2, :], w_down[e_, fg * FG:(fg + 1) * FG, :].rearrange("(k p) d -> p k d", p=128), 2, D)

        def gather(e_):
            for sc in range(8):
                S.dma("pool", lambda q, sc=sc: q.indirect_dma_start(out=xg[:, sc, :], out_offset=None, in_=x2b_d[:, :], in_offset=bass.IndirectOffsetOnAxis(ap=idxT[:, e_, sc:sc + 1], axis=0)), reads=[idxT], writes=[xg], owner=xg)

        def transposes(e_):
            xt_ = xgT[e_ % 2]
            for sc in range(8):
                for dk in range(8):
                    S.pe(lambda e, sc=sc, dk=dk: e.transpose(ptr[:, dk, :], xg[:, sc, dk * 128:(dk + 1) * 128], identb[:]), [xg, identb], [ptr])
                S.act(lambda e, sc=sc: e.copy(xt_[:, :, sc * 128:(sc + 1) * 128], ptr[:]), [ptr], [xt_])

        yi = 0; pi = 0
        gather(0)
        transposes(0)
        load_group(0, 0)
        for e_ in range(NE):
            xt_ = xgT[e_ % 2]
            if e_ + 1 < NE:
                gather(e_ + 1)
            for fg in range(NFG):
                wg_ = Wg[fg % 2]; wu_ = Wu[fg % 2]
                if fg + 1 < NFG:
                    load_group(e_, fg + 1)
                for fk in range(FG // 128):
                    fkg = fg * (FG // 128) + fk
                    for sh in range(2):
                        pg = bank[(pi % 2) * 2]; pu = bank[(pi % 2) * 2 + 1]; s_ = sg[pi % 2]; pi += 1
                        for dk in range(8):
                            S.pe(lambda e, pg=pg, dk=dk, fk=fk, sh=sh, wg_=wg_: e.matmul(pg[:], wg_[:, dk, fk * 128:(fk + 1) * 128], xt_[:, dk, sh * 512:(sh + 1) * 512], start=(dk == 0), stop=(dk == 7)), [wg_, xt_], [pg])
                        for dk in range(8):
                            S.pe(lambda e, pu=pu, dk=dk, fk=fk, sh=sh, wu_=wu_: e.matmul(pu[:], wu_[:, dk, fk * 128:(fk + 1) * 128], xt_[:, dk, sh * 512:(sh + 1) * 512], start=(dk == 0), stop=(dk == 7)), [wu_, xt_], [pu])
                        S.act(lambda e, s_=s_, pg=pg: e.activation(s_[:], pg[:], AF.Silu), [pg], [s_])
                        S.dve(lambda e, s_=s_, pu=pu, fkg=fkg, sh=sh: e.tensor_tensor(hT[:, fkg, sh * 512:(sh + 1) * 512], s_[:], pu[:], op=ALU.mult), [s_, pu], [hT])
            for sc in range(8):
                y_ = yo[yi % 2]; yi += 1
                for dh in range(2):
                    py = bank[4 + dh]
                    for fk in range(16):
                        S.pe(lambda e, py=py, fk=fk, sc=sc, dh=dh: e.matmul(py[:], hT[:, fk, sc * 128:(sc + 1) * 128], Wd[:, fk, dh * 512:(dh + 1) * 512], start=(fk == 0), stop=(fk == 15)), [hT, Wd], [py])
                    S.dve(lambda e, py=py, y_=y_, dh=dh, sc=sc: e.tensor_scalar(y_[:, dh * 512:(dh + 1) * 512], py[:], gate[:, e_, sc:sc + 1], None, op0=ALU.mult), [py, gate], [y_])
                S.dma("pool", lambda q, y_=y_, sc=sc: q.indirect_dma_start(out=acc_d[:, :], out_offset=bass.IndirectOffsetOnAxis(ap=idxT[:, e_, sc:sc + 1], axis=0), in_=y_[:], in_offset=None, compute_op=ALU.add), reads=[y_, idxT], writes=[accbuf], owner=y_)
            if e_ + 1 < NE:
                transposes(e_ + 1)
                load_group(e_ + 1, 0)


def phase_G(S, nc, env):
    acc_d = env["acc_d"]; out_d = env["out_d"]
    with Phase(S, "Z") as P:
        OG = row_bcast(S, P, "OG", env["ln_moe_g"]); OB = row_bcast(S, P, "OB", env["ln_moe_b"])
        at = P.ring("at", [128, D], F32, 4); ot = P.ring("ot", [128, D], F32, 4)
        sts = [ln_stats(S, P, None) for _ in range(4)]
        for tl in range(NT):
            a_ = at[tl % 4]; o_ = ot[tl % 4]; st, mv, rs = sts[tl % 4]
            S.dma("sp", lambda q, a_=a_, tl=tl: q.dma_start(out=a_[:], in_=acc_d[tl * 128:(tl + 1) * 128, :]), writes=[a_], owner=a_)
            emit_ln_stats(S, a_, a_.t, st, mv, rs)
            S.dve(lambda e, o_=o_, a_=a_, mv=mv, rs=rs: e.tensor_scalar(o_[:], a_[:], mv[:, 0:1], rs[:], op0=ALU.subtract, op1=ALU.mult), [a_, mv, rs], [o_])
            S.dve(lambda e, o_=o_: e.tensor_tensor(o_[:], o_[:], OG[:], op=ALU.mult), [o_, OG], [o_])
            S.pool(lambda e, o_=o_: e.tensor_tensor(o_[:], o_[:], OB[:], op=ALU.add), [o_, OB], [o_])
            S.dma("pool", lambda q, o_=o_, tl=tl: q.dma_start(out=out_d[tl * 128:(tl + 1) * 128, :], in_=o_[:]), reads=[o_], writes=[env["dbuf"]("out", tl)], owner=o_)


def host_consts():
    a = np.arange(128, dtype=np.float64)
    th = 2.0 * np.pi * np.outer(a, a) / 128.0
    Fm = np.concatenate([np.cos(th), -np.sin(th)], axis=1).astype(np.float32)
    th2 = 2.0 * np.pi * np.outer(a, a) / NFFT
    Tm = np.concatenate([np.cos(th2), -np.sin(th2)], axis=1).astype(np.float32)
    n = np.arange(L, dtype=np.float32)
    t = n / np.float32(L - 1)
    bands = 16
    f = np.linspace(1e-4, bands - 1, bands, dtype=np.float32)
    ang = (np.float32(2.0 * math.pi) * n / np.float32(L))[:, None] * f[None, :]
    feat = np.concatenate([t[:, None], np.cos(ang), -np.sin(ang)], axis=-1).astype(np.float32)
    featTS = np.zeros((NFFT, 33), np.float32)
    featTS[:L] = feat
    featTS[L + 1:] = feat[1:][::-1]
    tts = np.zeros(NFFT, np.float32)
    tts[:L] = t
    tts[L + 1:] = t[1:][::-1]
    tidx = np.zeros((128, NT, 2), np.float32)
    tidx[:, :, 0] = np.arange(NT, dtype=np.float32)[None, :]
    tidx[:, :, 1] = np.arange(128, dtype=np.float32)[:, None]
    return {
        "c_F": Fm, "c_T": Tm, "c_featT": np.ascontiguousarray(featTS.T), "c_tts": tts.reshape(128, 128).copy(),
        "c_ident": np.eye(128, dtype=np.float32),
        "c_iota": np.tile(np.arange(CAP, dtype=np.float32)[None, :], (128, 1)),
        "c_tidx": tidx,
        "c_blk8": np.kron(np.eye(16, dtype=np.float32), np.ones((8, 8), np.float32)),
    }


def make_in_maps(inputs, ncores=8):
    c = host_consts()
    f = lambda k: np.ascontiguousarray(np.asarray(inputs[k], dtype=np.float32))
    shared = {
        "ln_in_g": f("ln_in_g").reshape(D, 1), "ln_in_b": f("ln_in_b").reshape(D, 1),
        "ln_in_g_row": f("ln_in_g").reshape(1, D), "ln_in_b_row": f("ln_in_b").reshape(1, D),
        "w_in": f("w_in")[0], "b_gate": f("b_gate")[0].reshape(2 * D, 1),
        "conf_dw_w": f("conf_dw_w")[0], "conf_dw_b": f("conf_dw_b")[0].reshape(CW, 1),
        "conf_ln_g": f("conf_ln_g")[0].reshape(CW, 1), "conf_ln_b": f("conf_ln_b")[0].reshape(CW, 1),
        "conf_w_out": f("conf_w_out")[0],
        "hy_short_w": f("hy_short_w")[0], "hy_short_b": f("hy_short_b")[0].reshape(3 * HW_, 1),
        "hy_ffn_w1": f("hy_ffn_w1")[0], "hy_ffn_b1": f("hy_ffn_b1")[0].reshape(64, 1), "hy_freq1": f("hy_freq1")[0].reshape(64, 1),
        "hy_ffn_w2": f("hy_ffn_w2")[0], "hy_ffn_b2": f("hy_ffn_b2")[0].reshape(64, 1), "hy_freq2": f("hy_freq2")[0].reshape(64, 1),
        "hy_ffn_w3": f("hy_ffn_w3")[0], "hy_skip": f("hy_skip")[0].reshape(1, 2 * HW_),
        "hy_w_out": f("hy_w_out")[0], "w_mix_out": f("w_mix_out")[0],
        "ln_mix_g": f("ln_mix_g")[0].reshape(1, D), "ln_mix_b": f("ln_mix_b")[0].reshape(1, D),
        "xa_wq": f("xa_wq")[0], "xa_wk": f("xa_wk")[0], "xa_wv": f("xa_wv")[0], "xa_wo": f("xa_wo")[0],
        "ln_xa_g": f("ln_xa_g")[0].reshape(1, D), "ln_xa_b": f("ln_xa_b")[0].reshape(1, D),
        "moe_w_router": f("moe_w_router")[0],
        "moe_w_gate": f("moe_w_gate")[0], "moe_w_up": f("moe_w_up")[0], "moe_w_down": f("moe_w_down")[0],
        "ln_moe_g": f("ln_moe_g")[0].reshape(1, D), "ln_moe_b": f("ln_moe_b")[0].reshape(1, D),
    }
    shared.update(c)
    x = f("x"); mem = f("mem")
    maps = []
    for r in range(ncores):
        m = dict(shared)
        m["x"] = x[r % 4]
        m["mem"] = mem[r % 4]
        maps.append(m)
    return maps


def kernel(**inputs):
    nc = build_nc()
    maps = make_in_maps(inputs)
    res = run_bass_kernel_spmd(nc, maps, core_ids=list(range(8)))
    out = np.stack([np.asarray(res.results[r]["out"], dtype=np.float32) for r in range(4)], axis=0)
    return out
```
